# Optimizing a Trainium2 kernel written in Bass

```python
import math
import jax, jax.numpy as jnp
from jax import lax
import numpy as np

D_MODEL = 1024
BATCH = 32
SEQ = 2048
DEPTH = 1

HEAD_DIM = 64
HEADS_DIFF = 4
HEADS_MOBA = 8
N_HEADS_TOTAL = HEADS_DIFF + HEADS_MOBA
DIFF_WIDTH = HEADS_DIFF * 2 * HEAD_DIM
MOBA_WIDTH = HEADS_MOBA * HEAD_DIM
MIX_WIDTH = DIFF_WIDTH + MOBA_WIDTH
IN_PROJ_WIDTH = 3 * MIX_WIDTH
DIFF_Q_BLOCK = 128
MOBA_BLOCK = 256
MOBA_TOPK = 3
MOBA_Q_CHUNK = 16
NUM_BUCKETS = 32
MAX_DISTANCE = 2048
N_GROUPS = 4
EXPERTS_PER_GROUP = 8
N_EXPERTS = N_GROUPS * EXPERTS_PER_GROUP
EXPERTS_PER_TOKEN = 2
EXPERT_HIDDEN = 256
DISPATCH_CHUNK = 256
RMS_EPS = 1e-6
NEG_INF = -1e30

kernel_name = 'hybrid_diffattn_moba_hier_moe'


def rms_norm(x, g):
    xf = x.astype(jnp.float32)
    y = xf * lax.rsqrt(jnp.mean(xf * xf, axis=-1, keepdims=True) + RMS_EPS)
    return (y * g.astype(jnp.float32)).astype(x.dtype)


def t5_bucket(dist):
    n = jnp.maximum(dist, 0)
    max_exact = NUM_BUCKETS // 2
    nf = jnp.maximum(n, 1).astype(jnp.float32)
    large = max_exact + (jnp.log(nf / max_exact) / math.log(MAX_DISTANCE / max_exact)
                         * (NUM_BUCKETS - max_exact)).astype(jnp.int32)
    large = jnp.minimum(large, NUM_BUCKETS - 1)
    return jnp.where(n < max_exact, n, large)


def diff_attention(q, k, v, q_g, k_g, lq1, lk1, lq2, lk2, sub_g, bias_table, lambda_init):
    B, S, H = q.shape[:3]
    q = rms_norm(q, q_g).transpose(0, 2, 3, 1, 4)
    k = rms_norm(k, k_g).transpose(0, 2, 3, 1, 4)
    v = v.transpose(0, 2, 1, 3)
    f32 = jnp.float32
    lam = (jnp.exp(jnp.sum(lq1.astype(f32) * lk1.astype(f32)))
           - jnp.exp(jnp.sum(lq2.astype(f32) * lk2.astype(f32))) + lambda_init)
    scale = HEAD_DIM ** -0.5
    kpos = jnp.arange(S)

    def block(i):
        s0 = i * DIFF_Q_BLOCK
        qb = lax.dynamic_slice_in_dim(q, s0, DIFF_Q_BLOCK, axis=3)
        logits = jnp.einsum('bhmqd,bhmkd->bhmqk', qb, k, preferred_element_type=f32) * scale
        dist = (s0 + jnp.arange(DIFF_Q_BLOCK))[:, None] - kpos[None, :]
        bias = bias_table[t5_bucket(dist)].astype(f32).transpose(2, 0, 1)
        logits = jnp.where(dist >= 0, logits + bias[None, :, None], NEG_INF)
        p = jax.nn.softmax(logits, axis=-1)
        w = p[:, :, 0] - lam * p[:, :, 1]
        return jnp.einsum('bhqk,bhkc->bhqc', w.astype(v.dtype), v)

    out = lax.map(block, jnp.arange(S // DIFF_Q_BLOCK))
    out = out.transpose(1, 0, 3, 2, 4).reshape(B, S, H, 2 * HEAD_DIM)
    out = rms_norm(out, sub_g) * (1.0 - lambda_init)
    return out.reshape(B, S, H * 2 * HEAD_DIM)


def moba_attention(q, k, v, q_g, k_g, bias_table):
    B, S, H, d = q.shape
    f32 = jnp.float32
    q = rms_norm(q, q_g).transpose(0, 2, 1, 3)
    k = rms_norm(k, k_g).transpose(0, 2, 1, 3)
    v = v.transpose(0, 2, 1, 3)
    n_kb = -(-S // MOBA_BLOCK)
    pad = n_kb * MOBA_BLOCK - S
    k_blocks = jnp.pad(k, ((0, 0), (0, 0), (0, pad), (0, 0))).reshape(B, H, n_kb, MOBA_BLOCK, d)
    v_blocks = jnp.pad(v, ((0, 0), (0, 0), (0, pad), (0, 0))).reshape(B, H, n_kb, MOBA_BLOCK, d)
    k_mean = jnp.mean(k_blocks.astype(f32), axis=3)
    top = min(MOBA_TOPK, n_kb)
    scale = HEAD_DIM ** -0.5
    bi = jnp.arange(B)[:, None, None, None]
    hi = jnp.arange(H)[None, :, None, None]
    blk_off = jnp.arange(MOBA_BLOCK)

    def chunk(i):
        s0 = i * MOBA_Q_CHUNK
        qc = lax.dynamic_slice_in_dim(q, s0, MOBA_Q_CHUNK, axis=2)
        qpos = s0 + jnp.arange(MOBA_Q_CHUNK)
        own = s0 // MOBA_BLOCK
        gate = jnp.einsum('bhqd,bhnd->bhqn', qc.astype(f32), k_mean)
        gate = jnp.where(jnp.arange(n_kb) < own, gate, NEG_INF)
        _, sel = lax.top_k(gate, top)
        sel_valid = jnp.arange(top)[:, None] < own
        k_sel = k_blocks[bi, hi, sel]
        v_sel = v_blocks[bi, hi, sel]
        sel_logits = jnp.einsum('bhqd,bhqtkd->bhqtk', qc, k_sel, preferred_element_type=f32) * scale
        sel_pos = sel[..., None] * MOBA_BLOCK + blk_off
        sel_bias = bias_table[t5_bucket(qpos[:, None, None] - sel_pos), hi[..., None]].astype(f32)
        sel_logits = jnp.where(sel_valid, sel_logits + sel_bias, NEG_INF)
        k_own = lax.dynamic_index_in_dim(k_blocks, own, axis=2, keepdims=False)
        v_own = lax.dynamic_index_in_dim(v_blocks, own, axis=2, keepdims=False)
        dist_own = qpos[:, None] - (own * MOBA_BLOCK + blk_off)[None, :]
        own_bias = bias_table[t5_bucket(dist_own)].astype(f32).transpose(2, 0, 1)[None]
        own_logits = jnp.einsum('bhqd,bhkd->bhqk', qc, k_own, preferred_element_type=f32) * scale
        own_logits = jnp.where(dist_own >= 0, own_logits + own_bias, NEG_INF)
        logits = jnp.concatenate([sel_logits.reshape(B, H, MOBA_Q_CHUNK, top * MOBA_BLOCK), own_logits], axis=-1)
        p = jax.nn.softmax(logits, axis=-1).astype(v.dtype)
        p_sel = p[..., :top * MOBA_BLOCK].reshape(B, H, MOBA_Q_CHUNK, top, MOBA_BLOCK)
        p_own = p[..., top * MOBA_BLOCK:]
        return (jnp.einsum('bhqtk,bhqtkd->bhqd', p_sel, v_sel)
                + jnp.einsum('bhqk,bhkd->bhqd', p_own, v_own))

    out = lax.map(chunk, jnp.arange(S // MOBA_Q_CHUNK))
    return out.transpose(1, 0, 3, 2, 4).reshape(B, S, H * d)


def hier_moe(h, router_group, router_expert, w_gate, w_up, w_down):
    B, S, D = h.shape
    N = B * S
    t = h.reshape(N, D)
    f32 = jnp.float32
    g_logits = (t @ router_group).astype(f32)
    g_prob = jax.nn.softmax(g_logits, axis=-1)
    g = jnp.argmax(g_logits, axis=-1)
    p_group = jnp.take_along_axis(g_prob, g[:, None], axis=-1)
    e_logits_all = jnp.einsum('nd,gde->nge', t, router_expert).astype(f32)
    e_logits = jnp.take_along_axis(e_logits_all, g[:, None, None], axis=1)[:, 0]
    top_v, top_e = lax.top_k(e_logits, EXPERTS_PER_TOKEN)
    weight = p_group * jax.nn.softmax(top_v, axis=-1)
    expert_id = g[:, None] * EXPERTS_PER_GROUP + top_e
    A = N * EXPERTS_PER_TOKEN
    C = DISPATCH_CHUNK
    eid = expert_id.reshape(A)
    tok = jnp.repeat(jnp.arange(N, dtype=jnp.int32), EXPERTS_PER_TOKEN)
    w_flat = weight.reshape(A)
    order = jnp.argsort(eid)
    eid_s, tok_s, w_s = eid[order], tok[order], w_flat[order]
    counts = jax.ops.segment_sum(jnp.ones((A,), jnp.int32), eid, num_segments=N_EXPERTS)
    start = jnp.cumsum(counts) - counts
    pad_counts = ((counts + C - 1) // C) * C
    pad_end = jnp.cumsum(pad_counts)
    pad_start = pad_end - pad_counts
    dest = pad_start[eid_s] + (jnp.arange(A, dtype=jnp.int32) - start[eid_s])
    P = A + N_EXPERTS * C
    n_chunks = P // C
    slot_tok = jnp.zeros((P,), jnp.int32).at[dest].set(tok_s)
    slot_w = jnp.zeros((P,), f32).at[dest].set(w_s)
    chunk_e = jnp.minimum(jnp.searchsorted(pad_end, jnp.arange(n_chunks) * C, side='right'), N_EXPERTS - 1)

    def run(args):
        e, toks = args
        xc = t[toks]
        hid = jax.nn.silu(xc @ w_gate[e]) * (xc @ w_up[e])
        return hid @ w_down[e]

    out = lax.map(run, (chunk_e, slot_tok.reshape(n_chunks, C))).reshape(P, D)
    y = jnp.zeros((N, D), t.dtype).at[slot_tok].add((out * slot_w[:, None]).astype(t.dtype))
    return y.reshape(B, S, D)


def setup_inputs(seed: int = 0) -> dict:
    key = jax.random.key(seed)
    ks = jax.random.split(key, 20)
    f32 = jnp.float32

    def nrm(k, shape, scale):
        return jax.random.normal(k, shape, f32) * scale

    def gain(k, shape):
        return 1.0 + 0.05 * jax.random.normal(k, shape, f32)

    return {
        'x': nrm(ks[0], (BATCH, SEQ, D_MODEL), 1.0),
        'norm1_g': gain(ks[1], (DEPTH, D_MODEL)),
        'w_in': nrm(ks[2], (DEPTH, D_MODEL, IN_PROJ_WIDTH), D_MODEL ** -0.5),
        'diff_q_g': gain(ks[3], (DEPTH, HEAD_DIM)),
        'diff_k_g': gain(ks[4], (DEPTH, HEAD_DIM)),
        'lambda_q1': nrm(ks[5], (DEPTH, HEAD_DIM), 0.1),
        'lambda_k1': nrm(ks[6], (DEPTH, HEAD_DIM), 0.1),
        'lambda_q2': nrm(ks[7], (DEPTH, HEAD_DIM), 0.1),
        'lambda_k2': nrm(ks[8], (DEPTH, HEAD_DIM), 0.1),
        'diff_sub_g': gain(ks[9], (DEPTH, 2 * HEAD_DIM)),
        'moba_q_g': gain(ks[10], (DEPTH, HEAD_DIM)),
        'moba_k_g': gain(ks[11], (DEPTH, HEAD_DIM)),
        'rel_bias': nrm(ks[12], (NUM_BUCKETS, N_HEADS_TOTAL), 0.5),
        'w_out': nrm(ks[13], (DEPTH, MIX_WIDTH, D_MODEL), MIX_WIDTH ** -0.5),
        'norm2_g': gain(ks[14], (DEPTH, D_MODEL)),
        'router_group': nrm(ks[15], (DEPTH, D_MODEL, N_GROUPS), D_MODEL ** -0.5),
        'router_expert': nrm(ks[16], (DEPTH, N_GROUPS, D_MODEL, EXPERTS_PER_GROUP), D_MODEL ** -0.5),
        'w_gate': nrm(ks[17], (DEPTH, N_EXPERTS, D_MODEL, EXPERT_HIDDEN), D_MODEL ** -0.5),
        'w_up': nrm(ks[18], (DEPTH, N_EXPERTS, D_MODEL, EXPERT_HIDDEN), D_MODEL ** -0.5),
        'w_down': nrm(ks[19], (DEPTH, N_EXPERTS, EXPERT_HIDDEN, D_MODEL), EXPERT_HIDDEN ** -0.5),
    }


def reference(x, norm1_g, w_in, diff_q_g, diff_k_g, lambda_q1, lambda_k1, lambda_q2, lambda_k2,
              diff_sub_g, moba_q_g, moba_k_g, rel_bias, w_out, norm2_g, router_group,
              router_expert, w_gate, w_up, w_down):
    B, S, _ = x.shape
    bias_diff = rel_bias[:, :HEADS_DIFF]
    bias_moba = rel_bias[:, HEADS_DIFF:]
    splits = [DIFF_WIDTH, 2 * DIFF_WIDTH, 3 * DIFF_WIDTH,
              3 * DIFF_WIDTH + MOBA_WIDTH, 3 * DIFF_WIDTH + 2 * MOBA_WIDTH]
    for layer in range(DEPTH):
        lambda_init = 0.8 - 0.6 * math.exp(-0.3 * layer)
        h = rms_norm(x, norm1_g[layer])
        proj = h @ w_in[layer]
        q_d, k_d, v_d, q_m, k_m, v_m = jnp.split(proj, splits, axis=-1)
        y_d = diff_attention(q_d.reshape(B, S, HEADS_DIFF, 2, HEAD_DIM),
                             k_d.reshape(B, S, HEADS_DIFF, 2, HEAD_DIM),
                             v_d.reshape(B, S, HEADS_DIFF, 2 * HEAD_DIM),
                             diff_q_g[layer], diff_k_g[layer], lambda_q1[layer], lambda_k1[layer],
                             lambda_q2[layer], lambda_k2[layer], diff_sub_g[layer], bias_diff, lambda_init)
        y_m = moba_attention(q_m.reshape(B, S, HEADS_MOBA, HEAD_DIM),
                             k_m.reshape(B, S, HEADS_MOBA, HEAD_DIM),
                             v_m.reshape(B, S, HEADS_MOBA, HEAD_DIM),
                             moba_q_g[layer], moba_k_g[layer], bias_moba)
        x = x + jnp.concatenate([y_d, y_m], axis=-1) @ w_out[layer]
        x = x + hier_moe(rms_norm(x, norm2_g[layer]), router_group[layer], router_expert[layer],
                         w_gate[layer], w_up[layer], w_down[layer])
    return x
```

```python
import math
from contextlib import ExitStack

import numpy as np
import concourse.bass as bass
import concourse.mybir as mybir
from concourse.bass_utils import run_bass_kernel_spmd

F32 = mybir.dt.float32
BF16 = mybir.dt.bfloat16
I32 = mybir.dt.int32
AF = mybir.ActivationFunctionType
ALU = mybir.AluOpType
AX = mybir.AxisListType


class Buf:
    __slots__ = ("w", "r", "name")

    def __init__(self, name=""):
        self.w = None
        self.r = []
        self.name = name


class Prog:
    ENG = ("pe", "act", "dve", "pool", "sp")

    def __init__(self, nc, stack):
        self.nc = nc
        self.stack = stack
        self.ops = {e: [] for e in self.ENG}
        self.sem = {}
        self.cnt = {}
        self.seen = {e: {} for e in self.ENG}
        for e in self.ENG:
            self.newsem(e)

    def newsem(self, name):
        self.sem[name] = self.stack.enter_context(self.nc.semaphore("s_" + name))
        self.cnt[name] = 0

    def _waits(self, eng, reads, writes):
        need = {}

        def add(ev, raw):
            if ev is None:
                return
            sn, v = ev
            if sn == eng:
                if eng == "pe":
                    return
            if v > need.get(sn, 0):
                need[sn] = v

        for b in reads:
            add(b.w, True)
        for b in writes:
            add(b.w, False)
            for ev in b.r:
                add(ev, False)
        out = []
        for sn, v in need.items():
            if self.seen[eng].get(sn, 0) < v:
                self.seen[eng][sn] = v
                out.append((sn, v))
        return out

    def _mark(self, ev, reads, writes):
        for b in reads:
            b.r.append(ev)
        for b in writes:
            b.w = ev
            b.r = []

    def op(self, eng, fn, reads=(), writes=()):
        waits = self._waits(eng, reads, writes)
        self.cnt[eng] += 1
        ev = (eng, self.cnt[eng])
        self.ops[eng].append((fn, waits, (eng, 1)))
        self._mark(ev, reads, writes)
        return ev

    def dma(self, q, semname, fn, reads=(), writes=()):
        if semname == "dpar":
            self._npar = getattr(self, "_npar", 0) + 1
            semname = "dpar%d" % self._npar
        if semname not in self.sem:
            self.newsem(semname)
        waits = self._waits(q, reads, writes)
        self.cnt[semname] += 16
        ev = (semname, self.cnt[semname])
        self.ops[q].append((fn, waits, (semname, 16)))
        self._mark(ev, reads, writes)
        return ev

    def wait_all(self, eng, bufs):
        waits = self._waits(eng, bufs, ())
        self.ops[eng].append((None, waits, None))

    def barrier(self):
        waits = [(sn, v) for sn, v in self.cnt.items() if v > 0]
        for eng in self.ENG:
            w = [(sn, v) for sn, v in waits if self.seen[eng].get(sn, 0) < v and not (sn == eng and eng == "sp")]
            for sn, v in w:
                self.seen[eng][sn] = v
            self.ops[eng].append((None, w, None))

    def finish(self):
        waits = [(sn, v) for sn, v in self.cnt.items() if v > 0]
        self.ops["sp"].append((None, waits, None))

    def emit(self):
        nc = self.nc
        sem = self.sem
        ops = self.ops

        def run(e, name):
            for fn, waits, inc in ops[name]:
                for sn, v in waits:
                    e.wait_ge(sem[sn], v)
                if fn is None:
                    continue
                ins = fn(e)
                ins.then_inc(sem[inc[0]], inc[1])

        with nc.Block() as block:
            @block.tensor
            def _(e):
                run(e, "pe")

            @block.scalar
            def _(e):
                run(e, "act")

            @block.vector
            def _(e):
                run(e, "dve")

            @block.gpsimd
            def _(e):
                run(e, "pool")

            @block.sync
            def _(e):
                run(e, "sp")


S_LEN = 2048
D_MODEL = 1024
NT = 16
HEAD_DIM = 64
RMS_EPS = 1e-6
LAMBDA_INIT = 0.8 - 0.6 * math.exp(-0.3 * 0)
NEG_BIG = 30000.0
N_EXP = 32
CAP_CHUNK = 128


def _t5_bucket_np(n):
    n = np.maximum(n, 0)
    max_exact = 16
    nf = np.maximum(n, 1).astype(np.float32)
    large = max_exact + (np.log(nf / np.float32(max_exact)) / np.float32(math.log(2048 / max_exact))
                         * np.float32(32 - max_exact)).astype(np.int32)
    large = np.minimum(large, 31)
    return np.where(n < max_exact, n, large)


def _consts():
    d = np.arange(2048)
    b = _t5_bucket_np(d)
    onehot = np.zeros((32, 2048), np.float32)
    onehot[b, d] = 1.0
    cmask = None
    blk = np.zeros((8, 2048), np.float32)
    for n in range(8):
        blk[n, n * 256:(n + 1) * 256] = 1.0
    return onehot, cmask, blk


def bc_inner(ap, n):
    return bass.AP(tensor=ap.tensor, offset=ap.offset, ap=[list(a) for a in ap.ap] + [[0, n]])


class _Stop(Exception):
    pass


def build(nseq, stop_after_attn=False, dbg=0):
    nc = bass.Bass("TRN2", target_bir_lowering=False)
    NTOK = nseq * S_LEN
    dt_in = lambda name, shape: nc.dram_tensor(name, shape, F32, kind="ExternalInput")
    x = dt_in("x", [NTOK, D_MODEL])
    norm1_g = dt_in("norm1_g", [1, 1024])
    w_in = dt_in("w_in", [1, 1024, 3072])
    diff_q_g = dt_in("diff_q_g", [1, 64]); diff_k_g = dt_in("diff_k_g", [1, 64])
    lambda_q1 = dt_in("lambda_q1", [1, 64]); lambda_k1 = dt_in("lambda_k1", [1, 64])
    lambda_q2 = dt_in("lambda_q2", [1, 64]); lambda_k2 = dt_in("lambda_k2", [1, 64])
    diff_sub_g = dt_in("diff_sub_g", [1, 128])
    moba_q_g = dt_in("moba_q_g", [1, 64]); moba_k_g = dt_in("moba_k_g", [1, 64])
    rel_bias = dt_in("rel_bias", [32, 12])
    w_out = dt_in("w_out", [1, 1024, 1024])
    norm2_g = dt_in("norm2_g", [1, 1024])
    router_group = dt_in("router_group", [1, 1024, 4])
    router_expert = dt_in("router_expert", [1, 4, 1024, 8])
    w_gate = dt_in("w_gate", [1, 32, 1024, 256])
    w_up = dt_in("w_up", [1, 32, 1024, 256])
    w_down = dt_in("w_down", [1, 32, 256, 1024])
    c_onehot = dt_in("c_onehot", [32, 2048])
    c_blk = dt_in("c_blk", [8, 2048])
    out = nc.dram_tensor("out", [NTOK, D_MODEL], F32, kind="ExternalOutput")
    Bd = nc.dram_tensor("Bd", [12, 129, 2560], F32, kind="Internal")
    Wd = nc.dram_tensor("Wd", [8, 1024, 384], BF16, kind="Internal")
    x1d = nc.dram_tensor("x1d", [NTOK, D_MODEL], F32, kind="Internal")

    with ExitStack() as st:
        P = Prog(nc, st)
        sb = lambda name, shape, dt: st.enter_context(nc.sbuf_tensor(name, shape, dt))
        ps = lambda name, shape, dt: st.enter_context(nc.psum_tensor(name, shape, dt))
        out_bufs = []

        Sps = [ps("Sps0", [128, 512], F32), ps("Sps1", [128, 512], F32)]
        B_S = [Buf(), Buf()]
        Ops = [ps("Ops0", [128, 4, 256], F32), ps("Ops1", [128, 4, 256], F32)]
        B_O = [Buf(), Buf()]
        pj = ps("pj", [128, 512], F32); B_pj = Buf()
        ptr = ps("ptr", [128, 1024], BF16); B_ptr = Buf()

        ident_f = sb("ident_f", [128, 128], F32); B_idf = Buf()
        ident_b = sb("ident_b", [128, 128], BF16); B_idb = Buf()
        wg = [sb("wg0", [128, 8, 384], BF16), sb("wg1", [128, 8, 384], BF16)]; B_wg = [Buf(), Buf()]
        B_Wd = Buf()
        woutb = sb("woutb", [128, 8, 1024], BF16); B_wout = Buf()
        stage = [sb("stage0", [128, 1536], F32), sb("stage1", [128, 1536], F32)]
        B_stage = [Buf(), Buf()]
        g1c = sb("g1c", [128, 8], F32); B_g1 = Buf()
        xT = sb("xT", [128, 8, 2048], BF16); B_xT = [Buf() for _ in range(NT)]
        yT = sb("yT", [128, 8, 2048], BF16); B_yT = [[Buf() for _ in range(4)] for _ in range(8)]
        qTa = sb("qTa", [128, 2, 2048], BF16); B_qT = [[Buf() for _ in range(NT)] for _ in range(2)]
        kTa = sb("kTa", [128, 2, 2048], BF16); B_kT = [Buf(), Buf()]
        B_qaugrows = Buf()
        Vt = sb("Vt", [128, 16, 136], BF16); B_V = Buf()
        G = [sb("G0", [128, 2560], F32), sb("G1", [128, 2560], F32)]; B_G = [Buf(), Buf()]
        junk = sb("junk", [128, 1024], BF16); B_junk = Buf()
        xb = sb("xb", [128, 1024], BF16); B_xb = Buf()
        ss1 = sb("ss1", [128, 16], F32); B_ss1 = Buf()
        rstd1 = sb("rstd1", [128, 16], F32); B_rstd1 = Buf()
        qk = sb("qk", [128, 256], F32); B_qk = Buf()
        sq = sb("sq", [128, 256], F32); B_sq = Buf()
        ssh = sb("ssh", [128, 4], F32); B_ssh = Buf()
        rsh = sb("rsh", [128, 4], F32); B_rsh = Buf()
        qkn = sb("qkn", [128, 256], BF16); B_qkn = Buf()
        E = [sb("E0", [128, 512], F32), sb("E1", [128, 512], F32)]; B_E = [Buf(), Buf()]
        PT = [sb(f"PT{i}", [128, 512], BF16) for i in range(3)]; B_PT = [Buf() for _ in range(3)]
        rr = sb("rr", [128, 4], F32); B_rr = Buf()
        nlr = sb("nlr", [128, 4], F32); B_nlr = Buf()
        tmp1 = sb("tmp1", [128, 4, 128], F32); B_tmp1 = Buf()
        yd = sb("yd", [128, 4, 128], F32); B_yd = Buf()
        sq2 = sb("sq2", [128, 4, 128], F32); B_sq2 = Buf()
        ss2 = sb("ss2", [128, 4], F32); B_ss2 = Buf()
        rs2 = sb("rs2", [128, 4], F32); B_rs2 = Buf()
        ytok = sb("ytok", [128, 4, 128], BF16); B_ytok = Buf()
        sgb = sb("sgb", [128, 128], F32); B_sgb = Buf()
        g2b = sb("g2b", [128, 1024], F32); B_g2b = Buf()
        lamv = sb("lamv", [128, 4, 64], F32); B_lamv = Buf()
        lamp = sb("lamp", [128, 2, 64], F32); B_lamp = Buf()
        lams = sb("lams", [128, 4], F32); B_lams = Buf()
        gb = sb("gb", [128, 4, 64], F32); B_gq = Buf()
        gqk = sb("gqk", [128, 2, 64], F32); B_gqk = Buf()
        rb = sb("rb", [32, 12], F32); B_rb = Buf()
        oh = G[0][0:32, 0:2048]; B_oh = B_G[0]
        et = G[1][0:12, 0:2560]; B_et = B_G[1]
        B_Bd = Buf()
        B_x1d = Buf()
        km = sb("km", [64, 16], F32); B_km = Buf()
        kmf = sb("kmf", [64, 16], F32); B_kmf = Buf()
        kmhf = sb("kmhf", [64, 16], F32); B_kmhf = Buf()
        kmhl = sb("kmhl", [64, 2, 16], BF16); B_kmhl = Buf()
        g16 = sb("g16", [128, 16], F32); B_g16 = Buf()
        gate = sb("gate", [128, 8], F32); B_gate = Buf()
        mx8 = sb("mx8", [128, 8], F32); B_mx8 = Buf()
        selm = sb("selm", [128, 8], F32); B_selm = Buf()
        qaug = sb("qaug", [128, 128], BF16); B_qaug = Buf()

        P.op("pool", lambda e: e.memset(ident_f[:], 1.0), writes=[B_idf])
        P.op("pool", lambda e: e.affine_select(out=ident_f[:], in_=ident_f[:], pattern=[[-1, 128]],
                                               compare_op=ALU.is_equal, fill=0.0, base=0, channel_multiplier=1),
             reads=[B_idf], writes=[B_idf])
        P.op("dve", lambda e: e.tensor_copy(out=ident_b[:], in_=ident_f[:]), reads=[B_idf], writes=[B_idb])

        P.dma("sp", "dpar", lambda e: e.dma_start(out=g1c[:], in_=norm1_g.ap().rearrange("o (k p) -> p (o k)", p=128)),
              writes=[B_g1])
        w_in_v = w_in.ap().rearrange("o (k p) n -> p (o k) n", p=128)
        B_wst = [Buf(), Buf()]
        for kc in range(8):
            for T in range(2):
                s = T
                P.dma("sp", f"dst{s}", lambda e, kc=kc, s=s, T=T: e.dma_start(out=stage[s][:], in_=w_in_v[:, kc, T * 1536:(T + 1) * 1536]),
                      writes=[B_stage[s]])
                P.op("dve" if s == 0 else "pool",
                     lambda e, kc=kc, s=s: e.tensor_scalar_mul(out=qTa[:, s, 0:1536], in0=stage[s][:], scalar1=g1c[:, kc:kc + 1]),
                     reads=[B_stage[s], B_g1], writes=[B_wst[s]])
                for gg in range(4):
                    for seg in range(3):
                        P.dma("sp", f"dws{s}", lambda e, kc=kc, s=s, T=T, gg=gg, seg=seg: e.dma_start(
                            out=Wd.ap()[T * 4 + gg, kc * 128:(kc + 1) * 128, seg * 128:(seg + 1) * 128],
                            in_=qTa[:, s, seg * 512 + gg * 128: seg * 512 + gg * 128 + 128]),
                            reads=[B_wst[s]], writes=[B_Wd])
        w_out_v = w_out.ap().rearrange("o (k p) n -> p (o k) n", p=128)
        P.dma("pool", "dwo", lambda e: e.dma_start(out=woutb[:], in_=w_out_v), writes=[B_wout])

        P.dma("sp", "dpar", lambda e: e.dma_start(out=rb[:], in_=rel_bias.ap()), writes=[B_rb])
        P.dma("sp", "dpar", lambda e: e.dma_start(out=oh, in_=c_onehot.ap()), writes=[B_oh])
        P.op("pool", lambda e: e.memset(et[:, 0:512], 0.0), writes=[B_et])
        for c in range(4):
            P.op("pe", lambda e, c=c: e.matmul(out=pj[0:12, :], lhsT=rb[:, :], rhs=oh[:, c * 512:(c + 1) * 512],
                                               start=True, stop=True), reads=[B_rb, B_oh], writes=[B_pj])
            P.op("act", lambda e, c=c: e.activation(out=et[:, 512 + c * 512:512 + (c + 1) * 512], in_=pj[0:12, :], func=AF.Exp),
                 reads=[B_pj], writes=[B_et])
        et_ap = et
        esrc = bass.AP(tensor=et_ap.tensor, offset=et_ap.offset, ap=[list(et_ap.ap[0]), [0, 129], [1, 2560]])
        P.dma("sp", "dbd", lambda e: e.dma_start(out=Bd.ap(), in_=esrc), reads=[B_et], writes=[B_Bd])

        for i, t in enumerate([diff_q_g, diff_k_g, moba_q_g, moba_k_g]):
            P.dma("sp", "dpar", lambda e, i=i, t=t: e.dma_start(out=gb[:, i, :], in_=bass.AP(tensor=t, offset=0, ap=[[0, 128], [1, 64]])),
                  writes=[B_gq])
        P.op("dve", lambda e: e.tensor_tensor(out=gqk[:, 0, :], in0=gb[:, 0, :], in1=gb[:, 1, :], op=ALU.mult), reads=[B_gq], writes=[B_gqk])
        P.op("dve", lambda e: e.tensor_tensor(out=gqk[:, 1, :], in0=gb[:, 2, :], in1=gb[:, 3, :], op=ALU.mult), reads=[B_gq], writes=[B_gqk])
        P.dma("sp", "dpar", lambda e: e.dma_start(out=sgb[:], in_=bass.AP(tensor=diff_sub_g, offset=0, ap=[[0, 128], [1, 128]])),
              writes=[B_sgb])
        P.op("dve", lambda e: e.tensor_scalar_mul(out=sgb[:], in0=sgb[:], scalar1=float(1.0 - LAMBDA_INIT)), reads=[B_sgb], writes=[B_sgb])
        P.dma("sp", "dpar", lambda e: e.dma_start(out=g2b[:], in_=bass.AP(tensor=norm2_g, offset=0, ap=[[0, 128], [1, 1024]])),
              writes=[B_g2b])
        for i, t in enumerate([lambda_q1, lambda_k1, lambda_q2, lambda_k2]):
            P.dma("sp", "dpar", lambda e, i=i, t=t: e.dma_start(out=lamv[:, i, :], in_=bass.AP(tensor=t, offset=0, ap=[[0, 128], [1, 64]])),
                  writes=[B_lamv])
        P.op("dve", lambda e: e.tensor_tensor(out=lamp[:, 0, :], in0=lamv[:, 0, :], in1=lamv[:, 1, :], op=ALU.mult), reads=[B_lamv], writes=[B_lamp])
        P.op("dve", lambda e: e.tensor_tensor(out=lamp[:, 1, :], in0=lamv[:, 2, :], in1=lamv[:, 3, :], op=ALU.mult), reads=[B_lamv], writes=[B_lamp])
        P.op("dve", lambda e: e.tensor_reduce(out=lams[:, 0:2], in_=lamp[:], axis=AX.X, op=ALU.add), reads=[B_lamp], writes=[B_lams])
        P.op("act", lambda e: e.activation(out=lams[:, 0:2], in_=lams[:, 0:2], func=AF.Exp), reads=[B_lams], writes=[B_lams])
        P.op("dve", lambda e: e.tensor_sub(out=lams[:, 2:3], in0=lams[:, 0:1], in1=lams[:, 1:2]), reads=[B_lams], writes=[B_lams])
        P.op("dve", lambda e: e.tensor_scalar(out=lams[:, 3:4], in0=lams[:, 2:3], scalar1=float(LAMBDA_INIT), scalar2=-1.0,
                                              op0=ALU.add, op1=ALU.mult), reads=[B_lams], writes=[B_lams])
        nlam = lams[:, 3:4]

        P.op("pool", lambda e: e.memset(Vt[:], 0.0), writes=[B_V])
        P.op("pool", lambda e: e.memset(Vt[:, :, 64:65], 1.0), writes=[B_V])
        P.op("pool", lambda e: e.memset(Vt[:, :, 132:133], 1.0), writes=[B_V])
        P.op("pool", lambda e: e.memset(qaug[:], 0.0), writes=[B_qaug])
        P.op("pool", lambda e: e.memset(qTa[64:128, :, :], 0.0), writes=[B_qaugrows, B_wst[0], B_wst[1]])
        P.op("pool", lambda e: e.memset(kTa[64:128, :, :], 0.0), writes=[B_kT[0], B_kT[1]])
        for m in range(2):
            P.dma("pool", "dpar", lambda e, m=m: e.dma_start(out=kTa[64:72, m, :], in_=c_blk.ap()), writes=[B_kT[m]])

        def rsqrt_act(dst, src, n_feat, rbufs, wbufs):
            P.op("act", lambda e: e.activation(out=dst, in_=src, func=AF.Ln, bias=float(RMS_EPS), scale=1.0 / n_feat),
                 reads=rbufs, writes=wbufs)
            P.op("act", lambda e: e.activation(out=dst, in_=dst, func=AF.Exp, scale=-0.5), reads=wbufs, writes=wbufs)

        pt_ctr = [0]
        e_ctr = [0]
        o_ctr = [0]
        mul_ctr = [0]
        w_ctr = [0]
        _pass = [0]
        import os as _os
        _STG = int(_os.environ.get('DBG_STAGE', '0'))

        for sq_i in range(nseq if (dbg != 1 and dbg < 50) else 0):
            tok0 = sq_i * S_LEN
            for t in range(NT):
                s = t % 2
                r0 = tok0 + t * 128
                xt = stage[s][:, 0:1024]
                P.dma("sp", f"dst{s}", lambda e, xt=xt, r0=r0: e.dma_start(out=xt, in_=x.ap()[r0:r0 + 128, :]), writes=[B_stage[s]])
                P.op("act", lambda e, xt=xt, t=t: e.activation(out=junk[:], in_=xt, func=AF.Square, accum_out=ss1[:, t:t + 1]),
                     reads=[B_stage[s]], writes=[B_junk, B_ss1])
                P.op("pool", lambda e, xt=xt: e.tensor_copy(out=xb[:], in_=xt), reads=[B_stage[s]], writes=[B_xb])
                for kc in range(8):
                    P.op("pe", lambda e, kc=kc: e.transpose(out=ptr[:, kc * 128:(kc + 1) * 128], in_=xb[:, kc * 128:(kc + 1) * 128],
                                                            identity=ident_b[:]), reads=[B_xb, B_idb], writes=[B_ptr])
                P.op("dve", lambda e, t=t: e.tensor_copy(out=xT[:, :, t * 128:(t + 1) * 128],
                                                         in_=ptr[:].rearrange("p (k n) -> p k n", k=8)),
                     reads=[B_ptr], writes=[B_xT[t]])
            rsqrt_act(rstd1[:], ss1[:], 1024.0, [B_ss1], [B_rstd1])
            if dbg == 2:
                break

            for g in range(8):
                is_moba = g >= 4
                qoff = (1536 if is_moba else 0) + (g % 4) * 128
                K = 72 if is_moba else 64
                gcol = 2 if is_moba else 0
                heads = [4 + 2 * (g - 4), 4 + 2 * (g - 4) + 1] if is_moba else [g]
                for gi, h in enumerate(heads):
                    P.dma("sp", f"dG{gi}", lambda e, gi=gi, h=h: e.dma_start(
                        out=G[gi][:, 0:2432], in_=bass.AP(tensor=Bd, offset=h * 129 * 2560 + 128, ap=[[2559, 128], [1, 2432]])),
                        reads=[B_Bd], writes=[B_G[gi]])
                wsl = w_ctr[0] % 2; w_ctr[0] += 1
                P.dma("sp", f"dwg{wsl}", lambda e, wsl=wsl, g=g: e.dma_start(
                    out=wg[wsl][:], in_=Wd.ap()[g].rearrange("(k p) n -> p k n", p=128)), reads=[B_Wd], writes=[B_wg[wsl]])
                for t in (range(1, 2) if dbg == 38 else (list(range(0, 1)) * 2 if dbg == 39 else range(NT))):
                    if dbg == 30 or (dbg == 35 and t == 1) or (dbg == 36 and t == 2) or (dbg == 37 and t == 8):
                        break
                    _pass[0] += 1
                    for kc in range(8):
                        P.op("pe", lambda e, kc=kc, t=t, wsl=wsl: e.matmul(
                            out=pj[:, 0:384], lhsT=xT[:, kc, t * 128:(t + 1) * 128], rhs=wg[wsl][:, kc, :],
                            start=(kc == 0), stop=(kc == 7)), reads=[B_xT[t], B_wg[wsl]], writes=[B_pj])
                    if dbg == 39 and _pass[0] == 2 and _STG == 1:
                        break
                    P.op("dve", lambda e, t=t: e.tensor_scalar_mul(out=qk[:], in0=pj[:, 0:256], scalar1=rstd1[:, t:t + 1]),
                         reads=[B_pj, B_rstd1], writes=[B_qk])
                    if dbg == 305:
                        break
                    if dbg == 39 and _pass[0] == 2 and _STG == 2:
                        break
                    P.op("dve", lambda e, t=t: e.tensor_scalar_mul(
                        out=Vt[:, t, :].rearrange("p (a b) -> p a b", a=2)[:, :, 0:64],
                        in0=pj[:, 256:384].rearrange("p (a b) -> p a b", a=2), scalar1=rstd1[:, t:t + 1]),
                        reads=[B_pj, B_rstd1], writes=[B_V])
                    if dbg == 31:
                        break
                    if dbg == 39 and _pass[0] == 2 and _STG == 3:
                        break
                    P.op("pool", lambda e: e.tensor_tensor(out=sq[:], in0=qk[:], in1=qk[:], op=ALU.mult), reads=[B_qk], writes=[B_sq])
                    if dbg == 39 and _pass[0] == 2 and _STG == 4:
                        break
                    P.op("dve", lambda e: e.tensor_reduce(out=ssh[:], in_=sq[:].rearrange("p (a b) -> p a b", a=4), axis=AX.X, op=ALU.add),
                         reads=[B_sq], writes=[B_ssh])
                    if dbg == 39 and _pass[0] == 2 and _STG == 5:
                        break
                    rsqrt_act(rsh[:], ssh[:], 64.0, [B_ssh], [B_rsh])
                    if dbg == 39 and _pass[0] == 2 and _STG == 6:
                        break
                    for i in range(2):
                        P.op("dve", lambda e, i=i: e.tensor_scalar_mul(out=qkn[:, i * 64:(i + 1) * 64], in0=qk[:, i * 64:(i + 1) * 64],
                                                                       scalar1=rsh[:, i:i + 1]), reads=[B_qk, B_rsh], writes=[B_qkn])
                    for i in range(2, 4):
                        P.op("dve", lambda e, i=i, gi2=(1 if is_moba else 0): e.scalar_tensor_tensor(
                            out=qkn[:, i * 64:(i + 1) * 64], in0=qk[:, i * 64:(i + 1) * 64], scalar=rsh[:, i:i + 1],
                            in1=gqk[:, gi2, :], op0=ALU.mult, op1=ALU.mult), reads=[B_qk, B_rsh, B_gqk], writes=[B_qkn])
                    if dbg == 39 and _pass[0] == 2 and _STG == 7:
                        break
                    for i in range(4):
                        P.op("pe", lambda e, i=i: e.transpose(out=ptr[0:64, i * 128:(i + 1) * 128], in_=qkn[:, i * 64:(i + 1) * 64],
                                                              identity=ident_b[:]), reads=[B_qkn, B_idb], writes=[B_ptr])
                    if dbg == 33:
                        break
                    if dbg == 39 and _pass[0] == 2 and _STG == 8:
                        break
                    P.op("act", lambda e, t=t: e.copy(out=qTa[0:64, :, t * 128:(t + 1) * 128],
                                                      in_=ptr[0:64, 0:256].rearrange("p (a b) -> p a b", a=2)),
                         reads=[B_ptr], writes=[B_qT[0][t], B_qT[1][t]])
                    if dbg == 34:
                        break
                    if dbg == 39 and _pass[0] == 2 and _STG == 9:
                        break
                    P.op("act", lambda e, t=t: e.copy(out=kTa[0:64, :, t * 128:(t + 1) * 128],
                                                      in_=ptr[0:64, 256:512].rearrange("p (a b) -> p a b", a=2)),
                         reads=[B_ptr], writes=[B_kT[0], B_kT[1]])
                if dbg in (3, 30, 305, 31, 32, 33, 34, 35, 36, 37, 38, 39):
                    break
                if is_moba:
                    P.op("dve", lambda e: e.tensor_reduce(out=km[:], in_=kTa[0:64, :, :].rearrange("p m (a b) -> p (m a) b", a=8),
                                                          axis=AX.X, op=ALU.add), reads=[B_kT[0], B_kT[1]], writes=[B_km])
                    P.op("dve", lambda e: e.tensor_scalar_mul(out=kmf[:], in0=km[:], scalar1=1.0 / 256), reads=[B_km], writes=[B_kmf])
                    kview = kmhl[:, :, 0:8]
                    P.op("dve", lambda e: e.tensor_copy(out=kview, in_=kmf[:].rearrange("p (m a) -> p m a", m=2)),
                         reads=[B_kmf], writes=[B_kmhl])
                    P.op("dve", lambda e: e.tensor_copy(out=kmhf[:].rearrange("p (m a) -> p m a", m=2), in_=kview),
                         reads=[B_kmhl], writes=[B_kmhf])
                    P.op("dve", lambda e: e.tensor_sub(out=kmhl[:, :, 8:16], in0=kmf[:].rearrange("p (m a) -> p m a", m=2),
                                                       in1=kmhf[:].rearrange("p (m a) -> p m a", m=2)),
                         reads=[B_kmf, B_kmhf], writes=[B_kmhl])
                    for m in range(2):
                        for t in range(8, NT):
                            own = t // 2
                            P.op("pe", lambda e, m=m, t=t: e.matmul(out=pj[:, 0:16], lhsT=qTa[0:64, m, t * 128:(t + 1) * 128],
                                                                    rhs=kmhl[0:64, m, :], start=True, stop=True),
                                 reads=[B_qT[m][t], B_kmhl], writes=[B_pj])
                            P.op("act", lambda e: e.copy(out=g16[:], in_=pj[:, 0:16]), reads=[B_pj], writes=[B_g16])
                            P.op("pool", lambda e: e.memset(gate[:], -1e30), writes=[B_gate])
                            P.op("dve", lambda e, own=own: e.tensor_tensor(out=gate[:, 0:own], in0=g16[:, 0:own], in1=g16[:, 8:8 + own],
                                                                           op=ALU.add), reads=[B_g16], writes=[B_gate])
                            P.op("dve", lambda e: e.max(out=mx8[:], in_=gate[:]), reads=[B_gate], writes=[B_mx8])
                            P.op("dve", lambda e, own=own: e.tensor_scalar(out=selm[:, 0:own], in0=gate[:, 0:own], scalar1=mx8[:, 2:3],
                                                                           scalar2=None, op0=ALU.is_ge), reads=[B_gate, B_mx8], writes=[B_selm])
                            P.op("pool", lambda e: e.memset(qaug[:, 64:72], 0.0), writes=[B_qaug])
                            P.op("dve", lambda e, own=own: e.tensor_scalar(out=qaug[:, 64:64 + own], in0=selm[:, 0:own], scalar1=-1.0,
                                                                           scalar2=NEG_BIG, op0=ALU.add, op1=ALU.mult),
                                 reads=[B_selm], writes=[B_qaug])
                            P.op("pe", lambda e: e.transpose(out=ptr[:, 0:128], in_=qaug[:], identity=ident_b[:]),
                                 reads=[B_qaug, B_idb], writes=[B_ptr])
                            P.op("act", lambda e, m=m, t=t: e.copy(out=qTa[64:72, m, t * 128:(t + 1) * 128], in_=ptr[64:72, 0:128]),
                                 reads=[B_ptr], writes=[B_qT[m][t]])
                for qc in range(4):
                    for m in range(2):
                        o = o_ctr[0] % 2; o_ctr[0] += 1
                        gi = m if is_moba else 0
                        if is_moba:
                            v0, vw = m * 68, 65
                        else:
                            v0, vw = 0, 133
                        nk = 4 * qc + 4
                        for kt in range(nk):
                            i = e_ctr[0] % 2; e_ctr[0] += 1
                            j = pt_ctr[0] % 3; pt_ctr[0] += 1
                            P.op("pe", lambda e, i=i, m=m, kt=kt, qc=qc, K=K: e.matmul(
                                out=Sps[i][:], lhsT=kTa[0:K, m, kt * 128:(kt + 1) * 128], rhs=qTa[0:K, m, qc * 512:(qc + 1) * 512],
                                start=True, stop=True),
                                reads=[B_kT[m]] + [B_qT[m][qc * 4 + u] for u in range(4)] + [B_qaugrows], writes=[B_S[i]])
                            P.op("act", lambda e, i=i: e.activation(out=E[i][:], in_=Sps[i][:], func=AF.Exp, scale=HEAD_DIM ** -0.5),
                                 reads=[B_S[i]], writes=[B_E[i]])
                            c0 = (4 * qc - kt + 3) * 128
                            meng = "pool" if (mul_ctr[0] % 3 == 2) else "dve"; mul_ctr[0] += 1
                            P.op(meng, lambda e, i=i, j=j, gi=gi, c0=c0: e.tensor_tensor(out=PT[j][:], in0=E[i][:], in1=G[gi][:, c0:c0 + 512],
                                                                                       op=ALU.mult),
                                 reads=[B_E[i], B_G[gi]], writes=[B_PT[j]])
                            for jq in range(4):
                                if kt > 4 * qc + jq:
                                    continue
                                P.op("pe", lambda e, o=o, jq=jq, j=j, kt=kt, v0=v0, vw=vw, qc=qc: e.matmul(
                                    out=Ops[o][:, jq, 0:vw], lhsT=PT[j][:, jq * 128:(jq + 1) * 128], rhs=Vt[:, kt, v0:v0 + vw],
                                    start=(kt == 0 and jq in (0, 2)), stop=(kt == 4 * qc + jq), skip_group_check=True),
                                    reads=[B_PT[j], B_V], writes=[B_O[o]])
                        scol = 132 if not is_moba else 64
                        P.op("dve", lambda e, o=o, scol=scol: e.reciprocal(out=rr[:], in_=Ops[o][:, :, scol:scol + 1].rearrange("p a b -> p (a b)")),
                             reads=[B_O[o]], writes=[B_rr])
                        if not is_moba:
                            if m == 0:
                                for jq in range(4):
                                    P.op("dve", lambda e, o=o, jq=jq: e.tensor_scalar_mul(
                                        out=tmp1[:, jq, :].rearrange("p (a b) -> p a b", a=2),
                                        in0=Ops[o][:, jq, 0:136].rearrange("p (a b) -> p a b", a=2)[:, :, 0:64],
                                        scalar1=rr[:, jq:jq + 1]), reads=[B_O[o], B_rr], writes=[B_tmp1])
                            else:
                                P.op("dve", lambda e: e.tensor_scalar_mul(out=nlr[:], in0=rr[:], scalar1=nlam), reads=[B_rr, B_lams], writes=[B_nlr])
                                for jq in range(4):
                                    P.op("dve", lambda e, o=o, jq=jq: e.scalar_tensor_tensor(
                                        out=yd[:, jq, :].rearrange("p (a b) -> p a b", a=2),
                                        in0=Ops[o][:, jq, 0:136].rearrange("p (a b) -> p a b", a=2)[:, :, 0:64],
                                        scalar=nlr[:, jq:jq + 1], in1=tmp1[:, jq, :].rearrange("p (a b) -> p a b", a=2),
                                        op0=ALU.mult, op1=ALU.add), reads=[B_O[o], B_nlr, B_tmp1], writes=[B_yd])
                                P.op("pool", lambda e: e.tensor_tensor(out=sq2[:], in0=yd[:], in1=yd[:], op=ALU.mult), reads=[B_yd], writes=[B_sq2])
                                P.op("dve", lambda e: e.tensor_reduce(out=ss2[:], in_=sq2[:], axis=AX.X, op=ALU.add), reads=[B_sq2], writes=[B_ss2])
                                rsqrt_act(rs2[:], ss2[:], 128.0, [B_ss2], [B_rs2])
                                for jq in range(4):
                                    P.op("dve", lambda e, jq=jq: e.scalar_tensor_tensor(
                                        out=ytok[:, jq, :], in0=yd[:, jq, :], scalar=rs2[:, jq:jq + 1], in1=sgb[:],
                                        op0=ALU.mult, op1=ALU.mult), reads=[B_yd, B_rs2, B_sgb], writes=[B_ytok])
                        else:
                            for jq in range(4):
                                P.op("dve", lambda e, o=o, jq=jq, m=m: e.tensor_scalar_mul(
                                    out=ytok[:, jq, m * 64:(m + 1) * 64], in0=Ops[o][:, jq, 0:64], scalar1=rr[:, jq:jq + 1]),
                                    reads=[B_O[o], B_rr], writes=[B_ytok])
                    for jq in range(4):
                        P.op("pe", lambda e, jq=jq: e.transpose(out=ptr[:, jq * 128:(jq + 1) * 128], in_=ytok[:, jq, :], identity=ident_b[:]),
                             reads=[B_ytok, B_idb], writes=[B_ptr])
                    P.op("act", lambda e, g=g, qc=qc: e.copy(out=yT[:, g, qc * 512:(qc + 1) * 512], in_=ptr[:, 0:512]),
                         reads=[B_ptr], writes=[B_yT[g][qc]])

                if dbg == 4:
                    break
            if dbg in (3, 4, 30, 305, 31, 32, 33, 34, 35, 36, 37, 38, 39):
                break
            for t in range(NT):
                s = t % 2
                r0 = tok0 + t * 128
                xt = stage[s][:, 0:1024]
                P.dma("sp", f"dst{s}", lambda e, xt=xt, r0=r0: e.dma_start(out=xt, in_=x.ap()[r0:r0 + 128, :]), writes=[B_stage[s]])
                for half in range(2):
                    for kc in range(8):
                        P.op("pe", lambda e, half=half, kc=kc, t=t: e.matmul(
                            out=Sps[half][:], lhsT=yT[:, kc, t * 128:(t + 1) * 128], rhs=woutb[:, kc, half * 512:(half + 1) * 512],
                            start=(kc == 0), stop=(kc == 7)), reads=[B_yT[kc][t // 4], B_wout], writes=[B_S[half]])
                    P.op("dve", lambda e, half=half, xt=xt: e.tensor_tensor(
                        out=xt[:, half * 512:(half + 1) * 512], in0=Sps[half][:], in1=xt[:, half * 512:(half + 1) * 512], op=ALU.add),
                        reads=[B_S[half], B_stage[s]], writes=[B_stage[s]])
                if not stop_after_attn:
                    P.dma("sp", f"dout{s}", lambda e, xt=xt, r0=r0: e.dma_start(out=x1d.ap()[r0:r0 + 128, :], in_=xt),
                          reads=[B_stage[s]], writes=[B_x1d])
                if stop_after_attn:
                    ob = Buf()
                    P.dma("sp", f"dout{s}", lambda e, xt=xt, r0=r0: e.dma_start(out=out.ap()[r0:r0 + 128, :], in_=xt), reads=[B_stage[s]], writes=[ob])
                    out_bufs.append(ob)


        if not stop_after_attn and (dbg == 0 or dbg >= 50):
            P.barrier()
            acc = xT[:].rearrange("p k n -> p (k n)").bitcast(F32).rearrange("p (t n) -> p t n", t=8)
            h2Tb = yT
            Wgu = [yT[:, :, 1024:1536], yT[:, :, 1536:2048]]
            Wdn = [qTa[:, :, 0:1024], qTa[:, :, 1024:2048]]
            wst = [G[0][:, 0:2048], G[1][:, 0:2048]]
            h2f = stage[0][:, 0:1024]
            h2Tf = stage[1][:, 0:1024]
            sg = E[0][:, 0:256]
            hid = PT[0][:, 0:256]
            hidT = PT[1][:, 0:256]
            Rw = sb("Rw", [128, 8, 36], F32)
            lg = sb("lg", [128, 36], F32)
            wfull = sb("wfull", [128, 8, 32], F32)
            rt = sb("rt", [128, 64], F32)
            Bm = {k: Buf(k) for k in ["acc", "h2Tb", "wgu0", "wgu1", "wd0", "wd1", "wst0", "wst1", "h2f", "h2Tf", "sg", "hid",
                                      "hidT", "Rw", "lg", "wfull", "rt", "S0", "S1", "O0", "O1", "ptr", "pj", "junk", "g2b", "idb", "idf", "x1d"]}
            P.dma("sp", "dpar", lambda e: e.dma_start(out=Rw[:, :, 0:4], in_=router_group.ap().rearrange("o (k p) c -> p (o k) c", p=128)),
                  writes=[Bm["Rw"]])
            for gx in range(4):
                P.dma("sp", "dpar", lambda e, gx=gx: e.dma_start(out=Rw[:, :, 4 + 8 * gx:12 + 8 * gx],
                                                                 in_=router_expert.ap()[0, gx].rearrange("(k p) c -> p k c", p=128)),
                      writes=[Bm["Rw"]])
            Opf = [Ops[0][:].rearrange("p a b -> p (a b)"), Ops[1][:].rearrange("p a b -> p (a b)")]
            BO = [Bm["O0"], Bm["O1"]]
            BS = [Bm["S0"], Bm["S1"]]
            Bwgu = [Bm["wgu0"], Bm["wgu1"]]
            Bwd = [Bm["wd0"], Bm["wd1"]]
            Bwst = [Bm["wst0"], Bm["wst1"]]
            st_ctr = [0]
            s_ctr = [0]
            o_ctr2 = [0]
            NSB = NTOK // 1024
            for sbk in range(NSB):
                for ti in range(8):
                    if dbg == 50 and ti == 1:
                        break
                    r0 = sbk * 1024 + ti * 128
                    if dbg == 50 and _STG == 1:
                        break
                    P.dma("sp", f"dx1_{ti}", lambda e, ti=ti, r0=r0: e.dma_start(out=acc[:, ti, :], in_=x1d.ap()[r0:r0 + 128, :]),
                          reads=[Bm["x1d"]], writes=[Bm["acc"]])
                    if dbg == 50 and _STG == 2:
                        break
                    P.op("act", lambda e, ti=ti: e.activation(out=junk[:], in_=acc[:, ti, :], func=AF.Square, accum_out=rt[:, 0:1]),
                         reads=[Bm["acc"]], writes=[Bm["junk"], Bm["rt"]])
                    if dbg == 50 and _STG == 3:
                        break
                    P.op("act", lambda e: e.activation(out=rt[:, 1:2], in_=rt[:, 0:1], func=AF.Ln, bias=float(RMS_EPS), scale=1.0 / 1024),
                         reads=[Bm["rt"]], writes=[Bm["rt"]])
                    if dbg == 50 and _STG == 4:
                        break
                    P.op("act", lambda e: e.activation(out=rt[:, 1:2], in_=rt[:, 1:2], func=AF.Exp, scale=-0.5), reads=[Bm["rt"]], writes=[Bm["rt"]])
                    if dbg == 50 and _STG == 5:
                        break
                    P.op("dve", lambda e, ti=ti: e.scalar_tensor_tensor(out=h2f, in0=acc[:, ti, :], scalar=rt[:, 1:2], in1=g2b[:],
                                                                         op0=ALU.mult, op1=ALU.mult),
                         reads=[Bm["acc"], Bm["rt"], Bm["g2b"]], writes=[Bm["h2f"]])
                    if dbg == 50 and _STG == 6:
                        break
                    for kc in range(8):
                        P.op("pe", lambda e, kc=kc: e.transpose(out=Opf[1][:, kc * 128:(kc + 1) * 128], in_=h2f[:, kc * 128:(kc + 1) * 128],
                                                                identity=ident_f[:]), reads=[Bm["h2f"], Bm["idf"]], writes=[BO[1]])
                    if dbg == 50 and _STG == 7:
                        break
                    P.op("act", lambda e: e.copy(out=h2Tf, in_=Opf[1]), reads=[BO[1]], writes=[Bm["h2Tf"]])
                    if dbg == 50 and _STG == 8:
                        break
                    P.op("dve", lambda e, ti=ti: e.tensor_copy(out=h2Tb[:, :, ti * 128:(ti + 1) * 128],
                                                               in_=h2Tf.rearrange("p (k n) -> p k n", k=8)),
                         reads=[Bm["h2Tf"]], writes=[Bm["h2Tb"]])
                    if dbg == 50 and _STG == 9:
                        break
                    for kc in range(8):
                        P.op("pe", lambda e, kc=kc: e.matmul(out=pj[:, 0:36], lhsT=h2Tf[:, kc * 128:(kc + 1) * 128], rhs=Rw[:, kc, :],
                                                             start=(kc == 0), stop=(kc == 7)), reads=[Bm["h2Tf"], Bm["Rw"]], writes=[Bm["pj"]])
                    if dbg == 50 and _STG == 10:
                        break
                    P.op("act", lambda e: e.copy(out=lg[:], in_=pj[:, 0:36]), reads=[Bm["pj"]], writes=[Bm["lg"]])
                    R_ = [Bm["rt"]]; L_ = [Bm["lg"]]
                    if dbg == 50 and _STG == 11:
                        break
                    P.op("dve", lambda e: e.tensor_reduce(out=rt[:, 2:3], in_=lg[:, 0:4], axis=AX.X, op=ALU.max), reads=L_, writes=R_)
                    if dbg == 50 and _STG == 12:
                        break
                    P.op("dve", lambda e: e.tensor_scalar(out=rt[:, 8:12], in0=lg[:, 0:4], scalar1=rt[:, 2:3], scalar2=None, op0=ALU.is_equal),
                         reads=L_ + R_, writes=R_)
                    if dbg == 50 and _STG == 13:
                        break
                    P.op("dve", lambda e: e.tensor_scalar_mul(out=rt[:, 3:4], in0=rt[:, 2:3], scalar1=-1.0), reads=R_, writes=R_)
                    if dbg == 50 and _STG == 14:
                        break
                    P.op("dve", lambda e: e.tensor_scalar(out=rt[:, 12:16], in0=lg[:, 0:4], scalar1=rt[:, 2:3], scalar2=None, op0=ALU.subtract),
                         reads=L_ + R_, writes=R_)
                    if dbg == 50 and _STG == 15:
                        break
                    P.op("act", lambda e: e.activation(out=rt[:, 12:16], in_=rt[:, 12:16], func=AF.Exp, accum_out=rt[:, 4:5]),
                         reads=R_, writes=R_)
                    if dbg == 50 and _STG == 16:
                        break
                    P.op("dve", lambda e: e.reciprocal(out=rt[:, 5:6], in_=rt[:, 4:5]), reads=R_, writes=R_)
                    if dbg == 50 and _STG == 17:
                        break
                    P.op("dve", lambda e: e.tensor_scalar_mul(out=rt[:, 16:24], in0=lg[:, 4:12], scalar1=rt[:, 8:9]), reads=L_ + R_, writes=R_)
                    if dbg == 50 and _STG == 18:
                        break
                    for gx in range(1, 4):
                        P.op("dve", lambda e, gx=gx: e.scalar_tensor_tensor(out=rt[:, 16:24], in0=lg[:, 4 + 8 * gx:12 + 8 * gx],
                                                                            scalar=rt[:, 8 + gx:9 + gx], in1=rt[:, 16:24],
                                                                            op0=ALU.mult, op1=ALU.add), reads=L_ + R_, writes=R_)
                    if dbg == 50 and _STG == 19:
                        break
                    P.op("dve", lambda e: e.max(out=rt[:, 24:32], in_=rt[:, 16:24]), reads=R_, writes=R_)
                    if dbg == 50 and _STG == 20:
                        break
                    P.op("dve", lambda e: e.tensor_scalar(out=rt[:, 32:40], in0=rt[:, 16:24], scalar1=rt[:, 24:25], scalar2=None, op0=ALU.is_equal),
                         reads=R_, writes=R_)
                    if dbg == 50 and _STG == 21:
                        break
                    P.op("dve", lambda e: e.tensor_scalar(out=rt[:, 40:48], in0=rt[:, 16:24], scalar1=rt[:, 25:26], scalar2=None, op0=ALU.is_equal),
                         reads=R_, writes=R_)
                    if dbg == 50 and _STG == 22:
                        break
                    P.op("dve", lambda e: e.tensor_sub(out=rt[:, 48:49], in0=rt[:, 25:26], in1=rt[:, 24:25]), reads=R_, writes=R_)
                    if dbg == 50 and _STG == 23:
                        break
                    P.op("act", lambda e: e.activation(out=rt[:, 49:50], in_=rt[:, 48:49], func=AF.Exp), reads=R_, writes=R_)
                    if dbg == 50 and _STG == 24:
                        break
                    P.op("dve", lambda e: e.tensor_scalar_add(out=rt[:, 50:51], in0=rt[:, 49:50], scalar1=1.0), reads=R_, writes=R_)
                    if dbg == 50 and _STG == 25:
                        break
                    P.op("dve", lambda e: e.reciprocal(out=rt[:, 51:52], in_=rt[:, 50:51]), reads=R_, writes=R_)
                    if dbg == 50 and _STG == 26:
                        break
                    P.op("dve", lambda e: e.tensor_tensor(out=rt[:, 52:53], in0=rt[:, 49:50], in1=rt[:, 51:52], op=ALU.mult), reads=R_, writes=R_)
                    if dbg == 50 and _STG == 27:
                        break
                    P.op("dve", lambda e: e.tensor_scalar_mul(out=rt[:, 53:55], in0=rt[:, 51:53], scalar1=rt[:, 5:6]), reads=R_, writes=R_)
                    if dbg == 50 and _STG == 28:
                        break
                    P.op("dve", lambda e: e.tensor_scalar_mul(out=rt[:, 56:64], in0=rt[:, 32:40], scalar1=rt[:, 53:54]), reads=R_, writes=R_)
                    if dbg == 50 and _STG == 29:
                        break
                    P.op("dve", lambda e: e.scalar_tensor_tensor(out=rt[:, 56:64], in0=rt[:, 40:48], scalar=rt[:, 54:55], in1=rt[:, 56:64],
                                                                 op0=ALU.mult, op1=ALU.add), reads=R_, writes=R_)
                    if dbg == 50 and _STG == 30:
                        break
                    for gx in range(4):
                        P.op("dve", lambda e, gx=gx, ti=ti: e.tensor_scalar_mul(out=wfull[:, ti, 8 * gx:8 * gx + 8], in0=rt[:, 56:64],
                                                                                scalar1=rt[:, 8 + gx:9 + gx]), reads=R_, writes=[Bm["wfull"]])
                if dbg in (50, 51):
                    break
                for ex in range(N_EXP if dbg != 52 else 1):
                    slot = ex % 2
                    srcs = [(w_gate.ap()[0, ex].rearrange("(k p) f -> p k f", p=128), Wgu[slot][:, :, 0:256], 8, 256, Bwgu[slot]),
                            (w_up.ap()[0, ex].rearrange("(k p) f -> p k f", p=128), Wgu[slot][:, :, 256:512], 8, 256, Bwgu[slot]),
                            (w_down.ap()[0, ex].rearrange("(c p) n -> p c n", p=128), Wdn[slot], 2, 1024, Bwd[slot])]
                    for src, dst, a_, b_, bdst in srcs:
                        si = st_ctr[0] % 2; st_ctr[0] += 1
                        stv = wst[si].rearrange("p (a b) -> p a b", a=a_)
                        P.dma("sp", f"dwe{si}", lambda e, stv=stv, src=src: e.dma_start(out=stv, in_=src), writes=[Bwst[si]])
                        P.op("pool", lambda e, stv=stv, dst=dst: e.tensor_copy(out=dst, in_=stv), reads=[Bwst[si]], writes=[bdst])
                    def head(ti, i, slot=slot):
                        for kc in range(8):
                            P.op("pe", lambda e, kc=kc: e.matmul(
                                out=Sps[i][:], lhsT=h2Tb[:, kc, ti * 128:(ti + 1) * 128], rhs=Wgu[slot][:, kc, :],
                                start=(kc == 0), stop=(kc == 7)), reads=[Bm["h2Tb"], Bwgu[slot]], writes=[BS[i]])

                    def tail(ti, i, o, slot=slot, ex=ex):
                        P.op("act", lambda e: e.activation(out=sg, in_=Sps[i][:, 0:256], func=AF.Silu), reads=[BS[i]], writes=[Bm["sg"]])
                        P.op("dve", lambda e: e.scalar_tensor_tensor(
                            out=hid, in0=Sps[i][:, 256:512], scalar=wfull[:, ti, ex:ex + 1], in1=sg, op0=ALU.mult, op1=ALU.mult),
                            reads=[BS[i], Bm["wfull"], Bm["sg"]], writes=[Bm["hid"]])
                        for c in range(2):
                            P.op("pe", lambda e, c=c: e.transpose(out=ptr[:, c * 128:(c + 1) * 128], in_=hid[:, c * 128:(c + 1) * 128],
                                                                  identity=ident_b[:]), reads=[Bm["hid"], Bm["idb"]], writes=[Bm["ptr"]])
                        P.op("act", lambda e: e.copy(out=hidT, in_=ptr[:, 0:256]), reads=[Bm["ptr"]], writes=[Bm["hidT"]])
                        for half in range(2):
                            for c in range(2):
                                P.op("pe", lambda e, half=half, c=c: e.matmul(
                                    out=Opf[o][:, half * 512:(half + 1) * 512], lhsT=hidT[:, c * 128:(c + 1) * 128],
                                    rhs=Wdn[slot][:, c, half * 512:(half + 1) * 512], start=(c == 0), stop=(c == 1)),
                                    reads=[Bm["hidT"], Bwd[slot]], writes=[BO[o]])
                        P.op("dve", lambda e: e.tensor_tensor(out=acc[:, ti, :], in0=Opf[o], in1=acc[:, ti, :], op=ALU.add),
                             reads=[BO[o], Bm["acc"]], writes=[Bm["acc"]])

                    prev = None
                    for ti in range(8):
                        i = s_ctr[0] % 2; s_ctr[0] += 1
                        o = o_ctr2[0] % 2; o_ctr2[0] += 1
                        head(ti, i)
                        if prev is not None:
                            tail(*prev)
                        prev = (ti, i, o)
                    tail(*prev)
                if dbg == 52:
                    break
                for ti in range(8):
                    r0 = sbk * 1024 + ti * 128
                    ob = Buf()
                    P.dma("sp", "dfin", lambda e, ti=ti, r0=r0: e.dma_start(out=out.ap()[r0:r0 + 128, :], in_=acc[:, ti, :]),
                          reads=[Bm["acc"]], writes=[ob])
                    out_bufs.append(ob)

        P.wait_all("sp", out_bufs)
        P.finish()
        with nc.allow_non_contiguous_dma(reason="small parameter loads"):
            P.emit()
    return nc


_PARAM_KEYS = ["norm1_g", "w_in", "diff_q_g", "diff_k_g", "lambda_q1", "lambda_k1", "lambda_q2", "lambda_k2",
               "diff_sub_g", "moba_q_g", "moba_k_g", "rel_bias", "w_out", "norm2_g", "router_group",
               "router_expert", "w_gate", "w_up", "w_down"]


def run_cores(inputs, nseq, ncores, stop_after_attn=False, dbg=0):
    nc = build(nseq, stop_after_attn=stop_after_attn, dbg=dbg)
    onehot, cmask, blk = _consts()
    xs = np.ascontiguousarray(inputs["x"], dtype=np.float32).reshape(-1, D_MODEL)
    in_maps = []
    for c in range(ncores):
        m = {k: np.ascontiguousarray(inputs[k], dtype=np.float32) for k in _PARAM_KEYS}
        m["x"] = xs[c * nseq * S_LEN:(c + 1) * nseq * S_LEN]
        m["c_onehot"] = onehot; m["c_blk"] = blk
        in_maps.append(m)
    res = run_bass_kernel_spmd(nc, in_maps, core_ids=list(range(ncores)))
    return np.concatenate([np.asarray(r["out"]) for r in res.results], axis=0)


def kernel(**inputs):
    B = inputs["x"].shape[0]
    o = run_cores(inputs, B // 8, 8)
    return o.reshape(B, S_LEN, D_MODEL).astype(np.float32)
```

```python
import math
from contextlib import ExitStack

import numpy as np
import concourse.bass as bass
import concourse.mybir as mybir
from concourse.bass_utils import run_bass_kernel_spmd

F32 = mybir.dt.float32
BF16 = mybir.dt.bfloat16
I32 = mybir.dt.int32
AF = mybir.ActivationFunctionType
ALU = mybir.AluOpType
AX = mybir.AxisListType


class Buf:
    __slots__ = ("w", "r", "name")

    def __init__(self, name=""):
        self.w = None
        self.r = []
        self.name = name


class Prog:
    ENG = ("pe", "act", "dve", "pool", "sp")

    def __init__(self, nc, stack):
        self.nc = nc
        self.stack = stack
        self.ops = {e: [] for e in self.ENG}
        self.sem = {}
        self.cnt = {}
        self.seen = {e: {} for e in self.ENG}
        for e in self.ENG:
            self.newsem(e)

    def newsem(self, name):
        self.sem[name] = self.stack.enter_context(self.nc.semaphore("s_" + name))
        self.cnt[name] = 0

    def _waits(self, eng, reads, writes):
        need = {}

        def add(ev, raw):
            if ev is None:
                return
            sn, v = ev
            if sn == eng:
                if eng == "pe":
                    return
            if v > need.get(sn, 0):
                need[sn] = v

        for b in reads:
            add(b.w, True)
        for b in writes:
            add(b.w, False)
            for ev in b.r:
                add(ev, False)
        out = []
        for sn, v in need.items():
            if self.seen[eng].get(sn, 0) < v:
                self.seen[eng][sn] = v
                out.append((sn, v))
        return out

    def _mark(self, ev, reads, writes):
        for b in reads:
            b.r.append(ev)
        for b in writes:
            b.w = ev
            b.r = []

    def op(self, eng, fn, reads=(), writes=()):
        waits = self._waits(eng, reads, writes)
        self.cnt[eng] += 1
        ev = (eng, self.cnt[eng])
        self.ops[eng].append((fn, waits, (eng, 1)))
        self._mark(ev, reads, writes)
        return ev

    def dma(self, q, semname, fn, reads=(), writes=()):
        if semname == "dpar":
            self._npar = getattr(self, "_npar", 0) + 1
            semname = "dpar%d" % self._npar
        if semname not in self.sem:
            self.newsem(semname)
        waits = self._waits(q, reads, writes)
        self.cnt[semname] += 16
        ev = (semname, self.cnt[semname])
        self.ops[q].append((fn, waits, (semname, 16)))
        self._mark(ev, reads, writes)
        return ev

    def wait_all(self, eng, bufs):
        waits = self._waits(eng, bufs, ())
        self.ops[eng].append((None, waits, None))

    def barrier(self):
        waits = [(sn, v) for sn, v in self.cnt.items() if v > 0]
        for eng in self.ENG:
            w = [(sn, v) for sn, v in waits if self.seen[eng].get(sn, 0) < v and not (sn == eng and eng == "sp")]
            for sn, v in w:
                self.seen[eng][sn] = v
            self.ops[eng].append((None, w, None))

    def finish(self):
        waits = [(sn, v) for sn, v in self.cnt.items() if v > 0]
        self.ops["sp"].append((None, waits, None))

    def emit(self):
        nc = self.nc
        sem = self.sem
        ops = self.ops

        def run(e, name):
            for fn, waits, inc in ops[name]:
                for sn, v in waits:
                    e.wait_ge(sem[sn], v)
                if fn is None:
                    continue
                ins = fn(e)
                ins.then_inc(sem[inc[0]], inc[1])

        with nc.Block() as block:
            @block.tensor
            def _(e):
                run(e, "pe")

            @block.scalar
            def _(e):
                run(e, "act")

            @block.vector
            def _(e):
                run(e, "dve")

            @block.gpsimd
            def _(e):
                run(e, "pool")

            @block.sync
            def _(e):
                run(e, "sp")


S_LEN = 2048
D_MODEL = 1024
NT = 16
HEAD_DIM = 64
RMS_EPS = 1e-6
LAMBDA_INIT = 0.8 - 0.6 * math.exp(-0.3 * 0)
NEG_BIG = 30000.0
N_EXP = 32
CAP_CHUNK = 128


def _t5_bucket_np(n):
    n = np.maximum(n, 0)
    max_exact = 16
    nf = np.maximum(n, 1).astype(np.float32)
    large = max_exact + (np.log(nf / np.float32(max_exact)) / np.float32(math.log(2048 / max_exact))
                         * np.float32(32 - max_exact)).astype(np.int32)
    large = np.minimum(large, 31)
    return np.where(n < max_exact, n, large)


def _consts():
    d = np.arange(2048)
    b = _t5_bucket_np(d)
    onehot = np.zeros((32, 2048), np.float32)
    onehot[b, d] = 1.0
    cmask = None
    blk = np.zeros((8, 2048), np.float32)
    for n in range(8):
        blk[n, n * 256:(n + 1) * 256] = 1.0
    return onehot, cmask, blk


def bc_inner(ap, n):
    return bass.AP(tensor=ap.tensor, offset=ap.offset, ap=[list(a) for a in ap.ap] + [[0, n]])


class _Stop(Exception):
    pass


def build(nseq, stop_after_attn=False, dbg=0):
    nc = bass.Bass("TRN2", target_bir_lowering=False)
    NTOK = nseq * S_LEN
    dt_in = lambda name, shape: nc.dram_tensor(name, shape, F32, kind="ExternalInput")
    x = dt_in("x", [NTOK, D_MODEL])
    norm1_g = dt_in("norm1_g", [1, 1024])
    w_in = dt_in("w_in", [1, 1024, 3072])
    diff_q_g = dt_in("diff_q_g", [1, 64]); diff_k_g = dt_in("diff_k_g", [1, 64])
    lambda_q1 = dt_in("lambda_q1", [1, 64]); lambda_k1 = dt_in("lambda_k1", [1, 64])
    lambda_q2 = dt_in("lambda_q2", [1, 64]); lambda_k2 = dt_in("lambda_k2", [1, 64])
    diff_sub_g = dt_in("diff_sub_g", [1, 128])
    moba_q_g = dt_in("moba_q_g", [1, 64]); moba_k_g = dt_in("moba_k_g", [1, 64])
    rel_bias = dt_in("rel_bias", [32, 12])
    w_out = dt_in("w_out", [1, 1024, 1024])
    norm2_g = dt_in("norm2_g", [1, 1024])
    router_group = dt_in("router_group", [1, 1024, 4])
    router_expert = dt_in("router_expert", [1, 4, 1024, 8])
    w_gate = dt_in("w_gate", [1, 32, 1024, 256])
    w_up = dt_in("w_up", [1, 32, 1024, 256])
    w_down = dt_in("w_down", [1, 32, 256, 1024])
    c_onehot = dt_in("c_onehot", [32, 2048])
    c_blk = dt_in("c_blk", [8, 2048])
    out = nc.dram_tensor("out", [NTOK, D_MODEL], F32, kind="ExternalOutput")
    Bd = nc.dram_tensor("Bd", [12, 129, 2560], F32, kind="Internal")
    Wd = nc.dram_tensor("Wd", [8, 1024, 384], BF16, kind="Internal")
    x1d = nc.dram_tensor("x1d", [NTOK, D_MODEL], F32, kind="Internal")

    with ExitStack() as st:
        P = Prog(nc, st)
        sb = lambda name, shape, dt: st.enter_context(nc.sbuf_tensor(name, shape, dt))
        ps = lambda name, shape, dt: st.enter_context(nc.psum_tensor(name, shape, dt))
        out_bufs = []

        Sps = [ps("Sps0", [128, 512], F32), ps("Sps1", [128, 512], F32)]
        B_S = [Buf(), Buf()]
        Ops = [ps("Ops0", [128, 4, 256], F32), ps("Ops1", [128, 4, 256], F32)]
        B_O = [Buf(), Buf()]
        pj = ps("pj", [128, 512], F32); B_pj = Buf()
        ptr = ps("ptr", [128, 1024], BF16); B_ptr = Buf(); B_ptr1 = Buf()

        ident_f = sb("ident_f", [128, 128], F32); B_idf = Buf()
        ident_b = sb("ident_b", [128, 128], BF16); B_idb = Buf()
        wg = [sb("wg0", [128, 8, 384], BF16), sb("wg1", [128, 8, 384], BF16)]; B_wg = [Buf(), Buf()]
        B_Wd = Buf()
        woutb = sb("woutb", [128, 8, 1024], BF16); B_wout = Buf()
        stage = [sb("stage0", [128, 1536], F32), sb("stage1", [128, 1536], F32)]
        B_stage = [Buf(), Buf()]
        g1c = sb("g1c", [128, 8], F32); B_g1 = Buf()
        xT = sb("xT", [128, 8, 2048], BF16); B_xT = [Buf() for _ in range(NT)]
        yT = sb("yT", [128, 8, 2048], BF16); B_yT = [[Buf() for _ in range(4)] for _ in range(8)]
        qTa = sb("qTa", [128, 2, 2048], BF16); B_qT = [[Buf() for _ in range(NT)] for _ in range(2)]
        kTa = sb("kTa", [128, 2, 2048], BF16); B_kT = [Buf(), Buf()]
        B_qaugrows = Buf()
        Vt = sb("Vt", [128, 16, 136], BF16); B_V = Buf()
        G = [sb("G0", [128, 2560], F32), sb("G1", [128, 2560], F32)]; B_G = [Buf(), Buf()]
        junk = sb("junk", [128, 1024], BF16); B_junk = Buf()
        xb = sb("xb", [128, 1024], BF16); B_xb = Buf()
        ss1 = sb("ss1", [128, 16], F32); B_ss1 = Buf()
        rstd1 = sb("rstd1", [128, 16], F32); B_rstd1 = Buf()
        qk2 = [sb("qk", [128, 256], F32), sb("qkB", [128, 256], F32)]; B_qk2 = [Buf(), Buf()]
        sq2b = [sb("sq", [128, 256], F32), sb("sqB", [128, 256], F32)]; B_sq2b = [Buf(), Buf()]
        ssh2 = [sb("ssh", [128, 4], F32), sb("sshB", [128, 4], F32)]; B_ssh2 = [Buf(), Buf()]
        rsh2 = [sb("rsh", [128, 4], F32), sb("rshB", [128, 4], F32)]; B_rsh2 = [Buf(), Buf()]
        qkn2 = [sb("qkn", [128, 256], BF16), sb("qknB", [128, 256], BF16)]; B_qkn2 = [Buf(), Buf()]
        E = [sb("E0", [128, 512], F32), sb("E1", [128, 512], F32)]; B_E = [Buf(), Buf()]
        PT = [sb(f"PT{i}", [128, 512], BF16) for i in range(3)]; B_PT = [Buf() for _ in range(3)]
        rr = sb("rr", [128, 4], F32); B_rr = Buf()
        nlr = sb("nlr", [128, 4], F32); B_nlr = Buf()
        tmp1 = sb("tmp1", [128, 4, 128], F32); B_tmp1 = Buf()
        yd = sb("yd", [128, 4, 128], F32); B_yd = Buf()
        sq2 = sb("sq2", [128, 4, 128], F32); B_sq2 = Buf()
        ss2 = sb("ss2", [128, 4], F32); B_ss2 = Buf()
        rs2 = sb("rs2", [128, 4], F32); B_rs2 = Buf()
        ytok = sb("ytok", [128, 4, 128], BF16); B_ytok = Buf()
        sgb = sb("sgb", [128, 128], F32); B_sgb = Buf()
        g2b = sb("g2b", [128, 1024], F32); B_g2b = Buf()
        lamv = sb("lamv", [128, 4, 64], F32); B_lamv = Buf()
        lamp = sb("lamp", [128, 2, 64], F32); B_lamp = Buf()
        lams = sb("lams", [128, 4], F32); B_lams = Buf()
        gb = sb("gb", [128, 4, 64], F32); B_gq = Buf()
        gqk = sb("gqk", [128, 2, 64], F32); B_gqk = Buf()
        rb = sb("rb", [32, 12], F32); B_rb = Buf()
        oh = G[0][0:32, 0:2048]; B_oh = B_G[0]
        et = G[1][0:12, 0:2560]; B_et = B_G[1]
        B_Bd = Buf()
        B_x1d = Buf()
        km = sb("km", [64, 16], F32); B_km = Buf()
        kmf = sb("kmf", [64, 16], F32); B_kmf = Buf()
        kmhf = sb("kmhf", [64, 16], F32); B_kmhf = Buf()
        kmhl = sb("kmhl", [64, 2, 16], BF16); B_kmhl = Buf()
        g16 = sb("g16", [128, 16], F32); B_g16 = Buf()
        gate = sb("gate", [128, 8], F32); B_gate = Buf()
        mx8 = sb("mx8", [128, 8], F32); B_mx8 = Buf()
        selm = sb("selm", [128, 8], F32); B_selm = Buf()
        qaug = sb("qaug", [128, 128], BF16); B_qaug = Buf()

        P.op("pool", lambda e: e.memset(ident_f[:], 1.0), writes=[B_idf])
        P.op("pool", lambda e: e.affine_select(out=ident_f[:], in_=ident_f[:], pattern=[[-1, 128]],
                                               compare_op=ALU.is_equal, fill=0.0, base=0, channel_multiplier=1),
             reads=[B_idf], writes=[B_idf])
        P.op("dve", lambda e: e.tensor_copy(out=ident_b[:], in_=ident_f[:]), reads=[B_idf], writes=[B_idb])

        P.dma("sp", "dpar", lambda e: e.dma_start(out=g1c[:], in_=norm1_g.ap().rearrange("o (k p) -> p (o k)", p=128)),
              writes=[B_g1])
        w_in_v = w_in.ap().rearrange("o (k p) n -> p (o k) n", p=128)
        B_wst = [Buf(), Buf()]
        for kc in range(8):
            for T in range(2):
                s = T
                P.dma("sp", f"dst{s}", lambda e, kc=kc, s=s, T=T: e.dma_start(out=stage[s][:], in_=w_in_v[:, kc, T * 1536:(T + 1) * 1536]),
                      writes=[B_stage[s]])
                P.op("dve" if s == 0 else "pool",
                     lambda e, kc=kc, s=s: e.tensor_scalar_mul(out=qTa[:, s, 0:1536], in0=stage[s][:], scalar1=g1c[:, kc:kc + 1]),
                     reads=[B_stage[s], B_g1], writes=[B_wst[s]])
                for gg in range(4):
                    for seg in range(3):
                        P.dma("sp", f"dws{s}", lambda e, kc=kc, s=s, T=T, gg=gg, seg=seg: e.dma_start(
                            out=Wd.ap()[T * 4 + gg, kc * 128:(kc + 1) * 128, seg * 128:(seg + 1) * 128],
                            in_=qTa[:, s, seg * 512 + gg * 128: seg * 512 + gg * 128 + 128]),
                            reads=[B_wst[s]], writes=[B_Wd])
        w_out_v = w_out.ap().rearrange("o (k p) n -> p (o k) n", p=128)
        P.dma("pool", "dwo", lambda e: e.dma_start(out=woutb[:], in_=w_out_v), writes=[B_wout])

        P.dma("sp", "dpar", lambda e: e.dma_start(out=rb[:], in_=rel_bias.ap()), writes=[B_rb])
        P.dma("sp", "dpar", lambda e: e.dma_start(out=oh, in_=c_onehot.ap()), writes=[B_oh])
        P.op("pool", lambda e: e.memset(et[:, 0:512], 0.0), writes=[B_et])
        for c in range(4):
            P.op("pe", lambda e, c=c: e.matmul(out=pj[0:12, :], lhsT=rb[:, :], rhs=oh[:, c * 512:(c + 1) * 512],
                                               start=True, stop=True), reads=[B_rb, B_oh], writes=[B_pj])
            P.op("act", lambda e, c=c: e.activation(out=et[:, 512 + c * 512:512 + (c + 1) * 512], in_=pj[0:12, :], func=AF.Exp),
                 reads=[B_pj], writes=[B_et])
        et_ap = et
        esrc = bass.AP(tensor=et_ap.tensor, offset=et_ap.offset, ap=[list(et_ap.ap[0]), [0, 129], [1, 2560]])
        P.dma("sp", "dbd", lambda e: e.dma_start(out=Bd.ap(), in_=esrc), reads=[B_et], writes=[B_Bd])

        for i, t in enumerate([diff_q_g, diff_k_g, moba_q_g, moba_k_g]):
            P.dma("sp", "dpar", lambda e, i=i, t=t: e.dma_start(out=gb[:, i, :], in_=bass.AP(tensor=t, offset=0, ap=[[0, 128], [1, 64]])),
                  writes=[B_gq])
        P.op("dve", lambda e: e.tensor_tensor(out=gqk[:, 0, :], in0=gb[:, 0, :], in1=gb[:, 1, :], op=ALU.mult), reads=[B_gq], writes=[B_gqk])
        P.op("dve", lambda e: e.tensor_tensor(out=gqk[:, 1, :], in0=gb[:, 2, :], in1=gb[:, 3, :], op=ALU.mult), reads=[B_gq], writes=[B_gqk])
        P.dma("sp", "dpar", lambda e: e.dma_start(out=sgb[:], in_=bass.AP(tensor=diff_sub_g, offset=0, ap=[[0, 128], [1, 128]])),
              writes=[B_sgb])
        P.op("dve", lambda e: e.tensor_scalar_mul(out=sgb[:], in0=sgb[:], scalar1=float(1.0 - LAMBDA_INIT)), reads=[B_sgb], writes=[B_sgb])
        P.dma("sp", "dpar", lambda e: e.dma_start(out=g2b[:], in_=bass.AP(tensor=norm2_g, offset=0, ap=[[0, 128], [1, 1024]])),
              writes=[B_g2b])
        for i, t in enumerate([lambda_q1, lambda_k1, lambda_q2, lambda_k2]):
            P.dma("sp", "dpar", lambda e, i=i, t=t: e.dma_start(out=lamv[:, i, :], in_=bass.AP(tensor=t, offset=0, ap=[[0, 128], [1, 64]])),
                  writes=[B_lamv])
        P.op("dve", lambda e: e.tensor_tensor(out=lamp[:, 0, :], in0=lamv[:, 0, :], in1=lamv[:, 1, :], op=ALU.mult), reads=[B_lamv], writes=[B_lamp])
        P.op("dve", lambda e: e.tensor_tensor(out=lamp[:, 1, :], in0=lamv[:, 2, :], in1=lamv[:, 3, :], op=ALU.mult), reads=[B_lamv], writes=[B_lamp])
        P.op("dve", lambda e: e.tensor_reduce(out=lams[:, 0:2], in_=lamp[:], axis=AX.X, op=ALU.add), reads=[B_lamp], writes=[B_lams])
        P.op("act", lambda e: e.activation(out=lams[:, 0:2], in_=lams[:, 0:2], func=AF.Exp), reads=[B_lams], writes=[B_lams])
        P.op("dve", lambda e: e.tensor_sub(out=lams[:, 2:3], in0=lams[:, 0:1], in1=lams[:, 1:2]), reads=[B_lams], writes=[B_lams])
        P.op("dve", lambda e: e.tensor_scalar(out=lams[:, 3:4], in0=lams[:, 2:3], scalar1=float(LAMBDA_INIT), scalar2=-1.0,
                                              op0=ALU.add, op1=ALU.mult), reads=[B_lams], writes=[B_lams])
        nlam = lams[:, 3:4]

        P.op("pool", lambda e: e.memset(Vt[:], 0.0), writes=[B_V])
        P.op("pool", lambda e: e.memset(Vt[:, :, 64:65], 1.0), writes=[B_V])
        P.op("pool", lambda e: e.memset(Vt[:, :, 132:133], 1.0), writes=[B_V])
        P.op("pool", lambda e: e.memset(qaug[:], 0.0), writes=[B_qaug])
        P.op("pool", lambda e: e.memset(qTa[64:128, :, :], 0.0), writes=[B_qaugrows, B_wst[0], B_wst[1]])
        P.op("pool", lambda e: e.memset(kTa[64:128, :, :], 0.0), writes=[B_kT[0], B_kT[1]])
        for m in range(2):
            P.dma("pool", "dpar", lambda e, m=m: e.dma_start(out=kTa[64:72, m, :], in_=c_blk.ap()), writes=[B_kT[m]])

        def rsqrt_act(dst, src, n_feat, rbufs, wbufs):
            P.op("act", lambda e: e.activation(out=dst, in_=src, func=AF.Ln, bias=float(RMS_EPS), scale=1.0 / n_feat),
                 reads=rbufs, writes=wbufs)
            P.op("act", lambda e: e.activation(out=dst, in_=dst, func=AF.Exp, scale=-0.5), reads=wbufs, writes=wbufs)

        pt_ctr = [0]
        e_ctr = [0]
        o_ctr = [0]
        mul_ctr = [0]
        w_ctr = [0]
        _pass = [0]
        import os as _os
        _STG = int(_os.environ.get('DBG_STAGE', '0'))

        for sq_i in range(nseq if (dbg != 1 and dbg < 50) else 0):
            tok0 = sq_i * S_LEN
            for t in range(NT):
                s = t % 2
                r0 = tok0 + t * 128
                xt = stage[s][:, 0:1024]
                P.dma("sp", f"dst{s}", lambda e, xt=xt, r0=r0: e.dma_start(out=xt, in_=x.ap()[r0:r0 + 128, :]), writes=[B_stage[s]])
                P.op("act", lambda e, xt=xt, t=t: e.activation(out=junk[:], in_=xt, func=AF.Square, accum_out=ss1[:, t:t + 1]),
                     reads=[B_stage[s]], writes=[B_junk, B_ss1])
                P.op("pool", lambda e, xt=xt: e.tensor_copy(out=xb[:], in_=xt), reads=[B_stage[s]], writes=[B_xb])
                for kc in range(8):
                    P.op("pe", lambda e, kc=kc: e.transpose(out=ptr[:, kc * 128:(kc + 1) * 128], in_=xb[:, kc * 128:(kc + 1) * 128],
                                                            identity=ident_b[:]), reads=[B_xb, B_idb], writes=[B_ptr, B_ptr1])
                P.op("dve", lambda e, t=t: e.tensor_copy(out=xT[:, :, t * 128:(t + 1) * 128],
                                                         in_=ptr[:].rearrange("p (k n) -> p k n", k=8)),
                     reads=[B_ptr, B_ptr1], writes=[B_xT[t]])
            rsqrt_act(rstd1[:], ss1[:], 1024.0, [B_ss1], [B_rstd1])
            if dbg == 2:
                break

            for g in range(8):
                is_moba = g >= 4
                qoff = (1536 if is_moba else 0) + (g % 4) * 128
                K = 72 if is_moba else 64
                gcol = 2 if is_moba else 0
                heads = [4 + 2 * (g - 4), 4 + 2 * (g - 4) + 1] if is_moba else [g]
                for gi, h in enumerate(heads):
                    P.dma("sp", f"dG{gi}", lambda e, gi=gi, h=h: e.dma_start(
                        out=G[gi][:, 0:2432], in_=bass.AP(tensor=Bd, offset=h * 129 * 2560 + 128, ap=[[2559, 128], [1, 2432]])),
                        reads=[B_Bd], writes=[B_G[gi]])
                wsl = w_ctr[0] % 2; w_ctr[0] += 1
                P.dma("sp", f"dwg{wsl}", lambda e, wsl=wsl, g=g: e.dma_start(
                    out=wg[wsl][:], in_=Wd.ap()[g].rearrange("(k p) n -> p k n", p=128)), reads=[B_Wd], writes=[B_wg[wsl]])
                for t in range(NT):
                    p = t % 2
                    pjp = [pj, Sps[0]][p]; B_pjp = [B_pj, B_S[0]][p]
                    qk = qk2[p]; B_qk = B_qk2[p]; sq = sq2b[p]; B_sq = B_sq2b[p]
                    ssh = ssh2[p]; B_ssh = B_ssh2[p]; rsh = rsh2[p]; B_rsh = B_rsh2[p]; qkn = qkn2[p]; B_qkn = B_qkn2[p]
                    B_pt = [B_ptr, B_ptr1][p]; pc0 = p * 512
                    for kc in range(8):
                        P.op("pe", lambda e, kc=kc, t=t, wsl=wsl, pjp=pjp: e.matmul(
                            out=pjp[:, 0:384], lhsT=xT[:, kc, t * 128:(t + 1) * 128], rhs=wg[wsl][:, kc, :],
                            start=(kc == 0), stop=(kc == 7)), reads=[B_xT[t], B_wg[wsl]], writes=[B_pjp])
                    P.op("dve", lambda e, t=t, pjp=pjp, qk=qk: e.tensor_scalar_mul(out=qk[:], in0=pjp[:, 0:256], scalar1=rstd1[:, t:t + 1]),
                         reads=[B_pjp, B_rstd1], writes=[B_qk])
                    P.op("dve", lambda e, t=t, pjp=pjp: e.tensor_scalar_mul(
                        out=Vt[:, t, :].rearrange("p (a b) -> p a b", a=2)[:, :, 0:64],
                        in0=pjp[:, 256:384].rearrange("p (a b) -> p a b", a=2), scalar1=rstd1[:, t:t + 1]),
                        reads=[B_pjp, B_rstd1], writes=[B_V])
                    P.op("pool", lambda e, sq=sq, qk=qk: e.tensor_tensor(out=sq[:], in0=qk[:], in1=qk[:], op=ALU.mult), reads=[B_qk], writes=[B_sq])
                    P.op("dve", lambda e, ssh=ssh, sq=sq: e.tensor_reduce(out=ssh[:], in_=sq[:].rearrange("p (a b) -> p a b", a=4), axis=AX.X, op=ALU.add),
                         reads=[B_sq], writes=[B_ssh])
                    rsqrt_act(rsh[:], ssh[:], 64.0, [B_ssh], [B_rsh])
                    for i in range(2):
                        P.op("dve", lambda e, i=i, qkn=qkn, qk=qk, rsh=rsh: e.tensor_scalar_mul(
                            out=qkn[:, i * 64:(i + 1) * 64], in0=qk[:, i * 64:(i + 1) * 64], scalar1=rsh[:, i:i + 1]),
                            reads=[B_qk, B_rsh], writes=[B_qkn])
                    for i in range(2, 4):
                        P.op("dve", lambda e, i=i, gi2=(1 if is_moba else 0), qkn=qkn, qk=qk, rsh=rsh: e.scalar_tensor_tensor(
                            out=qkn[:, i * 64:(i + 1) * 64], in0=qk[:, i * 64:(i + 1) * 64], scalar=rsh[:, i:i + 1],
                            in1=gqk[:, gi2, :], op0=ALU.mult, op1=ALU.mult), reads=[B_qk, B_rsh, B_gqk], writes=[B_qkn])
                    for i in range(4):
                        P.op("pe", lambda e, i=i, qkn=qkn, pc0=pc0: e.transpose(
                            out=ptr[0:64, pc0 + i * 128:pc0 + (i + 1) * 128], in_=qkn[:, i * 64:(i + 1) * 64],
                            identity=ident_b[:]), reads=[B_qkn, B_idb], writes=[B_pt])
                    P.op("act", lambda e, t=t, pc0=pc0: e.copy(out=qTa[0:64, :, t * 128:(t + 1) * 128],
                                                              in_=ptr[0:64, pc0:pc0 + 256].rearrange("p (a b) -> p a b", a=2)),
                         reads=[B_pt], writes=[B_qT[0][t], B_qT[1][t]])
                    P.op("act", lambda e, t=t, pc0=pc0: e.copy(out=kTa[0:64, :, t * 128:(t + 1) * 128],
                                                              in_=ptr[0:64, pc0 + 256:pc0 + 512].rearrange("p (a b) -> p a b", a=2)),
                         reads=[B_pt], writes=[B_kT[0], B_kT[1]])
                if dbg in (3, 30, 305, 31, 32, 33, 34, 35, 36, 37, 38, 39):
                    break
                if is_moba:
                    P.op("dve", lambda e: e.tensor_reduce(out=km[:], in_=kTa[0:64, :, :].rearrange("p m (a b) -> p (m a) b", a=8),
                                                          axis=AX.X, op=ALU.add), reads=[B_kT[0], B_kT[1]], writes=[B_km])
                    P.op("dve", lambda e: e.tensor_scalar_mul(out=kmf[:], in0=km[:], scalar1=1.0 / 256), reads=[B_km], writes=[B_kmf])
                    kview = kmhl[:, :, 0:8]
                    P.op("dve", lambda e: e.tensor_copy(out=kview, in_=kmf[:].rearrange("p (m a) -> p m a", m=2)),
                         reads=[B_kmf], writes=[B_kmhl])
                    P.op("dve", lambda e: e.tensor_copy(out=kmhf[:].rearrange("p (m a) -> p m a", m=2), in_=kview),
                         reads=[B_kmhl], writes=[B_kmhf])
                    P.op("dve", lambda e: e.tensor_sub(out=kmhl[:, :, 8:16], in0=kmf[:].rearrange("p (m a) -> p m a", m=2),
                                                       in1=kmhf[:].rearrange("p (m a) -> p m a", m=2)),
                         reads=[B_kmf, B_kmhf], writes=[B_kmhl])
                    for m in range(2):
                        for t in range(8, NT):
                            own = t // 2
                            P.op("pe", lambda e, m=m, t=t: e.matmul(out=pj[:, 0:16], lhsT=qTa[0:64, m, t * 128:(t + 1) * 128],
                                                                    rhs=kmhl[0:64, m, :], start=True, stop=True),
                                 reads=[B_qT[m][t], B_kmhl], writes=[B_pj])
                            P.op("act", lambda e: e.copy(out=g16[:], in_=pj[:, 0:16]), reads=[B_pj], writes=[B_g16])
                            P.op("pool", lambda e: e.memset(gate[:], -1e30), writes=[B_gate])
                            P.op("dve", lambda e, own=own: e.tensor_tensor(out=gate[:, 0:own], in0=g16[:, 0:own], in1=g16[:, 8:8 + own],
                                                                           op=ALU.add), reads=[B_g16], writes=[B_gate])
                            P.op("dve", lambda e: e.max(out=mx8[:], in_=gate[:]), reads=[B_gate], writes=[B_mx8])
                            P.op("dve", lambda e, own=own: e.tensor_scalar(out=selm[:, 0:own], in0=gate[:, 0:own], scalar1=mx8[:, 2:3],
                                                                           scalar2=None, op0=ALU.is_ge), reads=[B_gate, B_mx8], writes=[B_selm])
                            P.op("pool", lambda e: e.memset(qaug[:, 64:72], 0.0), writes=[B_qaug])
                            P.op("dve", lambda e, own=own: e.tensor_scalar(out=qaug[:, 64:64 + own], in0=selm[:, 0:own], scalar1=-1.0,
                                                                           scalar2=NEG_BIG, op0=ALU.add, op1=ALU.mult),
                                 reads=[B_selm], writes=[B_qaug])
                            P.op("pe", lambda e: e.transpose(out=ptr[:, 0:128], in_=qaug[:], identity=ident_b[:]),
                                 reads=[B_qaug, B_idb], writes=[B_ptr])
                            P.op("act", lambda e, m=m, t=t: e.copy(out=qTa[64:72, m, t * 128:(t + 1) * 128], in_=ptr[64:72, 0:128]),
                                 reads=[B_ptr], writes=[B_qT[m][t]])
                steps = []
                for qc in range(4):
                    for m in range(2):
                        for kt in range(4 * qc + 4):
                            steps.append((qc, m, kt))
                st_info = {}

                def emit_S(idx, K=K, is_moba=is_moba, steps=steps, st_info=st_info):
                    qc, m, kt = steps[idx]
                    if kt == 0:
                        st_info[(qc, m)] = o_ctr[0] % 2; o_ctr[0] += 1
                    i = e_ctr[0] % 2; e_ctr[0] += 1
                    j = pt_ctr[0] % 3; pt_ctr[0] += 1
                    gi = m if is_moba else 0
                    P.op("pe", lambda e: e.matmul(
                        out=Sps[i][:], lhsT=kTa[0:K, m, kt * 128:(kt + 1) * 128], rhs=qTa[0:K, m, qc * 512:(qc + 1) * 512],
                        start=True, stop=True),
                        reads=[B_kT[m]] + [B_qT[m][qc * 4 + u] for u in range(4)] + [B_qaugrows], writes=[B_S[i]])
                    P.op("act", lambda e: e.activation(out=E[i][:], in_=Sps[i][:], func=AF.Exp, scale=HEAD_DIM ** -0.5),
                         reads=[B_S[i]], writes=[B_E[i]])
                    c0 = (4 * qc - kt + 3) * 128
                    meng = "pool" if (mul_ctr[0] % 3 == 2) else "dve"; mul_ctr[0] += 1
                    P.op(meng, lambda e: e.tensor_tensor(out=PT[j][:], in0=E[i][:], in1=G[gi][:, c0:c0 + 512], op=ALU.mult),
                         reads=[B_E[i], B_G[gi]], writes=[B_PT[j]])
                    return j

                def emit_PV(idx, j, is_moba=is_moba, steps=steps, st_info=st_info):
                    qc, m, kt = steps[idx]
                    o = st_info[(qc, m)]
                    if is_moba:
                        v0, vw = m * 68, 65
                    else:
                        v0, vw = 0, 133
                    for jq in range(4):
                        if kt > 4 * qc + jq:
                            continue
                        P.op("pe", lambda e, jq=jq: e.matmul(
                            out=Ops[o][:, jq, 0:vw], lhsT=PT[j][:, jq * 128:(jq + 1) * 128], rhs=Vt[:, kt, v0:v0 + vw],
                            start=(kt == 0 and jq in (0, 2)), stop=(kt == 4 * qc + jq), skip_group_check=True),
                            reads=[B_PT[j], B_V], writes=[B_O[o]])
                    if kt == 4 * qc + 3:
                        finalize(qc, m, o)

                def finalize(qc, m, o, g=g, is_moba=is_moba):
                    scol = 132 if not is_moba else 64
                    P.op("dve", lambda e: e.reciprocal(out=rr[:], in_=Ops[o][:, :, scol:scol + 1].rearrange("p a b -> p (a b)")),
                         reads=[B_O[o]], writes=[B_rr])
                    if not is_moba:
                        if m == 0:
                            for jq in range(4):
                                P.op("dve", lambda e, jq=jq: e.tensor_scalar_mul(
                                    out=tmp1[:, jq, :].rearrange("p (a b) -> p a b", a=2),
                                    in0=Ops[o][:, jq, 0:136].rearrange("p (a b) -> p a b", a=2)[:, :, 0:64],
                                    scalar1=rr[:, jq:jq + 1]), reads=[B_O[o], B_rr], writes=[B_tmp1])
                        else:
                            P.op("dve", lambda e: e.tensor_scalar_mul(out=nlr[:], in0=rr[:], scalar1=nlam), reads=[B_rr, B_lams], writes=[B_nlr])
                            for jq in range(4):
                                P.op("dve", lambda e, jq=jq: e.scalar_tensor_tensor(
                                    out=yd[:, jq, :].rearrange("p (a b) -> p a b", a=2),
                                    in0=Ops[o][:, jq, 0:136].rearrange("p (a b) -> p a b", a=2)[:, :, 0:64],
                                    scalar=nlr[:, jq:jq + 1], in1=tmp1[:, jq, :].rearrange("p (a b) -> p a b", a=2),
                                    op0=ALU.mult, op1=ALU.add), reads=[B_O[o], B_nlr, B_tmp1], writes=[B_yd])
                            P.op("pool", lambda e: e.tensor_tensor(out=sq2[:], in0=yd[:], in1=yd[:], op=ALU.mult), reads=[B_yd], writes=[B_sq2])
                            P.op("dve", lambda e: e.tensor_reduce(out=ss2[:], in_=sq2[:], axis=AX.X, op=ALU.add), reads=[B_sq2], writes=[B_ss2])
                            rsqrt_act(rs2[:], ss2[:], 128.0, [B_ss2], [B_rs2])
                            for jq in range(4):
                                P.op("dve", lambda e, jq=jq: e.scalar_tensor_tensor(
                                    out=ytok[:, jq, :], in0=yd[:, jq, :], scalar=rs2[:, jq:jq + 1], in1=sgb[:],
                                    op0=ALU.mult, op1=ALU.mult), reads=[B_yd, B_rs2, B_sgb], writes=[B_ytok])
                    else:
                        for jq in range(4):
                            P.op("dve", lambda e, jq=jq: e.tensor_scalar_mul(
                                out=ytok[:, jq, m * 64:(m + 1) * 64], in0=Ops[o][:, jq, 0:64], scalar1=rr[:, jq:jq + 1]),
                                reads=[B_O[o], B_rr], writes=[B_ytok])
                    if m == 1:
                        for jq in range(4):
                            P.op("pe", lambda e, jq=jq: e.transpose(out=ptr[:, jq * 128:(jq + 1) * 128], in_=ytok[:, jq, :], identity=ident_b[:]),
                                 reads=[B_ytok, B_idb], writes=[B_ptr])
                        P.op("act", lambda e: e.copy(out=yT[:, g, qc * 512:(qc + 1) * 512], in_=ptr[:, 0:512]),
                             reads=[B_ptr], writes=[B_yT[g][qc]])

                jprev = emit_S(0)
                for idx in range(len(steps)):
                    jnext = emit_S(idx + 1) if idx + 1 < len(steps) else None
                    emit_PV(idx, jprev)
                    jprev = jnext
                if dbg == 4:
                    break
            if dbg in (3, 4, 30, 305, 31, 32, 33, 34, 35, 36, 37, 38, 39):
                break
            for t in range(NT):
                s = t % 2
                r0 = tok0 + t * 128
                xt = stage[s][:, 0:1024]
                P.dma("sp", f"dst{s}", lambda e, xt=xt, r0=r0: e.dma_start(out=xt, in_=x.ap()[r0:r0 + 128, :]), writes=[B_stage[s]])
                for half in range(2):
                    for kc in range(8):
                        P.op("pe", lambda e, half=half, kc=kc, t=t: e.matmul(
                            out=Sps[half][:], lhsT=yT[:, kc, t * 128:(t + 1) * 128], rhs=woutb[:, kc, half * 512:(half + 1) * 512],
                            start=(kc == 0), stop=(kc == 7)), reads=[B_yT[kc][t // 4], B_wout], writes=[B_S[half]])
                    P.op("dve", lambda e, half=half, xt=xt: e.tensor_tensor(
                        out=xt[:, half * 512:(half + 1) * 512], in0=Sps[half][:], in1=xt[:, half * 512:(half + 1) * 512], op=ALU.add),
                        reads=[B_S[half], B_stage[s]], writes=[B_stage[s]])
                if not stop_after_attn:
                    P.dma("sp", f"dout{s}", lambda e, xt=xt, r0=r0: e.dma_start(out=x1d.ap()[r0:r0 + 128, :], in_=xt),
                          reads=[B_stage[s]], writes=[B_x1d])
                if stop_after_attn:
                    ob = Buf()
                    P.dma("sp", f"dout{s}", lambda e, xt=xt, r0=r0: e.dma_start(out=out.ap()[r0:r0 + 128, :], in_=xt), reads=[B_stage[s]], writes=[ob])
                    out_bufs.append(ob)


        if not stop_after_attn and (dbg == 0 or dbg >= 50):
            P.barrier()
            acc = xT[:].rearrange("p k n -> p (k n)").bitcast(F32).rearrange("p (t n) -> p t n", t=8)
            h2Tb = yT
            Wgu = [yT[:, :, 1024:1536], yT[:, :, 1536:2048]]
            Wdn = [qTa[:, :, 0:1024], qTa[:, :, 1024:2048]]
            wst = [G[0][:, 0:2048], G[1][:, 0:2048]]
            h2f = stage[0][:, 0:1024]
            h2Tf = stage[1][:, 0:1024]
            sg = E[0][:, 0:256]
            hid = PT[0][:, 0:256]
            hidT = PT[1][:, 0:256]
            Rw = sb("Rw", [128, 8, 36], F32)
            lg = sb("lg", [128, 36], F32)
            wfull = sb("wfull", [128, 8, 32], F32)
            rt = sb("rt", [128, 64], F32)
            Bm = {k: Buf(k) for k in ["acc", "h2Tb", "wgu0", "wgu1", "wd0", "wd1", "wst0", "wst1", "h2f", "h2Tf", "sg", "hid",
                                      "hidT", "Rw", "lg", "wfull", "rt", "S0", "S1", "O0", "O1", "ptr", "pj", "junk", "g2b", "idb", "idf", "x1d"]}
            P.dma("sp", "dpar", lambda e: e.dma_start(out=Rw[:, :, 0:4], in_=router_group.ap().rearrange("o (k p) c -> p (o k) c", p=128)),
                  writes=[Bm["Rw"]])
            for gx in range(4):
                P.dma("sp", "dpar", lambda e, gx=gx: e.dma_start(out=Rw[:, :, 4 + 8 * gx:12 + 8 * gx],
                                                                 in_=router_expert.ap()[0, gx].rearrange("(k p) c -> p k c", p=128)),
                      writes=[Bm["Rw"]])
            Opf = [Ops[0][:].rearrange("p a b -> p (a b)"), Ops[1][:].rearrange("p a b -> p (a b)")]
            BO = [Bm["O0"], Bm["O1"]]
            BS = [Bm["S0"], Bm["S1"]]
            Bwgu = [Bm["wgu0"], Bm["wgu1"]]
            Bwd = [Bm["wd0"], Bm["wd1"]]
            Bwst = [Bm["wst0"], Bm["wst1"]]
            st_ctr = [0]
            s_ctr = [0]
            o_ctr2 = [0]
            NSB = NTOK // 1024
            for sbk in range(NSB):
                for ti in range(8):
                    if dbg == 50 and ti == 1:
                        break
                    r0 = sbk * 1024 + ti * 128
                    if dbg == 50 and _STG == 1:
                        break
                    P.dma("sp", f"dx1_{ti}", lambda e, ti=ti, r0=r0: e.dma_start(out=acc[:, ti, :], in_=x1d.ap()[r0:r0 + 128, :]),
                          reads=[Bm["x1d"]], writes=[Bm["acc"]])
                    if dbg == 50 and _STG == 2:
                        break
                    P.op("act", lambda e, ti=ti: e.activation(out=junk[:], in_=acc[:, ti, :], func=AF.Square, accum_out=rt[:, 0:1]),
                         reads=[Bm["acc"]], writes=[Bm["junk"], Bm["rt"]])
                    if dbg == 50 and _STG == 3:
                        break
                    P.op("act", lambda e: e.activation(out=rt[:, 1:2], in_=rt[:, 0:1], func=AF.Ln, bias=float(RMS_EPS), scale=1.0 / 1024),
                         reads=[Bm["rt"]], writes=[Bm["rt"]])
                    if dbg == 50 and _STG == 4:
                        break
                    P.op("act", lambda e: e.activation(out=rt[:, 1:2], in_=rt[:, 1:2], func=AF.Exp, scale=-0.5), reads=[Bm["rt"]], writes=[Bm["rt"]])
                    if dbg == 50 and _STG == 5:
                        break
                    P.op("dve", lambda e, ti=ti: e.scalar_tensor_tensor(out=h2f, in0=acc[:, ti, :], scalar=rt[:, 1:2], in1=g2b[:],
                                                                         op0=ALU.mult, op1=ALU.mult),
                         reads=[Bm["acc"], Bm["rt"], Bm["g2b"]], writes=[Bm["h2f"]])
                    if dbg == 50 and _STG == 6:
                        break
                    for kc in range(8):
                        P.op("pe", lambda e, kc=kc: e.transpose(out=Opf[1][:, kc * 128:(kc + 1) * 128], in_=h2f[:, kc * 128:(kc + 1) * 128],
                                                                identity=ident_f[:]), reads=[Bm["h2f"], Bm["idf"]], writes=[BO[1]])
                    if dbg == 50 and _STG == 7:
                        break
                    P.op("act", lambda e: e.copy(out=h2Tf, in_=Opf[1]), reads=[BO[1]], writes=[Bm["h2Tf"]])
                    if dbg == 50 and _STG == 8:
                        break
                    P.op("dve", lambda e, ti=ti: e.tensor_copy(out=h2Tb[:, :, ti * 128:(ti + 1) * 128],
                                                               in_=h2Tf.rearrange("p (k n) -> p k n", k=8)),
                         reads=[Bm["h2Tf"]], writes=[Bm["h2Tb"]])
                    if dbg == 50 and _STG == 9:
                        break
                    for kc in range(8):
                        P.op("pe", lambda e, kc=kc: e.matmul(out=pj[:, 0:36], lhsT=h2Tf[:, kc * 128:(kc + 1) * 128], rhs=Rw[:, kc, :],
                                                             start=(kc == 0), stop=(kc == 7)), reads=[Bm["h2Tf"], Bm["Rw"]], writes=[Bm["pj"]])
                    if dbg == 50 and _STG == 10:
                        break
                    P.op("act", lambda e: e.copy(out=lg[:], in_=pj[:, 0:36]), reads=[Bm["pj"]], writes=[Bm["lg"]])
                    R_ = [Bm["rt"]]; L_ = [Bm["lg"]]
                    if dbg == 50 and _STG == 11:
                        break
                    P.op("dve", lambda e: e.tensor_reduce(out=rt[:, 2:3], in_=lg[:, 0:4], axis=AX.X, op=ALU.max), reads=L_, writes=R_)
                    if dbg == 50 and _STG == 12:
                        break
                    P.op("dve", lambda e: e.tensor_scalar(out=rt[:, 8:12], in0=lg[:, 0:4], scalar1=rt[:, 2:3], scalar2=None, op0=ALU.is_equal),
                         reads=L_ + R_, writes=R_)
                    if dbg == 50 and _STG == 13:
                        break
                    P.op("dve", lambda e: e.tensor_scalar_mul(out=rt[:, 3:4], in0=rt[:, 2:3], scalar1=-1.0), reads=R_, writes=R_)
                    if dbg == 50 and _STG == 14:
                        break
                    P.op("dve", lambda e: e.tensor_scalar(out=rt[:, 12:16], in0=lg[:, 0:4], scalar1=rt[:, 2:3], scalar2=None, op0=ALU.subtract),
                         reads=L_ + R_, writes=R_)
                    if dbg == 50 and _STG == 15:
                        break
                    P.op("act", lambda e: e.activation(out=rt[:, 12:16], in_=rt[:, 12:16], func=AF.Exp, accum_out=rt[:, 4:5]),
                         reads=R_, writes=R_)
                    if dbg == 50 and _STG == 16:
                        break
                    P.op("dve", lambda e: e.reciprocal(out=rt[:, 5:6], in_=rt[:, 4:5]), reads=R_, writes=R_)
                    if dbg == 50 and _STG == 17:
                        break
                    P.op("dve", lambda e: e.tensor_scalar_mul(out=rt[:, 16:24], in0=lg[:, 4:12], scalar1=rt[:, 8:9]), reads=L_ + R_, writes=R_)
                    if dbg == 50 and _STG == 18:
                        break
                    for gx in range(1, 4):
                        P.op("dve", lambda e, gx=gx: e.scalar_tensor_tensor(out=rt[:, 16:24], in0=lg[:, 4 + 8 * gx:12 + 8 * gx],
                                                                            scalar=rt[:, 8 + gx:9 + gx], in1=rt[:, 16:24],
                                                                            op0=ALU.mult, op1=ALU.add), reads=L_ + R_, writes=R_)
                    if dbg == 50 and _STG == 19:
                        break
                    P.op("dve", lambda e: e.max(out=rt[:, 24:32], in_=rt[:, 16:24]), reads=R_, writes=R_)
                    if dbg == 50 and _STG == 20:
                        break
                    P.op("dve", lambda e: e.tensor_scalar(out=rt[:, 32:40], in0=rt[:, 16:24], scalar1=rt[:, 24:25], scalar2=None, op0=ALU.is_equal),
                         reads=R_, writes=R_)
                    if dbg == 50 and _STG == 21:
                        break
                    P.op("dve", lambda e: e.tensor_scalar(out=rt[:, 40:48], in0=rt[:, 16:24], scalar1=rt[:, 25:26], scalar2=None, op0=ALU.is_equal),
                         reads=R_, writes=R_)
                    if dbg == 50 and _STG == 22:
                        break
                    P.op("dve", lambda e: e.tensor_sub(out=rt[:, 48:49], in0=rt[:, 25:26], in1=rt[:, 24:25]), reads=R_, writes=R_)
                    if dbg == 50 and _STG == 23:
                        break
                    P.op("act", lambda e: e.activation(out=rt[:, 49:50], in_=rt[:, 48:49], func=AF.Exp), reads=R_, writes=R_)
                    if dbg == 50 and _STG == 24:
                        break
                    P.op("dve", lambda e: e.tensor_scalar_add(out=rt[:, 50:51], in0=rt[:, 49:50], scalar1=1.0), reads=R_, writes=R_)
                    if dbg == 50 and _STG == 25:
                        break
                    P.op("dve", lambda e: e.reciprocal(out=rt[:, 51:52], in_=rt[:, 50:51]), reads=R_, writes=R_)
                    if dbg == 50 and _STG == 26:
                        break
                    P.op("dve", lambda e: e.tensor_tensor(out=rt[:, 52:53], in0=rt[:, 49:50], in1=rt[:, 51:52], op=ALU.mult), reads=R_, writes=R_)
                    if dbg == 50 and _STG == 27:
                        break
                    P.op("dve", lambda e: e.tensor_scalar_mul(out=rt[:, 53:55], in0=rt[:, 51:53], scalar1=rt[:, 5:6]), reads=R_, writes=R_)
                    if dbg == 50 and _STG == 28:
                        break
                    P.op("dve", lambda e: e.tensor_scalar_mul(out=rt[:, 56:64], in0=rt[:, 32:40], scalar1=rt[:, 53:54]), reads=R_, writes=R_)
                    if dbg == 50 and _STG == 29:
                        break
                    P.op("dve", lambda e: e.scalar_tensor_tensor(out=rt[:, 56:64], in0=rt[:, 40:48], scalar=rt[:, 54:55], in1=rt[:, 56:64],
                                                                 op0=ALU.mult, op1=ALU.add), reads=R_, writes=R_)
                    if dbg == 50 and _STG == 30:
                        break
                    for gx in range(4):
                        P.op("dve", lambda e, gx=gx, ti=ti: e.tensor_scalar_mul(out=wfull[:, ti, 8 * gx:8 * gx + 8], in0=rt[:, 56:64],
                                                                                scalar1=rt[:, 8 + gx:9 + gx]), reads=R_, writes=[Bm["wfull"]])
                if dbg in (50, 51):
                    break
                for ex in range(N_EXP if dbg != 52 else 1):
                    slot = ex % 2
                    srcs = [(w_gate.ap()[0, ex].rearrange("(k p) f -> p k f", p=128), Wgu[slot][:, :, 0:256], 8, 256, Bwgu[slot]),
                            (w_up.ap()[0, ex].rearrange("(k p) f -> p k f", p=128), Wgu[slot][:, :, 256:512], 8, 256, Bwgu[slot]),
                            (w_down.ap()[0, ex].rearrange("(c p) n -> p c n", p=128), Wdn[slot], 2, 1024, Bwd[slot])]
                    for src, dst, a_, b_, bdst in srcs:
                        si = st_ctr[0] % 2; st_ctr[0] += 1
                        stv = wst[si].rearrange("p (a b) -> p a b", a=a_)
                        P.dma("sp", f"dwe{si}", lambda e, stv=stv, src=src: e.dma_start(out=stv, in_=src), writes=[Bwst[si]])
                        P.op("pool", lambda e, stv=stv, dst=dst: e.tensor_copy(out=dst, in_=stv), reads=[Bwst[si]], writes=[bdst])
                    def head(ti, i, slot=slot):
                        for kc in range(8):
                            P.op("pe", lambda e, kc=kc: e.matmul(
                                out=Sps[i][:], lhsT=h2Tb[:, kc, ti * 128:(ti + 1) * 128], rhs=Wgu[slot][:, kc, :],
                                start=(kc == 0), stop=(kc == 7)), reads=[Bm["h2Tb"], Bwgu[slot]], writes=[BS[i]])

                    def tail(ti, i, o, slot=slot, ex=ex):
                        P.op("act", lambda e: e.activation(out=sg, in_=Sps[i][:, 0:256], func=AF.Silu), reads=[BS[i]], writes=[Bm["sg"]])
                        P.op("dve", lambda e: e.scalar_tensor_tensor(
                            out=hid, in0=Sps[i][:, 256:512], scalar=wfull[:, ti, ex:ex + 1], in1=sg, op0=ALU.mult, op1=ALU.mult),
                            reads=[BS[i], Bm["wfull"], Bm["sg"]], writes=[Bm["hid"]])
                        for c in range(2):
                            P.op("pe", lambda e, c=c: e.transpose(out=ptr[:, c * 128:(c + 1) * 128], in_=hid[:, c * 128:(c + 1) * 128],
                                                                  identity=ident_b[:]), reads=[Bm["hid"], Bm["idb"]], writes=[Bm["ptr"]])
                        P.op("act", lambda e: e.copy(out=hidT, in_=ptr[:, 0:256]), reads=[Bm["ptr"]], writes=[Bm["hidT"]])
                        for half in range(2):
                            for c in range(2):
                                P.op("pe", lambda e, half=half, c=c: e.matmul(
                                    out=Opf[o][:, half * 512:(half + 1) * 512], lhsT=hidT[:, c * 128:(c + 1) * 128],
                                    rhs=Wdn[slot][:, c, half * 512:(half + 1) * 512], start=(c == 0), stop=(c == 1)),
                                    reads=[Bm["hidT"], Bwd[slot]], writes=[BO[o]])
                        P.op("dve", lambda e: e.tensor_tensor(out=acc[:, ti, :], in0=Opf[o], in1=acc[:, ti, :], op=ALU.add),
                             reads=[BO[o], Bm["acc"]], writes=[Bm["acc"]])

                    prev = None
                    for ti in range(8):
                        i = s_ctr[0] % 2; s_ctr[0] += 1
                        o = o_ctr2[0] % 2; o_ctr2[0] += 1
                        head(ti, i)
                        if prev is not None:
                            tail(*prev)
                        prev = (ti, i, o)
                    tail(*prev)
                if dbg == 52:
                    break
                for ti in range(8):
                    r0 = sbk * 1024 + ti * 128
                    ob = Buf()
                    P.dma("sp", "dfin", lambda e, ti=ti, r0=r0: e.dma_start(out=out.ap()[r0:r0 + 128, :], in_=acc[:, ti, :]),
                          reads=[Bm["acc"]], writes=[ob])
                    out_bufs.append(ob)

        P.wait_all("sp", out_bufs)
        P.finish()
        with nc.allow_non_contiguous_dma(reason="small parameter loads"):
            P.emit()
    return nc


_PARAM_KEYS = ["norm1_g", "w_in", "diff_q_g", "diff_k_g", "lambda_q1", "lambda_k1", "lambda_q2", "lambda_k2",
               "diff_sub_g", "moba_q_g", "moba_k_g", "rel_bias", "w_out", "norm2_g", "router_group",
               "router_expert", "w_gate", "w_up", "w_down"]


def run_cores(inputs, nseq, ncores, stop_after_attn=False, dbg=0):
    nc = build(nseq, stop_after_attn=stop_after_attn, dbg=dbg)
    onehot, cmask, blk = _consts()
    xs = np.ascontiguousarray(inputs["x"], dtype=np.float32).reshape(-1, D_MODEL)
    in_maps = []
    for c in range(ncores):
        m = {k: np.ascontiguousarray(inputs[k], dtype=np.float32) for k in _PARAM_KEYS}
        m["x"] = xs[c * nseq * S_LEN:(c + 1) * nseq * S_LEN]
        m["c_onehot"] = onehot; m["c_blk"] = blk
        in_maps.append(m)
    res = run_bass_kernel_spmd(nc, in_maps, core_ids=list(range(ncores)))
    return np.concatenate([np.asarray(r["out"]) for r in res.results], axis=0)


def kernel(**inputs):
    B = inputs["x"].shape[0]
    o = run_cores(inputs, B // 8, 8)
    return o.reshape(B, S_LEN, D_MODEL).astype(np.float32)
```

```python
import math
from contextlib import ExitStack

import numpy as np
import concourse.bass as bass
import concourse.mybir as mybir
from concourse.bass_utils import run_bass_kernel_spmd

F32 = mybir.dt.float32
BF16 = mybir.dt.bfloat16
I32 = mybir.dt.int32
AF = mybir.ActivationFunctionType
ALU = mybir.AluOpType
AX = mybir.AxisListType


class Buf:
    __slots__ = ("w", "r", "name")

    def __init__(self, name=""):
        self.w = None
        self.r = []
        self.name = name


class Prog:
    ENG = ("pe", "act", "dve", "pool", "sp")

    def __init__(self, nc, stack):
        self.nc = nc
        self.stack = stack
        self.ops = {e: [] for e in self.ENG}
        self.sem = {}
        self.cnt = {}
        self.seen = {e: {} for e in self.ENG}
        for e in self.ENG:
            self.newsem(e)

    def newsem(self, name):
        self.sem[name] = self.stack.enter_context(self.nc.semaphore("s_" + name))
        self.cnt[name] = 0

    def _waits(self, eng, reads, writes):
        need = {}

        def add(ev, raw):
            if ev is None:
                return
            sn, v = ev
            if sn == eng:
                if eng == "pe":
                    return
            if v > need.get(sn, 0):
                need[sn] = v

        for b in reads:
            add(b.w, True)
        for b in writes:
            add(b.w, False)
            for ev in b.r:
                add(ev, False)
        out = []
        for sn, v in need.items():
            if self.seen[eng].get(sn, 0) < v:
                self.seen[eng][sn] = v
                out.append((sn, v))
        return out

    def _mark(self, ev, reads, writes):
        for b in reads:
            b.r.append(ev)
        for b in writes:
            b.w = ev
            b.r = []

    def op(self, eng, fn, reads=(), writes=()):
        waits = self._waits(eng, reads, writes)
        self.cnt[eng] += 1
        ev = (eng, self.cnt[eng])
        self.ops[eng].append((fn, waits, (eng, 1)))
        self._mark(ev, reads, writes)
        return ev

    def dma(self, q, semname, fn, reads=(), writes=()):
        if semname == "dpar":
            self._npar = getattr(self, "_npar", 0) + 1
            semname = "dpar%d" % self._npar
        if semname not in self.sem:
            self.newsem(semname)
        waits = self._waits(q, reads, writes)
        self.cnt[semname] += 16
        ev = (semname, self.cnt[semname])
        self.ops[q].append((fn, waits, (semname, 16)))
        self._mark(ev, reads, writes)
        return ev

    def wait_all(self, eng, bufs):
        waits = self._waits(eng, bufs, ())
        self.ops[eng].append((None, waits, None))

    def barrier(self):
        waits = [(sn, v) for sn, v in self.cnt.items() if v > 0]
        for eng in self.ENG:
            w = [(sn, v) for sn, v in waits if self.seen[eng].get(sn, 0) < v and not (sn == eng and eng == "sp")]
            for sn, v in w:
                self.seen[eng][sn] = v
            self.ops[eng].append((None, w, None))

    def finish(self):
        waits = [(sn, v) for sn, v in self.cnt.items() if v > 0]
        self.ops["sp"].append((None, waits, None))

    def emit(self):
        nc = self.nc
        sem = self.sem
        ops = self.ops

        def run(e, name):
            for fn, waits, inc in ops[name]:
                for sn, v in waits:
                    e.wait_ge(sem[sn], v)
                if fn is None:
                    continue
                ins = fn(e)
                ins.then_inc(sem[inc[0]], inc[1])

        with nc.Block() as block:
            @block.tensor
            def _(e):
                run(e, "pe")

            @block.scalar
            def _(e):
                run(e, "act")

            @block.vector
            def _(e):
                run(e, "dve")

            @block.gpsimd
            def _(e):
                run(e, "pool")

            @block.sync
            def _(e):
                run(e, "sp")


S_LEN = 2048
D_MODEL = 1024
NT = 16
HEAD_DIM = 64
RMS_EPS = 1e-6
LAMBDA_INIT = 0.8 - 0.6 * math.exp(-0.3 * 0)
NEG_BIG = 30000.0
N_EXP = 32
CAP_CHUNK = 128


def _t5_bucket_np(n):
    n = np.maximum(n, 0)
    max_exact = 16
    nf = np.maximum(n, 1).astype(np.float32)
    large = max_exact + (np.log(nf / np.float32(max_exact)) / np.float32(math.log(2048 / max_exact))
                         * np.float32(32 - max_exact)).astype(np.int32)
    large = np.minimum(large, 31)
    return np.where(n < max_exact, n, large)


def _consts():
    d = np.arange(2048)
    b = _t5_bucket_np(d)
    onehot = np.zeros((32, 2048), np.float32)
    onehot[b, d] = 1.0
    cmask = None
    blk = np.zeros((8, 2048), np.float32)
    for n in range(8):
        blk[n, n * 256:(n + 1) * 256] = 1.0
    return onehot, cmask, blk


def bc_inner(ap, n):
    return bass.AP(tensor=ap.tensor, offset=ap.offset, ap=[list(a) for a in ap.ap] + [[0, n]])


class _Stop(Exception):
    pass


def build(nseq, stop_after_attn=False, dbg=0):
    nc = bass.Bass("TRN2", target_bir_lowering=False)
    NTOK = nseq * S_LEN
    dt_in = lambda name, shape: nc.dram_tensor(name, shape, F32, kind="ExternalInput")
    x = dt_in("x", [NTOK, D_MODEL])
    norm1_g = dt_in("norm1_g", [1, 1024])
    w_in = dt_in("w_in", [1, 1024, 3072])
    diff_q_g = dt_in("diff_q_g", [1, 64]); diff_k_g = dt_in("diff_k_g", [1, 64])
    lambda_q1 = dt_in("lambda_q1", [1, 64]); lambda_k1 = dt_in("lambda_k1", [1, 64])
    lambda_q2 = dt_in("lambda_q2", [1, 64]); lambda_k2 = dt_in("lambda_k2", [1, 64])
    diff_sub_g = dt_in("diff_sub_g", [1, 128])
    moba_q_g = dt_in("moba_q_g", [1, 64]); moba_k_g = dt_in("moba_k_g", [1, 64])
    rel_bias = dt_in("rel_bias", [32, 12])
    w_out = dt_in("w_out", [1, 1024, 1024])
    norm2_g = dt_in("norm2_g", [1, 1024])
    router_group = dt_in("router_group", [1, 1024, 4])
    router_expert = dt_in("router_expert", [1, 4, 1024, 8])
    w_gate = dt_in("w_gate", [1, 32, 1024, 256])
    w_up = dt_in("w_up", [1, 32, 1024, 256])
    w_down = dt_in("w_down", [1, 32, 256, 1024])
    c_onehot = dt_in("c_onehot", [32, 2048])
    c_blk = dt_in("c_blk", [8, 2048])
    out = nc.dram_tensor("out", [NTOK, D_MODEL], F32, kind="ExternalOutput")
    Bd = nc.dram_tensor("Bd", [12, 129, 2560], F32, kind="Internal")
    Wd = nc.dram_tensor("Wd", [8, 1024, 384], BF16, kind="Internal")
    x1d = nc.dram_tensor("x1d", [NTOK, D_MODEL], F32, kind="Internal")

    with ExitStack() as st:
        P = Prog(nc, st)
        sb = lambda name, shape, dt: st.enter_context(nc.sbuf_tensor(name, shape, dt))
        ps = lambda name, shape, dt: st.enter_context(nc.psum_tensor(name, shape, dt))
        out_bufs = []

        Sps = [ps("Sps0", [128, 512], F32), ps("Sps1", [128, 512], F32)]
        B_S = [Buf(), Buf()]
        Ops = [ps("Ops0", [128, 4, 256], F32), ps("Ops1", [128, 4, 256], F32)]
        B_O = [Buf(), Buf()]
        pj = ps("pj", [128, 512], F32); B_pj = Buf()
        ptr = ps("ptr", [128, 1024], BF16); B_ptr = Buf(); B_ptr1 = Buf()

        ident_f = sb("ident_f", [128, 128], F32); B_idf = Buf()
        ident_b = sb("ident_b", [128, 128], BF16); B_idb = Buf()
        wg = [sb("wg0", [128, 8, 384], BF16), sb("wg1", [128, 8, 384], BF16)]; B_wg = [Buf(), Buf()]
        B_Wd = Buf()
        woutb = sb("woutb", [128, 8, 1024], BF16); B_wout = Buf()
        stage = [sb("stage0", [128, 1536], F32), sb("stage1", [128, 1536], F32)]
        B_stage = [Buf(), Buf()]
        g1c = sb("g1c", [128, 8], F32); B_g1 = Buf()
        xT = sb("xT", [128, 8, 2048], BF16); B_xT = [Buf() for _ in range(NT)]
        yT = sb("yT", [128, 8, 2048], BF16); B_yT = [[Buf() for _ in range(4)] for _ in range(8)]
        qTa = sb("qTa", [128, 2, 2048], BF16); B_qT = [[Buf() for _ in range(NT)] for _ in range(2)]
        kTa = sb("kTa", [128, 2, 2048], BF16); B_kT = [Buf(), Buf()]
        B_qaugrows = Buf()
        Vt = sb("Vt", [128, 16, 136], BF16); B_V = Buf()
        G = [sb("G0", [128, 2560], F32), sb("G1", [128, 2560], F32)]; B_G = [Buf(), Buf()]
        junk = sb("junk", [128, 1024], BF16); B_junk = Buf()
        xb = sb("xb", [128, 1024], BF16); B_xb = Buf()
        ss1 = sb("ss1", [128, 16], F32); B_ss1 = Buf()
        rstd1 = sb("rstd1", [128, 16], F32); B_rstd1 = Buf()
        qk2 = [sb("qk", [128, 256], F32), sb("qkB", [128, 256], F32)]; B_qk2 = [Buf(), Buf()]
        sq2b = [sb("sq", [128, 256], F32), sb("sqB", [128, 256], F32)]; B_sq2b = [Buf(), Buf()]
        ssh2 = [sb("ssh", [128, 4], F32), sb("sshB", [128, 4], F32)]; B_ssh2 = [Buf(), Buf()]
        rsh2 = [sb("rsh", [128, 4], F32), sb("rshB", [128, 4], F32)]; B_rsh2 = [Buf(), Buf()]
        qkn2 = [sb("qkn", [128, 256], BF16), sb("qknB", [128, 256], BF16)]; B_qkn2 = [Buf(), Buf()]
        E = [sb("E0", [128, 512], F32), sb("E1", [128, 512], F32), sb("E2", [128, 512], F32)]; B_E = [Buf(), Buf(), Buf()]
        PT = [sb(f"PT{i}", [128, 512], BF16) for i in range(4)]; B_PT = [Buf() for _ in range(4)]
        rr = sb("rr", [128, 4], F32); B_rr = Buf()
        nlr = sb("nlr", [128, 4], F32); B_nlr = Buf()
        tmp1 = sb("tmp1", [128, 4, 128], F32); B_tmp1 = Buf()
        yd = sb("yd", [128, 4, 128], F32); B_yd = Buf()
        sq2 = sb("sq2", [128, 4, 128], F32); B_sq2 = Buf()
        ss2 = sb("ss2", [128, 4], F32); B_ss2 = Buf()
        rs2 = sb("rs2", [128, 4], F32); B_rs2 = Buf()
        ytok = sb("ytok", [128, 4, 128], BF16); B_ytok = Buf()
        sgb = sb("sgb", [128, 128], F32); B_sgb = Buf()
        g2b = sb("g2b", [128, 1024], F32); B_g2b = Buf()
        lamv = sb("lamv", [128, 4, 64], F32); B_lamv = Buf()
        lamp = sb("lamp", [128, 2, 64], F32); B_lamp = Buf()
        lams = sb("lams", [128, 4], F32); B_lams = Buf()
        gb = sb("gb", [128, 4, 64], F32); B_gq = Buf()
        gqk = sb("gqk", [128, 2, 64], F32); B_gqk = Buf()
        rb = sb("rb", [32, 12], F32); B_rb = Buf()
        oh = G[0][0:32, 0:2048]; B_oh = B_G[0]
        et = G[1][0:12, 0:2560]; B_et = B_G[1]
        B_Bd = Buf()
        B_x1d = Buf()
        km = sb("km", [64, 16], F32); B_km = Buf()
        kmf = sb("kmf", [64, 16], F32); B_kmf = Buf()
        kmhf = sb("kmhf", [64, 16], F32); B_kmhf = Buf()
        kmhl = sb("kmhl", [64, 2, 16], BF16); B_kmhl = Buf()
        g16 = sb("g16", [128, 16], F32); B_g16 = Buf()
        gate = sb("gate", [128, 8], F32); B_gate = Buf()
        mx8 = sb("mx8", [128, 8], F32); B_mx8 = Buf()
        selm = sb("selm", [128, 8], F32); B_selm = Buf()
        qaug = sb("qaug", [128, 128], BF16); B_qaug = Buf()

        P.op("pool", lambda e: e.memset(ident_f[:], 1.0), writes=[B_idf])
        P.op("pool", lambda e: e.affine_select(out=ident_f[:], in_=ident_f[:], pattern=[[-1, 128]],
                                               compare_op=ALU.is_equal, fill=0.0, base=0, channel_multiplier=1),
             reads=[B_idf], writes=[B_idf])
        P.op("dve", lambda e: e.tensor_copy(out=ident_b[:], in_=ident_f[:]), reads=[B_idf], writes=[B_idb])

        P.dma("sp", "dpar", lambda e: e.dma_start(out=g1c[:], in_=norm1_g.ap().rearrange("o (k p) -> p (o k)", p=128)),
              writes=[B_g1])
        w_in_v = w_in.ap().rearrange("o (k p) n -> p (o k) n", p=128)
        B_wst = [Buf(), Buf()]
        for kc in range(8):
            for T in range(2):
                s = T
                P.dma("sp", f"dst{s}", lambda e, kc=kc, s=s, T=T: e.dma_start(out=stage[s][:], in_=w_in_v[:, kc, T * 1536:(T + 1) * 1536]),
                      writes=[B_stage[s]])
                P.op("dve" if s == 0 else "pool",
                     lambda e, kc=kc, s=s: e.tensor_scalar_mul(out=qTa[:, s, 0:1536], in0=stage[s][:], scalar1=g1c[:, kc:kc + 1]),
                     reads=[B_stage[s], B_g1], writes=[B_wst[s]])
                for gg in range(4):
                    for seg in range(3):
                        P.dma("sp", f"dws{s}", lambda e, kc=kc, s=s, T=T, gg=gg, seg=seg: e.dma_start(
                            out=Wd.ap()[T * 4 + gg, kc * 128:(kc + 1) * 128, seg * 128:(seg + 1) * 128],
                            in_=qTa[:, s, seg * 512 + gg * 128: seg * 512 + gg * 128 + 128]),
                            reads=[B_wst[s]], writes=[B_Wd])
        w_out_v = w_out.ap().rearrange("o (k p) n -> p (o k) n", p=128)
        P.dma("pool", "dwo", lambda e: e.dma_start(out=woutb[:], in_=w_out_v), writes=[B_wout])

        P.dma("sp", "dpar", lambda e: e.dma_start(out=rb[:], in_=rel_bias.ap()), writes=[B_rb])
        P.dma("sp", "dpar", lambda e: e.dma_start(out=oh, in_=c_onehot.ap()), writes=[B_oh])
        P.op("pool", lambda e: e.memset(et[:, 0:512], 0.0), writes=[B_et])
        for c in range(4):
            P.op("pe", lambda e, c=c: e.matmul(out=pj[0:12, :], lhsT=rb[:, :], rhs=oh[:, c * 512:(c + 1) * 512],
                                               start=True, stop=True), reads=[B_rb, B_oh], writes=[B_pj])
            P.op("act", lambda e, c=c: e.activation(out=et[:, 512 + c * 512:512 + (c + 1) * 512], in_=pj[0:12, :], func=AF.Exp),
                 reads=[B_pj], writes=[B_et])
        et_ap = et
        esrc = bass.AP(tensor=et_ap.tensor, offset=et_ap.offset, ap=[list(et_ap.ap[0]), [0, 129], [1, 2560]])
        P.dma("sp", "dbd", lambda e: e.dma_start(out=Bd.ap(), in_=esrc), reads=[B_et], writes=[B_Bd])

        for i, t in enumerate([diff_q_g, diff_k_g, moba_q_g, moba_k_g]):
            P.dma("sp", "dpar", lambda e, i=i, t=t: e.dma_start(out=gb[:, i, :], in_=bass.AP(tensor=t, offset=0, ap=[[0, 128], [1, 64]])),
                  writes=[B_gq])
        P.op("dve", lambda e: e.tensor_tensor(out=gqk[:, 0, :], in0=gb[:, 0, :], in1=gb[:, 1, :], op=ALU.mult), reads=[B_gq], writes=[B_gqk])
        P.op("dve", lambda e: e.tensor_tensor(out=gqk[:, 1, :], in0=gb[:, 2, :], in1=gb[:, 3, :], op=ALU.mult), reads=[B_gq], writes=[B_gqk])
        P.dma("sp", "dpar", lambda e: e.dma_start(out=sgb[:], in_=bass.AP(tensor=diff_sub_g, offset=0, ap=[[0, 128], [1, 128]])),
              writes=[B_sgb])
        P.op("dve", lambda e: e.tensor_scalar_mul(out=sgb[:], in0=sgb[:], scalar1=float(1.0 - LAMBDA_INIT)), reads=[B_sgb], writes=[B_sgb])
        P.dma("sp", "dpar", lambda e: e.dma_start(out=g2b[:], in_=bass.AP(tensor=norm2_g, offset=0, ap=[[0, 128], [1, 1024]])),
              writes=[B_g2b])
        for i, t in enumerate([lambda_q1, lambda_k1, lambda_q2, lambda_k2]):
            P.dma("sp", "dpar", lambda e, i=i, t=t: e.dma_start(out=lamv[:, i, :], in_=bass.AP(tensor=t, offset=0, ap=[[0, 128], [1, 64]])),
                  writes=[B_lamv])
        P.op("dve", lambda e: e.tensor_tensor(out=lamp[:, 0, :], in0=lamv[:, 0, :], in1=lamv[:, 1, :], op=ALU.mult), reads=[B_lamv], writes=[B_lamp])
        P.op("dve", lambda e: e.tensor_tensor(out=lamp[:, 1, :], in0=lamv[:, 2, :], in1=lamv[:, 3, :], op=ALU.mult), reads=[B_lamv], writes=[B_lamp])
        P.op("dve", lambda e: e.tensor_reduce(out=lams[:, 0:2], in_=lamp[:], axis=AX.X, op=ALU.add), reads=[B_lamp], writes=[B_lams])
        P.op("act", lambda e: e.activation(out=lams[:, 0:2], in_=lams[:, 0:2], func=AF.Exp), reads=[B_lams], writes=[B_lams])
        P.op("dve", lambda e: e.tensor_sub(out=lams[:, 2:3], in0=lams[:, 0:1], in1=lams[:, 1:2]), reads=[B_lams], writes=[B_lams])
        P.op("dve", lambda e: e.tensor_scalar(out=lams[:, 3:4], in0=lams[:, 2:3], scalar1=float(LAMBDA_INIT), scalar2=-1.0,
                                              op0=ALU.add, op1=ALU.mult), reads=[B_lams], writes=[B_lams])
        nlam = lams[:, 3:4]

        P.op("pool", lambda e: e.memset(Vt[:], 0.0), writes=[B_V])
        P.op("pool", lambda e: e.memset(Vt[:, :, 64:65], 1.0), writes=[B_V])
        P.op("pool", lambda e: e.memset(Vt[:, :, 132:133], 1.0), writes=[B_V])
        P.op("pool", lambda e: e.memset(qaug[:], 0.0), writes=[B_qaug])
        P.op("pool", lambda e: e.memset(qTa[64:128, :, :], 0.0), writes=[B_qaugrows, B_wst[0], B_wst[1]])
        P.op("pool", lambda e: e.memset(kTa[64:128, :, :], 0.0), writes=[B_kT[0], B_kT[1]])
        for m in range(2):
            P.dma("pool", "dpar", lambda e, m=m: e.dma_start(out=kTa[64:72, m, :], in_=c_blk.ap()), writes=[B_kT[m]])

        def rsqrt_act(dst, src, n_feat, rbufs, wbufs):
            P.op("act", lambda e: e.activation(out=dst, in_=src, func=AF.Ln, bias=float(RMS_EPS), scale=1.0 / n_feat),
                 reads=rbufs, writes=wbufs)
            P.op("act", lambda e: e.activation(out=dst, in_=dst, func=AF.Exp, scale=-0.5), reads=wbufs, writes=wbufs)

        pt_ctr = [0]
        e_ctr = [0]
        o_ctr = [0]
        mul_ctr = [0]
        w_ctr = [0]
        _pass = [0]
        import os as _os
        _STG = int(_os.environ.get('DBG_STAGE', '0'))

        for sq_i in range(nseq if (dbg != 1 and dbg < 50) else 0):
            tok0 = sq_i * S_LEN
            for t in range(NT):
                s = t % 2
                r0 = tok0 + t * 128
                xt = stage[s][:, 0:1024]
                P.dma("sp", f"dst{s}", lambda e, xt=xt, r0=r0: e.dma_start(out=xt, in_=x.ap()[r0:r0 + 128, :]), writes=[B_stage[s]])
                P.op("act", lambda e, xt=xt, t=t: e.activation(out=junk[:], in_=xt, func=AF.Square, accum_out=ss1[:, t:t + 1]),
                     reads=[B_stage[s]], writes=[B_junk, B_ss1])
                P.op("pool", lambda e, xt=xt: e.tensor_copy(out=xb[:], in_=xt), reads=[B_stage[s]], writes=[B_xb])
                for kc in range(8):
                    P.op("pe", lambda e, kc=kc: e.transpose(out=ptr[:, kc * 128:(kc + 1) * 128], in_=xb[:, kc * 128:(kc + 1) * 128],
                                                            identity=ident_b[:]), reads=[B_xb, B_idb], writes=[B_ptr, B_ptr1])
                P.op("dve", lambda e, t=t: e.tensor_copy(out=xT[:, :, t * 128:(t + 1) * 128],
                                                         in_=ptr[:].rearrange("p (k n) -> p k n", k=8)),
                     reads=[B_ptr, B_ptr1], writes=[B_xT[t]])
            rsqrt_act(rstd1[:], ss1[:], 1024.0, [B_ss1], [B_rstd1])
            if dbg == 2:
                break

            for g in range(8):
                is_moba = g >= 4
                qoff = (1536 if is_moba else 0) + (g % 4) * 128
                K = 72 if is_moba else 64
                gcol = 2 if is_moba else 0
                heads = [4 + 2 * (g - 4), 4 + 2 * (g - 4) + 1] if is_moba else [g]
                for gi, h in enumerate(heads):
                    P.dma("sp", f"dG{gi}", lambda e, gi=gi, h=h: e.dma_start(
                        out=G[gi][:, 0:2432], in_=bass.AP(tensor=Bd, offset=h * 129 * 2560 + 128, ap=[[2559, 128], [1, 2432]])),
                        reads=[B_Bd], writes=[B_G[gi]])
                wsl = w_ctr[0] % 2; w_ctr[0] += 1
                P.dma("sp", f"dwg{wsl}", lambda e, wsl=wsl, g=g: e.dma_start(
                    out=wg[wsl][:], in_=Wd.ap()[g].rearrange("(k p) n -> p k n", p=128)), reads=[B_Wd], writes=[B_wg[wsl]])
                for t in range(NT):
                    p = t % 2
                    pjp = [pj, Sps[0]][p]; B_pjp = [B_pj, B_S[0]][p]
                    qk = qk2[p]; B_qk = B_qk2[p]; sq = sq2b[p]; B_sq = B_sq2b[p]
                    ssh = ssh2[p]; B_ssh = B_ssh2[p]; rsh = rsh2[p]; B_rsh = B_rsh2[p]; qkn = qkn2[p]; B_qkn = B_qkn2[p]
                    B_pt = [B_ptr, B_ptr1][p]; pc0 = p * 512
                    for kc in range(8):
                        P.op("pe", lambda e, kc=kc, t=t, wsl=wsl, pjp=pjp: e.matmul(
                            out=pjp[:, 0:384], lhsT=xT[:, kc, t * 128:(t + 1) * 128], rhs=wg[wsl][:, kc, :],
                            start=(kc == 0), stop=(kc == 7)), reads=[B_xT[t], B_wg[wsl]], writes=[B_pjp])
                    P.op("dve", lambda e, t=t, pjp=pjp, qk=qk: e.tensor_scalar_mul(out=qk[:], in0=pjp[:, 0:256], scalar1=rstd1[:, t:t + 1]),
                         reads=[B_pjp, B_rstd1], writes=[B_qk])
                    P.op("dve", lambda e, t=t, pjp=pjp: e.tensor_scalar_mul(
                        out=Vt[:, t, :].rearrange("p (a b) -> p a b", a=2)[:, :, 0:64],
                        in0=pjp[:, 256:384].rearrange("p (a b) -> p a b", a=2), scalar1=rstd1[:, t:t + 1]),
                        reads=[B_pjp, B_rstd1], writes=[B_V])
                    P.op("pool", lambda e, sq=sq, qk=qk: e.tensor_tensor(out=sq[:], in0=qk[:], in1=qk[:], op=ALU.mult), reads=[B_qk], writes=[B_sq])
                    P.op("dve", lambda e, ssh=ssh, sq=sq: e.tensor_reduce(out=ssh[:], in_=sq[:].rearrange("p (a b) -> p a b", a=4), axis=AX.X, op=ALU.add),
                         reads=[B_sq], writes=[B_ssh])
                    rsqrt_act(rsh[:], ssh[:], 64.0, [B_ssh], [B_rsh])
                    for i in range(2):
                        P.op("dve", lambda e, i=i, qkn=qkn, qk=qk, rsh=rsh: e.tensor_scalar_mul(
                            out=qkn[:, i * 64:(i + 1) * 64], in0=qk[:, i * 64:(i + 1) * 64], scalar1=rsh[:, i:i + 1]),
                            reads=[B_qk, B_rsh], writes=[B_qkn])
                    for i in range(2, 4):
                        P.op("dve", lambda e, i=i, gi2=(1 if is_moba else 0), qkn=qkn, qk=qk, rsh=rsh: e.scalar_tensor_tensor(
                            out=qkn[:, i * 64:(i + 1) * 64], in0=qk[:, i * 64:(i + 1) * 64], scalar=rsh[:, i:i + 1],
                            in1=gqk[:, gi2, :], op0=ALU.mult, op1=ALU.mult), reads=[B_qk, B_rsh, B_gqk], writes=[B_qkn])
                    for i in range(4):
                        P.op("pe", lambda e, i=i, qkn=qkn, pc0=pc0: e.transpose(
                            out=ptr[0:64, pc0 + i * 128:pc0 + (i + 1) * 128], in_=qkn[:, i * 64:(i + 1) * 64],
                            identity=ident_b[:]), reads=[B_qkn, B_idb], writes=[B_pt])
                    P.op("act", lambda e, t=t, pc0=pc0: e.copy(out=qTa[0:64, :, t * 128:(t + 1) * 128],
                                                              in_=ptr[0:64, pc0:pc0 + 256].rearrange("p (a b) -> p a b", a=2)),
                         reads=[B_pt], writes=[B_qT[0][t], B_qT[1][t]])
                    P.op("act", lambda e, t=t, pc0=pc0: e.copy(out=kTa[0:64, :, t * 128:(t + 1) * 128],
                                                              in_=ptr[0:64, pc0 + 256:pc0 + 512].rearrange("p (a b) -> p a b", a=2)),
                         reads=[B_pt], writes=[B_kT[0], B_kT[1]])
                if dbg in (3, 30, 305, 31, 32, 33, 34, 35, 36, 37, 38, 39):
                    break
                if is_moba:
                    P.op("dve", lambda e: e.tensor_reduce(out=km[:], in_=kTa[0:64, :, :].rearrange("p m (a b) -> p (m a) b", a=8),
                                                          axis=AX.X, op=ALU.add), reads=[B_kT[0], B_kT[1]], writes=[B_km])
                    P.op("dve", lambda e: e.tensor_scalar_mul(out=kmf[:], in0=km[:], scalar1=1.0 / 256), reads=[B_km], writes=[B_kmf])
                    kview = kmhl[:, :, 0:8]
                    P.op("dve", lambda e: e.tensor_copy(out=kview, in_=kmf[:].rearrange("p (m a) -> p m a", m=2)),
                         reads=[B_kmf], writes=[B_kmhl])
                    P.op("dve", lambda e: e.tensor_copy(out=kmhf[:].rearrange("p (m a) -> p m a", m=2), in_=kview),
                         reads=[B_kmhl], writes=[B_kmhf])
                    P.op("dve", lambda e: e.tensor_sub(out=kmhl[:, :, 8:16], in0=kmf[:].rearrange("p (m a) -> p m a", m=2),
                                                       in1=kmhf[:].rearrange("p (m a) -> p m a", m=2)),
                         reads=[B_kmf, B_kmhf], writes=[B_kmhl])
                    for m in range(2):
                        for t in range(8, NT):
                            own = t // 2
                            P.op("pe", lambda e, m=m, t=t: e.matmul(out=pj[:, 0:16], lhsT=qTa[0:64, m, t * 128:(t + 1) * 128],
                                                                    rhs=kmhl[0:64, m, :], start=True, stop=True),
                                 reads=[B_qT[m][t], B_kmhl], writes=[B_pj])
                            P.op("act", lambda e: e.copy(out=g16[:], in_=pj[:, 0:16]), reads=[B_pj], writes=[B_g16])
                            P.op("pool", lambda e: e.memset(gate[:], -1e30), writes=[B_gate])
                            P.op("dve", lambda e, own=own: e.tensor_tensor(out=gate[:, 0:own], in0=g16[:, 0:own], in1=g16[:, 8:8 + own],
                                                                           op=ALU.add), reads=[B_g16], writes=[B_gate])
                            P.op("dve", lambda e: e.max(out=mx8[:], in_=gate[:]), reads=[B_gate], writes=[B_mx8])
                            P.op("dve", lambda e, own=own: e.tensor_scalar(out=selm[:, 0:own], in0=gate[:, 0:own], scalar1=mx8[:, 2:3],
                                                                           scalar2=None, op0=ALU.is_ge), reads=[B_gate, B_mx8], writes=[B_selm])
                            P.op("pool", lambda e: e.memset(qaug[:, 64:72], 0.0), writes=[B_qaug])
                            P.op("dve", lambda e, own=own: e.tensor_scalar(out=qaug[:, 64:64 + own], in0=selm[:, 0:own], scalar1=-1.0,
                                                                           scalar2=NEG_BIG, op0=ALU.add, op1=ALU.mult),
                                 reads=[B_selm], writes=[B_qaug])
                            P.op("pe", lambda e: e.transpose(out=ptr[:, 0:128], in_=qaug[:], identity=ident_b[:]),
                                 reads=[B_qaug, B_idb], writes=[B_ptr])
                            P.op("act", lambda e, m=m, t=t: e.copy(out=qTa[64:72, m, t * 128:(t + 1) * 128], in_=ptr[64:72, 0:128]),
                                 reads=[B_ptr], writes=[B_qT[m][t]])
                steps = []
                for qc in range(4):
                    for m in range(2):
                        for kt in range(4 * qc + 4):
                            steps.append((qc, m, kt))
                st_info = {}

                def emit_S(idx, K=K, is_moba=is_moba, steps=steps, st_info=st_info):
                    qc, m, kt = steps[idx]
                    if kt == 0:
                        st_info[(qc, m)] = o_ctr[0] % 2; o_ctr[0] += 1
                    i = e_ctr[0] % 3; e_ctr[0] += 1
                    j = pt_ctr[0] % 4; pt_ctr[0] += 1
                    gi = m if is_moba else 0
                    Sb = [Sps[0], Sps[1], pj][i]; B_Sb = [B_S[0], B_S[1], B_pj][i]
                    P.op("pe", lambda e: e.matmul(
                        out=Sb[:], lhsT=kTa[0:K, m, kt * 128:(kt + 1) * 128], rhs=qTa[0:K, m, qc * 512:(qc + 1) * 512],
                        start=True, stop=True),
                        reads=[B_kT[m]] + [B_qT[m][qc * 4 + u] for u in range(4)] + [B_qaugrows], writes=[B_Sb])
                    P.op("act", lambda e: e.activation(out=E[i][:], in_=Sb[:], func=AF.Exp, scale=HEAD_DIM ** -0.5),
                         reads=[B_Sb], writes=[B_E[i]])
                    c0 = (4 * qc - kt + 3) * 128
                    meng = "pool" if (mul_ctr[0] % 3 == 2) else "dve"; mul_ctr[0] += 1
                    P.op(meng, lambda e: e.tensor_tensor(out=PT[j][:], in0=E[i][:], in1=G[gi][:, c0:c0 + 512], op=ALU.mult),
                         reads=[B_E[i], B_G[gi]], writes=[B_PT[j]])
                    return j

                def emit_PV(idx, j, is_moba=is_moba, steps=steps, st_info=st_info):
                    qc, m, kt = steps[idx]
                    o = st_info[(qc, m)]
                    if is_moba:
                        v0, vw = m * 68, 65
                    else:
                        v0, vw = 0, 133
                    for jq in range(4):
                        if kt > 4 * qc + jq:
                            continue
                        P.op("pe", lambda e, jq=jq: e.matmul(
                            out=Ops[o][:, jq, 0:vw], lhsT=PT[j][:, jq * 128:(jq + 1) * 128], rhs=Vt[:, kt, v0:v0 + vw],
                            start=(kt == 0 and jq in (0, 2)), stop=(kt == 4 * qc + jq), skip_group_check=True),
                            reads=[B_PT[j], B_V], writes=[B_O[o]])
                    if kt == 4 * qc + 3:
                        finalize(qc, m, o)

                def finalize(qc, m, o, g=g, is_moba=is_moba):
                    scol = 132 if not is_moba else 64
                    P.op("dve", lambda e: e.reciprocal(out=rr[:], in_=Ops[o][:, :, scol:scol + 1].rearrange("p a b -> p (a b)")),
                         reads=[B_O[o]], writes=[B_rr])
                    if not is_moba:
                        if m == 0:
                            for jq in range(4):
                                P.op("dve", lambda e, jq=jq: e.tensor_scalar_mul(
                                    out=tmp1[:, jq, :].rearrange("p (a b) -> p a b", a=2),
                                    in0=Ops[o][:, jq, 0:136].rearrange("p (a b) -> p a b", a=2)[:, :, 0:64],
                                    scalar1=rr[:, jq:jq + 1]), reads=[B_O[o], B_rr], writes=[B_tmp1])
                        else:
                            P.op("dve", lambda e: e.tensor_scalar_mul(out=nlr[:], in0=rr[:], scalar1=nlam), reads=[B_rr, B_lams], writes=[B_nlr])
                            for jq in range(4):
                                P.op("dve", lambda e, jq=jq: e.scalar_tensor_tensor(
                                    out=yd[:, jq, :].rearrange("p (a b) -> p a b", a=2),
                                    in0=Ops[o][:, jq, 0:136].rearrange("p (a b) -> p a b", a=2)[:, :, 0:64],
                                    scalar=nlr[:, jq:jq + 1], in1=tmp1[:, jq, :].rearrange("p (a b) -> p a b", a=2),
                                    op0=ALU.mult, op1=ALU.add), reads=[B_O[o], B_nlr, B_tmp1], writes=[B_yd])
                            P.op("pool", lambda e: e.tensor_tensor(out=sq2[:], in0=yd[:], in1=yd[:], op=ALU.mult), reads=[B_yd], writes=[B_sq2])
                            P.op("dve", lambda e: e.tensor_reduce(out=ss2[:], in_=sq2[:], axis=AX.X, op=ALU.add), reads=[B_sq2], writes=[B_ss2])
                            rsqrt_act(rs2[:], ss2[:], 128.0, [B_ss2], [B_rs2])
                            for jq in range(4):
                                P.op("dve", lambda e, jq=jq: e.scalar_tensor_tensor(
                                    out=ytok[:, jq, :], in0=yd[:, jq, :], scalar=rs2[:, jq:jq + 1], in1=sgb[:],
                                    op0=ALU.mult, op1=ALU.mult), reads=[B_yd, B_rs2, B_sgb], writes=[B_ytok])
                    else:
                        for jq in range(4):
                            P.op("dve", lambda e, jq=jq: e.tensor_scalar_mul(
                                out=ytok[:, jq, m * 64:(m + 1) * 64], in0=Ops[o][:, jq, 0:64], scalar1=rr[:, jq:jq + 1]),
                                reads=[B_O[o], B_rr], writes=[B_ytok])
                    if m == 1:
                        for jq in range(4):
                            P.op("pe", lambda e, jq=jq: e.transpose(out=ptr[:, jq * 128:(jq + 1) * 128], in_=ytok[:, jq, :], identity=ident_b[:]),
                                 reads=[B_ytok, B_idb], writes=[B_ptr])
                        P.op("act", lambda e: e.copy(out=yT[:, g, qc * 512:(qc + 1) * 512], in_=ptr[:, 0:512]),
                             reads=[B_ptr], writes=[B_yT[g][qc]])

                LOOK = 2
                jq_ = []
                for idx in range(min(LOOK, len(steps))):
                    jq_.append(emit_S(idx))
                for idx in range(len(steps)):
                    if idx + LOOK < len(steps):
                        jq_.append(emit_S(idx + LOOK))
                    emit_PV(idx, jq_[idx])
                if dbg == 4:
                    break
            if dbg in (3, 4, 30, 305, 31, 32, 33, 34, 35, 36, 37, 38, 39):
                break
            for t in range(NT):
                s = t % 2
                r0 = tok0 + t * 128
                xt = stage[s][:, 0:1024]
                P.dma("sp", f"dst{s}", lambda e, xt=xt, r0=r0: e.dma_start(out=xt, in_=x.ap()[r0:r0 + 128, :]), writes=[B_stage[s]])
                for half in range(2):
                    for kc in range(8):
                        P.op("pe", lambda e, half=half, kc=kc, t=t: e.matmul(
                            out=Sps[half][:], lhsT=yT[:, kc, t * 128:(t + 1) * 128], rhs=woutb[:, kc, half * 512:(half + 1) * 512],
                            start=(kc == 0), stop=(kc == 7)), reads=[B_yT[kc][t // 4], B_wout], writes=[B_S[half]])
                    P.op("dve", lambda e, half=half, xt=xt: e.tensor_tensor(
                        out=xt[:, half * 512:(half + 1) * 512], in0=Sps[half][:], in1=xt[:, half * 512:(half + 1) * 512], op=ALU.add),
                        reads=[B_S[half], B_stage[s]], writes=[B_stage[s]])
                if not stop_after_attn:
                    P.dma("sp", f"dout{s}", lambda e, xt=xt, r0=r0: e.dma_start(out=x1d.ap()[r0:r0 + 128, :], in_=xt),
                          reads=[B_stage[s]], writes=[B_x1d])
                if stop_after_attn:
                    ob = Buf()
                    P.dma("sp", f"dout{s}", lambda e, xt=xt, r0=r0: e.dma_start(out=out.ap()[r0:r0 + 128, :], in_=xt), reads=[B_stage[s]], writes=[ob])
                    out_bufs.append(ob)


        if not stop_after_attn and (dbg == 0 or dbg >= 50):
            P.barrier()
            acc = xT[:].rearrange("p k n -> p (k n)").bitcast(F32).rearrange("p (t n) -> p t n", t=8)
            h2Tb = yT
            Wgu = [yT[:, :, 1024:1536], yT[:, :, 1536:2048]]
            Wdn = [qTa[:, :, 0:1024], qTa[:, :, 1024:2048]]
            wst = [G[0][:, 0:2048], G[1][:, 0:2048]]
            h2f = stage[0][:, 0:1024]
            h2Tf = stage[1][:, 0:1024]
            sgs = [E[0][:, 0:256], E[0][:, 256:512]]
            hids = [PT[0][:, 0:256], PT[0][:, 256:512]]
            hidTs = [PT[1][:, 0:256], PT[1][:, 256:512]]
            Bsg = [Buf(), Buf()]; Bhid = [Buf(), Buf()]; BhidT = [Buf(), Buf()]; Bptr2 = [Buf(), Buf()]
            Rw = sb("Rw", [128, 8, 36], F32)
            lg = sb("lg", [128, 36], F32)
            wfull = sb("wfull", [128, 8, 32], F32)
            rt = sb("rt", [128, 64], F32)
            Bm = {k: Buf(k) for k in ["acc", "h2Tb", "wgu0", "wgu1", "wd0", "wd1", "wst0", "wst1", "h2f", "h2Tf", "sg", "hid",
                                      "hidT", "Rw", "lg", "wfull", "rt", "S0", "S1", "O0", "O1", "ptr", "pj", "junk", "g2b", "idb", "idf", "x1d"]}
            P.dma("sp", "dpar", lambda e: e.dma_start(out=Rw[:, :, 0:4], in_=router_group.ap().rearrange("o (k p) c -> p (o k) c", p=128)),
                  writes=[Bm["Rw"]])
            for gx in range(4):
                P.dma("sp", "dpar", lambda e, gx=gx: e.dma_start(out=Rw[:, :, 4 + 8 * gx:12 + 8 * gx],
                                                                 in_=router_expert.ap()[0, gx].rearrange("(k p) c -> p k c", p=128)),
                      writes=[Bm["Rw"]])
            Opf = [Ops[0][:].rearrange("p a b -> p (a b)"), Ops[1][:].rearrange("p a b -> p (a b)")]
            BO = [Bm["O0"], Bm["O1"]]
            BS = [Bm["S0"], Bm["S1"]]
            Bwgu = [Bm["wgu0"], Bm["wgu1"]]
            Bwd = [Bm["wd0"], Bm["wd1"]]
            Bwst = [Bm["wst0"], Bm["wst1"]]
            st_ctr = [0]
            s_ctr = [0]
            o_ctr2 = [0]
            NSB = NTOK // 1024
            for sbk in range(NSB):
                for ti in range(8):
                    if dbg == 50 and ti == 1:
                        break
                    r0 = sbk * 1024 + ti * 128
                    if dbg == 50 and _STG == 1:
                        break
                    P.dma("sp", f"dx1_{ti}", lambda e, ti=ti, r0=r0: e.dma_start(out=acc[:, ti, :], in_=x1d.ap()[r0:r0 + 128, :]),
                          reads=[Bm["x1d"]], writes=[Bm["acc"]])
                    if dbg == 50 and _STG == 2:
                        break
                    P.op("act", lambda e, ti=ti: e.activation(out=junk[:], in_=acc[:, ti, :], func=AF.Square, accum_out=rt[:, 0:1]),
                         reads=[Bm["acc"]], writes=[Bm["junk"], Bm["rt"]])
                    if dbg == 50 and _STG == 3:
                        break
                    P.op("act", lambda e: e.activation(out=rt[:, 1:2], in_=rt[:, 0:1], func=AF.Ln, bias=float(RMS_EPS), scale=1.0 / 1024),
                         reads=[Bm["rt"]], writes=[Bm["rt"]])
                    if dbg == 50 and _STG == 4:
                        break
                    P.op("act", lambda e: e.activation(out=rt[:, 1:2], in_=rt[:, 1:2], func=AF.Exp, scale=-0.5), reads=[Bm["rt"]], writes=[Bm["rt"]])
                    if dbg == 50 and _STG == 5:
                        break
                    P.op("dve", lambda e, ti=ti: e.scalar_tensor_tensor(out=h2f, in0=acc[:, ti, :], scalar=rt[:, 1:2], in1=g2b[:],
                                                                         op0=ALU.mult, op1=ALU.mult),
                         reads=[Bm["acc"], Bm["rt"], Bm["g2b"]], writes=[Bm["h2f"]])
                    if dbg == 50 and _STG == 6:
                        break
                    for kc in range(8):
                        P.op("pe", lambda e, kc=kc: e.transpose(out=Opf[1][:, kc * 128:(kc + 1) * 128], in_=h2f[:, kc * 128:(kc + 1) * 128],
                                                                identity=ident_f[:]), reads=[Bm["h2f"], Bm["idf"]], writes=[BO[1]])
                    if dbg == 50 and _STG == 7:
                        break
                    P.op("act", lambda e: e.copy(out=h2Tf, in_=Opf[1]), reads=[BO[1]], writes=[Bm["h2Tf"]])
                    if dbg == 50 and _STG == 8:
                        break
                    P.op("dve", lambda e, ti=ti: e.tensor_copy(out=h2Tb[:, :, ti * 128:(ti + 1) * 128],
                                                               in_=h2Tf.rearrange("p (k n) -> p k n", k=8)),
                         reads=[Bm["h2Tf"]], writes=[Bm["h2Tb"]])
                    if dbg == 50 and _STG == 9:
                        break
                    for kc in range(8):
                        P.op("pe", lambda e, kc=kc: e.matmul(out=pj[:, 0:36], lhsT=h2Tf[:, kc * 128:(kc + 1) * 128], rhs=Rw[:, kc, :],
                                                             start=(kc == 0), stop=(kc == 7)), reads=[Bm["h2Tf"], Bm["Rw"]], writes=[Bm["pj"]])
                    if dbg == 50 and _STG == 10:
                        break
                    P.op("act", lambda e: e.copy(out=lg[:], in_=pj[:, 0:36]), reads=[Bm["pj"]], writes=[Bm["lg"]])
                    R_ = [Bm["rt"]]; L_ = [Bm["lg"]]
                    if dbg == 50 and _STG == 11:
                        break
                    P.op("dve", lambda e: e.tensor_reduce(out=rt[:, 2:3], in_=lg[:, 0:4], axis=AX.X, op=ALU.max), reads=L_, writes=R_)
                    if dbg == 50 and _STG == 12:
                        break
                    P.op("dve", lambda e: e.tensor_scalar(out=rt[:, 8:12], in0=lg[:, 0:4], scalar1=rt[:, 2:3], scalar2=None, op0=ALU.is_equal),
                         reads=L_ + R_, writes=R_)
                    if dbg == 50 and _STG == 13:
                        break
                    P.op("dve", lambda e: e.tensor_scalar_mul(out=rt[:, 3:4], in0=rt[:, 2:3], scalar1=-1.0), reads=R_, writes=R_)
                    if dbg == 50 and _STG == 14:
                        break
                    P.op("dve", lambda e: e.tensor_scalar(out=rt[:, 12:16], in0=lg[:, 0:4], scalar1=rt[:, 2:3], scalar2=None, op0=ALU.subtract),
                         reads=L_ + R_, writes=R_)
                    if dbg == 50 and _STG == 15:
                        break
                    P.op("act", lambda e: e.activation(out=rt[:, 12:16], in_=rt[:, 12:16], func=AF.Exp, accum_out=rt[:, 4:5]),
                         reads=R_, writes=R_)
                    if dbg == 50 and _STG == 16:
                        break
                    P.op("dve", lambda e: e.reciprocal(out=rt[:, 5:6], in_=rt[:, 4:5]), reads=R_, writes=R_)
                    if dbg == 50 and _STG == 17:
                        break
                    P.op("dve", lambda e: e.tensor_scalar_mul(out=rt[:, 16:24], in0=lg[:, 4:12], scalar1=rt[:, 8:9]), reads=L_ + R_, writes=R_)
                    if dbg == 50 and _STG == 18:
                        break
                    for gx in range(1, 4):
                        P.op("dve", lambda e, gx=gx: e.scalar_tensor_tensor(out=rt[:, 16:24], in0=lg[:, 4 + 8 * gx:12 + 8 * gx],
                                                                            scalar=rt[:, 8 + gx:9 + gx], in1=rt[:, 16:24],
                                                                            op0=ALU.mult, op1=ALU.add), reads=L_ + R_, writes=R_)
                    if dbg == 50 and _STG == 19:
                        break
                    P.op("dve", lambda e: e.max(out=rt[:, 24:32], in_=rt[:, 16:24]), reads=R_, writes=R_)
                    if dbg == 50 and _STG == 20:
                        break
                    P.op("dve", lambda e: e.tensor_scalar(out=rt[:, 32:40], in0=rt[:, 16:24], scalar1=rt[:, 24:25], scalar2=None, op0=ALU.is_equal),
                         reads=R_, writes=R_)
                    if dbg == 50 and _STG == 21:
                        break
                    P.op("dve", lambda e: e.tensor_scalar(out=rt[:, 40:48], in0=rt[:, 16:24], scalar1=rt[:, 25:26], scalar2=None, op0=ALU.is_equal),
                         reads=R_, writes=R_)
                    if dbg == 50 and _STG == 22:
                        break
                    P.op("dve", lambda e: e.tensor_sub(out=rt[:, 48:49], in0=rt[:, 25:26], in1=rt[:, 24:25]), reads=R_, writes=R_)
                    if dbg == 50 and _STG == 23:
                        break
                    P.op("act", lambda e: e.activation(out=rt[:, 49:50], in_=rt[:, 48:49], func=AF.Exp), reads=R_, writes=R_)
                    if dbg == 50 and _STG == 24:
                        break
                    P.op("dve", lambda e: e.tensor_scalar_add(out=rt[:, 50:51], in0=rt[:, 49:50], scalar1=1.0), reads=R_, writes=R_)
                    if dbg == 50 and _STG == 25:
                        break
                    P.op("dve", lambda e: e.reciprocal(out=rt[:, 51:52], in_=rt[:, 50:51]), reads=R_, writes=R_)
                    if dbg == 50 and _STG == 26:
                        break
                    P.op("dve", lambda e: e.tensor_tensor(out=rt[:, 52:53], in0=rt[:, 49:50], in1=rt[:, 51:52], op=ALU.mult), reads=R_, writes=R_)
                    if dbg == 50 and _STG == 27:
                        break
                    P.op("dve", lambda e: e.tensor_scalar_mul(out=rt[:, 53:55], in0=rt[:, 51:53], scalar1=rt[:, 5:6]), reads=R_, writes=R_)
                    if dbg == 50 and _STG == 28:
                        break
                    P.op("dve", lambda e: e.tensor_scalar_mul(out=rt[:, 56:64], in0=rt[:, 32:40], scalar1=rt[:, 53:54]), reads=R_, writes=R_)
                    if dbg == 50 and _STG == 29:
                        break
                    P.op("dve", lambda e: e.scalar_tensor_tensor(out=rt[:, 56:64], in0=rt[:, 40:48], scalar=rt[:, 54:55], in1=rt[:, 56:64],
                                                                 op0=ALU.mult, op1=ALU.add), reads=R_, writes=R_)
                    if dbg == 50 and _STG == 30:
                        break
                    for gx in range(4):
                        P.op("dve", lambda e, gx=gx, ti=ti: e.tensor_scalar_mul(out=wfull[:, ti, 8 * gx:8 * gx + 8], in0=rt[:, 56:64],
                                                                                scalar1=rt[:, 8 + gx:9 + gx]), reads=R_, writes=[Bm["wfull"]])
                if dbg in (50, 51):
                    break
                for ex in range(N_EXP if dbg != 52 else 1):
                    slot = ex % 2
                    srcs = [(w_gate.ap()[0, ex].rearrange("(k p) f -> p k f", p=128), Wgu[slot][:, :, 0:256], 8, 256, Bwgu[slot]),
                            (w_up.ap()[0, ex].rearrange("(k p) f -> p k f", p=128), Wgu[slot][:, :, 256:512], 8, 256, Bwgu[slot]),
                            (w_down.ap()[0, ex].rearrange("(c p) n -> p c n", p=128), Wdn[slot], 2, 1024, Bwd[slot])]
                    for src, dst, a_, b_, bdst in srcs:
                        si = st_ctr[0] % 2; st_ctr[0] += 1
                        stv = wst[si].rearrange("p (a b) -> p a b", a=a_)
                        P.dma("sp", f"dwe{si}", lambda e, stv=stv, src=src: e.dma_start(out=stv, in_=src), writes=[Bwst[si]])
                        P.op("pool", lambda e, stv=stv, dst=dst: e.tensor_copy(out=dst, in_=stv), reads=[Bwst[si]], writes=[bdst])
                    def head(ti, i, slot=slot):
                        for kc in range(8):
                            P.op("pe", lambda e, kc=kc: e.matmul(
                                out=Sps[i][:], lhsT=h2Tb[:, kc, ti * 128:(ti + 1) * 128], rhs=Wgu[slot][:, kc, :],
                                start=(kc == 0), stop=(kc == 7)), reads=[Bm["h2Tb"], Bwgu[slot]], writes=[BS[i]])

                    def mid(ti, i, o, b, slot=slot, ex=ex):
                        sg = sgs[b]; hid = hids[b]
                        P.op("act", lambda e: e.activation(out=sg, in_=Sps[i][:, 0:256], func=AF.Silu), reads=[BS[i]], writes=[Bsg[b]])
                        P.op("dve", lambda e: e.scalar_tensor_tensor(
                            out=hid, in0=Sps[i][:, 256:512], scalar=wfull[:, ti, ex:ex + 1], in1=sg, op0=ALU.mult, op1=ALU.mult),
                            reads=[BS[i], Bm["wfull"], Bsg[b]], writes=[Bhid[b]])
                        for c in range(2):
                            P.op("pe", lambda e, c=c: e.transpose(out=ptr[:, b * 256 + c * 128:b * 256 + (c + 1) * 128],
                                                                  in_=hid[:, c * 128:(c + 1) * 128],
                                                                  identity=ident_b[:]), reads=[Bhid[b], Bm["idb"]], writes=[Bptr2[b]])
                        P.op("act", lambda e: e.copy(out=hidTs[b], in_=ptr[:, b * 256:(b + 1) * 256]), reads=[Bptr2[b]], writes=[BhidT[b]])

                    def tail(ti, i, o, b, slot=slot, ex=ex):
                        hidT = hidTs[b]
                        for half in range(2):
                            for c in range(2):
                                P.op("pe", lambda e, half=half, c=c: e.matmul(
                                    out=Opf[o][:, half * 512:(half + 1) * 512], lhsT=hidT[:, c * 128:(c + 1) * 128],
                                    rhs=Wdn[slot][:, c, half * 512:(half + 1) * 512], start=(c == 0), stop=(c == 1)),
                                    reads=[BhidT[b], Bwd[slot]], writes=[BO[o]])
                        P.op("dve", lambda e: e.tensor_tensor(out=acc[:, ti, :], in0=Opf[o], in1=acc[:, ti, :], op=ALU.add),
                             reads=[BO[o], Bm["acc"]], writes=[Bm["acc"]])

                    infos = []
                    for ti in range(8):
                        i = s_ctr[0] % 2; s_ctr[0] += 1
                        o = o_ctr2[0] % 2; o_ctr2[0] += 1
                        infos.append((ti, i, o, ti % 2))
                    for k in range(8 + 2):
                        if k < 8:
                            head(infos[k][0], infos[k][1])
                        if 0 <= k - 1 < 8:
                            mid(*infos[k - 1])
                        if 0 <= k - 2 < 8:
                            tail(*infos[k - 2])
                if dbg == 52:
                    break
                for ti in range(8):
                    r0 = sbk * 1024 + ti * 128
                    ob = Buf()
                    P.dma("sp", "dfin", lambda e, ti=ti, r0=r0: e.dma_start(out=out.ap()[r0:r0 + 128, :], in_=acc[:, ti, :]),
                          reads=[Bm["acc"]], writes=[ob])
                    out_bufs.append(ob)

        P.wait_all("sp", out_bufs)
        P.finish()
        with nc.allow_non_contiguous_dma(reason="small parameter loads"):
            P.emit()
    return nc


_PARAM_KEYS = ["norm1_g", "w_in", "diff_q_g", "diff_k_g", "lambda_q1", "lambda_k1", "lambda_q2", "lambda_k2",
               "diff_sub_g", "moba_q_g", "moba_k_g", "rel_bias", "w_out", "norm2_g", "router_group",
               "router_expert", "w_gate", "w_up", "w_down"]


def run_cores(inputs, nseq, ncores, stop_after_attn=False, dbg=0):
    nc = build(nseq, stop_after_attn=stop_after_attn, dbg=dbg)
    onehot, cmask, blk = _consts()
    xs = np.ascontiguousarray(inputs["x"], dtype=np.float32).reshape(-1, D_MODEL)
    in_maps = []
    for c in range(ncores):
        m = {k: np.ascontiguousarray(inputs[k], dtype=np.float32) for k in _PARAM_KEYS}
        m["x"] = xs[c * nseq * S_LEN:(c + 1) * nseq * S_LEN]
        m["c_onehot"] = onehot; m["c_blk"] = blk
        in_maps.append(m)
    res = run_bass_kernel_spmd(nc, in_maps, core_ids=list(range(ncores)))
    return np.concatenate([np.asarray(r["out"]) for r in res.results], axis=0)


def kernel(**inputs):
    B = inputs["x"].shape[0]
    o = run_cores(inputs, B // 8, 8)
    return o.reshape(B, S_LEN, D_MODEL).astype(np.float32)
```

```python
import math
from contextlib import ExitStack

import numpy as np
import concourse.bass as bass
import concourse.mybir as mybir
from concourse.bass_utils import run_bass_kernel_spmd

F32 = mybir.dt.float32
BF16 = mybir.dt.bfloat16
I32 = mybir.dt.int32
AF = mybir.ActivationFunctionType
ALU = mybir.AluOpType
AX = mybir.AxisListType


class Buf:
    __slots__ = ("w", "r", "name")

    def __init__(self, name=""):
        self.w = None
        self.r = []
        self.name = name


class Prog:
    ENG = ("pe", "act", "dve", "pool", "sp")

    def __init__(self, nc, stack):
        self.nc = nc
        self.stack = stack
        self.ops = {e: [] for e in self.ENG}
        self.sem = {}
        self.cnt = {}
        self.seen = {e: {} for e in self.ENG}
        for e in self.ENG:
            self.newsem(e)

    def newsem(self, name):
        self.sem[name] = self.stack.enter_context(self.nc.semaphore("s_" + name))
        self.cnt[name] = 0

    def _waits(self, eng, reads, writes):
        need = {}

        def add(ev, raw):
            if ev is None:
                return
            sn, v = ev
            if sn == eng:
                if eng == "pe":
                    return
            if v > need.get(sn, 0):
                need[sn] = v

        for b in reads:
            add(b.w, True)
        for b in writes:
            add(b.w, False)
            for ev in b.r:
                add(ev, False)
        out = []
        for sn, v in need.items():
            if self.seen[eng].get(sn, 0) < v:
                self.seen[eng][sn] = v
                out.append((sn, v))
        return out

    def _mark(self, ev, reads, writes):
        for b in reads:
            b.r.append(ev)
        for b in writes:
            b.w = ev
            b.r = []

    def op(self, eng, fn, reads=(), writes=()):
        waits = self._waits(eng, reads, writes)
        self.cnt[eng] += 1
        ev = (eng, self.cnt[eng])
        self.ops[eng].append((fn, waits, (eng, 1)))
        self._mark(ev, reads, writes)
        return ev

    def dma(self, q, semname, fn, reads=(), writes=()):
        if semname == "dpar":
            self._npar = getattr(self, "_npar", 0) + 1
            semname = "dpar%d" % self._npar
        if semname not in self.sem:
            self.newsem(semname)
        waits = self._waits(q, reads, writes)
        self.cnt[semname] += 16
        ev = (semname, self.cnt[semname])
        self.ops[q].append((fn, waits, (semname, 16)))
        self._mark(ev, reads, writes)
        return ev

    def wait_all(self, eng, bufs):
        waits = self._waits(eng, bufs, ())
        self.ops[eng].append((None, waits, None))

    def barrier(self):
        waits = [(sn, v) for sn, v in self.cnt.items() if v > 0]
        for eng in self.ENG:
            w = [(sn, v) for sn, v in waits if self.seen[eng].get(sn, 0) < v and not (sn == eng and eng == "sp")]
            for sn, v in w:
                self.seen[eng][sn] = v
            self.ops[eng].append((None, w, None))

    def finish(self):
        waits = [(sn, v) for sn, v in self.cnt.items() if v > 0]
        self.ops["sp"].append((None, waits, None))

    def emit(self):
        nc = self.nc
        sem = self.sem
        ops = self.ops

        def run(e, name):
            for fn, waits, inc in ops[name]:
                for sn, v in waits:
                    e.wait_ge(sem[sn], v)
                if fn is None:
                    continue
                ins = fn(e)
                ins.then_inc(sem[inc[0]], inc[1])

        with nc.Block() as block:
            @block.tensor
            def _(e):
                run(e, "pe")

            @block.scalar
            def _(e):
                run(e, "act")

            @block.vector
            def _(e):
                run(e, "dve")

            @block.gpsimd
            def _(e):
                run(e, "pool")

            @block.sync
            def _(e):
                run(e, "sp")


S_LEN = 2048
D_MODEL = 1024
NT = 16
HEAD_DIM = 64
RMS_EPS = 1e-6
LAMBDA_INIT = 0.8 - 0.6 * math.exp(-0.3 * 0)
NEG_BIG = 30000.0
N_EXP = 32
MULMOD = 1000000
CAP_CHUNK = 128


def _t5_bucket_np(n):
    n = np.maximum(n, 0)
    max_exact = 16
    nf = np.maximum(n, 1).astype(np.float32)
    large = max_exact + (np.log(nf / np.float32(max_exact)) / np.float32(math.log(2048 / max_exact))
                         * np.float32(32 - max_exact)).astype(np.int32)
    large = np.minimum(large, 31)
    return np.where(n < max_exact, n, large)


def _consts():
    d = np.arange(2048)
    b = _t5_bucket_np(d)
    onehot = np.zeros((32, 2048), np.float32)
    onehot[b, d] = 1.0
    cmask = None
    blk = np.zeros((8, 2048), np.float32)
    for n in range(8):
        blk[n, n * 256:(n + 1) * 256] = 1.0
    return onehot, cmask, blk


def bc_inner(ap, n):
    return bass.AP(tensor=ap.tensor, offset=ap.offset, ap=[list(a) for a in ap.ap] + [[0, n]])


class _Stop(Exception):
    pass


def build(nseq, stop_after_attn=False, dbg=0):
    nc = bass.Bass("TRN2", target_bir_lowering=False)
    NTOK = nseq * S_LEN
    dt_in = lambda name, shape: nc.dram_tensor(name, shape, F32, kind="ExternalInput")
    x = dt_in("x", [NTOK, D_MODEL])
    norm1_g = dt_in("norm1_g", [1, 1024])
    w_in = dt_in("w_in", [1, 1024, 3072])
    diff_q_g = dt_in("diff_q_g", [1, 64]); diff_k_g = dt_in("diff_k_g", [1, 64])
    lambda_q1 = dt_in("lambda_q1", [1, 64]); lambda_k1 = dt_in("lambda_k1", [1, 64])
    lambda_q2 = dt_in("lambda_q2", [1, 64]); lambda_k2 = dt_in("lambda_k2", [1, 64])
    diff_sub_g = dt_in("diff_sub_g", [1, 128])
    moba_q_g = dt_in("moba_q_g", [1, 64]); moba_k_g = dt_in("moba_k_g", [1, 64])
    rel_bias = dt_in("rel_bias", [32, 12])
    w_out = dt_in("w_out", [1, 1024, 1024])
    norm2_g = dt_in("norm2_g", [1, 1024])
    router_group = dt_in("router_group", [1, 1024, 4])
    router_expert = dt_in("router_expert", [1, 4, 1024, 8])
    w_gate = dt_in("w_gate", [1, 32, 1024, 256])
    w_up = dt_in("w_up", [1, 32, 1024, 256])
    w_down = dt_in("w_down", [1, 32, 256, 1024])
    c_onehot = dt_in("c_onehot", [32, 2048])
    c_blk = dt_in("c_blk", [8, 2048])
    out = nc.dram_tensor("out", [NTOK, D_MODEL], F32, kind="ExternalOutput")
    Bd = nc.dram_tensor("Bd", [12, 129, 2560], F32, kind="Internal")
    Wd = nc.dram_tensor("Wd", [8, 1024, 384], BF16, kind="Internal")
    x1d = nc.dram_tensor("x1d", [NTOK, D_MODEL], F32, kind="Internal")

    with ExitStack() as st:
        P = Prog(nc, st)
        sb = lambda name, shape, dt: st.enter_context(nc.sbuf_tensor(name, shape, dt))
        ps = lambda name, shape, dt: st.enter_context(nc.psum_tensor(name, shape, dt))
        out_bufs = []

        Sps = [ps("Sps0", [128, 512], F32), ps("Sps1", [128, 512], F32)]
        B_S = [Buf(), Buf()]
        Ops = [ps("Ops0", [128, 4, 256], F32), ps("Ops1", [128, 4, 256], F32)]
        B_O = [Buf(), Buf()]
        pj = ps("pj", [128, 512], F32); B_pj = Buf()
        ptr = ps("ptr", [128, 1024], BF16); B_ptr = Buf(); B_ptr1 = Buf()

        ident_f = sb("ident_f", [128, 128], F32); B_idf = Buf()
        ident_b = sb("ident_b", [128, 128], BF16); B_idb = Buf()
        wg = [sb("wg0", [128, 8, 384], BF16), sb("wg1", [128, 8, 384], BF16)]; B_wg = [Buf(), Buf()]
        B_Wd = Buf()
        woutb = sb("woutb", [128, 8, 1024], BF16); B_wout = Buf()
        stage = [sb("stage0", [128, 1536], F32), sb("stage1", [128, 1536], F32)]
        B_stage = [Buf(), Buf()]
        g1c = sb("g1c", [128, 8], F32); B_g1 = Buf()
        xT = sb("xT", [128, 8, 2048], BF16); B_xT = [Buf() for _ in range(NT)]
        yT = sb("yT", [128, 8, 2048], BF16); B_yT = [[Buf() for _ in range(4)] for _ in range(8)]
        qTa = sb("qTa", [128, 2, 2048], BF16); B_qT = [[Buf() for _ in range(NT)] for _ in range(2)]
        kTa = sb("kTa", [128, 2, 2048], BF16); B_kT = [Buf(), Buf()]
        B_qaugrows = Buf()
        Vt = sb("Vt", [128, 16, 136], BF16); B_V = Buf()
        G = [sb("G0", [128, 2560], F32), sb("G1", [128, 2560], F32)]; B_G = [Buf(), Buf()]
        junk = sb("junk", [128, 1024], BF16); B_junk = Buf()
        xb = sb("xb", [128, 1024], BF16); B_xb = Buf()
        ss1 = sb("ss1", [128, 16], F32); B_ss1 = Buf()
        rstd1 = sb("rstd1", [128, 16], F32); B_rstd1 = Buf()
        NB4 = 4
        qk2 = [sb(f"bqk{i}", [128, 256], F32) for i in range(NB4)]; B_qk2 = [Buf() for _ in range(NB4)]
        sq2b = [sb(f"bsq{i}", [128, 256], F32) for i in range(NB4)]; B_sq2b = [Buf() for _ in range(NB4)]
        ssh2 = [sb(f"bssh{i}", [128, 4], F32) for i in range(NB4)]; B_ssh2 = [Buf() for _ in range(NB4)]
        rsh2 = [sb(f"brsh{i}", [128, 4], F32) for i in range(NB4)]; B_rsh2 = [Buf() for _ in range(NB4)]
        qkn2 = [sb(f"bqkn{i}", [128, 256], BF16) for i in range(NB4)]; B_qkn2 = [Buf() for _ in range(NB4)]
        E = [sb(f"E{i}", [128, 512], F32) for i in range(4)]; B_E = [Buf() for _ in range(4)]
        PT = [sb(f"PT{i}", [128, 512], BF16) for i in range(5)]; B_PT = [Buf() for _ in range(5)]
        rr = sb("rr", [128, 4], F32); B_rr = Buf()
        nlr = sb("nlr", [128, 4], F32); B_nlr = Buf()
        tmp1 = sb("tmp1", [128, 4, 128], F32); B_tmp1 = Buf()
        yd = sb("yd", [128, 4, 128], F32); B_yd = Buf()
        sq2 = sb("sq2", [128, 4, 128], F32); B_sq2 = Buf()
        ss2 = sb("ss2", [128, 4], F32); B_ss2 = Buf()
        rs2 = sb("rs2", [128, 4], F32); B_rs2 = Buf()
        ytok = sb("ytok", [128, 4, 128], BF16); B_ytok = Buf()
        sgb = sb("sgb", [128, 128], F32); B_sgb = Buf()
        g2b = sb("g2b", [128, 1024], F32); B_g2b = Buf()
        lamv = sb("lamv", [128, 4, 64], F32); B_lamv = Buf()
        lamp = sb("lamp", [128, 2, 64], F32); B_lamp = Buf()
        lams = sb("lams", [128, 4], F32); B_lams = Buf()
        gb = sb("gb", [128, 4, 64], F32); B_gq = Buf()
        gqk = sb("gqk", [128, 2, 64], F32); B_gqk = Buf()
        rb = sb("rb", [32, 12], F32); B_rb = Buf()
        oh = G[0][0:32, 0:2048]; B_oh = B_G[0]
        et = G[1][0:12, 0:2560]; B_et = B_G[1]
        B_Bd = Buf()
        B_x1d = Buf()
        km = sb("km", [64, 16], F32); B_km = Buf()
        kmf = sb("kmf", [64, 16], F32); B_kmf = Buf()
        kmhf = sb("kmhf", [64, 16], F32); B_kmhf = Buf()
        kmhl = sb("kmhl", [64, 2, 16], BF16); B_kmhl = Buf()
        g16 = sb("g16", [128, 16], F32); B_g16 = Buf()
        gate = sb("gate", [128, 8], F32); B_gate = Buf()
        mx8 = sb("mx8", [128, 8], F32); B_mx8 = Buf()
        selm = sb("selm", [128, 8], F32); B_selm = Buf()
        qaug = sb("qaug", [128, 128], BF16); B_qaug = Buf()

        P.op("pool", lambda e: e.memset(ident_f[:], 1.0), writes=[B_idf])
        P.op("pool", lambda e: e.affine_select(out=ident_f[:], in_=ident_f[:], pattern=[[-1, 128]],
                                               compare_op=ALU.is_equal, fill=0.0, base=0, channel_multiplier=1),
             reads=[B_idf], writes=[B_idf])
        P.op("dve", lambda e: e.tensor_copy(out=ident_b[:], in_=ident_f[:]), reads=[B_idf], writes=[B_idb])

        P.dma("sp", "dpar", lambda e: e.dma_start(out=g1c[:], in_=norm1_g.ap().rearrange("o (k p) -> p (o k)", p=128)),
              writes=[B_g1])
        w_in_v = w_in.ap().rearrange("o (k p) n -> p (o k) n", p=128)
        B_wst = [Buf(), Buf()]
        for kc in range(8):
            for T in range(2):
                s = T
                P.dma("sp", f"dst{s}", lambda e, kc=kc, s=s, T=T: e.dma_start(out=stage[s][:], in_=w_in_v[:, kc, T * 1536:(T + 1) * 1536]),
                      writes=[B_stage[s]])
                P.op("dve" if s == 0 else "pool",
                     lambda e, kc=kc, s=s: e.tensor_scalar_mul(out=qTa[:, s, 0:1536], in0=stage[s][:], scalar1=g1c[:, kc:kc + 1]),
                     reads=[B_stage[s], B_g1], writes=[B_wst[s]])
                for gg in range(4):
                    a = qTa[:, s, gg * 128:gg * 128 + 128]
                    src3 = bass.AP(tensor=a.tensor, offset=a.offset, ap=[list(a.ap[0]), [512, 3], [1, 128]])
                    P.dma("act", f"dws{s}", lambda e, kc=kc, T=T, gg=gg, src3=src3: e.dma_start(
                        out=Wd.ap()[T * 4 + gg, kc * 128:(kc + 1) * 128, :].rearrange("p (a b) -> p a b", a=3), in_=src3),
                        reads=[B_wst[s]], writes=[B_Wd])
        w_out_v = w_out.ap().rearrange("o (k p) n -> p (o k) n", p=128)
        P.dma("pool", "dwo", lambda e: e.dma_start(out=woutb[:], in_=w_out_v), writes=[B_wout])

        P.dma("sp", "dpar", lambda e: e.dma_start(out=rb[:], in_=rel_bias.ap()), writes=[B_rb])
        P.dma("sp", "dpar", lambda e: e.dma_start(out=oh, in_=c_onehot.ap()), writes=[B_oh])
        P.op("pool", lambda e: e.memset(et[:, 0:512], 0.0), writes=[B_et])
        for c in range(4):
            P.op("pe", lambda e, c=c: e.matmul(out=pj[0:12, :], lhsT=rb[:, :], rhs=oh[:, c * 512:(c + 1) * 512],
                                               start=True, stop=True), reads=[B_rb, B_oh], writes=[B_pj])
            P.op("act", lambda e, c=c: e.activation(out=et[:, 512 + c * 512:512 + (c + 1) * 512], in_=pj[0:12, :], func=AF.Exp),
                 reads=[B_pj], writes=[B_et])
        et_ap = et
        esrc = bass.AP(tensor=et_ap.tensor, offset=et_ap.offset, ap=[list(et_ap.ap[0]), [0, 129], [1, 2560]])
        P.dma("sp", "dbd", lambda e: e.dma_start(out=Bd.ap(), in_=esrc), reads=[B_et], writes=[B_Bd])

        for i, t in enumerate([diff_q_g, diff_k_g, moba_q_g, moba_k_g]):
            P.dma("sp", "dpar", lambda e, i=i, t=t: e.dma_start(out=gb[:, i, :], in_=bass.AP(tensor=t, offset=0, ap=[[0, 128], [1, 64]])),
                  writes=[B_gq])
        P.op("dve", lambda e: e.tensor_tensor(out=gqk[:, 0, :], in0=gb[:, 0, :], in1=gb[:, 1, :], op=ALU.mult), reads=[B_gq], writes=[B_gqk])
        P.op("dve", lambda e: e.tensor_tensor(out=gqk[:, 1, :], in0=gb[:, 2, :], in1=gb[:, 3, :], op=ALU.mult), reads=[B_gq], writes=[B_gqk])
        P.dma("sp", "dpar", lambda e: e.dma_start(out=sgb[:], in_=bass.AP(tensor=diff_sub_g, offset=0, ap=[[0, 128], [1, 128]])),
              writes=[B_sgb])
        P.op("dve", lambda e: e.tensor_scalar_mul(out=sgb[:], in0=sgb[:], scalar1=float(1.0 - LAMBDA_INIT)), reads=[B_sgb], writes=[B_sgb])
        P.dma("sp", "dpar", lambda e: e.dma_start(out=g2b[:], in_=bass.AP(tensor=norm2_g, offset=0, ap=[[0, 128], [1, 1024]])),
              writes=[B_g2b])
        for i, t in enumerate([lambda_q1, lambda_k1, lambda_q2, lambda_k2]):
            P.dma("sp", "dpar", lambda e, i=i, t=t: e.dma_start(out=lamv[:, i, :], in_=bass.AP(tensor=t, offset=0, ap=[[0, 128], [1, 64]])),
                  writes=[B_lamv])
        P.op("dve", lambda e: e.tensor_tensor(out=lamp[:, 0, :], in0=lamv[:, 0, :], in1=lamv[:, 1, :], op=ALU.mult), reads=[B_lamv], writes=[B_lamp])
        P.op("dve", lambda e: e.tensor_tensor(out=lamp[:, 1, :], in0=lamv[:, 2, :], in1=lamv[:, 3, :], op=ALU.mult), reads=[B_lamv], writes=[B_lamp])
        P.op("dve", lambda e: e.tensor_reduce(out=lams[:, 0:2], in_=lamp[:], axis=AX.X, op=ALU.add), reads=[B_lamp], writes=[B_lams])
        P.op("act", lambda e: e.activation(out=lams[:, 0:2], in_=lams[:, 0:2], func=AF.Exp), reads=[B_lams], writes=[B_lams])
        P.op("dve", lambda e: e.tensor_sub(out=lams[:, 2:3], in0=lams[:, 0:1], in1=lams[:, 1:2]), reads=[B_lams], writes=[B_lams])
        P.op("dve", lambda e: e.tensor_scalar(out=lams[:, 3:4], in0=lams[:, 2:3], scalar1=float(LAMBDA_INIT), scalar2=-1.0,
                                              op0=ALU.add, op1=ALU.mult), reads=[B_lams], writes=[B_lams])
        nlam = lams[:, 3:4]

        P.op("pool", lambda e: e.memset(Vt[:], 0.0), writes=[B_V])
        P.op("pool", lambda e: e.memset(Vt[:, :, 64:65], 1.0), writes=[B_V])
        P.op("pool", lambda e: e.memset(Vt[:, :, 132:133], 1.0), writes=[B_V])
        P.op("pool", lambda e: e.memset(qaug[:], 0.0), writes=[B_qaug])
        P.op("pool", lambda e: e.memset(qTa[64:128, :, :], 0.0), writes=[B_qaugrows, B_wst[0], B_wst[1]])
        P.op("pool", lambda e: e.memset(kTa[64:128, :, :], 0.0), writes=[B_kT[0], B_kT[1]])
        for m in range(2):
            P.dma("pool", "dpar", lambda e, m=m: e.dma_start(out=kTa[64:72, m, :], in_=c_blk.ap()), writes=[B_kT[m]])

        def rsqrt_act(dst, src, n_feat, rbufs, wbufs):
            P.op("act", lambda e: e.activation(out=dst, in_=src, func=AF.Ln, bias=float(RMS_EPS), scale=1.0 / n_feat),
                 reads=rbufs, writes=wbufs)
            P.op("act", lambda e: e.activation(out=dst, in_=dst, func=AF.Exp, scale=-0.5), reads=wbufs, writes=wbufs)

        pt_ctr = [0]
        e_ctr = [0]
        o_ctr = [0]
        mul_ctr = [0]
        w_ctr = [0]
        _pass = [0]
        import os as _os
        _STG = int(_os.environ.get('DBG_STAGE', '0'))

        for sq_i in range(nseq if (dbg != 1 and dbg < 50) else 0):
            tok0 = sq_i * S_LEN
            for t in range(NT):
                s = t % 2
                r0 = tok0 + t * 128
                xt = stage[s][:, 0:1024]
                P.dma("sp", f"dst{s}", lambda e, xt=xt, r0=r0: e.dma_start(out=xt, in_=x.ap()[r0:r0 + 128, :]), writes=[B_stage[s]])
                P.op("act", lambda e, xt=xt, t=t: e.activation(out=junk[:], in_=xt, func=AF.Square, accum_out=ss1[:, t:t + 1]),
                     reads=[B_stage[s]], writes=[B_junk, B_ss1])
                P.op("pool", lambda e, xt=xt: e.tensor_copy(out=xb[:], in_=xt), reads=[B_stage[s]], writes=[B_xb])
                for kc in range(8):
                    P.op("pe", lambda e, kc=kc: e.transpose(out=ptr[:, kc * 128:(kc + 1) * 128], in_=xb[:, kc * 128:(kc + 1) * 128],
                                                            identity=ident_b[:]), reads=[B_xb, B_idb], writes=[B_ptr, B_ptr1])
                P.op("dve", lambda e, t=t: e.tensor_copy(out=xT[:, :, t * 128:(t + 1) * 128],
                                                         in_=ptr[:].rearrange("p (k n) -> p k n", k=8)),
                     reads=[B_ptr, B_ptr1], writes=[B_xT[t]])
            rsqrt_act(rstd1[:], ss1[:], 1024.0, [B_ss1], [B_rstd1])
            if dbg == 2:
                break

            for g in range(8):
                is_moba = g >= 4
                qoff = (1536 if is_moba else 0) + (g % 4) * 128
                K = 72 if is_moba else 64
                gcol = 2 if is_moba else 0
                heads = [4 + 2 * (g - 4), 4 + 2 * (g - 4) + 1] if is_moba else [g]
                for gi, h in enumerate(heads):
                    P.dma("sp", f"dG{gi}", lambda e, gi=gi, h=h: e.dma_start(
                        out=G[gi][:, 0:2432], in_=bass.AP(tensor=Bd, offset=h * 129 * 2560 + 128, ap=[[2559, 128], [1, 2432]])),
                        reads=[B_Bd], writes=[B_G[gi]])
                wsl = w_ctr[0] % 2; w_ctr[0] += 1
                P.dma("sp", f"dwg{wsl}", lambda e, wsl=wsl, g=g: e.dma_start(
                    out=wg[wsl][:], in_=Wd.ap()[g].rearrange("(k p) n -> p k n", p=128)), reads=[B_Wd], writes=[B_wg[wsl]])
                for t in range(NT):
                    p = t % 2
                    p3 = t % 3; p4 = t % 4
                    pjp = [pj, Sps[0], Sps[1]][p3]; B_pjp = [B_pj, B_S[0], B_S[1]][p3]
                    qk = qk2[p4]; B_qk = B_qk2[p4]; sq = sq2b[p4]; B_sq = B_sq2b[p4]
                    ssh = ssh2[p4]; B_ssh = B_ssh2[p4]; rsh = rsh2[p4]; B_rsh = B_rsh2[p4]; qkn = qkn2[p4]; B_qkn = B_qkn2[p4]
                    B_pt = [B_ptr, B_ptr1][p]; pc0 = p * 512
                    for kc in range(8):
                        P.op("pe", lambda e, kc=kc, t=t, wsl=wsl, pjp=pjp: e.matmul(
                            out=pjp[:, 0:384], lhsT=xT[:, kc, t * 128:(t + 1) * 128], rhs=wg[wsl][:, kc, :],
                            start=(kc == 0), stop=(kc == 7)), reads=[B_xT[t], B_wg[wsl]], writes=[B_pjp])
                    P.op("dve", lambda e, t=t, pjp=pjp, qk=qk: e.tensor_scalar_mul(out=qk[:], in0=pjp[:, 0:256], scalar1=rstd1[:, t:t + 1]),
                         reads=[B_pjp, B_rstd1], writes=[B_qk])
                    P.op("dve", lambda e, t=t, pjp=pjp: e.tensor_scalar_mul(
                        out=Vt[:, t, :].rearrange("p (a b) -> p a b", a=2)[:, :, 0:64],
                        in0=pjp[:, 256:384].rearrange("p (a b) -> p a b", a=2), scalar1=rstd1[:, t:t + 1]),
                        reads=[B_pjp, B_rstd1], writes=[B_V])
                    P.op("pool", lambda e, sq=sq, qk=qk: e.tensor_tensor(out=sq[:], in0=qk[:], in1=qk[:], op=ALU.mult), reads=[B_qk], writes=[B_sq])
                    P.op("dve", lambda e, ssh=ssh, sq=sq: e.tensor_reduce(out=ssh[:], in_=sq[:].rearrange("p (a b) -> p a b", a=4), axis=AX.X, op=ALU.add),
                         reads=[B_sq], writes=[B_ssh])
                    rsqrt_act(rsh[:], ssh[:], 64.0, [B_ssh], [B_rsh])
                    for i in range(2):
                        P.op("dve", lambda e, i=i, qkn=qkn, qk=qk, rsh=rsh: e.tensor_scalar_mul(
                            out=qkn[:, i * 64:(i + 1) * 64], in0=qk[:, i * 64:(i + 1) * 64], scalar1=rsh[:, i:i + 1]),
                            reads=[B_qk, B_rsh], writes=[B_qkn])
                    for i in range(2, 4):
                        P.op("dve", lambda e, i=i, gi2=(1 if is_moba else 0), qkn=qkn, qk=qk, rsh=rsh: e.scalar_tensor_tensor(
                            out=qkn[:, i * 64:(i + 1) * 64], in0=qk[:, i * 64:(i + 1) * 64], scalar=rsh[:, i:i + 1],
                            in1=gqk[:, gi2, :], op0=ALU.mult, op1=ALU.mult), reads=[B_qk, B_rsh, B_gqk], writes=[B_qkn])
                    for i in range(4):
                        P.op("pe", lambda e, i=i, qkn=qkn, pc0=pc0: e.transpose(
                            out=ptr[0:64, pc0 + i * 128:pc0 + (i + 1) * 128], in_=qkn[:, i * 64:(i + 1) * 64],
                            identity=ident_b[:]), reads=[B_qkn, B_idb], writes=[B_pt])
                    P.op("act", lambda e, t=t, pc0=pc0: e.copy(out=qTa[0:64, :, t * 128:(t + 1) * 128],
                                                              in_=ptr[0:64, pc0:pc0 + 256].rearrange("p (a b) -> p a b", a=2)),
                         reads=[B_pt], writes=[B_qT[0][t], B_qT[1][t]])
                    P.op("act", lambda e, t=t, pc0=pc0: e.copy(out=kTa[0:64, :, t * 128:(t + 1) * 128],
                                                              in_=ptr[0:64, pc0 + 256:pc0 + 512].rearrange("p (a b) -> p a b", a=2)),
                         reads=[B_pt], writes=[B_kT[0], B_kT[1]])
                if dbg in (3, 30, 305, 31, 32, 33, 34, 35, 36, 37, 38, 39):
                    break
                if is_moba:
                    P.op("dve", lambda e: e.tensor_reduce(out=km[:], in_=kTa[0:64, :, :].rearrange("p m (a b) -> p (m a) b", a=8),
                                                          axis=AX.X, op=ALU.add), reads=[B_kT[0], B_kT[1]], writes=[B_km])
                    P.op("dve", lambda e: e.tensor_scalar_mul(out=kmf[:], in0=km[:], scalar1=1.0 / 256), reads=[B_km], writes=[B_kmf])
                    kview = kmhl[:, :, 0:8]
                    P.op("dve", lambda e: e.tensor_copy(out=kview, in_=kmf[:].rearrange("p (m a) -> p m a", m=2)),
                         reads=[B_kmf], writes=[B_kmhl])
                    P.op("dve", lambda e: e.tensor_copy(out=kmhf[:].rearrange("p (m a) -> p m a", m=2), in_=kview),
                         reads=[B_kmhl], writes=[B_kmhf])
                    P.op("dve", lambda e: e.tensor_sub(out=kmhl[:, :, 8:16], in0=kmf[:].rearrange("p (m a) -> p m a", m=2),
                                                       in1=kmhf[:].rearrange("p (m a) -> p m a", m=2)),
                         reads=[B_kmf, B_kmhf], writes=[B_kmhl])
                    for m in range(2):
                        for t in range(8, NT):
                            own = t // 2
                            P.op("pe", lambda e, m=m, t=t: e.matmul(out=pj[:, 0:16], lhsT=qTa[0:64, m, t * 128:(t + 1) * 128],
                                                                    rhs=kmhl[0:64, m, :], start=True, stop=True),
                                 reads=[B_qT[m][t], B_kmhl], writes=[B_pj])
                            P.op("act", lambda e: e.copy(out=g16[:], in_=pj[:, 0:16]), reads=[B_pj], writes=[B_g16])
                            P.op("pool", lambda e: e.memset(gate[:], -1e30), writes=[B_gate])
                            P.op("dve", lambda e, own=own: e.tensor_tensor(out=gate[:, 0:own], in0=g16[:, 0:own], in1=g16[:, 8:8 + own],
                                                                           op=ALU.add), reads=[B_g16], writes=[B_gate])
                            P.op("dve", lambda e: e.max(out=mx8[:], in_=gate[:]), reads=[B_gate], writes=[B_mx8])
                            P.op("dve", lambda e, own=own: e.tensor_scalar(out=selm[:, 0:own], in0=gate[:, 0:own], scalar1=mx8[:, 2:3],
                                                                           scalar2=None, op0=ALU.is_ge), reads=[B_gate, B_mx8], writes=[B_selm])
                            P.op("pool", lambda e: e.memset(qaug[:, 64:72], 0.0), writes=[B_qaug])
                            P.op("dve", lambda e, own=own: e.tensor_scalar(out=qaug[:, 64:64 + own], in0=selm[:, 0:own], scalar1=-1.0,
                                                                           scalar2=NEG_BIG, op0=ALU.add, op1=ALU.mult),
                                 reads=[B_selm], writes=[B_qaug])
                            P.op("pe", lambda e: e.transpose(out=ptr[:, 0:128], in_=qaug[:], identity=ident_b[:]),
                                 reads=[B_qaug, B_idb], writes=[B_ptr])
                            P.op("act", lambda e, m=m, t=t: e.copy(out=qTa[64:72, m, t * 128:(t + 1) * 128], in_=ptr[64:72, 0:128]),
                                 reads=[B_ptr], writes=[B_qT[m][t]])
                steps = []
                for qc in range(4):
                    for m in range(2):
                        for kt in range(4 * qc + 4):
                            steps.append((qc, m, kt))
                st_info = {}

                def emit_S(idx, K=K, is_moba=is_moba, steps=steps, st_info=st_info):
                    qc, m, kt = steps[idx]
                    if kt == 0:
                        st_info[(qc, m)] = o_ctr[0] % 2; o_ctr[0] += 1
                    i = e_ctr[0] % 4; e_ctr[0] += 1
                    j = pt_ctr[0] % 5; pt_ctr[0] += 1
                    gi = m if is_moba else 0
                    Sb = [Sps[0][:], Sps[1][:], pj[:], ptr[:].bitcast(F32)][i]
                    B_Sb = [[B_S[0]], [B_S[1]], [B_pj], [B_ptr, B_ptr1]][i]
                    P.op("pe", lambda e: e.matmul(
                        out=Sb, lhsT=kTa[0:K, m, kt * 128:(kt + 1) * 128], rhs=qTa[0:K, m, qc * 512:(qc + 1) * 512],
                        start=True, stop=True),
                        reads=[B_kT[m]] + [B_qT[m][qc * 4 + u] for u in range(4)] + [B_qaugrows], writes=B_Sb)
                    P.op("act", lambda e: e.activation(out=E[i][:], in_=Sb, func=AF.Exp, scale=HEAD_DIM ** -0.5),
                         reads=B_Sb, writes=[B_E[i]])
                    c0 = (4 * qc - kt + 3) * 128
                    meng = "pool" if (mul_ctr[0] % MULMOD == MULMOD - 1) else "dve"; mul_ctr[0] += 1
                    P.op(meng, lambda e: e.tensor_tensor(out=PT[j][:], in0=E[i][:], in1=G[gi][:, c0:c0 + 512], op=ALU.mult),
                         reads=[B_E[i], B_G[gi]], writes=[B_PT[j]])
                    return j

                def emit_PV(idx, j, is_moba=is_moba, steps=steps, st_info=st_info):
                    qc, m, kt = steps[idx]
                    o = st_info[(qc, m)]
                    if is_moba:
                        v0, vw = m * 68, 65
                    else:
                        v0, vw = 0, 133
                    for jq in range(4):
                        if kt > 4 * qc + jq:
                            continue
                        P.op("pe", lambda e, jq=jq: e.matmul(
                            out=Ops[o][:, jq, 0:vw], lhsT=PT[j][:, jq * 128:(jq + 1) * 128], rhs=Vt[:, kt, v0:v0 + vw],
                            start=(kt == 0 and jq in (0, 2)), stop=(kt == 4 * qc + jq), skip_group_check=True),
                            reads=[B_PT[j], B_V], writes=[B_O[o]])
                    if kt == 4 * qc + 3:
                        finalize(qc, m, o)

                def finalize(qc, m, o, g=g, is_moba=is_moba):
                    scol = 132 if not is_moba else 64
                    P.op("dve", lambda e: e.reciprocal(out=rr[:], in_=Ops[o][:, :, scol:scol + 1].rearrange("p a b -> p (a b)")),
                         reads=[B_O[o]], writes=[B_rr])
                    if not is_moba:
                        if m == 0:
                            for jq in range(4):
                                P.op("dve", lambda e, jq=jq: e.tensor_scalar_mul(
                                    out=tmp1[:, jq, :].rearrange("p (a b) -> p a b", a=2),
                                    in0=Ops[o][:, jq, 0:136].rearrange("p (a b) -> p a b", a=2)[:, :, 0:64],
                                    scalar1=rr[:, jq:jq + 1]), reads=[B_O[o], B_rr], writes=[B_tmp1])
                        else:
                            P.op("dve", lambda e: e.tensor_scalar_mul(out=nlr[:], in0=rr[:], scalar1=nlam), reads=[B_rr, B_lams], writes=[B_nlr])
                            for jq in range(4):
                                P.op("dve", lambda e, jq=jq: e.scalar_tensor_tensor(
                                    out=yd[:, jq, :].rearrange("p (a b) -> p a b", a=2),
                                    in0=Ops[o][:, jq, 0:136].rearrange("p (a b) -> p a b", a=2)[:, :, 0:64],
                                    scalar=nlr[:, jq:jq + 1], in1=tmp1[:, jq, :].rearrange("p (a b) -> p a b", a=2),
                                    op0=ALU.mult, op1=ALU.add), reads=[B_O[o], B_nlr, B_tmp1], writes=[B_yd])
                            P.op("pool", lambda e: e.tensor_tensor(out=sq2[:], in0=yd[:], in1=yd[:], op=ALU.mult), reads=[B_yd], writes=[B_sq2])
                            P.op("dve", lambda e: e.tensor_reduce(out=ss2[:], in_=sq2[:], axis=AX.X, op=ALU.add), reads=[B_sq2], writes=[B_ss2])
                            rsqrt_act(rs2[:], ss2[:], 128.0, [B_ss2], [B_rs2])
                            for jq in range(4):
                                P.op("dve", lambda e, jq=jq: e.scalar_tensor_tensor(
                                    out=ytok[:, jq, :], in0=yd[:, jq, :], scalar=rs2[:, jq:jq + 1], in1=sgb[:],
                                    op0=ALU.mult, op1=ALU.mult), reads=[B_yd, B_rs2, B_sgb], writes=[B_ytok])
                    else:
                        for jq in range(4):
                            P.op("dve", lambda e, jq=jq: e.tensor_scalar_mul(
                                out=ytok[:, jq, m * 64:(m + 1) * 64], in0=Ops[o][:, jq, 0:64], scalar1=rr[:, jq:jq + 1]),
                                reads=[B_O[o], B_rr], writes=[B_ytok])
                    if m == 1:
                        for jq in range(4):
                            P.op("pe", lambda e, jq=jq: e.transpose(out=ptr[:, jq * 128:(jq + 1) * 128], in_=ytok[:, jq, :], identity=ident_b[:]),
                                 reads=[B_ytok, B_idb], writes=[B_ptr, B_ptr1])
                        P.op("act", lambda e: e.copy(out=yT[:, g, qc * 512:(qc + 1) * 512], in_=ptr[:, 0:512]),
                             reads=[B_ptr, B_ptr1], writes=[B_yT[g][qc]])

                LOOK = 3
                jq_ = []
                for idx in range(min(LOOK, len(steps))):
                    jq_.append(emit_S(idx))
                for idx in range(len(steps)):
                    if idx + LOOK < len(steps):
                        jq_.append(emit_S(idx + LOOK))
                    emit_PV(idx, jq_[idx])
                if dbg == 4:
                    break
            if dbg in (3, 4, 30, 305, 31, 32, 33, 34, 35, 36, 37, 38, 39):
                break
            for t in range(NT):
                s = t % 2
                r0 = tok0 + t * 128
                xt = stage[s][:, 0:1024]
                P.dma("sp", f"dst{s}", lambda e, xt=xt, r0=r0: e.dma_start(out=xt, in_=x.ap()[r0:r0 + 128, :]), writes=[B_stage[s]])
                for half in range(2):
                    for kc in range(8):
                        P.op("pe", lambda e, half=half, kc=kc, t=t: e.matmul(
                            out=Sps[half][:], lhsT=yT[:, kc, t * 128:(t + 1) * 128], rhs=woutb[:, kc, half * 512:(half + 1) * 512],
                            start=(kc == 0), stop=(kc == 7)), reads=[B_yT[kc][t // 4], B_wout], writes=[B_S[half]])
                    P.op("dve", lambda e, half=half, xt=xt: e.tensor_tensor(
                        out=xt[:, half * 512:(half + 1) * 512], in0=Sps[half][:], in1=xt[:, half * 512:(half + 1) * 512], op=ALU.add),
                        reads=[B_S[half], B_stage[s]], writes=[B_stage[s]])
                if not stop_after_attn:
                    P.dma("sp", f"dout{s}", lambda e, xt=xt, r0=r0: e.dma_start(out=x1d.ap()[r0:r0 + 128, :], in_=xt),
                          reads=[B_stage[s]], writes=[B_x1d])
                if stop_after_attn:
                    ob = Buf()
                    P.dma("sp", f"dout{s}", lambda e, xt=xt, r0=r0: e.dma_start(out=out.ap()[r0:r0 + 128, :], in_=xt), reads=[B_stage[s]], writes=[ob])
                    out_bufs.append(ob)


        if not stop_after_attn and (dbg == 0 or dbg >= 50):
            P.barrier()
            acc = xT[:].rearrange("p k n -> p (k n)").bitcast(F32).rearrange("p (t n) -> p t n", t=8)
            h2Tb = yT
            Wgu = [yT[:, :, 1024:1536], yT[:, :, 1536:2048]]
            Wdn = [qTa[:, :, 0:1024], qTa[:, :, 1024:2048]]
            wst = [G[0][:, 0:2048], G[1][:, 0:2048]]
            h2f = stage[0][:, 0:1024]
            h2Tf = stage[1][:, 0:1024]
            sgs = [E[0][:, 0:256], E[0][:, 256:512]]
            hids = [PT[0][:, 0:256], PT[0][:, 256:512]]
            hidTs = [PT[1][:, 0:256], PT[1][:, 256:512]]
            Bsg = [Buf(), Buf()]; Bhid = [Buf(), Buf()]; BhidT = [Buf(), Buf()]; Bptr2 = [Buf(), Buf()]
            Rw = sb("Rw", [128, 8, 36], F32)
            lg = sb("lg", [128, 36], F32)
            wfull = sb("wfull", [128, 8, 32], F32)
            rt = sb("rt", [128, 64], F32)
            Bm = {k: Buf(k) for k in ["acc", "h2Tb", "wgu0", "wgu1", "wd0", "wd1", "wst0", "wst1", "h2f", "h2Tf", "sg", "hid",
                                      "hidT", "Rw", "lg", "wfull", "rt", "S0", "S1", "O0", "O1", "ptr", "pj", "junk", "g2b", "idb", "idf", "x1d"]}
            P.dma("sp", "dpar", lambda e: e.dma_start(out=Rw[:, :, 0:4], in_=router_group.ap().rearrange("o (k p) c -> p (o k) c", p=128)),
                  writes=[Bm["Rw"]])
            for gx in range(4):
                P.dma("sp", "dpar", lambda e, gx=gx: e.dma_start(out=Rw[:, :, 4 + 8 * gx:12 + 8 * gx],
                                                                 in_=router_expert.ap()[0, gx].rearrange("(k p) c -> p k c", p=128)),
                      writes=[Bm["Rw"]])
            Opf = [Ops[0][:].rearrange("p a b -> p (a b)"), Ops[1][:].rearrange("p a b -> p (a b)")]
            BO = [Bm["O0"], Bm["O1"]]
            BS = [Bm["S0"], Bm["S1"]]
            Bwgu = [Bm["wgu0"], Bm["wgu1"]]
            Bwd = [Bm["wd0"], Bm["wd1"]]
            Bwst = [Bm["wst0"], Bm["wst1"]]
            st_ctr = [0]
            s_ctr = [0]
            o_ctr2 = [0]
            NSB = NTOK // 1024
            for sbk in range(NSB):
                for ti in range(8):
                    if dbg == 50 and ti == 1:
                        break
                    r0 = sbk * 1024 + ti * 128
                    if dbg == 50 and _STG == 1:
                        break
                    P.dma("sp", f"dx1_{ti}", lambda e, ti=ti, r0=r0: e.dma_start(out=acc[:, ti, :], in_=x1d.ap()[r0:r0 + 128, :]),
                          reads=[Bm["x1d"]], writes=[Bm["acc"]])
                    if dbg == 50 and _STG == 2:
                        break
                    P.op("act", lambda e, ti=ti: e.activation(out=junk[:], in_=acc[:, ti, :], func=AF.Square, accum_out=rt[:, 0:1]),
                         reads=[Bm["acc"]], writes=[Bm["junk"], Bm["rt"]])
                    if dbg == 50 and _STG == 3:
                        break
                    P.op("act", lambda e: e.activation(out=rt[:, 1:2], in_=rt[:, 0:1], func=AF.Ln, bias=float(RMS_EPS), scale=1.0 / 1024),
                         reads=[Bm["rt"]], writes=[Bm["rt"]])
                    if dbg == 50 and _STG == 4:
                        break
                    P.op("act", lambda e: e.activation(out=rt[:, 1:2], in_=rt[:, 1:2], func=AF.Exp, scale=-0.5), reads=[Bm["rt"]], writes=[Bm["rt"]])
                    if dbg == 50 and _STG == 5:
                        break
                    P.op("dve", lambda e, ti=ti: e.scalar_tensor_tensor(out=h2f, in0=acc[:, ti, :], scalar=rt[:, 1:2], in1=g2b[:],
                                                                         op0=ALU.mult, op1=ALU.mult),
                         reads=[Bm["acc"], Bm["rt"], Bm["g2b"]], writes=[Bm["h2f"]])
                    if dbg == 50 and _STG == 6:
                        break
                    for kc in range(8):
                        P.op("pe", lambda e, kc=kc: e.transpose(out=Opf[1][:, kc * 128:(kc + 1) * 128], in_=h2f[:, kc * 128:(kc + 1) * 128],
                                                                identity=ident_f[:]), reads=[Bm["h2f"], Bm["idf"]], writes=[BO[1]])
                    if dbg == 50 and _STG == 7:
                        break
                    P.op("act", lambda e: e.copy(out=h2Tf, in_=Opf[1]), reads=[BO[1]], writes=[Bm["h2Tf"]])
                    if dbg == 50 and _STG == 8:
                        break
                    P.op("dve", lambda e, ti=ti: e.tensor_copy(out=h2Tb[:, :, ti * 128:(ti + 1) * 128],
                                                               in_=h2Tf.rearrange("p (k n) -> p k n", k=8)),
                         reads=[Bm["h2Tf"]], writes=[Bm["h2Tb"]])
                    if dbg == 50 and _STG == 9:
                        break
                    for kc in range(8):
                        P.op("pe", lambda e, kc=kc: e.matmul(out=pj[:, 0:36], lhsT=h2Tf[:, kc * 128:(kc + 1) * 128], rhs=Rw[:, kc, :],
                                                             start=(kc == 0), stop=(kc == 7)), reads=[Bm["h2Tf"], Bm["Rw"]], writes=[Bm["pj"]])
                    if dbg == 50 and _STG == 10:
                        break
                    P.op("act", lambda e: e.copy(out=lg[:], in_=pj[:, 0:36]), reads=[Bm["pj"]], writes=[Bm["lg"]])
                    R_ = [Bm["rt"]]; L_ = [Bm["lg"]]
                    if dbg == 50 and _STG == 11:
                        break
                    P.op("dve", lambda e: e.tensor_reduce(out=rt[:, 2:3], in_=lg[:, 0:4], axis=AX.X, op=ALU.max), reads=L_, writes=R_)
                    if dbg == 50 and _STG == 12:
                        break
                    P.op("dve", lambda e: e.tensor_scalar(out=rt[:, 8:12], in0=lg[:, 0:4], scalar1=rt[:, 2:3], scalar2=None, op0=ALU.is_equal),
                         reads=L_ + R_, writes=R_)
                    if dbg == 50 and _STG == 13:
                        break
                    P.op("dve", lambda e: e.tensor_scalar_mul(out=rt[:, 3:4], in0=rt[:, 2:3], scalar1=-1.0), reads=R_, writes=R_)
                    if dbg == 50 and _STG == 14:
                        break
                    P.op("dve", lambda e: e.tensor_scalar(out=rt[:, 12:16], in0=lg[:, 0:4], scalar1=rt[:, 2:3], scalar2=None, op0=ALU.subtract),
                         reads=L_ + R_, writes=R_)
                    if dbg == 50 and _STG == 15:
                        break
                    P.op("act", lambda e: e.activation(out=rt[:, 12:16], in_=rt[:, 12:16], func=AF.Exp, accum_out=rt[:, 4:5]),
                         reads=R_, writes=R_)
                    if dbg == 50 and _STG == 16:
                        break
                    P.op("dve", lambda e: e.reciprocal(out=rt[:, 5:6], in_=rt[:, 4:5]), reads=R_, writes=R_)
                    if dbg == 50 and _STG == 17:
                        break
                    P.op("dve", lambda e: e.tensor_scalar_mul(out=rt[:, 16:24], in0=lg[:, 4:12], scalar1=rt[:, 8:9]), reads=L_ + R_, writes=R_)
                    if dbg == 50 and _STG == 18:
                        break
                    for gx in range(1, 4):
                        P.op("dve", lambda e, gx=gx: e.scalar_tensor_tensor(out=rt[:, 16:24], in0=lg[:, 4 + 8 * gx:12 + 8 * gx],
                                                                            scalar=rt[:, 8 + gx:9 + gx], in1=rt[:, 16:24],
                                                                            op0=ALU.mult, op1=ALU.add), reads=L_ + R_, writes=R_)
                    if dbg == 50 and _STG == 19:
                        break
                    P.op("dve", lambda e: e.max(out=rt[:, 24:32], in_=rt[:, 16:24]), reads=R_, writes=R_)
                    if dbg == 50 and _STG == 20:
                        break
                    P.op("dve", lambda e: e.tensor_scalar(out=rt[:, 32:40], in0=rt[:, 16:24], scalar1=rt[:, 24:25], scalar2=None, op0=ALU.is_equal),
                         reads=R_, writes=R_)
                    if dbg == 50 and _STG == 21:
                        break
                    P.op("dve", lambda e: e.tensor_scalar(out=rt[:, 40:48], in0=rt[:, 16:24], scalar1=rt[:, 25:26], scalar2=None, op0=ALU.is_equal),
                         reads=R_, writes=R_)
                    if dbg == 50 and _STG == 22:
                        break
                    P.op("dve", lambda e: e.tensor_sub(out=rt[:, 48:49], in0=rt[:, 25:26], in1=rt[:, 24:25]), reads=R_, writes=R_)
                    if dbg == 50 and _STG == 23:
                        break
                    P.op("act", lambda e: e.activation(out=rt[:, 49:50], in_=rt[:, 48:49], func=AF.Exp), reads=R_, writes=R_)
                    if dbg == 50 and _STG == 24:
                        break
                    P.op("dve", lambda e: e.tensor_scalar_add(out=rt[:, 50:51], in0=rt[:, 49:50], scalar1=1.0), reads=R_, writes=R_)
                    if dbg == 50 and _STG == 25:
                        break
                    P.op("dve", lambda e: e.reciprocal(out=rt[:, 51:52], in_=rt[:, 50:51]), reads=R_, writes=R_)
                    if dbg == 50 and _STG == 26:
                        break
                    P.op("dve", lambda e: e.tensor_tensor(out=rt[:, 52:53], in0=rt[:, 49:50], in1=rt[:, 51:52], op=ALU.mult), reads=R_, writes=R_)
                    if dbg == 50 and _STG == 27:
                        break
                    P.op("dve", lambda e: e.tensor_scalar_mul(out=rt[:, 53:55], in0=rt[:, 51:53], scalar1=rt[:, 5:6]), reads=R_, writes=R_)
                    if dbg == 50 and _STG == 28:
                        break
                    P.op("dve", lambda e: e.tensor_scalar_mul(out=rt[:, 56:64], in0=rt[:, 32:40], scalar1=rt[:, 53:54]), reads=R_, writes=R_)
                    if dbg == 50 and _STG == 29:
                        break
                    P.op("dve", lambda e: e.scalar_tensor_tensor(out=rt[:, 56:64], in0=rt[:, 40:48], scalar=rt[:, 54:55], in1=rt[:, 56:64],
                                                                 op0=ALU.mult, op1=ALU.add), reads=R_, writes=R_)
                    if dbg == 50 and _STG == 30:
                        break
                    for gx in range(4):
                        P.op("dve", lambda e, gx=gx, ti=ti: e.tensor_scalar_mul(out=wfull[:, ti, 8 * gx:8 * gx + 8], in0=rt[:, 56:64],
                                                                                scalar1=rt[:, 8 + gx:9 + gx]), reads=R_, writes=[Bm["wfull"]])
                if dbg in (50, 51):
                    break
                for ex in range(N_EXP if dbg != 52 else 1):
                    slot = ex % 2
                    srcs = [(w_gate.ap()[0, ex].rearrange("(k p) f -> p k f", p=128), Wgu[slot][:, :, 0:256], 8, 256, Bwgu[slot]),
                            (w_up.ap()[0, ex].rearrange("(k p) f -> p k f", p=128), Wgu[slot][:, :, 256:512], 8, 256, Bwgu[slot]),
                            (w_down.ap()[0, ex].rearrange("(c p) n -> p c n", p=128), Wdn[slot], 2, 1024, Bwd[slot])]
                    for src, dst, a_, b_, bdst in srcs:
                        si = st_ctr[0] % 2; st_ctr[0] += 1
                        stv = wst[si].rearrange("p (a b) -> p a b", a=a_)
                        P.dma("sp", f"dwe{si}", lambda e, stv=stv, src=src: e.dma_start(out=stv, in_=src), writes=[Bwst[si]])
                        P.op("pool", lambda e, stv=stv, dst=dst: e.tensor_copy(out=dst, in_=stv), reads=[Bwst[si]], writes=[bdst])
                    def head(ti, i, slot=slot):
                        for kc in range(8):
                            P.op("pe", lambda e, kc=kc: e.matmul(
                                out=Sps[i][:], lhsT=h2Tb[:, kc, ti * 128:(ti + 1) * 128], rhs=Wgu[slot][:, kc, :],
                                start=(kc == 0), stop=(kc == 7)), reads=[Bm["h2Tb"], Bwgu[slot]], writes=[BS[i]])

                    def mid(ti, i, o, b, slot=slot, ex=ex):
                        sg = sgs[b]; hid = hids[b]
                        P.op("act", lambda e: e.activation(out=sg, in_=Sps[i][:, 0:256], func=AF.Silu), reads=[BS[i]], writes=[Bsg[b]])
                        P.op("dve", lambda e: e.scalar_tensor_tensor(
                            out=hid, in0=Sps[i][:, 256:512], scalar=wfull[:, ti, ex:ex + 1], in1=sg, op0=ALU.mult, op1=ALU.mult),
                            reads=[BS[i], Bm["wfull"], Bsg[b]], writes=[Bhid[b]])
                        for c in range(2):
                            P.op("pe", lambda e, c=c: e.transpose(out=ptr[:, b * 256 + c * 128:b * 256 + (c + 1) * 128],
                                                                  in_=hid[:, c * 128:(c + 1) * 128],
                                                                  identity=ident_b[:]), reads=[Bhid[b], Bm["idb"]], writes=[Bptr2[b]])
                        P.op("act", lambda e: e.copy(out=hidTs[b], in_=ptr[:, b * 256:(b + 1) * 256]), reads=[Bptr2[b]], writes=[BhidT[b]])

                    def tail(ti, i, o, b, slot=slot, ex=ex):
                        hidT = hidTs[b]
                        for half in range(2):
                            for c in range(2):
                                P.op("pe", lambda e, half=half, c=c: e.matmul(
                                    out=Opf[o][:, half * 512:(half + 1) * 512], lhsT=hidT[:, c * 128:(c + 1) * 128],
                                    rhs=Wdn[slot][:, c, half * 512:(half + 1) * 512], start=(c == 0), stop=(c == 1)),
                                    reads=[BhidT[b], Bwd[slot]], writes=[BO[o]])
                        P.op("dve", lambda e: e.tensor_tensor(out=acc[:, ti, :], in0=Opf[o], in1=acc[:, ti, :], op=ALU.add),
                             reads=[BO[o], Bm["acc"]], writes=[Bm["acc"]])

                    infos = []
                    for ti in range(8):
                        i = s_ctr[0] % 2; s_ctr[0] += 1
                        o = o_ctr2[0] % 2; o_ctr2[0] += 1
                        infos.append((ti, i, o, ti % 2))
                    for k in range(8 + 2):
                        if k < 8:
                            head(infos[k][0], infos[k][1])
                        if 0 <= k - 1 < 8:
                            mid(*infos[k - 1])
                        if 0 <= k - 2 < 8:
                            tail(*infos[k - 2])
                if dbg == 52:
                    break
                for ti in range(8):
                    r0 = sbk * 1024 + ti * 128
                    ob = Buf()
                    P.dma("sp", "dfin", lambda e, ti=ti, r0=r0: e.dma_start(out=out.ap()[r0:r0 + 128, :], in_=acc[:, ti, :]),
                          reads=[Bm["acc"]], writes=[ob])
                    out_bufs.append(ob)

        P.wait_all("sp", out_bufs)
        P.finish()
        with nc.allow_non_contiguous_dma(reason="small parameter loads"):
            P.emit()
    return nc


_PARAM_KEYS = ["norm1_g", "w_in", "diff_q_g", "diff_k_g", "lambda_q1", "lambda_k1", "lambda_q2", "lambda_k2",
               "diff_sub_g", "moba_q_g", "moba_k_g", "rel_bias", "w_out", "norm2_g", "router_group",
               "router_expert", "w_gate", "w_up", "w_down"]


def run_cores(inputs, nseq, ncores, stop_after_attn=False, dbg=0):
    nc = build(nseq, stop_after_attn=stop_after_attn, dbg=dbg)
    onehot, cmask, blk = _consts()
    xs = np.ascontiguousarray(inputs["x"], dtype=np.float32).reshape(-1, D_MODEL)
    in_maps = []
    for c in range(ncores):
        m = {k: np.ascontiguousarray(inputs[k], dtype=np.float32) for k in _PARAM_KEYS}
        m["x"] = xs[c * nseq * S_LEN:(c + 1) * nseq * S_LEN]
        m["c_onehot"] = onehot; m["c_blk"] = blk
        in_maps.append(m)
    res = run_bass_kernel_spmd(nc, in_maps, core_ids=list(range(ncores)))
    return np.concatenate([np.asarray(r["out"]) for r in res.results], axis=0)


def kernel(**inputs):
    B = inputs["x"].shape[0]
    o = run_cores(inputs, B // 8, 8)
    return o.reshape(B, S_LEN, D_MODEL).astype(np.float32)
```

```python
import math
from contextlib import ExitStack

import numpy as np
import concourse.bass as bass
import concourse.mybir as mybir
from concourse.bass_utils import run_bass_kernel_spmd

F32 = mybir.dt.float32
BF16 = mybir.dt.bfloat16
I32 = mybir.dt.int32
AF = mybir.ActivationFunctionType
ALU = mybir.AluOpType
AX = mybir.AxisListType


class Buf:
    __slots__ = ("w", "r", "name")

    def __init__(self, name=""):
        self.w = None
        self.r = []
        self.name = name


class Prog:
    ENG = ("pe", "act", "dve", "pool", "sp")

    def __init__(self, nc, stack):
        self.nc = nc
        self.stack = stack
        self.ops = {e: [] for e in self.ENG}
        self.sem = {}
        self.cnt = {}
        self.seen = {e: {} for e in self.ENG}
        for e in self.ENG:
            self.newsem(e)

    def newsem(self, name):
        self.sem[name] = self.stack.enter_context(self.nc.semaphore("s_" + name))
        self.cnt[name] = 0

    def _waits(self, eng, reads, writes):
        need = {}

        def add(ev, raw):
            if ev is None:
                return
            sn, v = ev
            if sn == eng:
                if eng == "pe" or not raw:
                    return
            if v > need.get(sn, 0):
                need[sn] = v

        for b in reads:
            add(b.w, True)
        for b in writes:
            add(b.w, False)
            for ev in b.r:
                add(ev, False)
        out = []
        for sn, v in need.items():
            if self.seen[eng].get(sn, 0) < v:
                self.seen[eng][sn] = v
                out.append((sn, v))
        return out

    def _mark(self, ev, reads, writes):
        for b in reads:
            b.r.append(ev)
        for b in writes:
            b.w = ev
            b.r = []

    def op(self, eng, fn, reads=(), writes=()):
        waits = self._waits(eng, reads, writes)
        self.cnt[eng] += 1
        ev = (eng, self.cnt[eng])
        self.ops[eng].append((fn, waits, (eng, 1)))
        self._mark(ev, reads, writes)
        return ev

    def dma(self, q, semname, fn, reads=(), writes=()):
        if semname == "dpar":
            self._npar = getattr(self, "_npar", 0) + 1
            semname = "dpar%d" % self._npar
        if semname not in self.sem:
            self.newsem(semname)
        waits = self._waits(q, reads, writes)
        self.cnt[semname] += 16
        ev = (semname, self.cnt[semname])
        self.ops[q].append((fn, waits, (semname, 16)))
        self._mark(ev, reads, writes)
        return ev

    def wait_all(self, eng, bufs):
        waits = self._waits(eng, bufs, ())
        self.ops[eng].append((None, waits, None))

    def barrier(self):
        waits = [(sn, v) for sn, v in self.cnt.items() if v > 0]
        for eng in self.ENG:
            w = [(sn, v) for sn, v in waits if self.seen[eng].get(sn, 0) < v and not (sn == eng and eng == "sp")]
            for sn, v in w:
                self.seen[eng][sn] = v
            self.ops[eng].append((None, w, None))

    def finish(self):
        waits = [(sn, v) for sn, v in self.cnt.items() if v > 0]
        self.ops["sp"].append((None, waits, None))

    def emit(self):
        nc = self.nc
        sem = self.sem
        ops = self.ops

        def run(e, name):
            for fn, waits, inc in ops[name]:
                for sn, v in waits:
                    e.wait_ge(sem[sn], v)
                if fn is None:
                    continue
                ins = fn(e)
                ins.then_inc(sem[inc[0]], inc[1])

        with nc.Block() as block:
            @block.tensor
            def _(e):
                run(e, "pe")

            @block.scalar
            def _(e):
                run(e, "act")

            @block.vector
            def _(e):
                run(e, "dve")

            @block.gpsimd
            def _(e):
                run(e, "pool")

            @block.sync
            def _(e):
                run(e, "sp")


S_LEN = 2048
D_MODEL = 1024
NT = 16
HEAD_DIM = 64
RMS_EPS = 1e-6
LAMBDA_INIT = 0.8 - 0.6 * math.exp(-0.3 * 0)
NEG_BIG = 30000.0
N_EXP = 32
MULMOD = 1000000
CAP_CHUNK = 128


def _t5_bucket_np(n):
    n = np.maximum(n, 0)
    max_exact = 16
    nf = np.maximum(n, 1).astype(np.float32)
    large = max_exact + (np.log(nf / np.float32(max_exact)) / np.float32(math.log(2048 / max_exact))
                         * np.float32(32 - max_exact)).astype(np.int32)
    large = np.minimum(large, 31)
    return np.where(n < max_exact, n, large)


def _consts():
    d = np.arange(2048)
    b = _t5_bucket_np(d)
    onehot = np.zeros((32, 2048), np.float32)
    onehot[b, d] = 1.0
    cmask = None
    blk = np.zeros((8, 2048), np.float32)
    for n in range(8):
        blk[n, n * 256:(n + 1) * 256] = 1.0
    return onehot, cmask, blk


def bc_inner(ap, n):
    return bass.AP(tensor=ap.tensor, offset=ap.offset, ap=[list(a) for a in ap.ap] + [[0, n]])


class _Stop(Exception):
    pass


def build(nseq, stop_after_attn=False, dbg=0):
    nc = bass.Bass("TRN2", target_bir_lowering=False)
    NTOK = nseq * S_LEN
    dt_in = lambda name, shape: nc.dram_tensor(name, shape, F32, kind="ExternalInput")
    x = dt_in("x", [NTOK, D_MODEL])
    norm1_g = dt_in("norm1_g", [1, 1024])
    w_in = dt_in("w_in", [1, 1024, 3072])
    diff_q_g = dt_in("diff_q_g", [1, 64]); diff_k_g = dt_in("diff_k_g", [1, 64])
    lambda_q1 = dt_in("lambda_q1", [1, 64]); lambda_k1 = dt_in("lambda_k1", [1, 64])
    lambda_q2 = dt_in("lambda_q2", [1, 64]); lambda_k2 = dt_in("lambda_k2", [1, 64])
    diff_sub_g = dt_in("diff_sub_g", [1, 128])
    moba_q_g = dt_in("moba_q_g", [1, 64]); moba_k_g = dt_in("moba_k_g", [1, 64])
    rel_bias = dt_in("rel_bias", [32, 12])
    w_out = dt_in("w_out", [1, 1024, 1024])
    norm2_g = dt_in("norm2_g", [1, 1024])
    router_group = dt_in("router_group", [1, 1024, 4])
    router_expert = dt_in("router_expert", [1, 4, 1024, 8])
    w_gate = dt_in("w_gate", [1, 32, 1024, 256])
    w_up = dt_in("w_up", [1, 32, 1024, 256])
    w_down = dt_in("w_down", [1, 32, 256, 1024])
    c_onehot = dt_in("c_onehot", [32, 2048])
    c_blk = dt_in("c_blk", [8, 2048])
    out = nc.dram_tensor("out", [NTOK, D_MODEL], F32, kind="ExternalOutput")
    Bd = nc.dram_tensor("Bd", [12, 129, 2560], F32, kind="Internal")
    Wd = nc.dram_tensor("Wd", [8, 1024, 384], BF16, kind="Internal")
    x1d = nc.dram_tensor("x1d", [NTOK, D_MODEL], F32, kind="Internal")

    with ExitStack() as st:
        P = Prog(nc, st)
        sb = lambda name, shape, dt: st.enter_context(nc.sbuf_tensor(name, shape, dt))
        ps = lambda name, shape, dt: st.enter_context(nc.psum_tensor(name, shape, dt))
        out_bufs = []

        Sps = [ps("Sps0", [128, 512], F32), ps("Sps1", [128, 512], F32)]
        B_S = [Buf(), Buf()]
        Ops = [ps("Ops0", [128, 4, 256], F32), ps("Ops1", [128, 4, 256], F32)]
        B_O = [Buf(), Buf()]
        pj = ps("pj", [128, 512], F32); B_pj = Buf()
        ptr = ps("ptr", [128, 1024], BF16); B_ptr = Buf(); B_ptr1 = Buf()

        ident_f = sb("ident_f", [128, 128], F32); B_idf = Buf()
        ident_b = sb("ident_b", [128, 128], BF16); B_idb = Buf()
        wg = [sb("wg0", [128, 8, 384], BF16), sb("wg1", [128, 8, 384], BF16)]; B_wg = [Buf(), Buf()]
        B_Wd = Buf()
        woutb = sb("woutb", [128, 8, 1024], BF16); B_wout = Buf()
        stage = [sb("stage0", [128, 1536], F32), sb("stage1", [128, 1536], F32)]
        B_stage = [Buf(), Buf()]
        g1c = sb("g1c", [128, 8], F32); B_g1 = Buf()
        xT = sb("xT", [128, 8, 2048], BF16); B_xT = [Buf() for _ in range(NT)]
        yT = sb("yT", [128, 8, 2048], BF16); B_yT = [[Buf() for _ in range(4)] for _ in range(8)]
        qTa = sb("qTa", [128, 2, 2048], BF16); B_qT = [[Buf() for _ in range(NT)] for _ in range(2)]
        kTa = sb("kTa", [128, 2, 2048], BF16); B_kT = [Buf(), Buf()]
        B_qaugrows = Buf()
        Vt = sb("Vt", [128, 16, 136], BF16); B_V = Buf()
        G = [sb("G0", [128, 2560], F32), sb("G1", [128, 2560], F32)]; B_G = [Buf(), Buf()]
        junk = sb("junk", [128, 1024], BF16); B_junk = Buf()
        xb = sb("xb", [128, 1024], BF16); B_xb = Buf()
        ss1 = sb("ss1", [128, 16], F32); B_ss1 = Buf()
        rstd1 = sb("rstd1", [128, 16], F32); B_rstd1 = Buf()
        NB4 = 4
        qk2 = [sb(f"bqk{i}", [128, 256], F32) for i in range(NB4)]; B_qk2 = [Buf() for _ in range(NB4)]
        sq2b = [sb(f"bsq{i}", [128, 256], F32) for i in range(NB4)]; B_sq2b = [Buf() for _ in range(NB4)]
        ssh2 = [sb(f"bssh{i}", [128, 4], F32) for i in range(NB4)]; B_ssh2 = [Buf() for _ in range(NB4)]
        rsh2 = [sb(f"brsh{i}", [128, 4], F32) for i in range(NB4)]; B_rsh2 = [Buf() for _ in range(NB4)]
        qkn2 = [sb(f"bqkn{i}", [128, 256], BF16) for i in range(NB4)]; B_qkn2 = [Buf() for _ in range(NB4)]
        E = [sb(f"E{i}", [128, 512], F32) for i in range(4)]; B_E = [Buf() for _ in range(4)]
        PT = [sb(f"PT{i}", [128, 512], BF16) for i in range(5)]; B_PT = [Buf() for _ in range(5)]
        rr = sb("rr", [128, 4], F32); B_rr = Buf()
        nlr = sb("nlr", [128, 4], F32); B_nlr = Buf()
        tmp1 = sb("tmp1", [128, 4, 128], F32); B_tmp1 = Buf()
        yd = sb("yd", [128, 4, 128], F32); B_yd = Buf()
        sq2 = sb("sq2", [128, 4, 128], F32); B_sq2 = Buf()
        ss2 = sb("ss2", [128, 4], F32); B_ss2 = Buf()
        rs2 = sb("rs2", [128, 4], F32); B_rs2 = Buf()
        ytok = sb("ytok", [128, 4, 128], BF16); B_ytok = Buf()
        sgb = sb("sgb", [128, 128], F32); B_sgb = Buf()
        g2b = sb("g2b", [128, 1024], F32); B_g2b = Buf()
        lamv = sb("lamv", [128, 4, 64], F32); B_lamv = Buf()
        lamp = sb("lamp", [128, 2, 64], F32); B_lamp = Buf()
        lams = sb("lams", [128, 4], F32); B_lams = Buf()
        gb = sb("gb", [128, 4, 64], F32); B_gq = Buf()
        gqk = sb("gqk", [128, 2, 64], F32); B_gqk = Buf()
        rb = sb("rb", [32, 12], F32); B_rb = Buf()
        oh = G[0][0:32, 0:2048]; B_oh = B_G[0]
        et = G[1][0:12, 0:2560]; B_et = B_G[1]
        B_Bd = Buf()
        B_x1d = Buf()
        km = sb("km", [64, 16], F32); B_km = Buf()
        kmf = sb("kmf", [64, 16], F32); B_kmf = Buf()
        kmhf = sb("kmhf", [64, 16], F32); B_kmhf = Buf()
        kmhl = sb("kmhl", [64, 2, 16], BF16); B_kmhl = Buf()
        g16 = sb("g16", [128, 16], F32); B_g16 = Buf()
        gate = sb("gate", [128, 8], F32); B_gate = Buf()
        mx8 = sb("mx8", [128, 8], F32); B_mx8 = Buf()
        selm = sb("selm", [128, 8], F32); B_selm = Buf()
        qaug = sb("qaug", [128, 128], BF16); B_qaug = Buf()

        P.op("pool", lambda e: e.memset(ident_f[:], 1.0), writes=[B_idf])
        P.op("pool", lambda e: e.affine_select(out=ident_f[:], in_=ident_f[:], pattern=[[-1, 128]],
                                               compare_op=ALU.is_equal, fill=0.0, base=0, channel_multiplier=1),
             reads=[B_idf], writes=[B_idf])
        P.op("dve", lambda e: e.tensor_copy(out=ident_b[:], in_=ident_f[:]), reads=[B_idf], writes=[B_idb])

        P.dma("sp", "dpar", lambda e: e.dma_start(out=g1c[:], in_=norm1_g.ap().rearrange("o (k p) -> p (o k)", p=128)),
              writes=[B_g1])
        w_in_v = w_in.ap().rearrange("o (k p) n -> p (o k) n", p=128)
        B_wst = [Buf(), Buf()]
        for kc in range(8):
            for T in range(2):
                s = T
                P.dma("sp", f"dst{s}", lambda e, kc=kc, s=s, T=T: e.dma_start(out=stage[s][:], in_=w_in_v[:, kc, T * 1536:(T + 1) * 1536]),
                      writes=[B_stage[s]])
                P.op("dve" if s == 0 else "pool",
                     lambda e, kc=kc, s=s: e.tensor_scalar_mul(out=qTa[:, s, 0:1536], in0=stage[s][:], scalar1=g1c[:, kc:kc + 1]),
                     reads=[B_stage[s], B_g1], writes=[B_wst[s]])
                for gg in range(4):
                    a = qTa[:, s, gg * 128:gg * 128 + 128]
                    src3 = bass.AP(tensor=a.tensor, offset=a.offset, ap=[list(a.ap[0]), [512, 3], [1, 128]])
                    P.dma("act", f"dws{s}", lambda e, kc=kc, T=T, gg=gg, src3=src3: e.dma_start(
                        out=Wd.ap()[T * 4 + gg, kc * 128:(kc + 1) * 128, :].rearrange("p (a b) -> p a b", a=3), in_=src3),
                        reads=[B_wst[s]], writes=[B_Wd])
        w_out_v = w_out.ap().rearrange("o (k p) n -> p (o k) n", p=128)
        P.dma("pool", "dwo", lambda e: e.dma_start(out=woutb[:], in_=w_out_v), writes=[B_wout])

        P.dma("sp", "dpar", lambda e: e.dma_start(out=rb[:], in_=rel_bias.ap()), writes=[B_rb])
        P.dma("sp", "dpar", lambda e: e.dma_start(out=oh, in_=c_onehot.ap()), writes=[B_oh])
        P.op("pool", lambda e: e.memset(et[:, 0:512], 0.0), writes=[B_et])
        for c in range(4):
            P.op("pe", lambda e, c=c: e.matmul(out=pj[0:12, :], lhsT=rb[:, :], rhs=oh[:, c * 512:(c + 1) * 512],
                                               start=True, stop=True), reads=[B_rb, B_oh], writes=[B_pj])
            P.op("act", lambda e, c=c: e.activation(out=et[:, 512 + c * 512:512 + (c + 1) * 512], in_=pj[0:12, :], func=AF.Exp),
                 reads=[B_pj], writes=[B_et])
        et_ap = et
        esrc = bass.AP(tensor=et_ap.tensor, offset=et_ap.offset, ap=[list(et_ap.ap[0]), [0, 129], [1, 2560]])
        P.dma("sp", "dbd", lambda e: e.dma_start(out=Bd.ap(), in_=esrc), reads=[B_et], writes=[B_Bd])

        for i, t in enumerate([diff_q_g, diff_k_g, moba_q_g, moba_k_g]):
            P.dma("sp", "dpar", lambda e, i=i, t=t: e.dma_start(out=gb[:, i, :], in_=bass.AP(tensor=t, offset=0, ap=[[0, 128], [1, 64]])),
                  writes=[B_gq])
        P.op("dve", lambda e: e.tensor_tensor(out=gqk[:, 0, :], in0=gb[:, 0, :], in1=gb[:, 1, :], op=ALU.mult), reads=[B_gq], writes=[B_gqk])
        P.op("dve", lambda e: e.tensor_tensor(out=gqk[:, 1, :], in0=gb[:, 2, :], in1=gb[:, 3, :], op=ALU.mult), reads=[B_gq], writes=[B_gqk])
        P.dma("sp", "dpar", lambda e: e.dma_start(out=sgb[:], in_=bass.AP(tensor=diff_sub_g, offset=0, ap=[[0, 128], [1, 128]])),
              writes=[B_sgb])
        P.op("dve", lambda e: e.tensor_scalar_mul(out=sgb[:], in0=sgb[:], scalar1=float(1.0 - LAMBDA_INIT)), reads=[B_sgb], writes=[B_sgb])
        P.dma("sp", "dpar", lambda e: e.dma_start(out=g2b[:], in_=bass.AP(tensor=norm2_g, offset=0, ap=[[0, 128], [1, 1024]])),
              writes=[B_g2b])
        for i, t in enumerate([lambda_q1, lambda_k1, lambda_q2, lambda_k2]):
            P.dma("sp", "dpar", lambda e, i=i, t=t: e.dma_start(out=lamv[:, i, :], in_=bass.AP(tensor=t, offset=0, ap=[[0, 128], [1, 64]])),
                  writes=[B_lamv])
        P.op("dve", lambda e: e.tensor_tensor(out=lamp[:, 0, :], in0=lamv[:, 0, :], in1=lamv[:, 1, :], op=ALU.mult), reads=[B_lamv], writes=[B_lamp])
        P.op("dve", lambda e: e.tensor_tensor(out=lamp[:, 1, :], in0=lamv[:, 2, :], in1=lamv[:, 3, :], op=ALU.mult), reads=[B_lamv], writes=[B_lamp])
        P.op("dve", lambda e: e.tensor_reduce(out=lams[:, 0:2], in_=lamp[:], axis=AX.X, op=ALU.add), reads=[B_lamp], writes=[B_lams])
        P.op("act", lambda e: e.activation(out=lams[:, 0:2], in_=lams[:, 0:2], func=AF.Exp), reads=[B_lams], writes=[B_lams])
        P.op("dve", lambda e: e.tensor_sub(out=lams[:, 2:3], in0=lams[:, 0:1], in1=lams[:, 1:2]), reads=[B_lams], writes=[B_lams])
        P.op("dve", lambda e: e.tensor_scalar(out=lams[:, 3:4], in0=lams[:, 2:3], scalar1=float(LAMBDA_INIT), scalar2=-1.0,
                                              op0=ALU.add, op1=ALU.mult), reads=[B_lams], writes=[B_lams])
        nlam = lams[:, 3:4]

        P.op("pool", lambda e: e.memset(Vt[:], 0.0), writes=[B_V])
        P.op("pool", lambda e: e.memset(Vt[:, :, 64:65], 1.0), writes=[B_V])
        P.op("pool", lambda e: e.memset(Vt[:, :, 132:133], 1.0), writes=[B_V])
        P.op("pool", lambda e: e.memset(qaug[:], 0.0), writes=[B_qaug])
        P.op("pool", lambda e: e.memset(qTa[64:128, :, :], 0.0), writes=[B_qaugrows, B_wst[0], B_wst[1]])
        P.op("pool", lambda e: e.memset(kTa[64:128, :, :], 0.0), writes=[B_kT[0], B_kT[1]])
        for m in range(2):
            P.dma("pool", "dpar", lambda e, m=m: e.dma_start(out=kTa[64:72, m, :], in_=c_blk.ap()), writes=[B_kT[m]])

        def rsqrt_act(dst, src, n_feat, rbufs, wbufs):
            P.op("act", lambda e: e.activation(out=dst, in_=src, func=AF.Ln, bias=float(RMS_EPS), scale=1.0 / n_feat),
                 reads=rbufs, writes=wbufs)
            P.op("act", lambda e: e.activation(out=dst, in_=dst, func=AF.Exp, scale=-0.5), reads=wbufs, writes=wbufs)

        pt_ctr = [0]
        e_ctr = [0]
        o_ctr = [0]
        mul_ctr = [0]
        w_ctr = [0]
        _pass = [0]
        import os as _os
        _STG = int(_os.environ.get('DBG_STAGE', '0'))

        for sq_i in range(nseq if (dbg != 1 and dbg < 50) else 0):
            tok0 = sq_i * S_LEN
            for t in range(NT):
                s = t % 2
                r0 = tok0 + t * 128
                xt = stage[s][:, 0:1024]
                P.dma("sp", f"dst{s}", lambda e, xt=xt, r0=r0: e.dma_start(out=xt, in_=x.ap()[r0:r0 + 128, :]), writes=[B_stage[s]])
                P.op("act", lambda e, xt=xt, t=t: e.activation(out=junk[:], in_=xt, func=AF.Square, accum_out=ss1[:, t:t + 1]),
                     reads=[B_stage[s]], writes=[B_junk, B_ss1])
                P.op("pool", lambda e, xt=xt: e.tensor_copy(out=xb[:], in_=xt), reads=[B_stage[s]], writes=[B_xb])
                for kc in range(8):
                    P.op("pe", lambda e, kc=kc: e.transpose(out=ptr[:, kc * 128:(kc + 1) * 128], in_=xb[:, kc * 128:(kc + 1) * 128],
                                                            identity=ident_b[:]), reads=[B_xb, B_idb], writes=[B_ptr, B_ptr1])
                P.op("dve", lambda e, t=t: e.tensor_copy(out=xT[:, :, t * 128:(t + 1) * 128],
                                                         in_=ptr[:].rearrange("p (k n) -> p k n", k=8)),
                     reads=[B_ptr, B_ptr1], writes=[B_xT[t]])
            rsqrt_act(rstd1[:], ss1[:], 1024.0, [B_ss1], [B_rstd1])
            if dbg == 2:
                break

            for g in range(8):
                is_moba = g >= 4
                qoff = (1536 if is_moba else 0) + (g % 4) * 128
                K = 72 if is_moba else 64
                gcol = 2 if is_moba else 0
                heads = [4 + 2 * (g - 4), 4 + 2 * (g - 4) + 1] if is_moba else [g]
                for gi, h in enumerate(heads):
                    P.dma("sp", f"dG{gi}", lambda e, gi=gi, h=h: e.dma_start(
                        out=G[gi][:, 0:2432], in_=bass.AP(tensor=Bd, offset=h * 129 * 2560 + 128, ap=[[2559, 128], [1, 2432]])),
                        reads=[B_Bd], writes=[B_G[gi]])
                wsl = w_ctr[0] % 2; w_ctr[0] += 1
                P.dma("sp", f"dwg{wsl}", lambda e, wsl=wsl, g=g: e.dma_start(
                    out=wg[wsl][:], in_=Wd.ap()[g].rearrange("(k p) n -> p k n", p=128)), reads=[B_Wd], writes=[B_wg[wsl]])
                for t in range(NT):
                    p = t % 2
                    p3 = t % 3; p4 = t % 4
                    pjp = [pj, Sps[0], Sps[1]][p3]; B_pjp = [B_pj, B_S[0], B_S[1]][p3]
                    qk = qk2[p4]; B_qk = B_qk2[p4]; sq = sq2b[p4]; B_sq = B_sq2b[p4]
                    ssh = ssh2[p4]; B_ssh = B_ssh2[p4]; rsh = rsh2[p4]; B_rsh = B_rsh2[p4]; qkn = qkn2[p4]; B_qkn = B_qkn2[p4]
                    B_pt = [B_ptr, B_ptr1][p]; pc0 = p * 512
                    for kc in range(8):
                        P.op("pe", lambda e, kc=kc, t=t, wsl=wsl, pjp=pjp: e.matmul(
                            out=pjp[:, 0:384], lhsT=xT[:, kc, t * 128:(t + 1) * 128], rhs=wg[wsl][:, kc, :],
                            start=(kc == 0), stop=(kc == 7)), reads=[B_xT[t], B_wg[wsl]], writes=[B_pjp])
                    P.op("dve", lambda e, t=t, pjp=pjp, qk=qk: e.tensor_scalar_mul(out=qk[:], in0=pjp[:, 0:256], scalar1=rstd1[:, t:t + 1]),
                         reads=[B_pjp, B_rstd1], writes=[B_qk])
                    P.op("dve", lambda e, t=t, pjp=pjp: e.tensor_scalar_mul(
                        out=Vt[:, t, :].rearrange("p (a b) -> p a b", a=2)[:, :, 0:64],
                        in0=pjp[:, 256:384].rearrange("p (a b) -> p a b", a=2), scalar1=rstd1[:, t:t + 1]),
                        reads=[B_pjp, B_rstd1], writes=[B_V])
                    P.op("pool", lambda e, sq=sq, qk=qk: e.tensor_tensor(out=sq[:], in0=qk[:], in1=qk[:], op=ALU.mult), reads=[B_qk], writes=[B_sq])
                    P.op("dve", lambda e, ssh=ssh, sq=sq: e.tensor_reduce(out=ssh[:], in_=sq[:].rearrange("p (a b) -> p a b", a=4), axis=AX.X, op=ALU.add),
                         reads=[B_sq], writes=[B_ssh])
                    rsqrt_act(rsh[:], ssh[:], 64.0, [B_ssh], [B_rsh])
                    for i in range(2):
                        P.op("dve", lambda e, i=i, qkn=qkn, qk=qk, rsh=rsh: e.tensor_scalar_mul(
                            out=qkn[:, i * 64:(i + 1) * 64], in0=qk[:, i * 64:(i + 1) * 64], scalar1=rsh[:, i:i + 1]),
                            reads=[B_qk, B_rsh], writes=[B_qkn])
                    for i in range(2, 4):
                        P.op("dve", lambda e, i=i, gi2=(1 if is_moba else 0), qkn=qkn, qk=qk, rsh=rsh: e.scalar_tensor_tensor(
                            out=qkn[:, i * 64:(i + 1) * 64], in0=qk[:, i * 64:(i + 1) * 64], scalar=rsh[:, i:i + 1],
                            in1=gqk[:, gi2, :], op0=ALU.mult, op1=ALU.mult), reads=[B_qk, B_rsh, B_gqk], writes=[B_qkn])
                    for i in range(4):
                        P.op("pe", lambda e, i=i, qkn=qkn, pc0=pc0: e.transpose(
                            out=ptr[0:64, pc0 + i * 128:pc0 + (i + 1) * 128], in_=qkn[:, i * 64:(i + 1) * 64],
                            identity=ident_b[:]), reads=[B_qkn, B_idb], writes=[B_pt])
                    P.op("act", lambda e, t=t, pc0=pc0: e.copy(out=qTa[0:64, :, t * 128:(t + 1) * 128],
                                                              in_=ptr[0:64, pc0:pc0 + 256].rearrange("p (a b) -> p a b", a=2)),
                         reads=[B_pt], writes=[B_qT[0][t], B_qT[1][t]])
                    P.op("act", lambda e, t=t, pc0=pc0: e.copy(out=kTa[0:64, :, t * 128:(t + 1) * 128],
                                                              in_=ptr[0:64, pc0 + 256:pc0 + 512].rearrange("p (a b) -> p a b", a=2)),
                         reads=[B_pt], writes=[B_kT[0], B_kT[1]])
                if dbg in (3, 30, 305, 31, 32, 33, 34, 35, 36, 37, 38, 39):
                    break
                if is_moba:
                    P.op("dve", lambda e: e.tensor_reduce(out=km[:], in_=kTa[0:64, :, :].rearrange("p m (a b) -> p (m a) b", a=8),
                                                          axis=AX.X, op=ALU.add), reads=[B_kT[0], B_kT[1]], writes=[B_km])
                    P.op("dve", lambda e: e.tensor_scalar_mul(out=kmf[:], in0=km[:], scalar1=1.0 / 256), reads=[B_km], writes=[B_kmf])
                    kview = kmhl[:, :, 0:8]
                    P.op("dve", lambda e: e.tensor_copy(out=kview, in_=kmf[:].rearrange("p (m a) -> p m a", m=2)),
                         reads=[B_kmf], writes=[B_kmhl])
                    P.op("dve", lambda e: e.tensor_copy(out=kmhf[:].rearrange("p (m a) -> p m a", m=2), in_=kview),
                         reads=[B_kmhl], writes=[B_kmhf])
                    P.op("dve", lambda e: e.tensor_sub(out=kmhl[:, :, 8:16], in0=kmf[:].rearrange("p (m a) -> p m a", m=2),
                                                       in1=kmhf[:].rearrange("p (m a) -> p m a", m=2)),
                         reads=[B_kmf, B_kmhf], writes=[B_kmhl])
                    for m in range(2):
                        for t in range(8, NT):
                            own = t // 2
                            P.op("pe", lambda e, m=m, t=t: e.matmul(out=pj[:, 0:16], lhsT=qTa[0:64, m, t * 128:(t + 1) * 128],
                                                                    rhs=kmhl[0:64, m, :], start=True, stop=True),
                                 reads=[B_qT[m][t], B_kmhl], writes=[B_pj])
                            P.op("act", lambda e: e.copy(out=g16[:], in_=pj[:, 0:16]), reads=[B_pj], writes=[B_g16])
                            P.op("pool", lambda e: e.memset(gate[:], -1e30), writes=[B_gate])
                            P.op("dve", lambda e, own=own: e.tensor_tensor(out=gate[:, 0:own], in0=g16[:, 0:own], in1=g16[:, 8:8 + own],
                                                                           op=ALU.add), reads=[B_g16], writes=[B_gate])
                            P.op("dve", lambda e: e.max(out=mx8[:], in_=gate[:]), reads=[B_gate], writes=[B_mx8])
                            P.op("dve", lambda e, own=own: e.tensor_scalar(out=selm[:, 0:own], in0=gate[:, 0:own], scalar1=mx8[:, 2:3],
                                                                           scalar2=None, op0=ALU.is_ge), reads=[B_gate, B_mx8], writes=[B_selm])
                            P.op("pool", lambda e: e.memset(qaug[:, 64:72], 0.0), writes=[B_qaug])
                            P.op("dve", lambda e, own=own: e.tensor_scalar(out=qaug[:, 64:64 + own], in0=selm[:, 0:own], scalar1=-1.0,
                                                                           scalar2=NEG_BIG, op0=ALU.add, op1=ALU.mult),
                                 reads=[B_selm], writes=[B_qaug])
                            P.op("pe", lambda e: e.transpose(out=ptr[:, 0:128], in_=qaug[:], identity=ident_b[:]),
                                 reads=[B_qaug, B_idb], writes=[B_ptr])
                            P.op("act", lambda e, m=m, t=t: e.copy(out=qTa[64:72, m, t * 128:(t + 1) * 128], in_=ptr[64:72, 0:128]),
                                 reads=[B_ptr], writes=[B_qT[m][t]])
                steps = []
                for qc in range(4):
                    for m in range(2):
                        for kt in range(4 * qc + 4):
                            steps.append((qc, m, kt))
                st_info = {}

                def emit_S(idx, K=K, is_moba=is_moba, steps=steps, st_info=st_info):
                    qc, m, kt = steps[idx]
                    if kt == 0:
                        st_info[(qc, m)] = o_ctr[0] % 2; o_ctr[0] += 1
                    i = e_ctr[0] % 4; e_ctr[0] += 1
                    j = pt_ctr[0] % 5; pt_ctr[0] += 1
                    gi = m if is_moba else 0
                    Sb = [Sps[0][:], Sps[1][:], pj[:], ptr[:].bitcast(F32)][i]
                    B_Sb = [[B_S[0]], [B_S[1]], [B_pj], [B_ptr, B_ptr1]][i]
                    P.op("pe", lambda e: e.matmul(
                        out=Sb, lhsT=kTa[0:K, m, kt * 128:(kt + 1) * 128], rhs=qTa[0:K, m, qc * 512:(qc + 1) * 512],
                        start=True, stop=True),
                        reads=[B_kT[m]] + [B_qT[m][qc * 4 + u] for u in range(4)] + [B_qaugrows], writes=B_Sb)
                    P.op("act", lambda e: e.activation(out=E[i][:], in_=Sb, func=AF.Exp, scale=HEAD_DIM ** -0.5),
                         reads=B_Sb, writes=[B_E[i]])
                    c0 = (4 * qc - kt + 3) * 128
                    meng = "pool" if (mul_ctr[0] % MULMOD == MULMOD - 1) else "dve"; mul_ctr[0] += 1
                    P.op(meng, lambda e: e.tensor_tensor(out=PT[j][:], in0=E[i][:], in1=G[gi][:, c0:c0 + 512], op=ALU.mult),
                         reads=[B_E[i], B_G[gi]], writes=[B_PT[j]])
                    return j

                def emit_PV(idx, j, is_moba=is_moba, steps=steps, st_info=st_info):
                    qc, m, kt = steps[idx]
                    o = st_info[(qc, m)]
                    if is_moba:
                        v0, vw = m * 68, 65
                    else:
                        v0, vw = 0, 133
                    for jq in range(4):
                        if kt > 4 * qc + jq:
                            continue
                        P.op("pe", lambda e, jq=jq: e.matmul(
                            out=Ops[o][:, jq, 0:vw], lhsT=PT[j][:, jq * 128:(jq + 1) * 128], rhs=Vt[:, kt, v0:v0 + vw],
                            start=(kt == 0 and jq in (0, 2)), stop=(kt == 4 * qc + jq), skip_group_check=True),
                            reads=[B_PT[j], B_V], writes=[B_O[o]])
                    if kt == 4 * qc + 3:
                        finalize(qc, m, o)

                def finalize(qc, m, o, g=g, is_moba=is_moba):
                    scol = 132 if not is_moba else 64
                    P.op("dve", lambda e: e.reciprocal(out=rr[:], in_=Ops[o][:, :, scol:scol + 1].rearrange("p a b -> p (a b)")),
                         reads=[B_O[o]], writes=[B_rr])
                    if not is_moba:
                        if m == 0:
                            for jq in range(4):
                                P.op("dve", lambda e, jq=jq: e.tensor_scalar_mul(
                                    out=tmp1[:, jq, :].rearrange("p (a b) -> p a b", a=2),
                                    in0=Ops[o][:, jq, 0:136].rearrange("p (a b) -> p a b", a=2)[:, :, 0:64],
                                    scalar1=rr[:, jq:jq + 1]), reads=[B_O[o], B_rr], writes=[B_tmp1])
                        else:
                            P.op("dve", lambda e: e.tensor_scalar_mul(out=nlr[:], in0=rr[:], scalar1=nlam), reads=[B_rr, B_lams], writes=[B_nlr])
                            for jq in range(4):
                                P.op("dve", lambda e, jq=jq: e.scalar_tensor_tensor(
                                    out=yd[:, jq, :].rearrange("p (a b) -> p a b", a=2),
                                    in0=Ops[o][:, jq, 0:136].rearrange("p (a b) -> p a b", a=2)[:, :, 0:64],
                                    scalar=nlr[:, jq:jq + 1], in1=tmp1[:, jq, :].rearrange("p (a b) -> p a b", a=2),
                                    op0=ALU.mult, op1=ALU.add), reads=[B_O[o], B_nlr, B_tmp1], writes=[B_yd])
                            P.op("pool", lambda e: e.tensor_tensor(out=sq2[:], in0=yd[:], in1=yd[:], op=ALU.mult), reads=[B_yd], writes=[B_sq2])
                            P.op("dve", lambda e: e.tensor_reduce(out=ss2[:], in_=sq2[:], axis=AX.X, op=ALU.add), reads=[B_sq2], writes=[B_ss2])
                            rsqrt_act(rs2[:], ss2[:], 128.0, [B_ss2], [B_rs2])
                            for jq in range(4):
                                P.op("dve", lambda e, jq=jq: e.scalar_tensor_tensor(
                                    out=ytok[:, jq, :], in0=yd[:, jq, :], scalar=rs2[:, jq:jq + 1], in1=sgb[:],
                                    op0=ALU.mult, op1=ALU.mult), reads=[B_yd, B_rs2, B_sgb], writes=[B_ytok])
                    else:
                        for jq in range(4):
                            P.op("dve", lambda e, jq=jq: e.tensor_scalar_mul(
                                out=ytok[:, jq, m * 64:(m + 1) * 64], in0=Ops[o][:, jq, 0:64], scalar1=rr[:, jq:jq + 1]),
                                reads=[B_O[o], B_rr], writes=[B_ytok])
                    if m == 1:
                        for jq in range(4):
                            P.op("pe", lambda e, jq=jq: e.transpose(out=ptr[:, jq * 128:(jq + 1) * 128], in_=ytok[:, jq, :], identity=ident_b[:]),
                                 reads=[B_ytok, B_idb], writes=[B_ptr, B_ptr1])
                        P.op("act", lambda e: e.copy(out=yT[:, g, qc * 512:(qc + 1) * 512], in_=ptr[:, 0:512]),
                             reads=[B_ptr, B_ptr1], writes=[B_yT[g][qc]])

                LOOK = 3
                jq_ = []
                for idx in range(min(LOOK, len(steps))):
                    jq_.append(emit_S(idx))
                for idx in range(len(steps)):
                    if idx + LOOK < len(steps):
                        jq_.append(emit_S(idx + LOOK))
                    emit_PV(idx, jq_[idx])
                if dbg == 4:
                    break
            if dbg in (3, 4, 30, 305, 31, 32, 33, 34, 35, 36, 37, 38, 39):
                break
            for t in range(NT):
                s = t % 2
                r0 = tok0 + t * 128
                xt = stage[s][:, 0:1024]
                P.dma("sp", f"dst{s}", lambda e, xt=xt, r0=r0: e.dma_start(out=xt, in_=x.ap()[r0:r0 + 128, :]), writes=[B_stage[s]])
                for half in range(2):
                    for kc in range(8):
                        P.op("pe", lambda e, half=half, kc=kc, t=t: e.matmul(
                            out=Sps[half][:], lhsT=yT[:, kc, t * 128:(t + 1) * 128], rhs=woutb[:, kc, half * 512:(half + 1) * 512],
                            start=(kc == 0), stop=(kc == 7)), reads=[B_yT[kc][t // 4], B_wout], writes=[B_S[half]])
                    P.op("dve", lambda e, half=half, xt=xt: e.tensor_tensor(
                        out=xt[:, half * 512:(half + 1) * 512], in0=Sps[half][:], in1=xt[:, half * 512:(half + 1) * 512], op=ALU.add),
                        reads=[B_S[half], B_stage[s]], writes=[B_stage[s]])
                if not stop_after_attn:
                    P.dma("sp", f"dout{s}", lambda e, xt=xt, r0=r0: e.dma_start(out=x1d.ap()[r0:r0 + 128, :], in_=xt),
                          reads=[B_stage[s]], writes=[B_x1d])
                if stop_after_attn:
                    ob = Buf()
                    P.dma("sp", f"dout{s}", lambda e, xt=xt, r0=r0: e.dma_start(out=out.ap()[r0:r0 + 128, :], in_=xt), reads=[B_stage[s]], writes=[ob])
                    out_bufs.append(ob)


        if not stop_after_attn and (dbg == 0 or dbg >= 50):
            P.barrier()
            acc = xT[:].rearrange("p k n -> p (k n)").bitcast(F32).rearrange("p (t n) -> p t n", t=8)
            h2Tb = yT
            Wgu = [yT[:, :, 1024:1536], yT[:, :, 1536:2048]]
            Wdn = [qTa[:, :, 0:1024], qTa[:, :, 1024:2048]]
            wst = [G[0][:, 0:2048], G[1][:, 0:2048]]
            h2f = stage[0][:, 0:1024]
            h2Tf = stage[1][:, 0:1024]
            sgs = [E[0][:, 0:256], E[0][:, 256:512]]
            hids = [PT[0][:, 0:256], PT[0][:, 256:512]]
            hidTs = [PT[1][:, 0:256], PT[1][:, 256:512]]
            Bsg = [Buf(), Buf()]; Bhid = [Buf(), Buf()]; BhidT = [Buf(), Buf()]; Bptr2 = [Buf(), Buf()]
            Rw = sb("Rw", [128, 8, 36], F32)
            lg = sb("lg", [128, 36], F32)
            wfull = sb("wfull", [128, 8, 32], F32)
            rt = sb("rt", [128, 64], F32)
            Bm = {k: Buf(k) for k in ["acc", "h2Tb", "wgu0", "wgu1", "wd0", "wd1", "wst0", "wst1", "h2f", "h2Tf", "sg", "hid",
                                      "hidT", "Rw", "lg", "wfull", "rt", "S0", "S1", "O0", "O1", "ptr", "pj", "junk", "g2b", "idb", "idf", "x1d"]}
            P.dma("sp", "dpar", lambda e: e.dma_start(out=Rw[:, :, 0:4], in_=router_group.ap().rearrange("o (k p) c -> p (o k) c", p=128)),
                  writes=[Bm["Rw"]])
            for gx in range(4):
                P.dma("sp", "dpar", lambda e, gx=gx: e.dma_start(out=Rw[:, :, 4 + 8 * gx:12 + 8 * gx],
                                                                 in_=router_expert.ap()[0, gx].rearrange("(k p) c -> p k c", p=128)),
                      writes=[Bm["Rw"]])
            Opf = [Ops[0][:].rearrange("p a b -> p (a b)"), Ops[1][:].rearrange("p a b -> p (a b)")]
            BO = [Bm["O0"], Bm["O1"]]
            BS = [Bm["S0"], Bm["S1"]]
            Bwgu = [Bm["wgu0"], Bm["wgu1"]]
            Bwd = [Bm["wd0"], Bm["wd1"]]
            Bwst = [Bm["wst0"], Bm["wst1"]]
            st_ctr = [0]
            s_ctr = [0]
            o_ctr2 = [0]
            NSB = NTOK // 1024
            for sbk in range(NSB):
                for ti in range(8):
                    if dbg == 50 and ti == 1:
                        break
                    r0 = sbk * 1024 + ti * 128
                    if dbg == 50 and _STG == 1:
                        break
                    P.dma("sp", f"dx1_{ti}", lambda e, ti=ti, r0=r0: e.dma_start(out=acc[:, ti, :], in_=x1d.ap()[r0:r0 + 128, :]),
                          reads=[Bm["x1d"]], writes=[Bm["acc"]])
                    if dbg == 50 and _STG == 2:
                        break
                    P.op("act", lambda e, ti=ti: e.activation(out=junk[:], in_=acc[:, ti, :], func=AF.Square, accum_out=rt[:, 0:1]),
                         reads=[Bm["acc"]], writes=[Bm["junk"], Bm["rt"]])
                    if dbg == 50 and _STG == 3:
                        break
                    P.op("act", lambda e: e.activation(out=rt[:, 1:2], in_=rt[:, 0:1], func=AF.Ln, bias=float(RMS_EPS), scale=1.0 / 1024),
                         reads=[Bm["rt"]], writes=[Bm["rt"]])
                    if dbg == 50 and _STG == 4:
                        break
                    P.op("act", lambda e: e.activation(out=rt[:, 1:2], in_=rt[:, 1:2], func=AF.Exp, scale=-0.5), reads=[Bm["rt"]], writes=[Bm["rt"]])
                    if dbg == 50 and _STG == 5:
                        break
                    P.op("dve", lambda e, ti=ti: e.scalar_tensor_tensor(out=h2f, in0=acc[:, ti, :], scalar=rt[:, 1:2], in1=g2b[:],
                                                                         op0=ALU.mult, op1=ALU.mult),
                         reads=[Bm["acc"], Bm["rt"], Bm["g2b"]], writes=[Bm["h2f"]])
                    if dbg == 50 and _STG == 6:
                        break
                    for kc in range(8):
                        P.op("pe", lambda e, kc=kc: e.transpose(out=Opf[1][:, kc * 128:(kc + 1) * 128], in_=h2f[:, kc * 128:(kc + 1) * 128],
                                                                identity=ident_f[:]), reads=[Bm["h2f"], Bm["idf"]], writes=[BO[1]])
                    if dbg == 50 and _STG == 7:
                        break
                    P.op("act", lambda e: e.copy(out=h2Tf, in_=Opf[1]), reads=[BO[1]], writes=[Bm["h2Tf"]])
                    if dbg == 50 and _STG == 8:
                        break
                    P.op("dve", lambda e, ti=ti: e.tensor_copy(out=h2Tb[:, :, ti * 128:(ti + 1) * 128],
                                                               in_=h2Tf.rearrange("p (k n) -> p k n", k=8)),
                         reads=[Bm["h2Tf"]], writes=[Bm["h2Tb"]])
                    if dbg == 50 and _STG == 9:
                        break
                    for kc in range(8):
                        P.op("pe", lambda e, kc=kc: e.matmul(out=pj[:, 0:36], lhsT=h2Tf[:, kc * 128:(kc + 1) * 128], rhs=Rw[:, kc, :],
                                                             start=(kc == 0), stop=(kc == 7)), reads=[Bm["h2Tf"], Bm["Rw"]], writes=[Bm["pj"]])
                    if dbg == 50 and _STG == 10:
                        break
                    P.op("act", lambda e: e.copy(out=lg[:], in_=pj[:, 0:36]), reads=[Bm["pj"]], writes=[Bm["lg"]])
                    R_ = [Bm["rt"]]; L_ = [Bm["lg"]]
                    if dbg == 50 and _STG == 11:
                        break
                    P.op("dve", lambda e: e.tensor_reduce(out=rt[:, 2:3], in_=lg[:, 0:4], axis=AX.X, op=ALU.max), reads=L_, writes=R_)
                    if dbg == 50 and _STG == 12:
                        break
                    P.op("dve", lambda e: e.tensor_scalar(out=rt[:, 8:12], in0=lg[:, 0:4], scalar1=rt[:, 2:3], scalar2=None, op0=ALU.is_equal),
                         reads=L_ + R_, writes=R_)
                    if dbg == 50 and _STG == 13:
                        break
                    P.op("dve", lambda e: e.tensor_scalar_mul(out=rt[:, 3:4], in0=rt[:, 2:3], scalar1=-1.0), reads=R_, writes=R_)
                    if dbg == 50 and _STG == 14:
                        break
                    P.op("dve", lambda e: e.tensor_scalar(out=rt[:, 12:16], in0=lg[:, 0:4], scalar1=rt[:, 2:3], scalar2=None, op0=ALU.subtract),
                         reads=L_ + R_, writes=R_)
                    if dbg == 50 and _STG == 15:
                        break
                    P.op("act", lambda e: e.activation(out=rt[:, 12:16], in_=rt[:, 12:16], func=AF.Exp, accum_out=rt[:, 4:5]),
                         reads=R_, writes=R_)
                    if dbg == 50 and _STG == 16:
                        break
                    P.op("dve", lambda e: e.reciprocal(out=rt[:, 5:6], in_=rt[:, 4:5]), reads=R_, writes=R_)
                    if dbg == 50 and _STG == 17:
                        break
                    P.op("dve", lambda e: e.tensor_scalar_mul(out=rt[:, 16:24], in0=lg[:, 4:12], scalar1=rt[:, 8:9]), reads=L_ + R_, writes=R_)
                    if dbg == 50 and _STG == 18:
                        break
                    for gx in range(1, 4):
                        P.op("dve", lambda e, gx=gx: e.scalar_tensor_tensor(out=rt[:, 16:24], in0=lg[:, 4 + 8 * gx:12 + 8 * gx],
                                                                            scalar=rt[:, 8 + gx:9 + gx], in1=rt[:, 16:24],
                                                                            op0=ALU.mult, op1=ALU.add), reads=L_ + R_, writes=R_)
                    if dbg == 50 and _STG == 19:
                        break
                    P.op("dve", lambda e: e.max(out=rt[:, 24:32], in_=rt[:, 16:24]), reads=R_, writes=R_)
                    if dbg == 50 and _STG == 20:
                        break
                    P.op("dve", lambda e: e.tensor_scalar(out=rt[:, 32:40], in0=rt[:, 16:24], scalar1=rt[:, 24:25], scalar2=None, op0=ALU.is_equal),
                         reads=R_, writes=R_)
                    if dbg == 50 and _STG == 21:
                        break
                    P.op("dve", lambda e: e.tensor_scalar(out=rt[:, 40:48], in0=rt[:, 16:24], scalar1=rt[:, 25:26], scalar2=None, op0=ALU.is_equal),
                         reads=R_, writes=R_)
                    if dbg == 50 and _STG == 22:
                        break
                    P.op("dve", lambda e: e.tensor_sub(out=rt[:, 48:49], in0=rt[:, 25:26], in1=rt[:, 24:25]), reads=R_, writes=R_)
                    if dbg == 50 and _STG == 23:
                        break
                    P.op("act", lambda e: e.activation(out=rt[:, 49:50], in_=rt[:, 48:49], func=AF.Exp), reads=R_, writes=R_)
                    if dbg == 50 and _STG == 24:
                        break
                    P.op("dve", lambda e: e.tensor_scalar_add(out=rt[:, 50:51], in0=rt[:, 49:50], scalar1=1.0), reads=R_, writes=R_)
                    if dbg == 50 and _STG == 25:
                        break
                    P.op("dve", lambda e: e.reciprocal(out=rt[:, 51:52], in_=rt[:, 50:51]), reads=R_, writes=R_)
                    if dbg == 50 and _STG == 26:
                        break
                    P.op("dve", lambda e: e.tensor_tensor(out=rt[:, 52:53], in0=rt[:, 49:50], in1=rt[:, 51:52], op=ALU.mult), reads=R_, writes=R_)
                    if dbg == 50 and _STG == 27:
                        break
                    P.op("dve", lambda e: e.tensor_scalar_mul(out=rt[:, 53:55], in0=rt[:, 51:53], scalar1=rt[:, 5:6]), reads=R_, writes=R_)
                    if dbg == 50 and _STG == 28:
                        break
                    P.op("dve", lambda e: e.tensor_scalar_mul(out=rt[:, 56:64], in0=rt[:, 32:40], scalar1=rt[:, 53:54]), reads=R_, writes=R_)
                    if dbg == 50 and _STG == 29:
                        break
                    P.op("dve", lambda e: e.scalar_tensor_tensor(out=rt[:, 56:64], in0=rt[:, 40:48], scalar=rt[:, 54:55], in1=rt[:, 56:64],
                                                                 op0=ALU.mult, op1=ALU.add), reads=R_, writes=R_)
                    if dbg == 50 and _STG == 30:
                        break
                    for gx in range(4):
                        P.op("dve", lambda e, gx=gx, ti=ti: e.tensor_scalar_mul(out=wfull[:, ti, 8 * gx:8 * gx + 8], in0=rt[:, 56:64],
                                                                                scalar1=rt[:, 8 + gx:9 + gx]), reads=R_, writes=[Bm["wfull"]])
                if dbg in (50, 51):
                    break
                for ex in range(N_EXP if dbg != 52 else 1):
                    slot = ex % 2
                    srcs = [(w_gate.ap()[0, ex].rearrange("(k p) f -> p k f", p=128), Wgu[slot][:, :, 0:256], 8, 256, Bwgu[slot]),
                            (w_up.ap()[0, ex].rearrange("(k p) f -> p k f", p=128), Wgu[slot][:, :, 256:512], 8, 256, Bwgu[slot]),
                            (w_down.ap()[0, ex].rearrange("(c p) n -> p c n", p=128), Wdn[slot], 2, 1024, Bwd[slot])]
                    for src, dst, a_, b_, bdst in srcs:
                        si = st_ctr[0] % 2; st_ctr[0] += 1
                        stv = wst[si].rearrange("p (a b) -> p a b", a=a_)
                        P.dma("sp", f"dwe{si}", lambda e, stv=stv, src=src: e.dma_start(out=stv, in_=src), writes=[Bwst[si]])
                        P.op("pool", lambda e, stv=stv, dst=dst: e.tensor_copy(out=dst, in_=stv), reads=[Bwst[si]], writes=[bdst])
                    def head(ti, i, slot=slot):
                        for kc in range(8):
                            P.op("pe", lambda e, kc=kc: e.matmul(
                                out=Sps[i][:], lhsT=h2Tb[:, kc, ti * 128:(ti + 1) * 128], rhs=Wgu[slot][:, kc, :],
                                start=(kc == 0), stop=(kc == 7)), reads=[Bm["h2Tb"], Bwgu[slot]], writes=[BS[i]])

                    def mid(ti, i, o, b, slot=slot, ex=ex):
                        sg = sgs[b]; hid = hids[b]
                        P.op("act", lambda e: e.activation(out=sg, in_=Sps[i][:, 0:256], func=AF.Silu), reads=[BS[i]], writes=[Bsg[b]])
                        P.op("dve", lambda e: e.scalar_tensor_tensor(
                            out=hid, in0=Sps[i][:, 256:512], scalar=wfull[:, ti, ex:ex + 1], in1=sg, op0=ALU.mult, op1=ALU.mult),
                            reads=[BS[i], Bm["wfull"], Bsg[b]], writes=[Bhid[b]])
                        for c in range(2):
                            P.op("pe", lambda e, c=c: e.transpose(out=ptr[:, b * 256 + c * 128:b * 256 + (c + 1) * 128],
                                                                  in_=hid[:, c * 128:(c + 1) * 128],
                                                                  identity=ident_b[:]), reads=[Bhid[b], Bm["idb"]], writes=[Bptr2[b]])
                        P.op("act", lambda e: e.copy(out=hidTs[b], in_=ptr[:, b * 256:(b + 1) * 256]), reads=[Bptr2[b]], writes=[BhidT[b]])

                    def tail(ti, i, o, b, slot=slot, ex=ex):
                        hidT = hidTs[b]
                        for half in range(2):
                            for c in range(2):
                                P.op("pe", lambda e, half=half, c=c: e.matmul(
                                    out=Opf[o][:, half * 512:(half + 1) * 512], lhsT=hidT[:, c * 128:(c + 1) * 128],
                                    rhs=Wdn[slot][:, c, half * 512:(half + 1) * 512], start=(c == 0), stop=(c == 1)),
                                    reads=[BhidT[b], Bwd[slot]], writes=[BO[o]])
                        P.op("dve", lambda e: e.tensor_tensor(out=acc[:, ti, :], in0=Opf[o], in1=acc[:, ti, :], op=ALU.add),
                             reads=[BO[o], Bm["acc"]], writes=[Bm["acc"]])

                    infos = []
                    for ti in range(8):
                        i = s_ctr[0] % 2; s_ctr[0] += 1
                        o = o_ctr2[0] % 2; o_ctr2[0] += 1
                        infos.append((ti, i, o, ti % 2))
                    for k in range(8 + 2):
                        if k < 8:
                            head(infos[k][0], infos[k][1])
                        if 0 <= k - 1 < 8:
                            mid(*infos[k - 1])
                        if 0 <= k - 2 < 8:
                            tail(*infos[k - 2])
                if dbg == 52:
                    break
                for ti in range(8):
                    r0 = sbk * 1024 + ti * 128
                    ob = Buf()
                    P.dma("sp", "dfin", lambda e, ti=ti, r0=r0: e.dma_start(out=out.ap()[r0:r0 + 128, :], in_=acc[:, ti, :]),
                          reads=[Bm["acc"]], writes=[ob])
                    out_bufs.append(ob)

        P.wait_all("sp", out_bufs)
        P.finish()
        with nc.allow_non_contiguous_dma(reason="small parameter loads"):
            P.emit()
    return nc


_PARAM_KEYS = ["norm1_g", "w_in", "diff_q_g", "diff_k_g", "lambda_q1", "lambda_k1", "lambda_q2", "lambda_k2",
               "diff_sub_g", "moba_q_g", "moba_k_g", "rel_bias", "w_out", "norm2_g", "router_group",
               "router_expert", "w_gate", "w_up", "w_down"]


def run_cores(inputs, nseq, ncores, stop_after_attn=False, dbg=0):
    nc = build(nseq, stop_after_attn=stop_after_attn, dbg=dbg)
    onehot, cmask, blk = _consts()
    xs = np.ascontiguousarray(inputs["x"], dtype=np.float32).reshape(-1, D_MODEL)
    in_maps = []
    for c in range(ncores):
        m = {k: np.ascontiguousarray(inputs[k], dtype=np.float32) for k in _PARAM_KEYS}
        m["x"] = xs[c * nseq * S_LEN:(c + 1) * nseq * S_LEN]
        m["c_onehot"] = onehot; m["c_blk"] = blk
        in_maps.append(m)
    res = run_bass_kernel_spmd(nc, in_maps, core_ids=list(range(ncores)))
    return np.concatenate([np.asarray(r["out"]) for r in res.results], axis=0)


def kernel(**inputs):
    B = inputs["x"].shape[0]
    o = run_cores(inputs, B // 8, 8)
    return o.reshape(B, S_LEN, D_MODEL).astype(np.float32)
```

```python
import math
from contextlib import ExitStack

import numpy as np
import concourse.bass as bass
import concourse.mybir as mybir
from concourse.bass_utils import run_bass_kernel_spmd

F32 = mybir.dt.float32
BF16 = mybir.dt.bfloat16
I32 = mybir.dt.int32
AF = mybir.ActivationFunctionType
ALU = mybir.AluOpType
AX = mybir.AxisListType


import os as _os0
_STRICT = bool(int(_os0.environ.get('BASS_STRICT_SYNC', '0')))


class Buf:
    __slots__ = ("w", "r", "name")

    def __init__(self, name=""):
        self.w = None
        self.r = []
        self.name = name


class Prog:
    ENG = ("pe", "act", "dve", "pool", "sp")

    def __init__(self, nc, stack):
        self.nc = nc
        self.stack = stack
        self.ops = {e: [] for e in self.ENG}
        self.sem = {}
        self.cnt = {}
        self.seen = {e: {} for e in self.ENG}
        for e in self.ENG:
            self.newsem(e)

    def newsem(self, name):
        self.sem[name] = self.stack.enter_context(self.nc.semaphore("s_" + name))
        self.cnt[name] = 0

    def _waits(self, eng, reads, writes):
        need = {}

        def add(ev, raw):
            if ev is None:
                return
            sn, v = ev
            if sn == eng:
                if eng == "pe" or (not raw and not _STRICT):
                    return
            if v > need.get(sn, 0):
                need[sn] = v

        for b in reads:
            add(b.w, True)
        for b in writes:
            add(b.w, False)
            for ev in b.r:
                add(ev, False)
        out = []
        for sn, v in need.items():
            if self.seen[eng].get(sn, 0) < v:
                self.seen[eng][sn] = v
                out.append((sn, v))
        return out

    def _mark(self, ev, reads, writes):
        for b in reads:
            b.r.append(ev)
        for b in writes:
            b.w = ev
            b.r = []

    def op(self, eng, fn, reads=(), writes=()):
        waits = self._waits(eng, reads, writes)
        self.cnt[eng] += 1
        ev = (eng, self.cnt[eng])
        self.ops[eng].append((fn, waits, (eng, 1)))
        self._mark(ev, reads, writes)
        return ev

    def dma(self, q, semname, fn, reads=(), writes=()):
        if semname == "dpar":
            self._npar = getattr(self, "_npar", 0) + 1
            semname = "dpar%d" % self._npar
        if semname not in self.sem:
            self.newsem(semname)
        waits = self._waits(q, reads, writes)
        self.cnt[semname] += 16
        ev = (semname, self.cnt[semname])
        self.ops[q].append((fn, waits, (semname, 16)))
        self._mark(ev, reads, writes)
        return ev

    def wait_all(self, eng, bufs):
        waits = self._waits(eng, bufs, ())
        self.ops[eng].append((None, waits, None))

    def barrier(self):
        waits = [(sn, v) for sn, v in self.cnt.items() if v > 0]
        for eng in self.ENG:
            w = [(sn, v) for sn, v in waits if self.seen[eng].get(sn, 0) < v and not (sn == eng and eng == "sp")]
            for sn, v in w:
                self.seen[eng][sn] = v
            self.ops[eng].append((None, w, None))

    def finish(self):
        waits = [(sn, v) for sn, v in self.cnt.items() if v > 0]
        self.ops["sp"].append((None, waits, None))

    def emit(self):
        nc = self.nc
        sem = self.sem
        ops = self.ops

        def run(e, name):
            for fn, waits, inc in ops[name]:
                for sn, v in waits:
                    e.wait_ge(sem[sn], v)
                if fn is None:
                    continue
                ins = fn(e)
                ins.then_inc(sem[inc[0]], inc[1])

        with nc.Block() as block:
            @block.tensor
            def _(e):
                run(e, "pe")

            @block.scalar
            def _(e):
                run(e, "act")

            @block.vector
            def _(e):
                run(e, "dve")

            @block.gpsimd
            def _(e):
                run(e, "pool")

            @block.sync
            def _(e):
                run(e, "sp")


S_LEN = 2048
D_MODEL = 1024
NT = 16
HEAD_DIM = 64
RMS_EPS = 1e-6
LAMBDA_INIT = 0.8 - 0.6 * math.exp(-0.3 * 0)
NEG_BIG = 30000.0
N_EXP = 32
MULMOD = 1000000
CAP_CHUNK = 128


def _t5_bucket_np(n):
    n = np.maximum(n, 0)
    max_exact = 16
    nf = np.maximum(n, 1).astype(np.float32)
    large = max_exact + (np.log(nf / np.float32(max_exact)) / np.float32(math.log(2048 / max_exact))
                         * np.float32(32 - max_exact)).astype(np.int32)
    large = np.minimum(large, 31)
    return np.where(n < max_exact, n, large)


def _consts():
    d = np.arange(2048)
    b = _t5_bucket_np(d)
    onehot = np.zeros((32, 2048), np.float32)
    onehot[b, d] = 1.0
    cmask = None
    blk = np.zeros((8, 2048), np.float32)
    for n in range(8):
        blk[n, n * 256:(n + 1) * 256] = 1.0
    return onehot, cmask, blk


def bc_inner(ap, n):
    return bass.AP(tensor=ap.tensor, offset=ap.offset, ap=[list(a) for a in ap.ap] + [[0, n]])


class _Stop(Exception):
    pass


def build(nseq, stop_after_attn=False, dbg=0):
    nc = bass.Bass("TRN2", target_bir_lowering=False)
    NTOK = nseq * S_LEN
    dt_in = lambda name, shape: nc.dram_tensor(name, shape, F32, kind="ExternalInput")
    x = dt_in("x", [NTOK, D_MODEL])
    norm1_g = dt_in("norm1_g", [1, 1024])
    w_in = dt_in("w_in", [1, 1024, 3072])
    diff_q_g = dt_in("diff_q_g", [1, 64]); diff_k_g = dt_in("diff_k_g", [1, 64])
    lambda_q1 = dt_in("lambda_q1", [1, 64]); lambda_k1 = dt_in("lambda_k1", [1, 64])
    lambda_q2 = dt_in("lambda_q2", [1, 64]); lambda_k2 = dt_in("lambda_k2", [1, 64])
    diff_sub_g = dt_in("diff_sub_g", [1, 128])
    moba_q_g = dt_in("moba_q_g", [1, 64]); moba_k_g = dt_in("moba_k_g", [1, 64])
    rel_bias = dt_in("rel_bias", [32, 12])
    w_out = dt_in("w_out", [1, 1024, 1024])
    norm2_g = dt_in("norm2_g", [1, 1024])
    router_group = dt_in("router_group", [1, 1024, 4])
    router_expert = dt_in("router_expert", [1, 4, 1024, 8])
    w_gate = dt_in("w_gate", [1, 32, 1024, 256])
    w_up = dt_in("w_up", [1, 32, 1024, 256])
    w_down = dt_in("w_down", [1, 32, 256, 1024])
    c_onehot = dt_in("c_onehot", [32, 2048])
    c_blk = dt_in("c_blk", [8, 2048])
    out = nc.dram_tensor("out", [NTOK, D_MODEL], F32, kind="ExternalOutput")
    Bd = nc.dram_tensor("Bd", [12, 129, 2560], F32, kind="Internal")
    Wd = nc.dram_tensor("Wd", [8, 1024, 384], BF16, kind="Internal")
    x1d = nc.dram_tensor("x1d", [NTOK, D_MODEL], F32, kind="Internal")

    with ExitStack() as st:
        P = Prog(nc, st)
        sb = lambda name, shape, dt: st.enter_context(nc.sbuf_tensor(name, shape, dt))
        ps = lambda name, shape, dt: st.enter_context(nc.psum_tensor(name, shape, dt))
        out_bufs = []

        Sps = [ps("Sps0", [128, 512], F32), ps("Sps1", [128, 512], F32)]
        B_S = [Buf(), Buf()]
        Ops = [ps("Ops0", [128, 4, 256], F32), ps("Ops1", [128, 4, 256], F32)]
        B_O = [Buf(), Buf()]
        pj = ps("pj", [128, 512], F32); B_pj = Buf()
        ptr = ps("ptr", [128, 1024], BF16); B_ptr = Buf(); B_ptr1 = Buf()

        ident_f = sb("ident_f", [128, 128], F32); B_idf = Buf()
        ident_b = sb("ident_b", [128, 128], BF16); B_idb = Buf()
        wg = [sb("wg0", [128, 8, 384], BF16), sb("wg1", [128, 8, 384], BF16)]; B_wg = [Buf(), Buf()]
        B_Wd = Buf()
        woutb = sb("woutb", [128, 8, 1024], BF16); B_wout = Buf()
        stage = [sb("stage0", [128, 1536], F32), sb("stage1", [128, 1536], F32)]
        B_stage = [Buf(), Buf()]
        g1c = sb("g1c", [128, 8], F32); B_g1 = Buf()
        xT = sb("xT", [128, 8, 2048], BF16); B_xT = [Buf() for _ in range(NT)]
        yT = sb("yT", [128, 8, 2048], BF16); B_yT = [[Buf() for _ in range(4)] for _ in range(8)]
        qTa = sb("qTa", [128, 2, 2048], BF16); B_qT = [[Buf() for _ in range(NT)] for _ in range(2)]
        kTa = sb("kTa", [128, 2, 2048], BF16); B_kT = [Buf(), Buf()]
        B_qaugrows = Buf()
        Vt = sb("Vt", [128, 16, 136], BF16); B_V = Buf()
        G = [sb("G0", [128, 2560], F32), sb("G1", [128, 2560], F32)]; B_G = [Buf(), Buf()]
        junk = sb("junk", [128, 1024], BF16); B_junk = Buf()
        xb = sb("xb", [128, 1024], BF16); B_xb = Buf()
        ss1 = sb("ss1", [128, 16], F32); B_ss1 = Buf()
        rstd1 = sb("rstd1", [128, 16], F32); B_rstd1 = Buf()
        NB4 = 4
        qk2 = [sb(f"bqk{i}", [128, 256], F32) for i in range(NB4)]; B_qk2 = [Buf() for _ in range(NB4)]
        sq2b = [sb(f"bsq{i}", [128, 256], F32) for i in range(NB4)]; B_sq2b = [Buf() for _ in range(NB4)]
        ssh2 = [sb(f"bssh{i}", [128, 4], F32) for i in range(NB4)]; B_ssh2 = [Buf() for _ in range(NB4)]
        rsh2 = [sb(f"brsh{i}", [128, 4], F32) for i in range(NB4)]; B_rsh2 = [Buf() for _ in range(NB4)]
        qkn2 = [sb(f"bqkn{i}", [128, 256], BF16) for i in range(NB4)]; B_qkn2 = [Buf() for _ in range(NB4)]
        E = [sb(f"E{i}", [128, 512], F32) for i in range(4)]; B_E = [Buf() for _ in range(4)]
        PT = [sb(f"PT{i}", [128, 512], BF16) for i in range(5)]; B_PT = [Buf() for _ in range(5)]
        rr = sb("rr", [128, 4], F32); B_rr = Buf()
        nlr = sb("nlr", [128, 4], F32); B_nlr = Buf()
        tmp1 = sb("tmp1", [128, 4, 128], F32); B_tmp1 = Buf()
        yd = sb("yd", [128, 4, 128], F32); B_yd = Buf()
        sq2 = sb("sq2", [128, 4, 128], F32); B_sq2 = Buf()
        ss2 = sb("ss2", [128, 4], F32); B_ss2 = Buf()
        rs2 = sb("rs2", [128, 4], F32); B_rs2 = Buf()
        ytok = sb("ytok", [128, 4, 128], BF16); B_ytok = Buf()
        sgb = sb("sgb", [128, 128], F32); B_sgb = Buf()
        g2b = sb("g2b", [128, 1024], F32); B_g2b = Buf()
        lamv = sb("lamv", [128, 4, 64], F32); B_lamv = Buf()
        lamp = sb("lamp", [128, 2, 64], F32); B_lamp = Buf()
        lams = sb("lams", [128, 4], F32); B_lams = Buf()
        gb = sb("gb", [128, 4, 64], F32); B_gq = Buf()
        gqk = sb("gqk", [128, 2, 64], F32); B_gqk = Buf()
        rb = sb("rb", [32, 12], F32); B_rb = Buf()
        oh = G[0][0:32, 0:2048]; B_oh = B_G[0]
        et = G[1][0:12, 0:2560]; B_et = B_G[1]
        B_Bd = Buf()
        B_x1d = Buf()
        km = sb("km", [64, 16], F32); B_km = Buf()
        kmf = sb("kmf", [64, 16], F32); B_kmf = Buf()
        kmhf = sb("kmhf", [64, 16], F32); B_kmhf = Buf()
        kmhl = sb("kmhl", [64, 2, 16], BF16); B_kmhl = Buf()
        g16 = sb("g16", [128, 16], F32); B_g16 = Buf()
        gate = sb("gate", [128, 8], F32); B_gate = Buf()
        mx8 = sb("mx8", [128, 8], F32); B_mx8 = Buf()
        selm = sb("selm", [128, 8], F32); B_selm = Buf()
        qaug = sb("qaug", [128, 128], BF16); B_qaug = Buf()

        P.op("pool", lambda e: e.memset(ident_f[:], 1.0), writes=[B_idf])
        P.op("pool", lambda e: e.affine_select(out=ident_f[:], in_=ident_f[:], pattern=[[-1, 128]],
                                               compare_op=ALU.is_equal, fill=0.0, base=0, channel_multiplier=1),
             reads=[B_idf], writes=[B_idf])
        P.op("dve", lambda e: e.tensor_copy(out=ident_b[:], in_=ident_f[:]), reads=[B_idf], writes=[B_idb])

        P.dma("sp", "dpar", lambda e: e.dma_start(out=g1c[:], in_=norm1_g.ap().rearrange("o (k p) -> p (o k)", p=128)),
              writes=[B_g1])
        w_in_v = w_in.ap().rearrange("o (k p) n -> p (o k) n", p=128)
        B_wst = [Buf(), Buf()]
        for kc in range(8):
            for T in range(2):
                s = T
                P.dma("sp", f"dst{s}", lambda e, kc=kc, s=s, T=T: e.dma_start(out=stage[s][:], in_=w_in_v[:, kc, T * 1536:(T + 1) * 1536]),
                      writes=[B_stage[s]])
                P.op("dve" if s == 0 else "pool",
                     lambda e, kc=kc, s=s: e.tensor_scalar_mul(out=qTa[:, s, 0:1536], in0=stage[s][:], scalar1=g1c[:, kc:kc + 1]),
                     reads=[B_stage[s], B_g1], writes=[B_wst[s]])
                for gg in range(4):
                    a = qTa[:, s, gg * 128:gg * 128 + 128]
                    src3 = bass.AP(tensor=a.tensor, offset=a.offset, ap=[list(a.ap[0]), [512, 3], [1, 128]])
                    P.dma("act", f"dws{s}", lambda e, kc=kc, T=T, gg=gg, src3=src3: e.dma_start(
                        out=Wd.ap()[T * 4 + gg, kc * 128:(kc + 1) * 128, :].rearrange("p (a b) -> p a b", a=3), in_=src3),
                        reads=[B_wst[s]], writes=[B_Wd])
        w_out_v = w_out.ap().rearrange("o (k p) n -> p (o k) n", p=128)
        P.dma("pool", "dwo", lambda e: e.dma_start(out=woutb[:], in_=w_out_v), writes=[B_wout])

        P.dma("sp", "dpar", lambda e: e.dma_start(out=rb[:], in_=rel_bias.ap()), writes=[B_rb])
        P.dma("sp", "dpar", lambda e: e.dma_start(out=oh, in_=c_onehot.ap()), writes=[B_oh])
        P.op("pool", lambda e: e.memset(et[:, 0:512], 0.0), writes=[B_et])
        for c in range(4):
            P.op("pe", lambda e, c=c: e.matmul(out=pj[0:12, :], lhsT=rb[:, :], rhs=oh[:, c * 512:(c + 1) * 512],
                                               start=True, stop=True), reads=[B_rb, B_oh], writes=[B_pj])
            P.op("act", lambda e, c=c: e.activation(out=et[:, 512 + c * 512:512 + (c + 1) * 512], in_=pj[0:12, :], func=AF.Exp),
                 reads=[B_pj], writes=[B_et])
        et_ap = et
        esrc = bass.AP(tensor=et_ap.tensor, offset=et_ap.offset, ap=[list(et_ap.ap[0]), [0, 129], [1, 2560]])
        P.dma("sp", "dbd", lambda e: e.dma_start(out=Bd.ap(), in_=esrc), reads=[B_et], writes=[B_Bd])

        for i, t in enumerate([diff_q_g, diff_k_g, moba_q_g, moba_k_g]):
            P.dma("sp", "dpar", lambda e, i=i, t=t: e.dma_start(out=gb[:, i, :], in_=bass.AP(tensor=t, offset=0, ap=[[0, 128], [1, 64]])),
                  writes=[B_gq])
        P.op("dve", lambda e: e.tensor_tensor(out=gqk[:, 0, :], in0=gb[:, 0, :], in1=gb[:, 1, :], op=ALU.mult), reads=[B_gq], writes=[B_gqk])
        P.op("dve", lambda e: e.tensor_tensor(out=gqk[:, 1, :], in0=gb[:, 2, :], in1=gb[:, 3, :], op=ALU.mult), reads=[B_gq], writes=[B_gqk])
        P.dma("sp", "dpar", lambda e: e.dma_start(out=sgb[:], in_=bass.AP(tensor=diff_sub_g, offset=0, ap=[[0, 128], [1, 128]])),
              writes=[B_sgb])
        P.op("dve", lambda e: e.tensor_scalar_mul(out=sgb[:], in0=sgb[:], scalar1=float(1.0 - LAMBDA_INIT)), reads=[B_sgb], writes=[B_sgb])
        P.dma("sp", "dpar", lambda e: e.dma_start(out=g2b[:], in_=bass.AP(tensor=norm2_g, offset=0, ap=[[0, 128], [1, 1024]])),
              writes=[B_g2b])
        for i, t in enumerate([lambda_q1, lambda_k1, lambda_q2, lambda_k2]):
            P.dma("sp", "dpar", lambda e, i=i, t=t: e.dma_start(out=lamv[:, i, :], in_=bass.AP(tensor=t, offset=0, ap=[[0, 128], [1, 64]])),
                  writes=[B_lamv])
        P.op("dve", lambda e: e.tensor_tensor(out=lamp[:, 0, :], in0=lamv[:, 0, :], in1=lamv[:, 1, :], op=ALU.mult), reads=[B_lamv], writes=[B_lamp])
        P.op("dve", lambda e: e.tensor_tensor(out=lamp[:, 1, :], in0=lamv[:, 2, :], in1=lamv[:, 3, :], op=ALU.mult), reads=[B_lamv], writes=[B_lamp])
        P.op("dve", lambda e: e.tensor_reduce(out=lams[:, 0:2], in_=lamp[:], axis=AX.X, op=ALU.add), reads=[B_lamp], writes=[B_lams])
        P.op("act", lambda e: e.activation(out=lams[:, 0:2], in_=lams[:, 0:2], func=AF.Exp), reads=[B_lams], writes=[B_lams])
        P.op("dve", lambda e: e.tensor_sub(out=lams[:, 2:3], in0=lams[:, 0:1], in1=lams[:, 1:2]), reads=[B_lams], writes=[B_lams])
        P.op("dve", lambda e: e.tensor_scalar(out=lams[:, 3:4], in0=lams[:, 2:3], scalar1=float(LAMBDA_INIT), scalar2=-1.0,
                                              op0=ALU.add, op1=ALU.mult), reads=[B_lams], writes=[B_lams])
        nlam = lams[:, 3:4]

        P.op("pool", lambda e: e.memset(Vt[:], 0.0), writes=[B_V])
        P.op("pool", lambda e: e.memset(Vt[:, :, 64:65], 1.0), writes=[B_V])
        P.op("pool", lambda e: e.memset(Vt[:, :, 132:133], 1.0), writes=[B_V])
        P.op("pool", lambda e: e.memset(qaug[:], 0.0), writes=[B_qaug])
        P.op("pool", lambda e: e.memset(qTa[64:128, :, :], 0.0), writes=[B_qaugrows, B_wst[0], B_wst[1]])
        P.op("pool", lambda e: e.memset(kTa[64:128, :, :], 0.0), writes=[B_kT[0], B_kT[1]])
        for m in range(2):
            P.dma("pool", "dpar", lambda e, m=m: e.dma_start(out=kTa[64:72, m, :], in_=c_blk.ap()), writes=[B_kT[m]])

        def rsqrt_act(dst, src, n_feat, rbufs, wbufs):
            P.op("act", lambda e: e.activation(out=dst, in_=src, func=AF.Ln, bias=float(RMS_EPS), scale=1.0 / n_feat),
                 reads=rbufs, writes=wbufs)
            P.op("act", lambda e: e.activation(out=dst, in_=dst, func=AF.Exp, scale=-0.5), reads=wbufs, writes=wbufs)

        pt_ctr = [0]
        e_ctr = [0]
        o_ctr = [0]
        mul_ctr = [0]
        w_ctr = [0]
        _pass = [0]
        import os as _os
        _STG = int(_os.environ.get('DBG_STAGE', '0'))

        for sq_i in range(nseq if (dbg != 1 and dbg < 50) else 0):
            tok0 = sq_i * S_LEN
            for t in range(NT):
                s = t % 2
                r0 = tok0 + t * 128
                xt = stage[s][:, 0:1024]
                P.dma("sp", f"dst{s}", lambda e, xt=xt, r0=r0: e.dma_start(out=xt, in_=x.ap()[r0:r0 + 128, :]), writes=[B_stage[s]])
                P.op("act", lambda e, xt=xt, t=t: e.activation(out=junk[:], in_=xt, func=AF.Square, accum_out=ss1[:, t:t + 1]),
                     reads=[B_stage[s]], writes=[B_junk, B_ss1])
                P.op("pool", lambda e, xt=xt: e.tensor_copy(out=xb[:], in_=xt), reads=[B_stage[s]], writes=[B_xb])
                for kc in range(8):
                    P.op("pe", lambda e, kc=kc: e.transpose(out=ptr[:, kc * 128:(kc + 1) * 128], in_=xb[:, kc * 128:(kc + 1) * 128],
                                                            identity=ident_b[:]), reads=[B_xb, B_idb], writes=[B_ptr, B_ptr1])
                P.op("dve", lambda e, t=t: e.tensor_copy(out=xT[:, :, t * 128:(t + 1) * 128],
                                                         in_=ptr[:].rearrange("p (k n) -> p k n", k=8)),
                     reads=[B_ptr, B_ptr1], writes=[B_xT[t]])
            rsqrt_act(rstd1[:], ss1[:], 1024.0, [B_ss1], [B_rstd1])
            if dbg == 2:
                break

            for g in range(8):
                is_moba = g >= 4
                qoff = (1536 if is_moba else 0) + (g % 4) * 128
                K = 72 if is_moba else 64
                gcol = 2 if is_moba else 0
                heads = [4 + 2 * (g - 4), 4 + 2 * (g - 4) + 1] if is_moba else [g]
                for gi, h in enumerate(heads):
                    P.dma("sp", f"dG{gi}", lambda e, gi=gi, h=h: e.dma_start(
                        out=G[gi][:, 0:2432], in_=bass.AP(tensor=Bd, offset=h * 129 * 2560 + 128, ap=[[2559, 128], [1, 2432]])),
                        reads=[B_Bd], writes=[B_G[gi]])
                wsl = w_ctr[0] % 2; w_ctr[0] += 1
                P.dma("sp", f"dwg{wsl}", lambda e, wsl=wsl, g=g: e.dma_start(
                    out=wg[wsl][:], in_=Wd.ap()[g].rearrange("(k p) n -> p k n", p=128)), reads=[B_Wd], writes=[B_wg[wsl]])
                def bsel(t):
                    p = t % 2; p3 = t % 3; p4 = t % 4
                    return dict(pjp=[pj, Sps[0], Sps[1]][p3], B_pjp=[B_pj, B_S[0], B_S[1]][p3],
                                qk=qk2[p4], B_qk=B_qk2[p4], sq=sq2b[p4], B_sq=B_sq2b[p4], ssh=ssh2[p4], B_ssh=B_ssh2[p4],
                                rsh=rsh2[p4], B_rsh=B_rsh2[p4], qkn=qkn2[p4], B_qkn=B_qkn2[p4],
                                B_pt=[B_ptr, B_ptr1], pc0=p * 512)

                def st0(t, wsl=wsl):
                    d = bsel(t); pjp = d["pjp"]
                    for kc in range(8):
                        P.op("pe", lambda e, kc=kc: e.matmul(
                            out=pjp[:, 0:384], lhsT=xT[:, kc, t * 128:(t + 1) * 128], rhs=wg[wsl][:, kc, :],
                            start=(kc == 0), stop=(kc == 7)), reads=[B_xT[t], B_wg[wsl]], writes=[d["B_pjp"]])

                def st1(t):
                    d = bsel(t); pjp = d["pjp"]; qk = d["qk"]; sq = d["sq"]
                    P.op("dve", lambda e: e.tensor_scalar_mul(out=qk[:], in0=pjp[:, 0:256], scalar1=rstd1[:, t:t + 1]),
                         reads=[d["B_pjp"], B_rstd1], writes=[d["B_qk"]])
                    P.op("dve", lambda e: e.tensor_scalar_mul(
                        out=Vt[:, t, :].rearrange("p (a b) -> p a b", a=2)[:, :, 0:64],
                        in0=pjp[:, 256:384].rearrange("p (a b) -> p a b", a=2), scalar1=rstd1[:, t:t + 1]),
                        reads=[d["B_pjp"], B_rstd1], writes=[B_V])
                    P.op("pool", lambda e: e.tensor_tensor(out=sq[:], in0=qk[:], in1=qk[:], op=ALU.mult), reads=[d["B_qk"]], writes=[d["B_sq"]])

                def st2(t):
                    d = bsel(t); sq = d["sq"]; ssh = d["ssh"]; rsh = d["rsh"]
                    P.op("dve", lambda e: e.tensor_reduce(out=ssh[:], in_=sq[:].rearrange("p (a b) -> p a b", a=4), axis=AX.X, op=ALU.add),
                         reads=[d["B_sq"]], writes=[d["B_ssh"]])
                    rsqrt_act(rsh[:], ssh[:], 64.0, [d["B_ssh"]], [d["B_rsh"]])

                def st3(t, gi2=(1 if is_moba else 0)):
                    d = bsel(t); qk = d["qk"]; rsh = d["rsh"]; qkn = d["qkn"]
                    for i in range(2):
                        P.op("dve", lambda e, i=i: e.tensor_scalar_mul(
                            out=qkn[:, i * 64:(i + 1) * 64], in0=qk[:, i * 64:(i + 1) * 64], scalar1=rsh[:, i:i + 1]),
                            reads=[d["B_qk"], d["B_rsh"]], writes=[d["B_qkn"]])
                    for i in range(2, 4):
                        P.op("dve", lambda e, i=i: e.scalar_tensor_tensor(
                            out=qkn[:, i * 64:(i + 1) * 64], in0=qk[:, i * 64:(i + 1) * 64], scalar=rsh[:, i:i + 1],
                            in1=gqk[:, gi2, :], op0=ALU.mult, op1=ALU.mult), reads=[d["B_qk"], d["B_rsh"], B_gqk], writes=[d["B_qkn"]])

                def st4(t):
                    d = bsel(t); qkn = d["qkn"]; pc0 = d["pc0"]
                    for i in range(4):
                        P.op("pe", lambda e, i=i: e.transpose(
                            out=ptr[0:64, pc0 + i * 128:pc0 + (i + 1) * 128], in_=qkn[:, i * 64:(i + 1) * 64],
                            identity=ident_b[:]), reads=[d["B_qkn"], B_idb], writes=d["B_pt"])

                def st5(t):
                    d = bsel(t); pc0 = d["pc0"]
                    P.op("act", lambda e: e.copy(out=qTa[0:64, :, t * 128:(t + 1) * 128],
                                                 in_=ptr[0:64, pc0:pc0 + 256].rearrange("p (a b) -> p a b", a=2)),
                         reads=d["B_pt"], writes=[B_qT[0][t], B_qT[1][t]])
                    P.op("act", lambda e: e.copy(out=kTa[0:64, :, t * 128:(t + 1) * 128],
                                                 in_=ptr[0:64, pc0 + 256:pc0 + 512].rearrange("p (a b) -> p a b", a=2)),
                         reads=d["B_pt"], writes=[B_kT[0], B_kT[1]])

                stages_b = [(st0, 0), (st1, 1), (st2, 2), (st3, 3), (st5, 5), (st4, 4)]
                for kk in range(NT + 5):
                    for fn, lag in stages_b:
                        tt = kk - lag
                        if 0 <= tt < NT:
                            fn(tt)
                if dbg in (3, 30, 305, 31, 32, 33, 34, 35, 36, 37, 38, 39):
                    break
                if is_moba:
                    P.op("dve", lambda e: e.tensor_reduce(out=km[:], in_=kTa[0:64, :, :].rearrange("p m (a b) -> p (m a) b", a=8),
                                                          axis=AX.X, op=ALU.add), reads=[B_kT[0], B_kT[1]], writes=[B_km])
                    P.op("dve", lambda e: e.tensor_scalar_mul(out=kmf[:], in0=km[:], scalar1=1.0 / 256), reads=[B_km], writes=[B_kmf])
                    kview = kmhl[:, :, 0:8]
                    P.op("dve", lambda e: e.tensor_copy(out=kview, in_=kmf[:].rearrange("p (m a) -> p m a", m=2)),
                         reads=[B_kmf], writes=[B_kmhl])
                    P.op("dve", lambda e: e.tensor_copy(out=kmhf[:].rearrange("p (m a) -> p m a", m=2), in_=kview),
                         reads=[B_kmhl], writes=[B_kmhf])
                    P.op("dve", lambda e: e.tensor_sub(out=kmhl[:, :, 8:16], in0=kmf[:].rearrange("p (m a) -> p m a", m=2),
                                                       in1=kmhf[:].rearrange("p (m a) -> p m a", m=2)),
                         reads=[B_kmf, B_kmhf], writes=[B_kmhl])
                    for m in range(2):
                        for t in range(8, NT):
                            own = t // 2
                            P.op("pe", lambda e, m=m, t=t: e.matmul(out=pj[:, 0:16], lhsT=qTa[0:64, m, t * 128:(t + 1) * 128],
                                                                    rhs=kmhl[0:64, m, :], start=True, stop=True),
                                 reads=[B_qT[m][t], B_kmhl], writes=[B_pj])
                            P.op("act", lambda e: e.copy(out=g16[:], in_=pj[:, 0:16]), reads=[B_pj], writes=[B_g16])
                            P.op("pool", lambda e: e.memset(gate[:], -1e30), writes=[B_gate])
                            P.op("dve", lambda e, own=own: e.tensor_tensor(out=gate[:, 0:own], in0=g16[:, 0:own], in1=g16[:, 8:8 + own],
                                                                           op=ALU.add), reads=[B_g16], writes=[B_gate])
                            P.op("dve", lambda e: e.max(out=mx8[:], in_=gate[:]), reads=[B_gate], writes=[B_mx8])
                            P.op("dve", lambda e, own=own: e.tensor_scalar(out=selm[:, 0:own], in0=gate[:, 0:own], scalar1=mx8[:, 2:3],
                                                                           scalar2=None, op0=ALU.is_ge), reads=[B_gate, B_mx8], writes=[B_selm])
                            P.op("pool", lambda e: e.memset(qaug[:, 64:72], 0.0), writes=[B_qaug])
                            P.op("dve", lambda e, own=own: e.tensor_scalar(out=qaug[:, 64:64 + own], in0=selm[:, 0:own], scalar1=-1.0,
                                                                           scalar2=NEG_BIG, op0=ALU.add, op1=ALU.mult),
                                 reads=[B_selm], writes=[B_qaug])
                            P.op("pe", lambda e: e.transpose(out=ptr[:, 0:128], in_=qaug[:], identity=ident_b[:]),
                                 reads=[B_qaug, B_idb], writes=[B_ptr])
                            P.op("act", lambda e, m=m, t=t: e.copy(out=qTa[64:72, m, t * 128:(t + 1) * 128], in_=ptr[64:72, 0:128]),
                                 reads=[B_ptr], writes=[B_qT[m][t]])
                steps = []
                for qc in range(4):
                    for m in range(2):
                        for kt in range(4 * qc + 4):
                            steps.append((qc, m, kt))
                st_info = {}

                def emit_S(idx, K=K, is_moba=is_moba, steps=steps, st_info=st_info):
                    qc, m, kt = steps[idx]
                    if kt == 0:
                        st_info[(qc, m)] = o_ctr[0] % 2; o_ctr[0] += 1
                    i = e_ctr[0] % 4; e_ctr[0] += 1
                    j = pt_ctr[0] % 5; pt_ctr[0] += 1
                    gi = m if is_moba else 0
                    Sb = [Sps[0][:], Sps[1][:], pj[:], ptr[:].bitcast(F32)][i]
                    B_Sb = [[B_S[0]], [B_S[1]], [B_pj], [B_ptr, B_ptr1]][i]
                    P.op("pe", lambda e: e.matmul(
                        out=Sb, lhsT=kTa[0:K, m, kt * 128:(kt + 1) * 128], rhs=qTa[0:K, m, qc * 512:(qc + 1) * 512],
                        start=True, stop=True),
                        reads=[B_kT[m]] + [B_qT[m][qc * 4 + u] for u in range(4)] + [B_qaugrows], writes=B_Sb)
                    P.op("act", lambda e: e.activation(out=E[i][:], in_=Sb, func=AF.Exp, scale=HEAD_DIM ** -0.5),
                         reads=B_Sb, writes=[B_E[i]])
                    c0 = (4 * qc - kt + 3) * 128
                    meng = "pool" if (mul_ctr[0] % MULMOD == MULMOD - 1) else "dve"; mul_ctr[0] += 1
                    P.op(meng, lambda e: e.tensor_tensor(out=PT[j][:], in0=E[i][:], in1=G[gi][:, c0:c0 + 512], op=ALU.mult),
                         reads=[B_E[i], B_G[gi]], writes=[B_PT[j]])
                    return j

                def emit_PV(idx, j, is_moba=is_moba, steps=steps, st_info=st_info):
                    qc, m, kt = steps[idx]
                    o = st_info[(qc, m)]
                    if is_moba:
                        v0, vw = m * 68, 65
                    else:
                        v0, vw = 0, 133
                    for jq in range(4):
                        if kt > 4 * qc + jq:
                            continue
                        P.op("pe", lambda e, jq=jq: e.matmul(
                            out=Ops[o][:, jq, 0:vw], lhsT=PT[j][:, jq * 128:(jq + 1) * 128], rhs=Vt[:, kt, v0:v0 + vw],
                            start=(kt == 0 and jq in (0, 2)), stop=(kt == 4 * qc + jq), skip_group_check=True),
                            reads=[B_PT[j], B_V], writes=[B_O[o]])
                    if kt == 4 * qc + 3:
                        finalize(qc, m, o)

                def finalize(qc, m, o, g=g, is_moba=is_moba):
                    scol = 132 if not is_moba else 64
                    P.op("dve", lambda e: e.reciprocal(out=rr[:], in_=Ops[o][:, :, scol:scol + 1].rearrange("p a b -> p (a b)")),
                         reads=[B_O[o]], writes=[B_rr])
                    if not is_moba:
                        if m == 0:
                            for jq in range(4):
                                P.op("dve", lambda e, jq=jq: e.tensor_scalar_mul(
                                    out=tmp1[:, jq, :].rearrange("p (a b) -> p a b", a=2),
                                    in0=Ops[o][:, jq, 0:136].rearrange("p (a b) -> p a b", a=2)[:, :, 0:64],
                                    scalar1=rr[:, jq:jq + 1]), reads=[B_O[o], B_rr], writes=[B_tmp1])
                        else:
                            P.op("dve", lambda e: e.tensor_scalar_mul(out=nlr[:], in0=rr[:], scalar1=nlam), reads=[B_rr, B_lams], writes=[B_nlr])
                            for jq in range(4):
                                P.op("dve", lambda e, jq=jq: e.scalar_tensor_tensor(
                                    out=yd[:, jq, :].rearrange("p (a b) -> p a b", a=2),
                                    in0=Ops[o][:, jq, 0:136].rearrange("p (a b) -> p a b", a=2)[:, :, 0:64],
                                    scalar=nlr[:, jq:jq + 1], in1=tmp1[:, jq, :].rearrange("p (a b) -> p a b", a=2),
                                    op0=ALU.mult, op1=ALU.add), reads=[B_O[o], B_nlr, B_tmp1], writes=[B_yd])
                            P.op("pool", lambda e: e.tensor_tensor(out=sq2[:], in0=yd[:], in1=yd[:], op=ALU.mult), reads=[B_yd], writes=[B_sq2])
                            P.op("dve", lambda e: e.tensor_reduce(out=ss2[:], in_=sq2[:], axis=AX.X, op=ALU.add), reads=[B_sq2], writes=[B_ss2])
                            rsqrt_act(rs2[:], ss2[:], 128.0, [B_ss2], [B_rs2])
                            for jq in range(4):
                                P.op("dve", lambda e, jq=jq: e.scalar_tensor_tensor(
                                    out=ytok[:, jq, :], in0=yd[:, jq, :], scalar=rs2[:, jq:jq + 1], in1=sgb[:],
                                    op0=ALU.mult, op1=ALU.mult), reads=[B_yd, B_rs2, B_sgb], writes=[B_ytok])
                    else:
                        for jq in range(4):
                            P.op("dve", lambda e, jq=jq: e.tensor_scalar_mul(
                                out=ytok[:, jq, m * 64:(m + 1) * 64], in0=Ops[o][:, jq, 0:64], scalar1=rr[:, jq:jq + 1]),
                                reads=[B_O[o], B_rr], writes=[B_ytok])
                    if m == 1:
                        for jq in range(4):
                            P.op("pe", lambda e, jq=jq: e.transpose(out=ptr[:, jq * 128:(jq + 1) * 128], in_=ytok[:, jq, :], identity=ident_b[:]),
                                 reads=[B_ytok, B_idb], writes=[B_ptr, B_ptr1])
                        P.op("act", lambda e: e.copy(out=yT[:, g, qc * 512:(qc + 1) * 512], in_=ptr[:, 0:512]),
                             reads=[B_ptr, B_ptr1], writes=[B_yT[g][qc]])

                LOOK = 3
                jq_ = []
                for idx in range(min(LOOK, len(steps))):
                    jq_.append(emit_S(idx))
                for idx in range(len(steps)):
                    if idx + LOOK < len(steps):
                        jq_.append(emit_S(idx + LOOK))
                    emit_PV(idx, jq_[idx])
                if dbg == 4:
                    break
            if dbg in (3, 4, 30, 305, 31, 32, 33, 34, 35, 36, 37, 38, 39):
                break
            for t in range(NT):
                s = t % 2
                r0 = tok0 + t * 128
                xt = stage[s][:, 0:1024]
                P.dma("sp", f"dst{s}", lambda e, xt=xt, r0=r0: e.dma_start(out=xt, in_=x.ap()[r0:r0 + 128, :]), writes=[B_stage[s]])
                for half in range(2):
                    for kc in range(8):
                        P.op("pe", lambda e, half=half, kc=kc, t=t: e.matmul(
                            out=Sps[half][:], lhsT=yT[:, kc, t * 128:(t + 1) * 128], rhs=woutb[:, kc, half * 512:(half + 1) * 512],
                            start=(kc == 0), stop=(kc == 7)), reads=[B_yT[kc][t // 4], B_wout], writes=[B_S[half]])
                    P.op("dve", lambda e, half=half, xt=xt: e.tensor_tensor(
                        out=xt[:, half * 512:(half + 1) * 512], in0=Sps[half][:], in1=xt[:, half * 512:(half + 1) * 512], op=ALU.add),
                        reads=[B_S[half], B_stage[s]], writes=[B_stage[s]])
                if not stop_after_attn:
                    P.dma("sp", f"dout{s}", lambda e, xt=xt, r0=r0: e.dma_start(out=x1d.ap()[r0:r0 + 128, :], in_=xt),
                          reads=[B_stage[s]], writes=[B_x1d])
                if stop_after_attn:
                    ob = Buf()
                    P.dma("sp", f"dout{s}", lambda e, xt=xt, r0=r0: e.dma_start(out=out.ap()[r0:r0 + 128, :], in_=xt), reads=[B_stage[s]], writes=[ob])
                    out_bufs.append(ob)


        if not stop_after_attn and (dbg == 0 or dbg >= 50):
            P.barrier()
            acc = xT[:].rearrange("p k n -> p (k n)").bitcast(F32).rearrange("p (t n) -> p t n", t=8)
            h2Tb = yT
            Wgu = [yT[:, :, 1024:1536], yT[:, :, 1536:2048]]
            Wdn = [qTa[:, :, 0:1024], qTa[:, :, 1024:2048]]
            wst = [G[0][:, 0:2048], G[1][:, 0:2048]]
            h2f = stage[0][:, 0:1024]
            h2Tf = stage[1][:, 0:1024]
            sgs = [E[0][:, 0:256], E[0][:, 256:512]]
            hids = [PT[0][:, 0:256], PT[0][:, 256:512]]
            hidTs = [PT[1][:, 0:256], PT[1][:, 256:512]]
            Bsg = [Buf(), Buf()]; Bhid = [Buf(), Buf()]; BhidT = [Buf(), Buf()]; _bp = Buf(); Bptr2 = [_bp, _bp]
            Rw = sb("Rw", [128, 8, 36], F32)
            lg = sb("lg", [128, 36], F32)
            wfull = sb("wfull", [128, 8, 32], F32)
            rt = sb("rt", [128, 64], F32)
            Bm = {k: Buf(k) for k in ["acc", "h2Tb", "wgu0", "wgu1", "wd0", "wd1", "wst0", "wst1", "h2f", "h2Tf", "sg", "hid",
                                      "hidT", "Rw", "lg", "wfull", "rt", "S0", "S1", "O0", "O1", "ptr", "pj", "junk", "g2b", "idb", "idf", "x1d"]}
            P.dma("sp", "dpar", lambda e: e.dma_start(out=Rw[:, :, 0:4], in_=router_group.ap().rearrange("o (k p) c -> p (o k) c", p=128)),
                  writes=[Bm["Rw"]])
            for gx in range(4):
                P.dma("sp", "dpar", lambda e, gx=gx: e.dma_start(out=Rw[:, :, 4 + 8 * gx:12 + 8 * gx],
                                                                 in_=router_expert.ap()[0, gx].rearrange("(k p) c -> p k c", p=128)),
                      writes=[Bm["Rw"]])
            Opf = [Ops[0][:].rearrange("p a b -> p (a b)"), Ops[1][:].rearrange("p a b -> p (a b)")]
            BO = [Bm["O0"], Bm["O1"]]
            BS = [Bm["S0"], Bm["S1"]]
            Bwgu = [Bm["wgu0"], Bm["wgu1"]]
            Bwd = [Bm["wd0"], Bm["wd1"]]
            Bwst = [Bm["wst0"], Bm["wst1"]]
            st_ctr = [0]
            s_ctr = [0]
            o_ctr2 = [0]
            NSB = NTOK // 1024
            for sbk in range(NSB):
                for ti in range(8):
                    if dbg == 50 and ti == 1:
                        break
                    r0 = sbk * 1024 + ti * 128
                    if dbg == 50 and _STG == 1:
                        break
                    P.dma("sp", f"dx1_{ti}", lambda e, ti=ti, r0=r0: e.dma_start(out=acc[:, ti, :], in_=x1d.ap()[r0:r0 + 128, :]),
                          reads=[Bm["x1d"]], writes=[Bm["acc"]])
                    if dbg == 50 and _STG == 2:
                        break
                    P.op("act", lambda e, ti=ti: e.activation(out=junk[:], in_=acc[:, ti, :], func=AF.Square, accum_out=rt[:, 0:1]),
                         reads=[Bm["acc"]], writes=[Bm["junk"], Bm["rt"]])
                    if dbg == 50 and _STG == 3:
                        break
                    P.op("act", lambda e: e.activation(out=rt[:, 1:2], in_=rt[:, 0:1], func=AF.Ln, bias=float(RMS_EPS), scale=1.0 / 1024),
                         reads=[Bm["rt"]], writes=[Bm["rt"]])
                    if dbg == 50 and _STG == 4:
                        break
                    P.op("act", lambda e: e.activation(out=rt[:, 1:2], in_=rt[:, 1:2], func=AF.Exp, scale=-0.5), reads=[Bm["rt"]], writes=[Bm["rt"]])
                    if dbg == 50 and _STG == 5:
                        break
                    P.op("dve", lambda e, ti=ti: e.scalar_tensor_tensor(out=h2f, in0=acc[:, ti, :], scalar=rt[:, 1:2], in1=g2b[:],
                                                                         op0=ALU.mult, op1=ALU.mult),
                         reads=[Bm["acc"], Bm["rt"], Bm["g2b"]], writes=[Bm["h2f"]])
                    if dbg == 50 and _STG == 6:
                        break
                    for kc in range(8):
                        P.op("pe", lambda e, kc=kc: e.transpose(out=Opf[1][:, kc * 128:(kc + 1) * 128], in_=h2f[:, kc * 128:(kc + 1) * 128],
                                                                identity=ident_f[:]), reads=[Bm["h2f"], Bm["idf"]], writes=[BO[1]])
                    if dbg == 50 and _STG == 7:
                        break
                    P.op("act", lambda e: e.copy(out=h2Tf, in_=Opf[1]), reads=[BO[1]], writes=[Bm["h2Tf"]])
                    if dbg == 50 and _STG == 8:
                        break
                    P.op("dve", lambda e, ti=ti: e.tensor_copy(out=h2Tb[:, :, ti * 128:(ti + 1) * 128],
                                                               in_=h2Tf.rearrange("p (k n) -> p k n", k=8)),
                         reads=[Bm["h2Tf"]], writes=[Bm["h2Tb"]])
                    if dbg == 50 and _STG == 9:
                        break
                    for kc in range(8):
                        P.op("pe", lambda e, kc=kc: e.matmul(out=pj[:, 0:36], lhsT=h2Tf[:, kc * 128:(kc + 1) * 128], rhs=Rw[:, kc, :],
                                                             start=(kc == 0), stop=(kc == 7)), reads=[Bm["h2Tf"], Bm["Rw"]], writes=[Bm["pj"]])
                    if dbg == 50 and _STG == 10:
                        break
                    P.op("act", lambda e: e.copy(out=lg[:], in_=pj[:, 0:36]), reads=[Bm["pj"]], writes=[Bm["lg"]])
                    R_ = [Bm["rt"]]; L_ = [Bm["lg"]]
                    if dbg == 50 and _STG == 11:
                        break
                    P.op("dve", lambda e: e.tensor_reduce(out=rt[:, 2:3], in_=lg[:, 0:4], axis=AX.X, op=ALU.max), reads=L_, writes=R_)
                    if dbg == 50 and _STG == 12:
                        break
                    P.op("dve", lambda e: e.tensor_scalar(out=rt[:, 8:12], in0=lg[:, 0:4], scalar1=rt[:, 2:3], scalar2=None, op0=ALU.is_equal),
                         reads=L_ + R_, writes=R_)
                    if dbg == 50 and _STG == 13:
                        break
                    P.op("dve", lambda e: e.tensor_scalar_mul(out=rt[:, 3:4], in0=rt[:, 2:3], scalar1=-1.0), reads=R_, writes=R_)
                    if dbg == 50 and _STG == 14:
                        break
                    P.op("dve", lambda e: e.tensor_scalar(out=rt[:, 12:16], in0=lg[:, 0:4], scalar1=rt[:, 2:3], scalar2=None, op0=ALU.subtract),
                         reads=L_ + R_, writes=R_)
                    if dbg == 50 and _STG == 15:
                        break
                    P.op("act", lambda e: e.activation(out=rt[:, 12:16], in_=rt[:, 12:16], func=AF.Exp, accum_out=rt[:, 4:5]),
                         reads=R_, writes=R_)
                    if dbg == 50 and _STG == 16:
                        break
                    P.op("dve", lambda e: e.reciprocal(out=rt[:, 5:6], in_=rt[:, 4:5]), reads=R_, writes=R_)
                    if dbg == 50 and _STG == 17:
                        break
                    P.op("dve", lambda e: e.tensor_scalar_mul(out=rt[:, 16:24], in0=lg[:, 4:12], scalar1=rt[:, 8:9]), reads=L_ + R_, writes=R_)
                    if dbg == 50 and _STG == 18:
                        break
                    for gx in range(1, 4):
                        P.op("dve", lambda e, gx=gx: e.scalar_tensor_tensor(out=rt[:, 16:24], in0=lg[:, 4 + 8 * gx:12 + 8 * gx],
                                                                            scalar=rt[:, 8 + gx:9 + gx], in1=rt[:, 16:24],
                                                                            op0=ALU.mult, op1=ALU.add), reads=L_ + R_, writes=R_)
                    if dbg == 50 and _STG == 19:
                        break
                    P.op("dve", lambda e: e.max(out=rt[:, 24:32], in_=rt[:, 16:24]), reads=R_, writes=R_)
                    if dbg == 50 and _STG == 20:
                        break
                    P.op("dve", lambda e: e.tensor_scalar(out=rt[:, 32:40], in0=rt[:, 16:24], scalar1=rt[:, 24:25], scalar2=None, op0=ALU.is_equal),
                         reads=R_, writes=R_)
                    if dbg == 50 and _STG == 21:
                        break
                    P.op("dve", lambda e: e.tensor_scalar(out=rt[:, 40:48], in0=rt[:, 16:24], scalar1=rt[:, 25:26], scalar2=None, op0=ALU.is_equal),
                         reads=R_, writes=R_)
                    if dbg == 50 and _STG == 22:
                        break
                    P.op("dve", lambda e: e.tensor_sub(out=rt[:, 48:49], in0=rt[:, 25:26], in1=rt[:, 24:25]), reads=R_, writes=R_)
                    if dbg == 50 and _STG == 23:
                        break
                    P.op("act", lambda e: e.activation(out=rt[:, 49:50], in_=rt[:, 48:49], func=AF.Exp), reads=R_, writes=R_)
                    if dbg == 50 and _STG == 24:
                        break
                    P.op("dve", lambda e: e.tensor_scalar_add(out=rt[:, 50:51], in0=rt[:, 49:50], scalar1=1.0), reads=R_, writes=R_)
                    if dbg == 50 and _STG == 25:
                        break
                    P.op("dve", lambda e: e.reciprocal(out=rt[:, 51:52], in_=rt[:, 50:51]), reads=R_, writes=R_)
                    if dbg == 50 and _STG == 26:
                        break
                    P.op("dve", lambda e: e.tensor_tensor(out=rt[:, 52:53], in0=rt[:, 49:50], in1=rt[:, 51:52], op=ALU.mult), reads=R_, writes=R_)
                    if dbg == 50 and _STG == 27:
                        break
                    P.op("dve", lambda e: e.tensor_scalar_mul(out=rt[:, 53:55], in0=rt[:, 51:53], scalar1=rt[:, 5:6]), reads=R_, writes=R_)
                    if dbg == 50 and _STG == 28:
                        break
                    P.op("dve", lambda e: e.tensor_scalar_mul(out=rt[:, 56:64], in0=rt[:, 32:40], scalar1=rt[:, 53:54]), reads=R_, writes=R_)
                    if dbg == 50 and _STG == 29:
                        break
                    P.op("dve", lambda e: e.scalar_tensor_tensor(out=rt[:, 56:64], in0=rt[:, 40:48], scalar=rt[:, 54:55], in1=rt[:, 56:64],
                                                                 op0=ALU.mult, op1=ALU.add), reads=R_, writes=R_)
                    if dbg == 50 and _STG == 30:
                        break
                    for gx in range(4):
                        P.op("dve", lambda e, gx=gx, ti=ti: e.tensor_scalar_mul(out=wfull[:, ti, 8 * gx:8 * gx + 8], in0=rt[:, 56:64],
                                                                                scalar1=rt[:, 8 + gx:9 + gx]), reads=R_, writes=[Bm["wfull"]])
                if dbg in (50, 51):
                    break
                for ex in range(N_EXP if dbg != 52 else 1):
                    slot = ex % 2
                    srcs = [(w_gate.ap()[0, ex].rearrange("(k p) f -> p k f", p=128), Wgu[slot][:, :, 0:256], 8, 256, Bwgu[slot]),
                            (w_up.ap()[0, ex].rearrange("(k p) f -> p k f", p=128), Wgu[slot][:, :, 256:512], 8, 256, Bwgu[slot]),
                            (w_down.ap()[0, ex].rearrange("(c p) n -> p c n", p=128), Wdn[slot], 2, 1024, Bwd[slot])]
                    for src, dst, a_, b_, bdst in srcs:
                        si = st_ctr[0] % 2; st_ctr[0] += 1
                        stv = wst[si].rearrange("p (a b) -> p a b", a=a_)
                        P.dma("sp", f"dwe{si}", lambda e, stv=stv, src=src: e.dma_start(out=stv, in_=src), writes=[Bwst[si]])
                        P.op("pool", lambda e, stv=stv, dst=dst: e.tensor_copy(out=dst, in_=stv), reads=[Bwst[si]], writes=[bdst])
                    def head(ti, i, slot=slot):
                        for kc in range(8):
                            P.op("pe", lambda e, kc=kc: e.matmul(
                                out=Sps[i][:], lhsT=h2Tb[:, kc, ti * 128:(ti + 1) * 128], rhs=Wgu[slot][:, kc, :],
                                start=(kc == 0), stop=(kc == 7)), reads=[Bm["h2Tb"], Bwgu[slot]], writes=[BS[i]])

                    def mid(ti, i, o, b, slot=slot, ex=ex):
                        sg = sgs[b]; hid = hids[b]
                        P.op("act", lambda e: e.activation(out=sg, in_=Sps[i][:, 0:256], func=AF.Silu), reads=[BS[i]], writes=[Bsg[b]])
                        P.op("dve", lambda e: e.scalar_tensor_tensor(
                            out=hid, in0=Sps[i][:, 256:512], scalar=wfull[:, ti, ex:ex + 1], in1=sg, op0=ALU.mult, op1=ALU.mult),
                            reads=[BS[i], Bm["wfull"], Bsg[b]], writes=[Bhid[b]])
                        for c in range(2):
                            P.op("pe", lambda e, c=c: e.transpose(out=ptr[:, b * 256 + c * 128:b * 256 + (c + 1) * 128],
                                                                  in_=hid[:, c * 128:(c + 1) * 128],
                                                                  identity=ident_b[:]), reads=[Bhid[b], Bm["idb"]], writes=[Bptr2[b]])
                        P.op("act", lambda e: e.copy(out=hidTs[b], in_=ptr[:, b * 256:(b + 1) * 256]), reads=[Bptr2[b]], writes=[BhidT[b]])

                    def tail(ti, i, o, b, slot=slot, ex=ex):
                        hidT = hidTs[b]
                        for half in range(2):
                            for c in range(2):
                                P.op("pe", lambda e, half=half, c=c: e.matmul(
                                    out=Opf[o][:, half * 512:(half + 1) * 512], lhsT=hidT[:, c * 128:(c + 1) * 128],
                                    rhs=Wdn[slot][:, c, half * 512:(half + 1) * 512], start=(c == 0), stop=(c == 1)),
                                    reads=[BhidT[b], Bwd[slot]], writes=[BO[o]])
                        P.op("dve", lambda e: e.tensor_tensor(out=acc[:, ti, :], in0=Opf[o], in1=acc[:, ti, :], op=ALU.add),
                             reads=[BO[o], Bm["acc"]], writes=[Bm["acc"]])

                    infos = []
                    for ti in range(8):
                        i = s_ctr[0] % 2; s_ctr[0] += 1
                        o = o_ctr2[0] % 2; o_ctr2[0] += 1
                        infos.append((ti, i, o, ti % 2))
                    for k in range(8 + 2):
                        if k < 8:
                            head(infos[k][0], infos[k][1])
                        if 0 <= k - 1 < 8:
                            mid(*infos[k - 1])
                        if 0 <= k - 2 < 8:
                            tail(*infos[k - 2])
                if dbg == 52:
                    break
                for ti in range(8):
                    r0 = sbk * 1024 + ti * 128
                    ob = Buf()
                    P.dma("sp", "dfin", lambda e, ti=ti, r0=r0: e.dma_start(out=out.ap()[r0:r0 + 128, :], in_=acc[:, ti, :]),
                          reads=[Bm["acc"]], writes=[ob])
                    out_bufs.append(ob)

        P.wait_all("sp", out_bufs)
        P.finish()
        with nc.allow_non_contiguous_dma(reason="small parameter loads"):
            P.emit()
    return nc


_PARAM_KEYS = ["norm1_g", "w_in", "diff_q_g", "diff_k_g", "lambda_q1", "lambda_k1", "lambda_q2", "lambda_k2",
               "diff_sub_g", "moba_q_g", "moba_k_g", "rel_bias", "w_out", "norm2_g", "router_group",
               "router_expert", "w_gate", "w_up", "w_down"]


def run_cores(inputs, nseq, ncores, stop_after_attn=False, dbg=0):
    nc = build(nseq, stop_after_attn=stop_after_attn, dbg=dbg)
    onehot, cmask, blk = _consts()
    xs = np.ascontiguousarray(inputs["x"], dtype=np.float32).reshape(-1, D_MODEL)
    in_maps = []
    for c in range(ncores):
        m = {k: np.ascontiguousarray(inputs[k], dtype=np.float32) for k in _PARAM_KEYS}
        m["x"] = xs[c * nseq * S_LEN:(c + 1) * nseq * S_LEN]
        m["c_onehot"] = onehot; m["c_blk"] = blk
        in_maps.append(m)
    res = run_bass_kernel_spmd(nc, in_maps, core_ids=list(range(ncores)))
    return np.concatenate([np.asarray(r["out"]) for r in res.results], axis=0)


def kernel(**inputs):
    B = inputs["x"].shape[0]
    o = run_cores(inputs, B // 8, 8)
    return o.reshape(B, S_LEN, D_MODEL).astype(np.float32)
```

```python
import math
from contextlib import ExitStack

import numpy as np
import concourse.bass as bass
import concourse.mybir as mybir
from concourse.bass_utils import run_bass_kernel_spmd

F32 = mybir.dt.float32
BF16 = mybir.dt.bfloat16
I32 = mybir.dt.int32
AF = mybir.ActivationFunctionType
ALU = mybir.AluOpType
AX = mybir.AxisListType


import os as _os0
_STRICT = bool(int(_os0.environ.get('BASS_STRICT_SYNC', '0')))


class Buf:
    __slots__ = ("w", "r", "name")

    def __init__(self, name=""):
        self.w = None
        self.r = []
        self.name = name


class Prog:
    ENG = ("pe", "act", "dve", "pool", "sp")

    def __init__(self, nc, stack):
        self.nc = nc
        self.stack = stack
        self.ops = {e: [] for e in self.ENG}
        self.sem = {}
        self.cnt = {}
        self.seen = {e: {} for e in self.ENG}
        for e in self.ENG:
            self.newsem(e)

    def newsem(self, name):
        self.sem[name] = self.stack.enter_context(self.nc.semaphore("s_" + name))
        self.cnt[name] = 0

    def _waits(self, eng, reads, writes):
        need = {}

        def add(ev, raw):
            if ev is None:
                return
            sn, v = ev
            if sn == eng:
                if eng == "pe" or (not raw and not _STRICT):
                    return
            if v > need.get(sn, 0):
                need[sn] = v

        for b in reads:
            add(b.w, True)
        for b in writes:
            add(b.w, False)
            for ev in b.r:
                add(ev, False)
        out = []
        for sn, v in need.items():
            if self.seen[eng].get(sn, 0) < v:
                self.seen[eng][sn] = v
                out.append((sn, v))
        return out

    def _mark(self, ev, reads, writes):
        for b in reads:
            b.r.append(ev)
        for b in writes:
            b.w = ev
            b.r = []

    def op(self, eng, fn, reads=(), writes=()):
        waits = self._waits(eng, reads, writes)
        self.cnt[eng] += 1
        ev = (eng, self.cnt[eng])
        self.ops[eng].append((fn, waits, (eng, 1)))
        self._mark(ev, reads, writes)
        return ev

    def dma(self, q, semname, fn, reads=(), writes=()):
        if semname == "dpar":
            self._npar = getattr(self, "_npar", 0) + 1
            semname = "dpar%d" % self._npar
        if semname not in self.sem:
            self.newsem(semname)
        waits = self._waits(q, reads, writes)
        self.cnt[semname] += 16
        ev = (semname, self.cnt[semname])
        self.ops[q].append((fn, waits, (semname, 16)))
        self._mark(ev, reads, writes)
        return ev

    def wait_all(self, eng, bufs):
        waits = self._waits(eng, bufs, ())
        self.ops[eng].append((None, waits, None))

    def barrier(self):
        waits = [(sn, v) for sn, v in self.cnt.items() if v > 0]
        for eng in self.ENG:
            w = [(sn, v) for sn, v in waits if self.seen[eng].get(sn, 0) < v and not (sn == eng and eng == "sp")]
            for sn, v in w:
                self.seen[eng][sn] = v
            self.ops[eng].append((None, w, None))

    def finish(self):
        waits = [(sn, v) for sn, v in self.cnt.items() if v > 0]
        self.ops["sp"].append((None, waits, None))

    def emit(self):
        nc = self.nc
        sem = self.sem
        ops = self.ops

        def run(e, name):
            for fn, waits, inc in ops[name]:
                for sn, v in waits:
                    e.wait_ge(sem[sn], v)
                if fn is None:
                    continue
                ins = fn(e)
                ins.then_inc(sem[inc[0]], inc[1])

        with nc.Block() as block:
            @block.tensor
            def _(e):
                run(e, "pe")

            @block.scalar
            def _(e):
                run(e, "act")

            @block.vector
            def _(e):
                run(e, "dve")

            @block.gpsimd
            def _(e):
                run(e, "pool")

            @block.sync
            def _(e):
                run(e, "sp")


S_LEN = 2048
D_MODEL = 1024
NT = 16
HEAD_DIM = 64
RMS_EPS = 1e-6
LAMBDA_INIT = 0.8 - 0.6 * math.exp(-0.3 * 0)
NEG_BIG = 30000.0
N_EXP = 32
MULMOD = 1000000
CAP_CHUNK = 128


def _t5_bucket_np(n):
    n = np.maximum(n, 0)
    max_exact = 16
    nf = np.maximum(n, 1).astype(np.float32)
    large = max_exact + (np.log(nf / np.float32(max_exact)) / np.float32(math.log(2048 / max_exact))
                         * np.float32(32 - max_exact)).astype(np.int32)
    large = np.minimum(large, 31)
    return np.where(n < max_exact, n, large)


def _consts():
    d = np.arange(2048)
    b = _t5_bucket_np(d)
    onehot = np.zeros((32, 2048), np.float32)
    onehot[b, d] = 1.0
    cmask = None
    blk = np.zeros((8, 2048), np.float32)
    for n in range(8):
        blk[n, n * 256:(n + 1) * 256] = 1.0
    return onehot, cmask, blk


def bc_inner(ap, n):
    return bass.AP(tensor=ap.tensor, offset=ap.offset, ap=[list(a) for a in ap.ap] + [[0, n]])


class _Stop(Exception):
    pass


def build(nseq, stop_after_attn=False, dbg=0):
    nc = bass.Bass("TRN2", target_bir_lowering=False)
    NTOK = nseq * S_LEN
    dt_in = lambda name, shape: nc.dram_tensor(name, shape, F32, kind="ExternalInput")
    x = dt_in("x", [NTOK, D_MODEL])
    norm1_g = dt_in("norm1_g", [1, 1024])
    w_in = dt_in("w_in", [1, 1024, 3072])
    diff_q_g = dt_in("diff_q_g", [1, 64]); diff_k_g = dt_in("diff_k_g", [1, 64])
    lambda_q1 = dt_in("lambda_q1", [1, 64]); lambda_k1 = dt_in("lambda_k1", [1, 64])
    lambda_q2 = dt_in("lambda_q2", [1, 64]); lambda_k2 = dt_in("lambda_k2", [1, 64])
    diff_sub_g = dt_in("diff_sub_g", [1, 128])
    moba_q_g = dt_in("moba_q_g", [1, 64]); moba_k_g = dt_in("moba_k_g", [1, 64])
    rel_bias = dt_in("rel_bias", [32, 12])
    w_out = dt_in("w_out", [1, 1024, 1024])
    norm2_g = dt_in("norm2_g", [1, 1024])
    router_group = dt_in("router_group", [1, 1024, 4])
    router_expert = dt_in("router_expert", [1, 4, 1024, 8])
    w_gate = dt_in("w_gate", [1, 32, 1024, 256])
    w_up = dt_in("w_up", [1, 32, 1024, 256])
    w_down = dt_in("w_down", [1, 32, 256, 1024])
    c_onehot = dt_in("c_onehot", [32, 2048])
    c_blk = dt_in("c_blk", [8, 2048])
    out = nc.dram_tensor("out", [NTOK, D_MODEL], F32, kind="ExternalOutput")
    Bd = nc.dram_tensor("Bd", [12, 129, 2560], F32, kind="Internal")
    Wd = nc.dram_tensor("Wd", [8, 1024, 384], BF16, kind="Internal")
    x1d = nc.dram_tensor("x1d", [NTOK, D_MODEL], F32, kind="Internal")

    with ExitStack() as st:
        P = Prog(nc, st)
        sb = lambda name, shape, dt: st.enter_context(nc.sbuf_tensor(name, shape, dt))
        ps = lambda name, shape, dt: st.enter_context(nc.psum_tensor(name, shape, dt))
        out_bufs = []

        Sps = [ps("Sps0", [128, 512], F32), ps("Sps1", [128, 512], F32)]
        B_S = [Buf(), Buf()]
        Ops = [ps("Ops0", [128, 4, 256], F32), ps("Ops1", [128, 4, 256], F32)]
        B_O = [Buf(), Buf()]
        pj = ps("pj", [128, 512], F32); B_pj = Buf()
        ptr = ps("ptr", [128, 1024], BF16); B_ptr = Buf(); B_ptr1 = Buf()

        ident_f = sb("ident_f", [128, 128], F32); B_idf = Buf()
        ident_b = sb("ident_b", [128, 128], BF16); B_idb = Buf()
        wg = [sb("wg0", [128, 8, 384], BF16), sb("wg1", [128, 8, 384], BF16)]; B_wg = [Buf(), Buf()]
        B_Wd = Buf()
        woutb = sb("woutb", [128, 8, 1024], BF16); B_wout = Buf()
        stage = [sb("stage0", [128, 1536], F32), sb("stage1", [128, 1536], F32)]
        B_stage = [Buf(), Buf()]
        g1c = sb("g1c", [128, 8], F32); B_g1 = Buf()
        xT = sb("xT", [128, 8, 2048], BF16); B_xT = [Buf() for _ in range(NT)]
        yT = sb("yT", [128, 8, 2048], BF16); B_yT = [[Buf() for _ in range(4)] for _ in range(8)]
        qTa = sb("qTa", [128, 2, 2048], BF16); B_qT = [[Buf() for _ in range(NT)] for _ in range(2)]
        kTa = sb("kTa", [128, 2, 2048], BF16); B_kT = [Buf(), Buf()]
        B_qaugrows = Buf()
        Vt = sb("Vt", [128, 16, 136], BF16); B_V = Buf()
        G = [sb("G0", [128, 2560], F32), sb("G1", [128, 2560], F32)]; B_G = [Buf(), Buf()]
        junk = sb("junk", [128, 1024], BF16); B_junk = Buf()
        xb = sb("xb", [128, 1024], BF16); B_xb = Buf()
        ss1 = sb("ss1", [128, 16], F32); B_ss1 = Buf()
        rstd1 = sb("rstd1", [128, 16], F32); B_rstd1 = Buf()
        NB4 = 4
        qk2 = [sb(f"bqk{i}", [128, 256], F32) for i in range(NB4)]; B_qk2 = [Buf() for _ in range(NB4)]
        sq2b = [sb(f"bsq{i}", [128, 256], F32) for i in range(NB4)]; B_sq2b = [Buf() for _ in range(NB4)]
        ssh2 = [sb(f"bssh{i}", [128, 4], F32) for i in range(NB4)]; B_ssh2 = [Buf() for _ in range(NB4)]
        rsh2 = [sb(f"brsh{i}", [128, 4], F32) for i in range(NB4)]; B_rsh2 = [Buf() for _ in range(NB4)]
        qkn2 = [sb(f"bqkn{i}", [128, 256], BF16) for i in range(NB4)]; B_qkn2 = [Buf() for _ in range(NB4)]
        E = [sb(f"E{i}", [128, 512], F32) for i in range(4)]; B_E = [Buf() for _ in range(4)]
        PT = [sb(f"PT{i}", [128, 512], BF16) for i in range(5)]; B_PT = [Buf() for _ in range(5)]
        rr = sb("rr", [128, 4], F32); B_rr = Buf()
        nlr = sb("nlr", [128, 4], F32); B_nlr = Buf()
        tmp1 = sb("tmp1", [128, 4, 128], F32); B_tmp1 = Buf()
        yd = sb("yd", [128, 4, 128], F32); B_yd = Buf()
        sq2 = sb("sq2", [128, 4, 128], F32); B_sq2 = Buf()
        ss2 = sb("ss2", [128, 4], F32); B_ss2 = Buf()
        rs2 = sb("rs2", [128, 4], F32); B_rs2 = Buf()
        ytok = sb("ytok", [128, 4, 128], BF16); B_ytok = Buf()
        sgb = sb("sgb", [128, 128], F32); B_sgb = Buf()
        g2b = sb("g2b", [128, 1024], F32); B_g2b = Buf()
        lamv = sb("lamv", [128, 4, 64], F32); B_lamv = Buf()
        lamp = sb("lamp", [128, 2, 64], F32); B_lamp = Buf()
        lams = sb("lams", [128, 4], F32); B_lams = Buf()
        gb = sb("gb", [128, 4, 64], F32); B_gq = Buf()
        gqk = sb("gqk", [128, 2, 64], F32); B_gqk = Buf()
        rb = sb("rb", [32, 12], F32); B_rb = Buf()
        oh = G[0][0:32, 0:2048]; B_oh = B_G[0]
        et = G[1][0:12, 0:2560]; B_et = B_G[1]
        B_Bd = Buf()
        B_x1d = Buf()
        km = sb("km", [64, 16], F32); B_km = Buf()
        kmf = sb("kmf", [64, 16], F32); B_kmf = Buf()
        kmhf = sb("kmhf", [64, 16], F32); B_kmhf = Buf()
        kmhl = sb("kmhl", [64, 2, 16], BF16); B_kmhl = Buf()
        g16 = sb("g16", [128, 16], F32); B_g16 = Buf()
        gate = sb("gate", [128, 8], F32); B_gate = Buf()
        mx8 = sb("mx8", [128, 8], F32); B_mx8 = Buf()
        selm = sb("selm", [128, 8], F32); B_selm = Buf()
        qaug = sb("qaug", [128, 128], BF16); B_qaug = Buf()

        P.op("pool", lambda e: e.memset(ident_f[:], 1.0), writes=[B_idf])
        P.op("pool", lambda e: e.affine_select(out=ident_f[:], in_=ident_f[:], pattern=[[-1, 128]],
                                               compare_op=ALU.is_equal, fill=0.0, base=0, channel_multiplier=1),
             reads=[B_idf], writes=[B_idf])
        P.op("dve", lambda e: e.tensor_copy(out=ident_b[:], in_=ident_f[:]), reads=[B_idf], writes=[B_idb])

        P.dma("sp", "dpar", lambda e: e.dma_start(out=g1c[:], in_=norm1_g.ap().rearrange("o (k p) -> p (o k)", p=128)),
              writes=[B_g1])
        w_in_v = w_in.ap().rearrange("o (k p) n -> p (o k) n", p=128)
        B_wst = [Buf(), Buf()]
        for kc in range(8):
            for T in range(2):
                s = T
                P.dma("sp", f"dst{s}", lambda e, kc=kc, s=s, T=T: e.dma_start(out=stage[s][:], in_=w_in_v[:, kc, T * 1536:(T + 1) * 1536]),
                      writes=[B_stage[s]])
                P.op("dve" if s == 0 else "pool",
                     lambda e, kc=kc, s=s: e.tensor_scalar_mul(out=qTa[:, s, 0:1536], in0=stage[s][:], scalar1=g1c[:, kc:kc + 1]),
                     reads=[B_stage[s], B_g1], writes=[B_wst[s]])
                for gg in range(4):
                    a = qTa[:, s, gg * 128:gg * 128 + 128]
                    src3 = bass.AP(tensor=a.tensor, offset=a.offset, ap=[list(a.ap[0]), [512, 3], [1, 128]])
                    P.dma("act", f"dws{s}", lambda e, kc=kc, T=T, gg=gg, src3=src3: e.dma_start(
                        out=Wd.ap()[T * 4 + gg, kc * 128:(kc + 1) * 128, :].rearrange("p (a b) -> p a b", a=3), in_=src3),
                        reads=[B_wst[s]], writes=[B_Wd])
        w_out_v = w_out.ap().rearrange("o (k p) n -> p (o k) n", p=128)
        P.dma("pool", "dwo", lambda e: e.dma_start(out=woutb[:], in_=w_out_v), writes=[B_wout])

        P.dma("sp", "dpar", lambda e: e.dma_start(out=rb[:], in_=rel_bias.ap()), writes=[B_rb])
        P.dma("sp", "dpar", lambda e: e.dma_start(out=oh, in_=c_onehot.ap()), writes=[B_oh])
        P.op("pool", lambda e: e.memset(et[:, 0:512], 0.0), writes=[B_et])
        for c in range(4):
            P.op("pe", lambda e, c=c: e.matmul(out=pj[0:12, :], lhsT=rb[:, :], rhs=oh[:, c * 512:(c + 1) * 512],
                                               start=True, stop=True), reads=[B_rb, B_oh], writes=[B_pj])
            P.op("act", lambda e, c=c: e.activation(out=et[:, 512 + c * 512:512 + (c + 1) * 512], in_=pj[0:12, :], func=AF.Exp),
                 reads=[B_pj], writes=[B_et])
        et_ap = et
        esrc = bass.AP(tensor=et_ap.tensor, offset=et_ap.offset, ap=[list(et_ap.ap[0]), [0, 129], [1, 2560]])
        P.dma("sp", "dbd", lambda e: e.dma_start(out=Bd.ap(), in_=esrc), reads=[B_et], writes=[B_Bd])

        for i, t in enumerate([diff_q_g, diff_k_g, moba_q_g, moba_k_g]):
            P.dma("sp", "dpar", lambda e, i=i, t=t: e.dma_start(out=gb[:, i, :], in_=bass.AP(tensor=t, offset=0, ap=[[0, 128], [1, 64]])),
                  writes=[B_gq])
        P.op("dve", lambda e: e.tensor_tensor(out=gqk[:, 0, :], in0=gb[:, 0, :], in1=gb[:, 1, :], op=ALU.mult), reads=[B_gq], writes=[B_gqk])
        P.op("dve", lambda e: e.tensor_tensor(out=gqk[:, 1, :], in0=gb[:, 2, :], in1=gb[:, 3, :], op=ALU.mult), reads=[B_gq], writes=[B_gqk])
        P.dma("sp", "dpar", lambda e: e.dma_start(out=sgb[:], in_=bass.AP(tensor=diff_sub_g, offset=0, ap=[[0, 128], [1, 128]])),
              writes=[B_sgb])
        P.op("dve", lambda e: e.tensor_scalar_mul(out=sgb[:], in0=sgb[:], scalar1=float(1.0 - LAMBDA_INIT)), reads=[B_sgb], writes=[B_sgb])
        P.dma("sp", "dpar", lambda e: e.dma_start(out=g2b[:], in_=bass.AP(tensor=norm2_g, offset=0, ap=[[0, 128], [1, 1024]])),
              writes=[B_g2b])
        for i, t in enumerate([lambda_q1, lambda_k1, lambda_q2, lambda_k2]):
            P.dma("sp", "dpar", lambda e, i=i, t=t: e.dma_start(out=lamv[:, i, :], in_=bass.AP(tensor=t, offset=0, ap=[[0, 128], [1, 64]])),
                  writes=[B_lamv])
        P.op("dve", lambda e: e.tensor_tensor(out=lamp[:, 0, :], in0=lamv[:, 0, :], in1=lamv[:, 1, :], op=ALU.mult), reads=[B_lamv], writes=[B_lamp])
        P.op("dve", lambda e: e.tensor_tensor(out=lamp[:, 1, :], in0=lamv[:, 2, :], in1=lamv[:, 3, :], op=ALU.mult), reads=[B_lamv], writes=[B_lamp])
        P.op("dve", lambda e: e.tensor_reduce(out=lams[:, 0:2], in_=lamp[:], axis=AX.X, op=ALU.add), reads=[B_lamp], writes=[B_lams])
        P.op("act", lambda e: e.activation(out=lams[:, 0:2], in_=lams[:, 0:2], func=AF.Exp), reads=[B_lams], writes=[B_lams])
        P.op("dve", lambda e: e.tensor_sub(out=lams[:, 2:3], in0=lams[:, 0:1], in1=lams[:, 1:2]), reads=[B_lams], writes=[B_lams])
        P.op("dve", lambda e: e.tensor_scalar(out=lams[:, 3:4], in0=lams[:, 2:3], scalar1=float(LAMBDA_INIT), scalar2=-1.0,
                                              op0=ALU.add, op1=ALU.mult), reads=[B_lams], writes=[B_lams])
        nlam = lams[:, 3:4]

        P.op("pool", lambda e: e.memset(Vt[:], 0.0), writes=[B_V])
        P.op("pool", lambda e: e.memset(Vt[:, :, 64:65], 1.0), writes=[B_V])
        P.op("pool", lambda e: e.memset(Vt[:, :, 132:133], 1.0), writes=[B_V])
        P.op("pool", lambda e: e.memset(qaug[:], 0.0), writes=[B_qaug])
        P.op("pool", lambda e: e.memset(qTa[64:128, :, :], 0.0), writes=[B_qaugrows, B_wst[0], B_wst[1]])
        P.op("pool", lambda e: e.memset(kTa[64:128, :, :], 0.0), writes=[B_kT[0], B_kT[1]])
        for m in range(2):
            P.dma("pool", "dpar", lambda e, m=m: e.dma_start(out=kTa[64:72, m, :], in_=c_blk.ap()), writes=[B_kT[m]])

        def rsqrt_act(dst, src, n_feat, rbufs, wbufs):
            P.op("act", lambda e: e.activation(out=dst, in_=src, func=AF.Ln, bias=float(RMS_EPS), scale=1.0 / n_feat),
                 reads=rbufs, writes=wbufs)
            P.op("act", lambda e: e.activation(out=dst, in_=dst, func=AF.Exp, scale=-0.5), reads=wbufs, writes=wbufs)

        pt_ctr = [0]
        e_ctr = [0]
        o_ctr = [0]
        mul_ctr = [0]
        w_ctr = [0]
        _pass = [0]
        import os as _os
        _STG = int(_os.environ.get('DBG_STAGE', '0'))

        for sq_i in range(nseq if (dbg != 1 and dbg < 50) else 0):
            tok0 = sq_i * S_LEN
            for t in range(NT):
                s = t % 2
                r0 = tok0 + t * 128
                xt = stage[s][:, 0:1024]
                P.dma("sp", f"dst{s}", lambda e, xt=xt, r0=r0: e.dma_start(out=xt, in_=x.ap()[r0:r0 + 128, :]), writes=[B_stage[s]])
                P.op("act", lambda e, xt=xt, t=t: e.activation(out=junk[:], in_=xt, func=AF.Square, accum_out=ss1[:, t:t + 1]),
                     reads=[B_stage[s]], writes=[B_junk, B_ss1])
                P.op("pool", lambda e, xt=xt: e.tensor_copy(out=xb[:], in_=xt), reads=[B_stage[s]], writes=[B_xb])
                for kc in range(8):
                    P.op("pe", lambda e, kc=kc: e.transpose(out=ptr[:, kc * 128:(kc + 1) * 128], in_=xb[:, kc * 128:(kc + 1) * 128],
                                                            identity=ident_b[:]), reads=[B_xb, B_idb], writes=[B_ptr, B_ptr1])
                P.op("dve", lambda e, t=t: e.tensor_copy(out=xT[:, :, t * 128:(t + 1) * 128],
                                                         in_=ptr[:].rearrange("p (k n) -> p k n", k=8)),
                     reads=[B_ptr, B_ptr1], writes=[B_xT[t]])
            rsqrt_act(rstd1[:], ss1[:], 1024.0, [B_ss1], [B_rstd1])
            if dbg == 2:
                break

            for g in range(8):
                is_moba = g >= 4
                qoff = (1536 if is_moba else 0) + (g % 4) * 128
                K = 72 if is_moba else 64
                gcol = 2 if is_moba else 0
                heads = [4 + 2 * (g - 4), 4 + 2 * (g - 4) + 1] if is_moba else [g]
                for gi, h in enumerate(heads):
                    P.dma("sp", f"dG{gi}", lambda e, gi=gi, h=h: e.dma_start(
                        out=G[gi][:, 0:2432], in_=bass.AP(tensor=Bd, offset=h * 129 * 2560 + 128, ap=[[2559, 128], [1, 2432]])),
                        reads=[B_Bd], writes=[B_G[gi]])
                wsl = w_ctr[0] % 2; w_ctr[0] += 1
                P.dma("sp", f"dwg{wsl}", lambda e, wsl=wsl, g=g: e.dma_start(
                    out=wg[wsl][:], in_=Wd.ap()[g].rearrange("(k p) n -> p k n", p=128)), reads=[B_Wd], writes=[B_wg[wsl]])
                def bsel(t):
                    p = t % 2; p3 = t % 3; p4 = t % 4
                    return dict(pjp=[pj, Sps[0], Sps[1]][p3], B_pjp=[B_pj, B_S[0], B_S[1]][p3],
                                qk=qk2[p4], B_qk=B_qk2[p4], sq=sq2b[p4], B_sq=B_sq2b[p4], ssh=ssh2[p4], B_ssh=B_ssh2[p4],
                                rsh=rsh2[p4], B_rsh=B_rsh2[p4], qkn=qkn2[p4], B_qkn=B_qkn2[p4],
                                B_pt=[B_ptr, B_ptr1], pc0=p * 512)

                def st0(t, wsl=wsl):
                    d = bsel(t); pjp = d["pjp"]
                    for kc in range(8):
                        P.op("pe", lambda e, kc=kc: e.matmul(
                            out=pjp[:, 0:384], lhsT=xT[:, kc, t * 128:(t + 1) * 128], rhs=wg[wsl][:, kc, :],
                            start=(kc == 0), stop=(kc == 7)), reads=[B_xT[t], B_wg[wsl]], writes=[d["B_pjp"]])

                def st1(t):
                    d = bsel(t); pjp = d["pjp"]; qk = d["qk"]; sq = d["sq"]
                    P.op("dve", lambda e: e.tensor_scalar_mul(out=qk[:], in0=pjp[:, 0:256], scalar1=rstd1[:, t:t + 1]),
                         reads=[d["B_pjp"], B_rstd1], writes=[d["B_qk"]])
                    P.op("dve", lambda e: e.tensor_scalar_mul(
                        out=Vt[:, t, :].rearrange("p (a b) -> p a b", a=2)[:, :, 0:64],
                        in0=pjp[:, 256:384].rearrange("p (a b) -> p a b", a=2), scalar1=rstd1[:, t:t + 1]),
                        reads=[d["B_pjp"], B_rstd1], writes=[B_V])
                    P.op("pool", lambda e: e.tensor_tensor(out=sq[:], in0=qk[:], in1=qk[:], op=ALU.mult), reads=[d["B_qk"]], writes=[d["B_sq"]])

                def st2(t):
                    d = bsel(t); sq = d["sq"]; ssh = d["ssh"]; rsh = d["rsh"]
                    P.op("dve", lambda e: e.tensor_reduce(out=ssh[:], in_=sq[:].rearrange("p (a b) -> p a b", a=4), axis=AX.X, op=ALU.add),
                         reads=[d["B_sq"]], writes=[d["B_ssh"]])
                    rsqrt_act(rsh[:], ssh[:], 64.0, [d["B_ssh"]], [d["B_rsh"]])

                def st3(t, gi2=(1 if is_moba else 0)):
                    d = bsel(t); qk = d["qk"]; rsh = d["rsh"]; qkn = d["qkn"]
                    for i in range(2):
                        P.op("dve", lambda e, i=i: e.tensor_scalar_mul(
                            out=qkn[:, i * 64:(i + 1) * 64], in0=qk[:, i * 64:(i + 1) * 64], scalar1=rsh[:, i:i + 1]),
                            reads=[d["B_qk"], d["B_rsh"]], writes=[d["B_qkn"]])
                    for i in range(2, 4):
                        P.op("dve", lambda e, i=i: e.scalar_tensor_tensor(
                            out=qkn[:, i * 64:(i + 1) * 64], in0=qk[:, i * 64:(i + 1) * 64], scalar=rsh[:, i:i + 1],
                            in1=gqk[:, gi2, :], op0=ALU.mult, op1=ALU.mult), reads=[d["B_qk"], d["B_rsh"], B_gqk], writes=[d["B_qkn"]])

                def st4(t):
                    d = bsel(t); qkn = d["qkn"]; pc0 = d["pc0"]
                    for i in range(4):
                        P.op("pe", lambda e, i=i: e.transpose(
                            out=ptr[0:64, pc0 + i * 128:pc0 + (i + 1) * 128], in_=qkn[:, i * 64:(i + 1) * 64],
                            identity=ident_b[:]), reads=[d["B_qkn"], B_idb], writes=d["B_pt"])

                def st5(t):
                    d = bsel(t); pc0 = d["pc0"]
                    P.op("act", lambda e: e.copy(out=qTa[0:64, :, t * 128:(t + 1) * 128],
                                                 in_=ptr[0:64, pc0:pc0 + 256].rearrange("p (a b) -> p a b", a=2)),
                         reads=d["B_pt"], writes=[B_qT[0][t], B_qT[1][t]])
                    P.op("act", lambda e: e.copy(out=kTa[0:64, :, t * 128:(t + 1) * 128],
                                                 in_=ptr[0:64, pc0 + 256:pc0 + 512].rearrange("p (a b) -> p a b", a=2)),
                         reads=d["B_pt"], writes=[B_kT[0], B_kT[1]])

                stages_b = [(st0, 0), (st1, 1), (st2, 2), (st3, 3), (st5, 5), (st4, 4)]
                for kk in range(NT + 5):
                    for fn, lag in stages_b:
                        tt = kk - lag
                        if 0 <= tt < NT:
                            fn(tt)
                if dbg in (3, 30, 305, 31, 32, 33, 34, 35, 36, 37, 38, 39):
                    break
                if is_moba:
                    P.op("dve", lambda e: e.tensor_reduce(out=km[:], in_=kTa[0:64, :, :].rearrange("p m (a b) -> p (m a) b", a=8),
                                                          axis=AX.X, op=ALU.add), reads=[B_kT[0], B_kT[1]], writes=[B_km])
                    P.op("dve", lambda e: e.tensor_scalar_mul(out=kmf[:], in0=km[:], scalar1=1.0 / 256), reads=[B_km], writes=[B_kmf])
                    kview = kmhl[:, :, 0:8]
                    P.op("dve", lambda e: e.tensor_copy(out=kview, in_=kmf[:].rearrange("p (m a) -> p m a", m=2)),
                         reads=[B_kmf], writes=[B_kmhl])
                    P.op("dve", lambda e: e.tensor_copy(out=kmhf[:].rearrange("p (m a) -> p m a", m=2), in_=kview),
                         reads=[B_kmhl], writes=[B_kmhf])
                    P.op("dve", lambda e: e.tensor_sub(out=kmhl[:, :, 8:16], in0=kmf[:].rearrange("p (m a) -> p m a", m=2),
                                                       in1=kmhf[:].rearrange("p (m a) -> p m a", m=2)),
                         reads=[B_kmf, B_kmhf], writes=[B_kmhl])
                    for m in range(2):
                        for t in range(8, NT):
                            own = t // 2
                            P.op("pe", lambda e, m=m, t=t: e.matmul(out=pj[:, 0:16], lhsT=qTa[0:64, m, t * 128:(t + 1) * 128],
                                                                    rhs=kmhl[0:64, m, :], start=True, stop=True),
                                 reads=[B_qT[m][t], B_kmhl], writes=[B_pj])
                            P.op("act", lambda e: e.copy(out=g16[:], in_=pj[:, 0:16]), reads=[B_pj], writes=[B_g16])
                            P.op("pool", lambda e: e.memset(gate[:], -1e30), writes=[B_gate])
                            P.op("dve", lambda e, own=own: e.tensor_tensor(out=gate[:, 0:own], in0=g16[:, 0:own], in1=g16[:, 8:8 + own],
                                                                           op=ALU.add), reads=[B_g16], writes=[B_gate])
                            P.op("dve", lambda e: e.max(out=mx8[:], in_=gate[:]), reads=[B_gate], writes=[B_mx8])
                            P.op("dve", lambda e, own=own: e.tensor_scalar(out=selm[:, 0:own], in0=gate[:, 0:own], scalar1=mx8[:, 2:3],
                                                                           scalar2=None, op0=ALU.is_ge), reads=[B_gate, B_mx8], writes=[B_selm])
                            P.op("pool", lambda e: e.memset(qaug[:, 64:72], 0.0), writes=[B_qaug])
                            P.op("dve", lambda e, own=own: e.tensor_scalar(out=qaug[:, 64:64 + own], in0=selm[:, 0:own], scalar1=-1.0,
                                                                           scalar2=NEG_BIG, op0=ALU.add, op1=ALU.mult),
                                 reads=[B_selm], writes=[B_qaug])
                            P.op("pe", lambda e: e.transpose(out=ptr[:, 0:128], in_=qaug[:], identity=ident_b[:]),
                                 reads=[B_qaug, B_idb], writes=[B_ptr])
                            P.op("act", lambda e, m=m, t=t: e.copy(out=qTa[64:72, m, t * 128:(t + 1) * 128], in_=ptr[64:72, 0:128]),
                                 reads=[B_ptr], writes=[B_qT[m][t]])
                steps = []
                for qc in range(4):
                    for m in range(2):
                        for kt in range(4 * qc + 4):
                            steps.append((qc, m, kt))
                st_info = {}

                def emit_S(idx, K=K, is_moba=is_moba, steps=steps, st_info=st_info):
                    qc, m, kt = steps[idx]
                    if kt == 0:
                        st_info[(qc, m)] = o_ctr[0] % 2; o_ctr[0] += 1
                    i = e_ctr[0] % 4; e_ctr[0] += 1
                    j = pt_ctr[0] % 5; pt_ctr[0] += 1
                    gi = m if is_moba else 0
                    Sb = [Sps[0][:], Sps[1][:], pj[:], ptr[:].bitcast(F32)][i]
                    B_Sb = [[B_S[0]], [B_S[1]], [B_pj], [B_ptr, B_ptr1]][i]
                    P.op("pe", lambda e: e.matmul(
                        out=Sb, lhsT=kTa[0:K, m, kt * 128:(kt + 1) * 128], rhs=qTa[0:K, m, qc * 512:(qc + 1) * 512],
                        start=True, stop=True),
                        reads=[B_kT[m]] + [B_qT[m][qc * 4 + u] for u in range(4)] + [B_qaugrows], writes=B_Sb)
                    P.op("act", lambda e: e.activation(out=E[i][:], in_=Sb, func=AF.Exp, scale=HEAD_DIM ** -0.5),
                         reads=B_Sb, writes=[B_E[i]])
                    c0 = (4 * qc - kt + 3) * 128
                    meng = "pool" if (mul_ctr[0] % MULMOD == MULMOD - 1) else "dve"; mul_ctr[0] += 1
                    P.op(meng, lambda e: e.tensor_tensor(out=PT[j][:], in0=E[i][:], in1=G[gi][:, c0:c0 + 512], op=ALU.mult),
                         reads=[B_E[i], B_G[gi]], writes=[B_PT[j]])
                    return j

                def emit_PV(idx, j, is_moba=is_moba, steps=steps, st_info=st_info):
                    qc, m, kt = steps[idx]
                    o = st_info[(qc, m)]
                    if is_moba:
                        v0, vw = m * 68, 65
                    else:
                        v0, vw = 0, 133
                    for jq in range(4):
                        if kt > 4 * qc + jq:
                            continue
                        P.op("pe", lambda e, jq=jq: e.matmul(
                            out=Ops[o][:, jq, 0:vw], lhsT=PT[j][:, jq * 128:(jq + 1) * 128], rhs=Vt[:, kt, v0:v0 + vw],
                            start=(kt == 0 and jq in (0, 2)), stop=(kt == 4 * qc + jq), skip_group_check=True),
                            reads=[B_PT[j], B_V], writes=[B_O[o]])
                    if kt == 4 * qc + 3:
                        finalize(qc, m, o)

                def finalize(qc, m, o, g=g, is_moba=is_moba):
                    scol = 132 if not is_moba else 64
                    P.op("dve", lambda e: e.reciprocal(out=rr[:], in_=Ops[o][:, :, scol:scol + 1].rearrange("p a b -> p (a b)")),
                         reads=[B_O[o]], writes=[B_rr])
                    if not is_moba:
                        if m == 0:
                            for jq in range(4):
                                P.op("dve", lambda e, jq=jq: e.tensor_scalar_mul(
                                    out=tmp1[:, jq, :].rearrange("p (a b) -> p a b", a=2),
                                    in0=Ops[o][:, jq, 0:136].rearrange("p (a b) -> p a b", a=2)[:, :, 0:64],
                                    scalar1=rr[:, jq:jq + 1]), reads=[B_O[o], B_rr], writes=[B_tmp1])
                        else:
                            P.op("dve", lambda e: e.tensor_scalar_mul(out=nlr[:], in0=rr[:], scalar1=nlam), reads=[B_rr, B_lams], writes=[B_nlr])
                            for jq in range(4):
                                P.op("dve", lambda e, jq=jq: e.scalar_tensor_tensor(
                                    out=yd[:, jq, :].rearrange("p (a b) -> p a b", a=2),
                                    in0=Ops[o][:, jq, 0:136].rearrange("p (a b) -> p a b", a=2)[:, :, 0:64],
                                    scalar=nlr[:, jq:jq + 1], in1=tmp1[:, jq, :].rearrange("p (a b) -> p a b", a=2),
                                    op0=ALU.mult, op1=ALU.add), reads=[B_O[o], B_nlr, B_tmp1], writes=[B_yd])
                            P.op("pool", lambda e: e.tensor_tensor(out=sq2[:], in0=yd[:], in1=yd[:], op=ALU.mult), reads=[B_yd], writes=[B_sq2])
                            P.op("dve", lambda e: e.tensor_reduce(out=ss2[:], in_=sq2[:], axis=AX.X, op=ALU.add), reads=[B_sq2], writes=[B_ss2])
                            rsqrt_act(rs2[:], ss2[:], 128.0, [B_ss2], [B_rs2])
                            for jq in range(4):
                                P.op("dve", lambda e, jq=jq: e.scalar_tensor_tensor(
                                    out=ytok[:, jq, :], in0=yd[:, jq, :], scalar=rs2[:, jq:jq + 1], in1=sgb[:],
                                    op0=ALU.mult, op1=ALU.mult), reads=[B_yd, B_rs2, B_sgb], writes=[B_ytok])
                    else:
                        for jq in range(4):
                            P.op("dve", lambda e, jq=jq: e.tensor_scalar_mul(
                                out=ytok[:, jq, m * 64:(m + 1) * 64], in0=Ops[o][:, jq, 0:64], scalar1=rr[:, jq:jq + 1]),
                                reads=[B_O[o], B_rr], writes=[B_ytok])
                    if m == 1:
                        for jq in range(4):
                            P.op("pe", lambda e, jq=jq: e.transpose(out=ptr[:, jq * 128:(jq + 1) * 128], in_=ytok[:, jq, :], identity=ident_b[:]),
                                 reads=[B_ytok, B_idb], writes=[B_ptr, B_ptr1])
                        P.op("act", lambda e: e.copy(out=yT[:, g, qc * 512:(qc + 1) * 512], in_=ptr[:, 0:512]),
                             reads=[B_ptr, B_ptr1], writes=[B_yT[g][qc]])

                LOOK = 3
                jq_ = []
                for idx in range(min(LOOK, len(steps))):
                    jq_.append(emit_S(idx))
                for idx in range(len(steps)):
                    if idx + LOOK < len(steps):
                        jq_.append(emit_S(idx + LOOK))
                    emit_PV(idx, jq_[idx])
                if dbg == 4:
                    break
            if dbg in (3, 4, 30, 305, 31, 32, 33, 34, 35, 36, 37, 38, 39):
                break
            for t in range(NT):
                s = t % 2
                r0 = tok0 + t * 128
                xt = stage[s][:, 0:1024]
                P.dma("sp", f"dst{s}", lambda e, xt=xt, r0=r0: e.dma_start(out=xt, in_=x.ap()[r0:r0 + 128, :]), writes=[B_stage[s]])
                for half in range(2):
                    for kc in range(8):
                        P.op("pe", lambda e, half=half, kc=kc, t=t: e.matmul(
                            out=Sps[half][:], lhsT=yT[:, kc, t * 128:(t + 1) * 128], rhs=woutb[:, kc, half * 512:(half + 1) * 512],
                            start=(kc == 0), stop=(kc == 7)), reads=[B_yT[kc][t // 4], B_wout], writes=[B_S[half]])
                    P.op("dve", lambda e, half=half, xt=xt: e.tensor_tensor(
                        out=xt[:, half * 512:(half + 1) * 512], in0=Sps[half][:], in1=xt[:, half * 512:(half + 1) * 512], op=ALU.add),
                        reads=[B_S[half], B_stage[s]], writes=[B_stage[s]])
                if not stop_after_attn:
                    P.dma("sp", f"dout{s}", lambda e, xt=xt, r0=r0: e.dma_start(out=x1d.ap()[r0:r0 + 128, :], in_=xt),
                          reads=[B_stage[s]], writes=[B_x1d])
                if stop_after_attn:
                    ob = Buf()
                    P.dma("sp", f"dout{s}", lambda e, xt=xt, r0=r0: e.dma_start(out=out.ap()[r0:r0 + 128, :], in_=xt), reads=[B_stage[s]], writes=[ob])
                    out_bufs.append(ob)


        if not stop_after_attn and (dbg == 0 or dbg >= 50):
            P.barrier()
            acc = xT[:].rearrange("p k n -> p (k n)").bitcast(F32).rearrange("p (t n) -> p t n", t=8)
            h2Tb = yT
            Wgu = [yT[:, :, 1024:1536], yT[:, :, 1536:2048]]
            Wdn = [qTa[:, :, 0:1024], qTa[:, :, 1024:2048]]
            wst = [G[0][:, 0:2048], G[1][:, 0:2048]]
            h2f = stage[0][:, 0:1024]
            h2Tf = stage[1][:, 0:1024]
            sgs = [E[0][:, 0:256], E[0][:, 256:512]]
            hids = [PT[0][:, 0:256], PT[0][:, 256:512]]
            hidTs = [PT[1][:, 0:256], PT[1][:, 256:512]]
            Bsg = [Buf(), Buf()]; Bhid = [Buf(), Buf()]; BhidT = [Buf(), Buf()]; _bp = Buf(); Bptr2 = [_bp, _bp]
            Rw = sb("Rw", [128, 8, 36], F32)
            lg8 = sb("lg8", [128, 8, 36], F32)
            rt8 = sb("rt8", [128, 480], F32)
            wfull = sb("wfull", [128, 8, 32], F32)
            rt = sb("rt", [128, 64], F32)
            Bm = {k: Buf(k) for k in ["acc", "h2Tb", "wgu0", "wgu1", "wd0", "wd1", "wst0", "wst1", "h2f", "h2Tf", "sg", "hid",
                                      "hidT", "Rw", "lg", "wfull", "rt", "S0", "S1", "O0", "O1", "ptr", "pj", "junk", "g2b", "idb", "idf", "x1d"]}
            P.dma("sp", "dpar", lambda e: e.dma_start(out=Rw[:, :, 0:4], in_=router_group.ap().rearrange("o (k p) c -> p (o k) c", p=128)),
                  writes=[Bm["Rw"]])
            for gx in range(4):
                P.dma("sp", "dpar", lambda e, gx=gx: e.dma_start(out=Rw[:, :, 4 + 8 * gx:12 + 8 * gx],
                                                                 in_=router_expert.ap()[0, gx].rearrange("(k p) c -> p k c", p=128)),
                      writes=[Bm["Rw"]])
            Opf = [Ops[0][:].rearrange("p a b -> p (a b)"), Ops[1][:].rearrange("p a b -> p (a b)")]
            BO = [Bm["O0"], Bm["O1"]]
            BS = [Bm["S0"], Bm["S1"]]
            Bwgu = [Bm["wgu0"], Bm["wgu1"]]
            Bwd = [Bm["wd0"], Bm["wd1"]]
            Bwst = [Bm["wst0"], Bm["wst1"]]
            st_ctr = [0]
            s_ctr = [0]
            o_ctr2 = [0]
            NSB = NTOK // 1024
            for sbk in range(NSB):
                for ti in range(8):
                    if dbg == 50 and ti == 1:
                        break
                    r0 = sbk * 1024 + ti * 128
                    if dbg == 50 and _STG == 1:
                        break
                    P.dma("sp", f"dx1_{ti}", lambda e, ti=ti, r0=r0: e.dma_start(out=acc[:, ti, :], in_=x1d.ap()[r0:r0 + 128, :]),
                          reads=[Bm["x1d"]], writes=[Bm["acc"]])
                    if dbg == 50 and _STG == 2:
                        break
                    P.op("act", lambda e, ti=ti: e.activation(out=junk[:], in_=acc[:, ti, :], func=AF.Square, accum_out=rt[:, 0:1]),
                         reads=[Bm["acc"]], writes=[Bm["junk"], Bm["rt"]])
                    if dbg == 50 and _STG == 3:
                        break
                    P.op("act", lambda e: e.activation(out=rt[:, 1:2], in_=rt[:, 0:1], func=AF.Ln, bias=float(RMS_EPS), scale=1.0 / 1024),
                         reads=[Bm["rt"]], writes=[Bm["rt"]])
                    if dbg == 50 and _STG == 4:
                        break
                    P.op("act", lambda e: e.activation(out=rt[:, 1:2], in_=rt[:, 1:2], func=AF.Exp, scale=-0.5), reads=[Bm["rt"]], writes=[Bm["rt"]])
                    if dbg == 50 and _STG == 5:
                        break
                    P.op("dve", lambda e, ti=ti: e.scalar_tensor_tensor(out=h2f, in0=acc[:, ti, :], scalar=rt[:, 1:2], in1=g2b[:],
                                                                         op0=ALU.mult, op1=ALU.mult),
                         reads=[Bm["acc"], Bm["rt"], Bm["g2b"]], writes=[Bm["h2f"]])
                    if dbg == 50 and _STG == 6:
                        break
                    for kc in range(8):
                        P.op("pe", lambda e, kc=kc: e.transpose(out=Opf[1][:, kc * 128:(kc + 1) * 128], in_=h2f[:, kc * 128:(kc + 1) * 128],
                                                                identity=ident_f[:]), reads=[Bm["h2f"], Bm["idf"]], writes=[BO[1]])
                    if dbg == 50 and _STG == 7:
                        break
                    P.op("act", lambda e: e.copy(out=h2Tf, in_=Opf[1]), reads=[BO[1]], writes=[Bm["h2Tf"]])
                    if dbg == 50 and _STG == 8:
                        break
                    P.op("dve", lambda e, ti=ti: e.tensor_copy(out=h2Tb[:, :, ti * 128:(ti + 1) * 128],
                                                               in_=h2Tf.rearrange("p (k n) -> p k n", k=8)),
                         reads=[Bm["h2Tf"]], writes=[Bm["h2Tb"]])
                    if dbg == 50 and _STG == 9:
                        break
                    for kc in range(8):
                        P.op("pe", lambda e, kc=kc: e.matmul(out=pj[:, 0:36], lhsT=h2Tf[:, kc * 128:(kc + 1) * 128], rhs=Rw[:, kc, :],
                                                             start=(kc == 0), stop=(kc == 7)), reads=[Bm["h2Tf"], Bm["Rw"]], writes=[Bm["pj"]])
                    if dbg == 50 and _STG == 10:
                        break
                    P.op("act", lambda e, ti=ti: e.copy(out=lg8[:, ti, :], in_=pj[:, 0:36]), reads=[Bm["pj"]], writes=[Bm["lg"]])
                R_ = [Bm["rt"]]; L_ = [Bm["lg"]]
                def r3(c0, n):
                    return rt8[:, c0:c0 + 8 * n].rearrange("p (t n) -> p t n", t=8)
                def r2(c0):
                    return rt8[:, c0:c0 + 8]
                GMAX, GSUM, PG, M1V, M2V, DD, ED, W1, W2, W1P, W2P = [8 * i for i in range(11)]
                GONE, SH, ESEL, TMP, M1, M2, W8 = 96, 128, 160, 224, 288, 352, 416
                glog = lg8[:, :, 0:4]
                P.op("dve", lambda e: e.tensor_reduce(out=r2(GMAX), in_=glog, axis=AX.X, op=ALU.max), reads=L_, writes=R_)
                P.op("dve", lambda e: e.tensor_tensor(out=r3(GONE, 4), in0=glog, in1=bc_inner(r2(GMAX), 4), op=ALU.is_equal), reads=L_ + R_, writes=R_)
                P.op("dve", lambda e: e.tensor_tensor(out=r3(SH, 4), in0=glog, in1=bc_inner(r2(GMAX), 4), op=ALU.subtract), reads=L_ + R_, writes=R_)
                P.op("act", lambda e: e.activation(out=r3(SH, 4), in_=r3(SH, 4), func=AF.Exp), reads=R_, writes=R_)
                P.op("dve", lambda e: e.tensor_reduce(out=r2(GSUM), in_=r3(SH, 4), axis=AX.X, op=ALU.add), reads=R_, writes=R_)
                P.op("dve", lambda e: e.reciprocal(out=r2(PG), in_=r2(GSUM)), reads=R_, writes=R_)
                for gx in range(4):
                    dst = r3(ESEL, 8) if gx == 0 else r3(TMP, 8)
                    P.op("dve", lambda e, gx=gx, dst=dst: e.tensor_tensor(
                        out=dst, in0=lg8[:, :, 4 + 8 * gx:12 + 8 * gx], in1=bc_inner(r3(GONE, 4)[:, :, gx], 8), op=ALU.mult),
                        reads=L_ + R_, writes=R_)
                    if gx > 0:
                        P.op("dve", lambda e: e.tensor_tensor(out=r3(ESEL, 8), in0=r3(ESEL, 8), in1=r3(TMP, 8), op=ALU.add), reads=R_, writes=R_)
                P.op("dve", lambda e: e.tensor_reduce(out=r2(M1V), in_=r3(ESEL, 8), axis=AX.X, op=ALU.max), reads=R_, writes=R_)
                P.op("dve", lambda e: e.tensor_tensor(out=r3(M1, 8), in0=r3(ESEL, 8), in1=bc_inner(r2(M1V), 8), op=ALU.is_equal), reads=R_, writes=R_)
                P.op("dve", lambda e: e.scalar_tensor_tensor(out=r3(TMP, 8), in0=r3(M1, 8), scalar=-1e30, in1=r3(ESEL, 8),
                                                             op0=ALU.mult, op1=ALU.add), reads=R_, writes=R_)
                P.op("dve", lambda e: e.tensor_reduce(out=r2(M2V), in_=r3(TMP, 8), axis=AX.X, op=ALU.max), reads=R_, writes=R_)
                P.op("dve", lambda e: e.tensor_tensor(out=r3(M2, 8), in0=r3(ESEL, 8), in1=bc_inner(r2(M2V), 8), op=ALU.is_equal), reads=R_, writes=R_)
                P.op("dve", lambda e: e.tensor_sub(out=r2(DD), in0=r2(M2V), in1=r2(M1V)), reads=R_, writes=R_)
                P.op("act", lambda e: e.activation(out=r2(ED), in_=r2(DD), func=AF.Exp), reads=R_, writes=R_)
                P.op("dve", lambda e: e.tensor_scalar_add(out=r2(W1), in0=r2(ED), scalar1=1.0), reads=R_, writes=R_)
                P.op("dve", lambda e: e.reciprocal(out=r2(W1), in_=r2(W1)), reads=R_, writes=R_)
                P.op("dve", lambda e: e.tensor_tensor(out=r2(W2), in0=r2(ED), in1=r2(W1), op=ALU.mult), reads=R_, writes=R_)
                P.op("dve", lambda e: e.tensor_tensor(out=r2(W1P), in0=r2(W1), in1=r2(PG), op=ALU.mult), reads=R_, writes=R_)
                P.op("dve", lambda e: e.tensor_tensor(out=r2(W2P), in0=r2(W2), in1=r2(PG), op=ALU.mult), reads=R_, writes=R_)
                P.op("dve", lambda e: e.tensor_tensor(out=r3(W8, 8), in0=r3(M1, 8), in1=bc_inner(r2(W1P), 8), op=ALU.mult), reads=R_, writes=R_)
                P.op("dve", lambda e: e.tensor_tensor(out=r3(TMP, 8), in0=r3(M2, 8), in1=bc_inner(r2(W2P), 8), op=ALU.mult), reads=R_, writes=R_)
                P.op("dve", lambda e: e.tensor_tensor(out=r3(W8, 8), in0=r3(W8, 8), in1=r3(TMP, 8), op=ALU.add), reads=R_, writes=R_)
                for gx in range(4):
                    P.op("dve", lambda e, gx=gx: e.tensor_tensor(out=wfull[:, :, 8 * gx:8 * gx + 8], in0=r3(W8, 8),
                                                                 in1=bc_inner(r3(GONE, 4)[:, :, gx], 8), op=ALU.mult),
                         reads=R_, writes=[Bm["wfull"]])
                if dbg in (50, 51):
                    break
                for ex in range(N_EXP if dbg != 52 else 1):
                    slot = ex % 2
                    srcs = [(w_gate.ap()[0, ex].rearrange("(k p) f -> p k f", p=128), Wgu[slot][:, :, 0:256], 8, 256, Bwgu[slot]),
                            (w_up.ap()[0, ex].rearrange("(k p) f -> p k f", p=128), Wgu[slot][:, :, 256:512], 8, 256, Bwgu[slot]),
                            (w_down.ap()[0, ex].rearrange("(c p) n -> p c n", p=128), Wdn[slot], 2, 1024, Bwd[slot])]
                    for src, dst, a_, b_, bdst in srcs:
                        si = st_ctr[0] % 2; st_ctr[0] += 1
                        stv = wst[si].rearrange("p (a b) -> p a b", a=a_)
                        P.dma("sp", f"dwe{si}", lambda e, stv=stv, src=src: e.dma_start(out=stv, in_=src), writes=[Bwst[si]])
                        P.op("pool", lambda e, stv=stv, dst=dst: e.tensor_copy(out=dst, in_=stv), reads=[Bwst[si]], writes=[bdst])
                    def head(ti, i, slot=slot):
                        for kc in range(8):
                            P.op("pe", lambda e, kc=kc: e.matmul(
                                out=Sps[i][:], lhsT=h2Tb[:, kc, ti * 128:(ti + 1) * 128], rhs=Wgu[slot][:, kc, :],
                                start=(kc == 0), stop=(kc == 7)), reads=[Bm["h2Tb"], Bwgu[slot]], writes=[BS[i]])

                    def mid(ti, i, o, b, slot=slot, ex=ex):
                        sg = sgs[b]; hid = hids[b]
                        P.op("act", lambda e: e.activation(out=sg, in_=Sps[i][:, 0:256], func=AF.Silu), reads=[BS[i]], writes=[Bsg[b]])
                        P.op("dve", lambda e: e.scalar_tensor_tensor(
                            out=hid, in0=Sps[i][:, 256:512], scalar=wfull[:, ti, ex:ex + 1], in1=sg, op0=ALU.mult, op1=ALU.mult),
                            reads=[BS[i], Bm["wfull"], Bsg[b]], writes=[Bhid[b]])
                        for c in range(2):
                            P.op("pe", lambda e, c=c: e.transpose(out=ptr[:, b * 256 + c * 128:b * 256 + (c + 1) * 128],
                                                                  in_=hid[:, c * 128:(c + 1) * 128],
                                                                  identity=ident_b[:]), reads=[Bhid[b], Bm["idb"]], writes=[Bptr2[b]])
                        P.op("act", lambda e: e.copy(out=hidTs[b], in_=ptr[:, b * 256:(b + 1) * 256]), reads=[Bptr2[b]], writes=[BhidT[b]])

                    def tail(ti, i, o, b, slot=slot, ex=ex):
                        hidT = hidTs[b]
                        for half in range(2):
                            for c in range(2):
                                P.op("pe", lambda e, half=half, c=c: e.matmul(
                                    out=Opf[o][:, half * 512:(half + 1) * 512], lhsT=hidT[:, c * 128:(c + 1) * 128],
                                    rhs=Wdn[slot][:, c, half * 512:(half + 1) * 512], start=(c == 0), stop=(c == 1)),
                                    reads=[BhidT[b], Bwd[slot]], writes=[BO[o]])
                        P.op("dve", lambda e: e.tensor_tensor(out=acc[:, ti, :], in0=Opf[o], in1=acc[:, ti, :], op=ALU.add),
                             reads=[BO[o], Bm["acc"]], writes=[Bm["acc"]])

                    infos = []
                    for ti in range(8):
                        i = s_ctr[0] % 2; s_ctr[0] += 1
                        o = o_ctr2[0] % 2; o_ctr2[0] += 1
                        infos.append((ti, i, o, ti % 2))
                    for k in range(8 + 2):
                        if k < 8:
                            head(infos[k][0], infos[k][1])
                        if 0 <= k - 1 < 8:
                            mid(*infos[k - 1])
                        if 0 <= k - 2 < 8:
                            tail(*infos[k - 2])
                if dbg == 52:
                    break
                for ti in range(8):
                    r0 = sbk * 1024 + ti * 128
                    ob = Buf()
                    P.dma("sp", "dfin", lambda e, ti=ti, r0=r0: e.dma_start(out=out.ap()[r0:r0 + 128, :], in_=acc[:, ti, :]),
                          reads=[Bm["acc"]], writes=[ob])
                    out_bufs.append(ob)

        P.wait_all("sp", out_bufs)
        P.finish()
        with nc.allow_non_contiguous_dma(reason="small parameter loads"):
            P.emit()
    return nc


_PARAM_KEYS = ["norm1_g", "w_in", "diff_q_g", "diff_k_g", "lambda_q1", "lambda_k1", "lambda_q2", "lambda_k2",
               "diff_sub_g", "moba_q_g", "moba_k_g", "rel_bias", "w_out", "norm2_g", "router_group",
               "router_expert", "w_gate", "w_up", "w_down"]


def run_cores(inputs, nseq, ncores, stop_after_attn=False, dbg=0):
    nc = build(nseq, stop_after_attn=stop_after_attn, dbg=dbg)
    onehot, cmask, blk = _consts()
    xs = np.ascontiguousarray(inputs["x"], dtype=np.float32).reshape(-1, D_MODEL)
    in_maps = []
    for c in range(ncores):
        m = {k: np.ascontiguousarray(inputs[k], dtype=np.float32) for k in _PARAM_KEYS}
        m["x"] = xs[c * nseq * S_LEN:(c + 1) * nseq * S_LEN]
        m["c_onehot"] = onehot; m["c_blk"] = blk
        in_maps.append(m)
    res = run_bass_kernel_spmd(nc, in_maps, core_ids=list(range(ncores)))
    return np.concatenate([np.asarray(r["out"]) for r in res.results], axis=0)


def kernel(**inputs):
    B = inputs["x"].shape[0]
    o = run_cores(inputs, B // 8, 8)
    return o.reshape(B, S_LEN, D_MODEL).astype(np.float32)
```

```python
import math
from contextlib import ExitStack

import numpy as np
import concourse.bass as bass
import concourse.mybir as mybir
from concourse.bass_utils import run_bass_kernel_spmd

F32 = mybir.dt.float32
BF16 = mybir.dt.bfloat16
I32 = mybir.dt.int32
AF = mybir.ActivationFunctionType
ALU = mybir.AluOpType
AX = mybir.AxisListType


import os as _os0
_STRICT = bool(int(_os0.environ.get('BASS_STRICT_SYNC', '0')))


class Buf:
    __slots__ = ("w", "r", "name")

    def __init__(self, name=""):
        self.w = None
        self.r = []
        self.name = name


class Prog:
    ENG = ("pe", "act", "dve", "pool", "sp")

    def __init__(self, nc, stack):
        self.nc = nc
        self.stack = stack
        self.ops = {e: [] for e in self.ENG}
        self.sem = {}
        self.cnt = {}
        self.seen = {e: {} for e in self.ENG}
        for e in self.ENG:
            self.newsem(e)

    def newsem(self, name):
        self.sem[name] = self.stack.enter_context(self.nc.semaphore("s_" + name))
        self.cnt[name] = 0

    def _waits(self, eng, reads, writes):
        need = {}

        def add(ev, raw):
            if ev is None:
                return
            sn, v = ev
            if sn == eng:
                if eng == "pe" or (not raw and not _STRICT):
                    return
            if v > need.get(sn, 0):
                need[sn] = v

        for b in reads:
            add(b.w, True)
        for b in writes:
            add(b.w, False)
            for ev in b.r:
                add(ev, False)
        out = []
        for sn, v in need.items():
            if self.seen[eng].get(sn, 0) < v:
                self.seen[eng][sn] = v
                out.append((sn, v))
        return out

    def _mark(self, ev, reads, writes):
        for b in reads:
            b.r.append(ev)
        for b in writes:
            b.w = ev
            b.r = []

    def op(self, eng, fn, reads=(), writes=()):
        waits = self._waits(eng, reads, writes)
        self.cnt[eng] += 1
        ev = (eng, self.cnt[eng])
        self.ops[eng].append((fn, waits, (eng, 1)))
        self._mark(ev, reads, writes)
        return ev

    def dma(self, q, semname, fn, reads=(), writes=()):
        if semname == "dpar":
            self._npar = getattr(self, "_npar", 0) + 1
            semname = "dpar%d" % self._npar
        if semname not in self.sem:
            self.newsem(semname)
        waits = self._waits(q, reads, writes)
        self.cnt[semname] += 16
        ev = (semname, self.cnt[semname])
        self.ops[q].append((fn, waits, (semname, 16)))
        self._mark(ev, reads, writes)
        return ev

    def wait_all(self, eng, bufs):
        waits = self._waits(eng, bufs, ())
        self.ops[eng].append((None, waits, None))

    def barrier(self):
        waits = [(sn, v) for sn, v in self.cnt.items() if v > 0]
        for eng in self.ENG:
            w = [(sn, v) for sn, v in waits if self.seen[eng].get(sn, 0) < v and not (sn == eng and eng == "sp")]
            for sn, v in w:
                self.seen[eng][sn] = v
            self.ops[eng].append((None, w, None))

    def finish(self):
        waits = [(sn, v) for sn, v in self.cnt.items() if v > 0]
        self.ops["sp"].append((None, waits, None))

    def emit(self):
        nc = self.nc
        sem = self.sem
        ops = self.ops

        def run(e, name):
            for fn, waits, inc in ops[name]:
                for sn, v in waits:
                    e.wait_ge(sem[sn], v)
                if fn is None:
                    continue
                ins = fn(e)
                ins.then_inc(sem[inc[0]], inc[1])

        with nc.Block() as block:
            @block.tensor
            def _(e):
                run(e, "pe")

            @block.scalar
            def _(e):
                run(e, "act")

            @block.vector
            def _(e):
                run(e, "dve")

            @block.gpsimd
            def _(e):
                run(e, "pool")

            @block.sync
            def _(e):
                run(e, "sp")


S_LEN = 2048
D_MODEL = 1024
NT = 16
HEAD_DIM = 64
RMS_EPS = 1e-6
LAMBDA_INIT = 0.8 - 0.6 * math.exp(-0.3 * 0)
NEG_BIG = 30000.0
N_EXP = 32
MULMOD = 1000000
CAP_CHUNK = 128


def _t5_bucket_np(n):
    n = np.maximum(n, 0)
    max_exact = 16
    nf = np.maximum(n, 1).astype(np.float32)
    large = max_exact + (np.log(nf / np.float32(max_exact)) / np.float32(math.log(2048 / max_exact))
                         * np.float32(32 - max_exact)).astype(np.int32)
    large = np.minimum(large, 31)
    return np.where(n < max_exact, n, large)


def _consts():
    d = np.arange(2048)
    b = _t5_bucket_np(d)
    onehot = np.zeros((32, 2048), np.float32)
    onehot[b, d] = 1.0
    cmask = None
    blk = np.zeros((8, 2048), np.float32)
    for n in range(8):
        blk[n, n * 256:(n + 1) * 256] = 1.0
    return onehot, cmask, blk


def bc_inner(ap, n):
    return bass.AP(tensor=ap.tensor, offset=ap.offset, ap=[list(a) for a in ap.ap] + [[0, n]])


class _Stop(Exception):
    pass


def build(nseq, stop_after_attn=False, dbg=0):
    nc = bass.Bass("TRN2", target_bir_lowering=False)
    NTOK = nseq * S_LEN
    dt_in = lambda name, shape: nc.dram_tensor(name, shape, F32, kind="ExternalInput")
    x = dt_in("x", [NTOK, D_MODEL])
    norm1_g = dt_in("norm1_g", [1, 1024])
    w_in = dt_in("w_in", [1, 1024, 3072])
    diff_q_g = dt_in("diff_q_g", [1, 64]); diff_k_g = dt_in("diff_k_g", [1, 64])
    lambda_q1 = dt_in("lambda_q1", [1, 64]); lambda_k1 = dt_in("lambda_k1", [1, 64])
    lambda_q2 = dt_in("lambda_q2", [1, 64]); lambda_k2 = dt_in("lambda_k2", [1, 64])
    diff_sub_g = dt_in("diff_sub_g", [1, 128])
    moba_q_g = dt_in("moba_q_g", [1, 64]); moba_k_g = dt_in("moba_k_g", [1, 64])
    rel_bias = dt_in("rel_bias", [32, 12])
    w_out = dt_in("w_out", [1, 1024, 1024])
    norm2_g = dt_in("norm2_g", [1, 1024])
    router_group = dt_in("router_group", [1, 1024, 4])
    router_expert = dt_in("router_expert", [1, 4, 1024, 8])
    w_gate = dt_in("w_gate", [1, 32, 1024, 256])
    w_up = dt_in("w_up", [1, 32, 1024, 256])
    w_down = dt_in("w_down", [1, 32, 256, 1024])
    c_onehot = dt_in("c_onehot", [32, 2048])
    c_blk = dt_in("c_blk", [8, 2048])
    out = nc.dram_tensor("out", [NTOK, D_MODEL], F32, kind="ExternalOutput")
    Bd = nc.dram_tensor("Bd", [12, 129, 2560], F32, kind="Internal")
    Wd = nc.dram_tensor("Wd", [8, 1024, 384], BF16, kind="Internal")
    x1d = nc.dram_tensor("x1d", [NTOK, D_MODEL], F32, kind="Internal")

    with ExitStack() as st:
        P = Prog(nc, st)
        sb = lambda name, shape, dt: st.enter_context(nc.sbuf_tensor(name, shape, dt))
        ps = lambda name, shape, dt: st.enter_context(nc.psum_tensor(name, shape, dt))
        out_bufs = []

        Sps = [ps("Sps0", [128, 512], F32), ps("Sps1", [128, 512], F32)]
        B_S = [Buf(), Buf()]
        Ops = [ps("Ops0", [128, 4, 256], F32), ps("Ops1", [128, 4, 256], F32)]
        B_O = [Buf(), Buf()]
        pj = ps("pj", [128, 512], F32); B_pj = Buf()
        ptr = ps("ptr", [128, 1024], BF16); B_ptr = Buf(); B_ptr1 = Buf()

        ident_f = sb("ident_f", [128, 128], F32); B_idf = Buf()
        ident_b = sb("ident_b", [128, 128], BF16); B_idb = Buf()
        wg = [sb("wg0", [128, 8, 384], BF16), sb("wg1", [128, 8, 384], BF16)]; B_wg = [Buf(), Buf()]
        B_Wd = Buf()
        woutb = sb("woutb", [128, 8, 1024], BF16); B_wout = Buf()
        stage = [sb("stage0", [128, 1536], F32), sb("stage1", [128, 1536], F32)]
        B_stage = [Buf(), Buf()]
        g1c = sb("g1c", [128, 8], F32); B_g1 = Buf()
        xT = sb("xT", [128, 8, 2048], BF16); B_xT = [Buf() for _ in range(NT)]
        yT = sb("yT", [128, 8, 2048], BF16); B_yT = [[Buf() for _ in range(4)] for _ in range(8)]
        qTa = sb("qTa", [128, 2, 2048], BF16); B_qT = [[Buf() for _ in range(NT)] for _ in range(2)]
        kTa = sb("kTa", [128, 2, 2048], BF16); B_kT = [Buf(), Buf()]
        B_qaugrows = Buf()
        Vt = sb("Vt", [128, 16, 136], BF16); B_V = Buf()
        G = [sb("G0", [128, 2560], F32), sb("G1", [128, 2560], F32)]; B_G = [Buf(), Buf()]
        junk = sb("junk", [128, 1024], BF16); B_junk = Buf()
        xb = sb("xb", [128, 1024], BF16); B_xb = Buf()
        ss1 = sb("ss1", [128, 16], F32); B_ss1 = Buf()
        rstd1 = sb("rstd1", [128, 16], F32); B_rstd1 = Buf()
        NB4 = 4
        qk2 = [sb(f"bqk{i}", [128, 256], F32) for i in range(NB4)]; B_qk2 = [Buf() for _ in range(NB4)]
        sq2b = [sb(f"bsq{i}", [128, 256], F32) for i in range(NB4)]; B_sq2b = [Buf() for _ in range(NB4)]
        ssh2 = [sb(f"bssh{i}", [128, 4], F32) for i in range(NB4)]; B_ssh2 = [Buf() for _ in range(NB4)]
        rsh2 = [sb(f"brsh{i}", [128, 4], F32) for i in range(NB4)]; B_rsh2 = [Buf() for _ in range(NB4)]
        qkn2 = [sb(f"bqkn{i}", [128, 256], BF16) for i in range(NB4)]; B_qkn2 = [Buf() for _ in range(NB4)]
        E = [sb(f"E{i}", [128, 512], F32) for i in range(4)]; B_E = [Buf() for _ in range(4)]
        PT = [sb(f"PT{i}", [128, 512], BF16) for i in range(5)]; B_PT = [Buf() for _ in range(5)]
        rr = sb("rr", [128, 4], F32); B_rr = Buf()
        nlr = sb("nlr", [128, 4], F32); B_nlr = Buf()
        tmp1 = sb("tmp1", [128, 4, 128], F32); B_tmp1 = Buf()
        yd = sb("yd", [128, 4, 128], F32); B_yd = Buf()
        sq2 = sb("sq2", [128, 4, 128], F32); B_sq2 = Buf()
        ss2 = sb("ss2", [128, 4], F32); B_ss2 = Buf()
        rs2 = sb("rs2", [128, 4], F32); B_rs2 = Buf()
        ytok = sb("ytok", [128, 4, 128], BF16); B_ytok = Buf()
        sgb = sb("sgb", [128, 128], F32); B_sgb = Buf()
        g2b = sb("g2b", [128, 1024], F32); B_g2b = Buf()
        lamv = sb("lamv", [128, 4, 64], F32); B_lamv = Buf()
        lamp = sb("lamp", [128, 2, 64], F32); B_lamp = Buf()
        lams = sb("lams", [128, 4], F32); B_lams = Buf()
        gb = sb("gb", [128, 4, 64], F32); B_gq = Buf()
        gqk = sb("gqk", [128, 2, 64], F32); B_gqk = Buf()
        rb = sb("rb", [32, 12], F32); B_rb = Buf()
        oh = G[0][0:32, 0:2048]; B_oh = B_G[0]
        et = G[1][0:12, 0:2560]; B_et = B_G[1]
        B_Bd = Buf()
        B_x1d = Buf()
        km = sb("km", [64, 16], F32); B_km = Buf()
        kmf = sb("kmf", [64, 16], F32); B_kmf = Buf()
        kmhf = sb("kmhf", [64, 16], F32); B_kmhf = Buf()
        kmhl = sb("kmhl", [64, 2, 16], BF16); B_kmhl = Buf()
        g16 = sb("g16", [128, 16], F32); B_g16 = Buf()
        gate = sb("gate", [128, 8], F32); B_gate = Buf()
        mx8 = sb("mx8", [128, 8], F32); B_mx8 = Buf()
        selm = sb("selm", [128, 8], F32); B_selm = Buf()
        qaug = sb("qaug", [128, 128], BF16); B_qaug = Buf()

        P.op("pool", lambda e: e.memset(ident_f[:], 1.0), writes=[B_idf])
        P.op("pool", lambda e: e.affine_select(out=ident_f[:], in_=ident_f[:], pattern=[[-1, 128]],
                                               compare_op=ALU.is_equal, fill=0.0, base=0, channel_multiplier=1),
             reads=[B_idf], writes=[B_idf])
        P.op("dve", lambda e: e.tensor_copy(out=ident_b[:], in_=ident_f[:]), reads=[B_idf], writes=[B_idb])

        P.dma("sp", "dpar", lambda e: e.dma_start(out=g1c[:], in_=norm1_g.ap().rearrange("o (k p) -> p (o k)", p=128)),
              writes=[B_g1])
        w_in_v = w_in.ap().rearrange("o (k p) n -> p (o k) n", p=128)
        B_wst = [Buf(), Buf()]
        for kc in range(8):
            for T in range(2):
                s = T
                P.dma("sp", f"dst{s}", lambda e, kc=kc, s=s, T=T: e.dma_start(out=stage[s][:], in_=w_in_v[:, kc, T * 1536:(T + 1) * 1536]),
                      writes=[B_stage[s]])
                P.op("dve" if s == 0 else "pool",
                     lambda e, kc=kc, s=s: e.tensor_scalar_mul(out=qTa[:, s, 0:1536], in0=stage[s][:], scalar1=g1c[:, kc:kc + 1]),
                     reads=[B_stage[s], B_g1], writes=[B_wst[s]])
                for gg in range(4):
                    a = qTa[:, s, gg * 128:gg * 128 + 128]
                    src3 = bass.AP(tensor=a.tensor, offset=a.offset, ap=[list(a.ap[0]), [512, 3], [1, 128]])
                    P.dma("act", f"dws{s}", lambda e, kc=kc, T=T, gg=gg, src3=src3: e.dma_start(
                        out=Wd.ap()[T * 4 + gg, kc * 128:(kc + 1) * 128, :].rearrange("p (a b) -> p a b", a=3), in_=src3),
                        reads=[B_wst[s]], writes=[B_Wd])
        w_out_v = w_out.ap().rearrange("o (k p) n -> p (o k) n", p=128)
        P.dma("pool", "dwo", lambda e: e.dma_start(out=woutb[:], in_=w_out_v), writes=[B_wout])

        P.dma("sp", "dpar", lambda e: e.dma_start(out=rb[:], in_=rel_bias.ap()), writes=[B_rb])
        P.dma("sp", "dpar", lambda e: e.dma_start(out=oh, in_=c_onehot.ap()), writes=[B_oh])
        P.op("pool", lambda e: e.memset(et[:, 0:512], 0.0), writes=[B_et])
        for c in range(4):
            P.op("pe", lambda e, c=c: e.matmul(out=pj[0:12, :], lhsT=rb[:, :], rhs=oh[:, c * 512:(c + 1) * 512],
                                               start=True, stop=True), reads=[B_rb, B_oh], writes=[B_pj])
            P.op("act", lambda e, c=c: e.activation(out=et[:, 512 + c * 512:512 + (c + 1) * 512], in_=pj[0:12, :], func=AF.Exp),
                 reads=[B_pj], writes=[B_et])
        et_ap = et
        esrc = bass.AP(tensor=et_ap.tensor, offset=et_ap.offset, ap=[list(et_ap.ap[0]), [0, 129], [1, 2560]])
        P.dma("sp", "dbd", lambda e: e.dma_start(out=Bd.ap(), in_=esrc), reads=[B_et], writes=[B_Bd])

        for i, t in enumerate([diff_q_g, diff_k_g, moba_q_g, moba_k_g]):
            P.dma("sp", "dpar", lambda e, i=i, t=t: e.dma_start(out=gb[:, i, :], in_=bass.AP(tensor=t, offset=0, ap=[[0, 128], [1, 64]])),
                  writes=[B_gq])
        P.op("dve", lambda e: e.tensor_tensor(out=gqk[:, 0, :], in0=gb[:, 0, :], in1=gb[:, 1, :], op=ALU.mult), reads=[B_gq], writes=[B_gqk])
        P.op("dve", lambda e: e.tensor_tensor(out=gqk[:, 1, :], in0=gb[:, 2, :], in1=gb[:, 3, :], op=ALU.mult), reads=[B_gq], writes=[B_gqk])
        P.dma("sp", "dpar", lambda e: e.dma_start(out=sgb[:], in_=bass.AP(tensor=diff_sub_g, offset=0, ap=[[0, 128], [1, 128]])),
              writes=[B_sgb])
        P.op("dve", lambda e: e.tensor_scalar_mul(out=sgb[:], in0=sgb[:], scalar1=float(1.0 - LAMBDA_INIT)), reads=[B_sgb], writes=[B_sgb])
        P.dma("sp", "dpar", lambda e: e.dma_start(out=g2b[:], in_=bass.AP(tensor=norm2_g, offset=0, ap=[[0, 128], [1, 1024]])),
              writes=[B_g2b])
        for i, t in enumerate([lambda_q1, lambda_k1, lambda_q2, lambda_k2]):
            P.dma("sp", "dpar", lambda e, i=i, t=t: e.dma_start(out=lamv[:, i, :], in_=bass.AP(tensor=t, offset=0, ap=[[0, 128], [1, 64]])),
                  writes=[B_lamv])
        P.op("dve", lambda e: e.tensor_tensor(out=lamp[:, 0, :], in0=lamv[:, 0, :], in1=lamv[:, 1, :], op=ALU.mult), reads=[B_lamv], writes=[B_lamp])
        P.op("dve", lambda e: e.tensor_tensor(out=lamp[:, 1, :], in0=lamv[:, 2, :], in1=lamv[:, 3, :], op=ALU.mult), reads=[B_lamv], writes=[B_lamp])
        P.op("dve", lambda e: e.tensor_reduce(out=lams[:, 0:2], in_=lamp[:], axis=AX.X, op=ALU.add), reads=[B_lamp], writes=[B_lams])
        P.op("act", lambda e: e.activation(out=lams[:, 0:2], in_=lams[:, 0:2], func=AF.Exp), reads=[B_lams], writes=[B_lams])
        P.op("dve", lambda e: e.tensor_sub(out=lams[:, 2:3], in0=lams[:, 0:1], in1=lams[:, 1:2]), reads=[B_lams], writes=[B_lams])
        P.op("dve", lambda e: e.tensor_scalar(out=lams[:, 3:4], in0=lams[:, 2:3], scalar1=float(LAMBDA_INIT), scalar2=-1.0,
                                              op0=ALU.add, op1=ALU.mult), reads=[B_lams], writes=[B_lams])
        nlam = lams[:, 3:4]

        P.op("pool", lambda e: e.memset(Vt[:], 0.0), writes=[B_V])
        P.op("pool", lambda e: e.memset(Vt[:, :, 64:65], 1.0), writes=[B_V])
        P.op("pool", lambda e: e.memset(Vt[:, :, 132:133], 1.0), writes=[B_V])
        P.op("pool", lambda e: e.memset(qaug[:], 0.0), writes=[B_qaug])
        P.op("pool", lambda e: e.memset(qTa[64:128, :, :], 0.0), writes=[B_qaugrows, B_wst[0], B_wst[1]])
        P.op("pool", lambda e: e.memset(kTa[64:128, :, :], 0.0), writes=[B_kT[0], B_kT[1]])
        for m in range(2):
            P.dma("pool", "dpar", lambda e, m=m: e.dma_start(out=kTa[64:72, m, :], in_=c_blk.ap()), writes=[B_kT[m]])

        def rsqrt_act(dst, src, n_feat, rbufs, wbufs):
            P.op("act", lambda e: e.activation(out=dst, in_=src, func=AF.Ln, bias=float(RMS_EPS), scale=1.0 / n_feat),
                 reads=rbufs, writes=wbufs)
            P.op("act", lambda e: e.activation(out=dst, in_=dst, func=AF.Exp, scale=-0.5), reads=wbufs, writes=wbufs)

        pt_ctr = [0]
        e_ctr = [0]
        o_ctr = [0]
        mul_ctr = [0]
        w_ctr = [0]
        _pass = [0]
        import os as _os
        _STG = int(_os.environ.get('DBG_STAGE', '0'))

        for sq_i in range(nseq if (dbg != 1 and dbg < 50) else 0):
            tok0 = sq_i * S_LEN
            for t in range(NT):
                s = t % 2
                r0 = tok0 + t * 128
                xt = stage[s][:, 0:1024]
                P.dma("sp", f"dst{s}", lambda e, xt=xt, r0=r0: e.dma_start(out=xt, in_=x.ap()[r0:r0 + 128, :]), writes=[B_stage[s]])
                P.op("act", lambda e, xt=xt, t=t: e.activation(out=junk[:], in_=xt, func=AF.Square, accum_out=ss1[:, t:t + 1]),
                     reads=[B_stage[s]], writes=[B_junk, B_ss1])
                P.op("pool", lambda e, xt=xt: e.tensor_copy(out=xb[:], in_=xt), reads=[B_stage[s]], writes=[B_xb])
                for kc in range(8):
                    P.op("pe", lambda e, kc=kc: e.transpose(out=ptr[:, kc * 128:(kc + 1) * 128], in_=xb[:, kc * 128:(kc + 1) * 128],
                                                            identity=ident_b[:]), reads=[B_xb, B_idb], writes=[B_ptr, B_ptr1])
                P.op("dve", lambda e, t=t: e.tensor_copy(out=xT[:, :, t * 128:(t + 1) * 128],
                                                         in_=ptr[:].rearrange("p (k n) -> p k n", k=8)),
                     reads=[B_ptr, B_ptr1], writes=[B_xT[t]])
            rsqrt_act(rstd1[:], ss1[:], 1024.0, [B_ss1], [B_rstd1])
            if dbg == 2:
                break

            for g in range(8):
                is_moba = g >= 4
                qoff = (1536 if is_moba else 0) + (g % 4) * 128
                K = 72 if is_moba else 64
                gcol = 2 if is_moba else 0
                heads = [4 + 2 * (g - 4), 4 + 2 * (g - 4) + 1] if is_moba else [g]
                for gi, h in enumerate(heads):
                    P.dma("sp", f"dG{gi}", lambda e, gi=gi, h=h: e.dma_start(
                        out=G[gi][:, 0:2432], in_=bass.AP(tensor=Bd, offset=h * 129 * 2560 + 128, ap=[[2559, 128], [1, 2432]])),
                        reads=[B_Bd], writes=[B_G[gi]])
                wsl = w_ctr[0] % 2; w_ctr[0] += 1
                P.dma("sp", f"dwg{wsl}", lambda e, wsl=wsl, g=g: e.dma_start(
                    out=wg[wsl][:], in_=Wd.ap()[g].rearrange("(k p) n -> p k n", p=128)), reads=[B_Wd], writes=[B_wg[wsl]])
                def bsel(t):
                    p = t % 2; p3 = t % 3; p4 = t % 4
                    return dict(pjp=[pj, Sps[0], Sps[1]][p3], B_pjp=[B_pj, B_S[0], B_S[1]][p3],
                                qk=qk2[p4], B_qk=B_qk2[p4], sq=sq2b[p4], B_sq=B_sq2b[p4], ssh=ssh2[p4], B_ssh=B_ssh2[p4],
                                rsh=rsh2[p4], B_rsh=B_rsh2[p4], qkn=qkn2[p4], B_qkn=B_qkn2[p4],
                                B_pt=[B_ptr, B_ptr1], pc0=p * 512)

                def st0(t, wsl=wsl):
                    d = bsel(t); pjp = d["pjp"]
                    for kc in range(8):
                        P.op("pe", lambda e, kc=kc: e.matmul(
                            out=pjp[:, 0:384], lhsT=xT[:, kc, t * 128:(t + 1) * 128], rhs=wg[wsl][:, kc, :],
                            start=(kc == 0), stop=(kc == 7)), reads=[B_xT[t], B_wg[wsl]], writes=[d["B_pjp"]])

                def st1(t):
                    d = bsel(t); pjp = d["pjp"]; qk = d["qk"]; sq = d["sq"]
                    P.op("dve", lambda e: e.tensor_scalar_mul(out=qk[:], in0=pjp[:, 0:256], scalar1=rstd1[:, t:t + 1]),
                         reads=[d["B_pjp"], B_rstd1], writes=[d["B_qk"]])
                    P.op("dve", lambda e: e.tensor_scalar_mul(
                        out=Vt[:, t, :].rearrange("p (a b) -> p a b", a=2)[:, :, 0:64],
                        in0=pjp[:, 256:384].rearrange("p (a b) -> p a b", a=2), scalar1=rstd1[:, t:t + 1]),
                        reads=[d["B_pjp"], B_rstd1], writes=[B_V])
                    P.op("pool", lambda e: e.tensor_tensor(out=sq[:], in0=qk[:], in1=qk[:], op=ALU.mult), reads=[d["B_qk"]], writes=[d["B_sq"]])

                def st2(t):
                    d = bsel(t); sq = d["sq"]; ssh = d["ssh"]; rsh = d["rsh"]
                    P.op("dve", lambda e: e.tensor_reduce(out=ssh[:], in_=sq[:].rearrange("p (a b) -> p a b", a=4), axis=AX.X, op=ALU.add),
                         reads=[d["B_sq"]], writes=[d["B_ssh"]])
                    rsqrt_act(rsh[:], ssh[:], 64.0, [d["B_ssh"]], [d["B_rsh"]])

                def st3(t, gi2=(1 if is_moba else 0)):
                    d = bsel(t); qk = d["qk"]; rsh = d["rsh"]; qkn = d["qkn"]
                    for i in range(2):
                        P.op("dve", lambda e, i=i: e.tensor_scalar_mul(
                            out=qkn[:, i * 64:(i + 1) * 64], in0=qk[:, i * 64:(i + 1) * 64], scalar1=rsh[:, i:i + 1]),
                            reads=[d["B_qk"], d["B_rsh"]], writes=[d["B_qkn"]])
                    for i in range(2, 4):
                        P.op("dve", lambda e, i=i: e.scalar_tensor_tensor(
                            out=qkn[:, i * 64:(i + 1) * 64], in0=qk[:, i * 64:(i + 1) * 64], scalar=rsh[:, i:i + 1],
                            in1=gqk[:, gi2, :], op0=ALU.mult, op1=ALU.mult), reads=[d["B_qk"], d["B_rsh"], B_gqk], writes=[d["B_qkn"]])

                def st4(t):
                    d = bsel(t); qkn = d["qkn"]; pc0 = d["pc0"]
                    for i in range(4):
                        P.op("pe", lambda e, i=i: e.transpose(
                            out=ptr[0:64, pc0 + i * 128:pc0 + (i + 1) * 128], in_=qkn[:, i * 64:(i + 1) * 64],
                            identity=ident_b[:]), reads=[d["B_qkn"], B_idb], writes=d["B_pt"])

                def st5(t):
                    d = bsel(t); pc0 = d["pc0"]
                    P.op("act", lambda e: e.copy(out=qTa[0:64, :, t * 128:(t + 1) * 128],
                                                 in_=ptr[0:64, pc0:pc0 + 256].rearrange("p (a b) -> p a b", a=2)),
                         reads=d["B_pt"], writes=[B_qT[0][t], B_qT[1][t]])
                    P.op("act", lambda e: e.copy(out=kTa[0:64, :, t * 128:(t + 1) * 128],
                                                 in_=ptr[0:64, pc0 + 256:pc0 + 512].rearrange("p (a b) -> p a b", a=2)),
                         reads=d["B_pt"], writes=[B_kT[0], B_kT[1]])

                stages_b = [(st0, 0), (st1, 1), (st2, 2), (st3, 3), (st5, 5), (st4, 4)]
                for kk in range(NT + 5):
                    for fn, lag in stages_b:
                        tt = kk - lag
                        if 0 <= tt < NT:
                            fn(tt)
                if dbg in (3, 30, 305, 31, 32, 33, 34, 35, 36, 37, 38, 39):
                    break
                if is_moba:
                    P.op("dve", lambda e: e.tensor_reduce(out=km[:], in_=kTa[0:64, :, :].rearrange("p m (a b) -> p (m a) b", a=8),
                                                          axis=AX.X, op=ALU.add), reads=[B_kT[0], B_kT[1]], writes=[B_km])
                    P.op("dve", lambda e: e.tensor_scalar_mul(out=kmf[:], in0=km[:], scalar1=1.0 / 256), reads=[B_km], writes=[B_kmf])
                    kview = kmhl[:, :, 0:8]
                    P.op("dve", lambda e: e.tensor_copy(out=kview, in_=kmf[:].rearrange("p (m a) -> p m a", m=2)),
                         reads=[B_kmf], writes=[B_kmhl])
                    P.op("dve", lambda e: e.tensor_copy(out=kmhf[:].rearrange("p (m a) -> p m a", m=2), in_=kview),
                         reads=[B_kmhl], writes=[B_kmhf])
                    P.op("dve", lambda e: e.tensor_sub(out=kmhl[:, :, 8:16], in0=kmf[:].rearrange("p (m a) -> p m a", m=2),
                                                       in1=kmhf[:].rearrange("p (m a) -> p m a", m=2)),
                         reads=[B_kmf, B_kmhf], writes=[B_kmhl])
                    for m in range(2):
                        for t in range(8, NT):
                            own = t // 2
                            P.op("pe", lambda e, m=m, t=t: e.matmul(out=pj[:, 0:16], lhsT=qTa[0:64, m, t * 128:(t + 1) * 128],
                                                                    rhs=kmhl[0:64, m, :], start=True, stop=True),
                                 reads=[B_qT[m][t], B_kmhl], writes=[B_pj])
                            P.op("act", lambda e: e.copy(out=g16[:], in_=pj[:, 0:16]), reads=[B_pj], writes=[B_g16])
                            P.op("pool", lambda e: e.memset(gate[:], -1e30), writes=[B_gate])
                            P.op("dve", lambda e, own=own: e.tensor_tensor(out=gate[:, 0:own], in0=g16[:, 0:own], in1=g16[:, 8:8 + own],
                                                                           op=ALU.add), reads=[B_g16], writes=[B_gate])
                            P.op("dve", lambda e: e.max(out=mx8[:], in_=gate[:]), reads=[B_gate], writes=[B_mx8])
                            P.op("dve", lambda e, own=own: e.tensor_scalar(out=selm[:, 0:own], in0=gate[:, 0:own], scalar1=mx8[:, 2:3],
                                                                           scalar2=None, op0=ALU.is_ge), reads=[B_gate, B_mx8], writes=[B_selm])
                            P.op("pool", lambda e: e.memset(qaug[:, 64:72], 0.0), writes=[B_qaug])
                            P.op("dve", lambda e, own=own: e.tensor_scalar(out=qaug[:, 64:64 + own], in0=selm[:, 0:own], scalar1=-1.0,
                                                                           scalar2=NEG_BIG, op0=ALU.add, op1=ALU.mult),
                                 reads=[B_selm], writes=[B_qaug])
                            P.op("pe", lambda e: e.transpose(out=ptr[:, 0:128], in_=qaug[:], identity=ident_b[:]),
                                 reads=[B_qaug, B_idb], writes=[B_ptr])
                            P.op("act", lambda e, m=m, t=t: e.copy(out=qTa[64:72, m, t * 128:(t + 1) * 128], in_=ptr[64:72, 0:128]),
                                 reads=[B_ptr], writes=[B_qT[m][t]])
                steps = []
                for qc in range(4):
                    for m in range(2):
                        for kt in range(4 * qc + 4):
                            steps.append((qc, m, kt))
                st_info = {}

                def emit_S(idx, K=K, is_moba=is_moba, steps=steps, st_info=st_info):
                    qc, m, kt = steps[idx]
                    if kt == 0:
                        st_info[(qc, m)] = o_ctr[0] % 2; o_ctr[0] += 1
                    i = e_ctr[0] % 4; e_ctr[0] += 1
                    j = pt_ctr[0] % 5; pt_ctr[0] += 1
                    gi = m if is_moba else 0
                    Sb = [Sps[0][:], Sps[1][:], pj[:], ptr[:].bitcast(F32)][i]
                    B_Sb = [[B_S[0]], [B_S[1]], [B_pj], [B_ptr, B_ptr1]][i]
                    P.op("pe", lambda e: e.matmul(
                        out=Sb, lhsT=kTa[0:K, m, kt * 128:(kt + 1) * 128], rhs=qTa[0:K, m, qc * 512:(qc + 1) * 512],
                        start=True, stop=True),
                        reads=[B_kT[m]] + [B_qT[m][qc * 4 + u] for u in range(4)] + [B_qaugrows], writes=B_Sb)
                    P.op("act", lambda e: e.activation(out=E[i][:], in_=Sb, func=AF.Exp, scale=HEAD_DIM ** -0.5),
                         reads=B_Sb, writes=[B_E[i]])
                    c0 = (4 * qc - kt + 3) * 128
                    meng = "pool" if (mul_ctr[0] % MULMOD == MULMOD - 1) else "dve"; mul_ctr[0] += 1
                    P.op(meng, lambda e: e.tensor_tensor(out=PT[j][:], in0=E[i][:], in1=G[gi][:, c0:c0 + 512], op=ALU.mult),
                         reads=[B_E[i], B_G[gi]], writes=[B_PT[j]])
                    return j

                pending_fin = []

                def emit_PV(idx, j, is_moba=is_moba, steps=steps, st_info=st_info, pending_fin=pending_fin):
                    qc, m, kt = steps[idx]
                    o = st_info[(qc, m)]
                    if is_moba:
                        v0, vw = m * 68, 65
                    else:
                        v0, vw = 0, 133
                    for jq in range(4):
                        if kt > 4 * qc + jq:
                            continue
                        P.op("pe", lambda e, jq=jq: e.matmul(
                            out=Ops[o][:, jq, 0:vw], lhsT=PT[j][:, jq * 128:(jq + 1) * 128], rhs=Vt[:, kt, v0:v0 + vw],
                            start=(kt == 0 and jq in (0, 2)), stop=(kt == 4 * qc + jq), skip_group_check=True),
                            reads=[B_PT[j], B_V], writes=[B_O[o]])
                    if kt == 4 * qc + 3:
                        pending_fin.append((idx + 2, qc, m, o))

                def finalize(qc, m, o, g=g, is_moba=is_moba):
                    scol = 132 if not is_moba else 64
                    P.op("dve", lambda e: e.reciprocal(out=rr[:], in_=Ops[o][:, :, scol:scol + 1].rearrange("p a b -> p (a b)")),
                         reads=[B_O[o]], writes=[B_rr])
                    if not is_moba:
                        if m == 0:
                            for jq in range(4):
                                P.op("dve", lambda e, jq=jq: e.tensor_scalar_mul(
                                    out=tmp1[:, jq, :].rearrange("p (a b) -> p a b", a=2),
                                    in0=Ops[o][:, jq, 0:136].rearrange("p (a b) -> p a b", a=2)[:, :, 0:64],
                                    scalar1=rr[:, jq:jq + 1]), reads=[B_O[o], B_rr], writes=[B_tmp1])
                        else:
                            P.op("dve", lambda e: e.tensor_scalar_mul(out=nlr[:], in0=rr[:], scalar1=nlam), reads=[B_rr, B_lams], writes=[B_nlr])
                            for jq in range(4):
                                P.op("dve", lambda e, jq=jq: e.scalar_tensor_tensor(
                                    out=yd[:, jq, :].rearrange("p (a b) -> p a b", a=2),
                                    in0=Ops[o][:, jq, 0:136].rearrange("p (a b) -> p a b", a=2)[:, :, 0:64],
                                    scalar=nlr[:, jq:jq + 1], in1=tmp1[:, jq, :].rearrange("p (a b) -> p a b", a=2),
                                    op0=ALU.mult, op1=ALU.add), reads=[B_O[o], B_nlr, B_tmp1], writes=[B_yd])
                            P.op("pool", lambda e: e.tensor_tensor(out=sq2[:], in0=yd[:], in1=yd[:], op=ALU.mult), reads=[B_yd], writes=[B_sq2])
                            P.op("dve", lambda e: e.tensor_reduce(out=ss2[:], in_=sq2[:], axis=AX.X, op=ALU.add), reads=[B_sq2], writes=[B_ss2])
                            rsqrt_act(rs2[:], ss2[:], 128.0, [B_ss2], [B_rs2])
                            for jq in range(4):
                                P.op("dve", lambda e, jq=jq: e.scalar_tensor_tensor(
                                    out=ytok[:, jq, :], in0=yd[:, jq, :], scalar=rs2[:, jq:jq + 1], in1=sgb[:],
                                    op0=ALU.mult, op1=ALU.mult), reads=[B_yd, B_rs2, B_sgb], writes=[B_ytok])
                    else:
                        for jq in range(4):
                            P.op("dve", lambda e, jq=jq: e.tensor_scalar_mul(
                                out=ytok[:, jq, m * 64:(m + 1) * 64], in0=Ops[o][:, jq, 0:64], scalar1=rr[:, jq:jq + 1]),
                                reads=[B_O[o], B_rr], writes=[B_ytok])
                    if m == 1:
                        for jq in range(4):
                            P.op("pe", lambda e, jq=jq: e.transpose(out=ptr[:, jq * 128:(jq + 1) * 128], in_=ytok[:, jq, :], identity=ident_b[:]),
                                 reads=[B_ytok, B_idb], writes=[B_ptr, B_ptr1])
                        P.op("act", lambda e: e.copy(out=yT[:, g, qc * 512:(qc + 1) * 512], in_=ptr[:, 0:512]),
                             reads=[B_ptr, B_ptr1], writes=[B_yT[g][qc]])

                LOOK = 3
                jq_ = []
                for idx in range(min(LOOK, len(steps))):
                    jq_.append(emit_S(idx))
                for idx in range(len(steps)):
                    if idx + LOOK < len(steps):
                        jq_.append(emit_S(idx + LOOK))
                    emit_PV(idx, jq_[idx])
                    while pending_fin and pending_fin[0][0] <= idx:
                        _, fq, fm, fo = pending_fin.pop(0)
                        finalize(fq, fm, fo)
                while pending_fin:
                    _, fq, fm, fo = pending_fin.pop(0)
                    finalize(fq, fm, fo)
                if dbg == 4:
                    break
            if dbg in (3, 4, 30, 305, 31, 32, 33, 34, 35, 36, 37, 38, 39):
                break
            for t in range(NT):
                s = t % 2
                r0 = tok0 + t * 128
                xt = stage[s][:, 0:1024]
                P.dma("sp", f"dst{s}", lambda e, xt=xt, r0=r0: e.dma_start(out=xt, in_=x.ap()[r0:r0 + 128, :]), writes=[B_stage[s]])
                for half in range(2):
                    for kc in range(8):
                        P.op("pe", lambda e, half=half, kc=kc, t=t: e.matmul(
                            out=Sps[half][:], lhsT=yT[:, kc, t * 128:(t + 1) * 128], rhs=woutb[:, kc, half * 512:(half + 1) * 512],
                            start=(kc == 0), stop=(kc == 7)), reads=[B_yT[kc][t // 4], B_wout], writes=[B_S[half]])
                    P.op("dve", lambda e, half=half, xt=xt: e.tensor_tensor(
                        out=xt[:, half * 512:(half + 1) * 512], in0=Sps[half][:], in1=xt[:, half * 512:(half + 1) * 512], op=ALU.add),
                        reads=[B_S[half], B_stage[s]], writes=[B_stage[s]])
                if not stop_after_attn:
                    P.dma("sp", f"dout{s}", lambda e, xt=xt, r0=r0: e.dma_start(out=x1d.ap()[r0:r0 + 128, :], in_=xt),
                          reads=[B_stage[s]], writes=[B_x1d])
                if stop_after_attn:
                    ob = Buf()
                    P.dma("sp", f"dout{s}", lambda e, xt=xt, r0=r0: e.dma_start(out=out.ap()[r0:r0 + 128, :], in_=xt), reads=[B_stage[s]], writes=[ob])
                    out_bufs.append(ob)


        if not stop_after_attn and (dbg == 0 or dbg >= 50):
            P.barrier()
            acc = xT[:].rearrange("p k n -> p (k n)").bitcast(F32).rearrange("p (t n) -> p t n", t=8)
            h2Tb = yT
            Wgu = [yT[:, :, 1024:1536], yT[:, :, 1536:2048]]
            Wdn = [qTa[:, :, 0:1024], qTa[:, :, 1024:2048]]
            wst = [G[0][:, 0:2048], G[1][:, 0:2048]]
            h2f = stage[0][:, 0:1024]
            h2Tf = stage[1][:, 0:1024]
            sgs = [E[0][:, 0:256], E[0][:, 256:512]]
            hids = [PT[0][:, 0:256], PT[0][:, 256:512]]
            hidTs = [PT[1][:, 0:256], PT[1][:, 256:512]]
            Bsg = [Buf(), Buf()]; Bhid = [Buf(), Buf()]; BhidT = [Buf(), Buf()]; _bp = Buf(); Bptr2 = [_bp, _bp]
            Rw = sb("Rw", [128, 8, 36], F32)
            lg8 = sb("lg8", [128, 8, 36], F32)
            rt8 = sb("rt8", [128, 480], F32)
            wfull = sb("wfull", [128, 8, 32], F32)
            rt = sb("rt", [128, 64], F32)
            Bm = {k: Buf(k) for k in ["acc", "h2Tb", "wgu0", "wgu1", "wd0", "wd1", "wst0", "wst1", "h2f", "h2Tf", "sg", "hid",
                                      "hidT", "Rw", "lg", "wfull", "rt", "S0", "S1", "O0", "O1", "ptr", "pj", "junk", "g2b", "idb", "idf", "x1d"]}
            P.dma("sp", "dpar", lambda e: e.dma_start(out=Rw[:, :, 0:4], in_=router_group.ap().rearrange("o (k p) c -> p (o k) c", p=128)),
                  writes=[Bm["Rw"]])
            for gx in range(4):
                P.dma("sp", "dpar", lambda e, gx=gx: e.dma_start(out=Rw[:, :, 4 + 8 * gx:12 + 8 * gx],
                                                                 in_=router_expert.ap()[0, gx].rearrange("(k p) c -> p k c", p=128)),
                      writes=[Bm["Rw"]])
            Opf = [Ops[0][:].rearrange("p a b -> p (a b)"), Ops[1][:].rearrange("p a b -> p (a b)")]
            BO = [Bm["O0"], Bm["O1"]]
            BS = [Bm["S0"], Bm["S1"]]
            Bwgu = [Bm["wgu0"], Bm["wgu1"]]
            Bwd = [Bm["wd0"], Bm["wd1"]]
            Bwst = [Bm["wst0"], Bm["wst1"]]
            st_ctr = [0]
            s_ctr = [0]
            o_ctr2 = [0]
            NSB = NTOK // 1024
            for sbk in range(NSB):
                for ti in range(8):
                    if dbg == 50 and ti == 1:
                        break
                    r0 = sbk * 1024 + ti * 128
                    if dbg == 50 and _STG == 1:
                        break
                    P.dma("sp", f"dx1_{ti}", lambda e, ti=ti, r0=r0: e.dma_start(out=acc[:, ti, :], in_=x1d.ap()[r0:r0 + 128, :]),
                          reads=[Bm["x1d"]], writes=[Bm["acc"]])
                    if dbg == 50 and _STG == 2:
                        break
                    P.op("act", lambda e, ti=ti: e.activation(out=junk[:], in_=acc[:, ti, :], func=AF.Square, accum_out=rt[:, 0:1]),
                         reads=[Bm["acc"]], writes=[Bm["junk"], Bm["rt"]])
                    if dbg == 50 and _STG == 3:
                        break
                    P.op("act", lambda e: e.activation(out=rt[:, 1:2], in_=rt[:, 0:1], func=AF.Ln, bias=float(RMS_EPS), scale=1.0 / 1024),
                         reads=[Bm["rt"]], writes=[Bm["rt"]])
                    if dbg == 50 and _STG == 4:
                        break
                    P.op("act", lambda e: e.activation(out=rt[:, 1:2], in_=rt[:, 1:2], func=AF.Exp, scale=-0.5), reads=[Bm["rt"]], writes=[Bm["rt"]])
                    if dbg == 50 and _STG == 5:
                        break
                    P.op("dve", lambda e, ti=ti: e.scalar_tensor_tensor(out=h2f, in0=acc[:, ti, :], scalar=rt[:, 1:2], in1=g2b[:],
                                                                         op0=ALU.mult, op1=ALU.mult),
                         reads=[Bm["acc"], Bm["rt"], Bm["g2b"]], writes=[Bm["h2f"]])
                    if dbg == 50 and _STG == 6:
                        break
                    for kc in range(8):
                        P.op("pe", lambda e, kc=kc: e.transpose(out=Opf[1][:, kc * 128:(kc + 1) * 128], in_=h2f[:, kc * 128:(kc + 1) * 128],
                                                                identity=ident_f[:]), reads=[Bm["h2f"], Bm["idf"]], writes=[BO[1]])
                    if dbg == 50 and _STG == 7:
                        break
                    P.op("act", lambda e: e.copy(out=h2Tf, in_=Opf[1]), reads=[BO[1]], writes=[Bm["h2Tf"]])
                    if dbg == 50 and _STG == 8:
                        break
                    P.op("dve", lambda e, ti=ti: e.tensor_copy(out=h2Tb[:, :, ti * 128:(ti + 1) * 128],
                                                               in_=h2Tf.rearrange("p (k n) -> p k n", k=8)),
                         reads=[Bm["h2Tf"]], writes=[Bm["h2Tb"]])
                    if dbg == 50 and _STG == 9:
                        break
                    for kc in range(8):
                        P.op("pe", lambda e, kc=kc: e.matmul(out=pj[:, 0:36], lhsT=h2Tf[:, kc * 128:(kc + 1) * 128], rhs=Rw[:, kc, :],
                                                             start=(kc == 0), stop=(kc == 7)), reads=[Bm["h2Tf"], Bm["Rw"]], writes=[Bm["pj"]])
                    if dbg == 50 and _STG == 10:
                        break
                    P.op("act", lambda e, ti=ti: e.copy(out=lg8[:, ti, :], in_=pj[:, 0:36]), reads=[Bm["pj"]], writes=[Bm["lg"]])
                R_ = [Bm["rt"]]; L_ = [Bm["lg"]]
                def r3(c0, n):
                    return rt8[:, c0:c0 + 8 * n].rearrange("p (t n) -> p t n", t=8)
                def r2(c0):
                    return rt8[:, c0:c0 + 8]
                GMAX, GSUM, PG, M1V, M2V, DD, ED, W1, W2, W1P, W2P = [8 * i for i in range(11)]
                GONE, SH, ESEL, TMP, M1, M2, W8 = 96, 128, 160, 224, 288, 352, 416
                glog = lg8[:, :, 0:4]
                P.op("dve", lambda e: e.tensor_reduce(out=r2(GMAX), in_=glog, axis=AX.X, op=ALU.max), reads=L_, writes=R_)
                P.op("dve", lambda e: e.tensor_tensor(out=r3(GONE, 4), in0=glog, in1=bc_inner(r2(GMAX), 4), op=ALU.is_equal), reads=L_ + R_, writes=R_)
                P.op("dve", lambda e: e.tensor_tensor(out=r3(SH, 4), in0=glog, in1=bc_inner(r2(GMAX), 4), op=ALU.subtract), reads=L_ + R_, writes=R_)
                P.op("act", lambda e: e.activation(out=r3(SH, 4), in_=r3(SH, 4), func=AF.Exp), reads=R_, writes=R_)
                P.op("dve", lambda e: e.tensor_reduce(out=r2(GSUM), in_=r3(SH, 4), axis=AX.X, op=ALU.add), reads=R_, writes=R_)
                P.op("dve", lambda e: e.reciprocal(out=r2(PG), in_=r2(GSUM)), reads=R_, writes=R_)
                for gx in range(4):
                    dst = r3(ESEL, 8) if gx == 0 else r3(TMP, 8)
                    P.op("dve", lambda e, gx=gx, dst=dst: e.tensor_tensor(
                        out=dst, in0=lg8[:, :, 4 + 8 * gx:12 + 8 * gx], in1=bc_inner(r3(GONE, 4)[:, :, gx], 8), op=ALU.mult),
                        reads=L_ + R_, writes=R_)
                    if gx > 0:
                        P.op("dve", lambda e: e.tensor_tensor(out=r3(ESEL, 8), in0=r3(ESEL, 8), in1=r3(TMP, 8), op=ALU.add), reads=R_, writes=R_)
                P.op("dve", lambda e: e.tensor_reduce(out=r2(M1V), in_=r3(ESEL, 8), axis=AX.X, op=ALU.max), reads=R_, writes=R_)
                P.op("dve", lambda e: e.tensor_tensor(out=r3(M1, 8), in0=r3(ESEL, 8), in1=bc_inner(r2(M1V), 8), op=ALU.is_equal), reads=R_, writes=R_)
                P.op("dve", lambda e: e.scalar_tensor_tensor(out=r3(TMP, 8), in0=r3(M1, 8), scalar=-1e30, in1=r3(ESEL, 8),
                                                             op0=ALU.mult, op1=ALU.add), reads=R_, writes=R_)
                P.op("dve", lambda e: e.tensor_reduce(out=r2(M2V), in_=r3(TMP, 8), axis=AX.X, op=ALU.max), reads=R_, writes=R_)
                P.op("dve", lambda e: e.tensor_tensor(out=r3(M2, 8), in0=r3(ESEL, 8), in1=bc_inner(r2(M2V), 8), op=ALU.is_equal), reads=R_, writes=R_)
                P.op("dve", lambda e: e.tensor_sub(out=r2(DD), in0=r2(M2V), in1=r2(M1V)), reads=R_, writes=R_)
                P.op("act", lambda e: e.activation(out=r2(ED), in_=r2(DD), func=AF.Exp), reads=R_, writes=R_)
                P.op("dve", lambda e: e.tensor_scalar_add(out=r2(W1), in0=r2(ED), scalar1=1.0), reads=R_, writes=R_)
                P.op("dve", lambda e: e.reciprocal(out=r2(W1), in_=r2(W1)), reads=R_, writes=R_)
                P.op("dve", lambda e: e.tensor_tensor(out=r2(W2), in0=r2(ED), in1=r2(W1), op=ALU.mult), reads=R_, writes=R_)
                P.op("dve", lambda e: e.tensor_tensor(out=r2(W1P), in0=r2(W1), in1=r2(PG), op=ALU.mult), reads=R_, writes=R_)
                P.op("dve", lambda e: e.tensor_tensor(out=r2(W2P), in0=r2(W2), in1=r2(PG), op=ALU.mult), reads=R_, writes=R_)
                P.op("dve", lambda e: e.tensor_tensor(out=r3(W8, 8), in0=r3(M1, 8), in1=bc_inner(r2(W1P), 8), op=ALU.mult), reads=R_, writes=R_)
                P.op("dve", lambda e: e.tensor_tensor(out=r3(TMP, 8), in0=r3(M2, 8), in1=bc_inner(r2(W2P), 8), op=ALU.mult), reads=R_, writes=R_)
                P.op("dve", lambda e: e.tensor_tensor(out=r3(W8, 8), in0=r3(W8, 8), in1=r3(TMP, 8), op=ALU.add), reads=R_, writes=R_)
                for gx in range(4):
                    P.op("dve", lambda e, gx=gx: e.tensor_tensor(out=wfull[:, :, 8 * gx:8 * gx + 8], in0=r3(W8, 8),
                                                                 in1=bc_inner(r3(GONE, 4)[:, :, gx], 8), op=ALU.mult),
                         reads=R_, writes=[Bm["wfull"]])
                if dbg in (50, 51):
                    break
                for ex in range(N_EXP if dbg != 52 else 1):
                    slot = ex % 2
                    srcs = [(w_gate.ap()[0, ex].rearrange("(k p) f -> p k f", p=128), Wgu[slot][:, :, 0:256], 8, 256, Bwgu[slot]),
                            (w_up.ap()[0, ex].rearrange("(k p) f -> p k f", p=128), Wgu[slot][:, :, 256:512], 8, 256, Bwgu[slot]),
                            (w_down.ap()[0, ex].rearrange("(c p) n -> p c n", p=128), Wdn[slot], 2, 1024, Bwd[slot])]
                    for src, dst, a_, b_, bdst in srcs:
                        si = st_ctr[0] % 2; st_ctr[0] += 1
                        stv = wst[si].rearrange("p (a b) -> p a b", a=a_)
                        P.dma("sp", f"dwe{si}", lambda e, stv=stv, src=src: e.dma_start(out=stv, in_=src), writes=[Bwst[si]])
                        P.op("pool", lambda e, stv=stv, dst=dst: e.tensor_copy(out=dst, in_=stv), reads=[Bwst[si]], writes=[bdst])
                    def head(ti, i, slot=slot):
                        for kc in range(8):
                            P.op("pe", lambda e, kc=kc: e.matmul(
                                out=Sps[i][:], lhsT=h2Tb[:, kc, ti * 128:(ti + 1) * 128], rhs=Wgu[slot][:, kc, :],
                                start=(kc == 0), stop=(kc == 7)), reads=[Bm["h2Tb"], Bwgu[slot]], writes=[BS[i]])

                    def mid(ti, i, o, b, slot=slot, ex=ex):
                        sg = sgs[b]; hid = hids[b]
                        P.op("act", lambda e: e.activation(out=sg, in_=Sps[i][:, 0:256], func=AF.Silu), reads=[BS[i]], writes=[Bsg[b]])
                        P.op("dve", lambda e: e.scalar_tensor_tensor(
                            out=hid, in0=Sps[i][:, 256:512], scalar=wfull[:, ti, ex:ex + 1], in1=sg, op0=ALU.mult, op1=ALU.mult),
                            reads=[BS[i], Bm["wfull"], Bsg[b]], writes=[Bhid[b]])
                        for c in range(2):
                            P.op("pe", lambda e, c=c: e.transpose(out=ptr[:, b * 256 + c * 128:b * 256 + (c + 1) * 128],
                                                                  in_=hid[:, c * 128:(c + 1) * 128],
                                                                  identity=ident_b[:]), reads=[Bhid[b], Bm["idb"]], writes=[Bptr2[b]])
                        P.op("act", lambda e: e.copy(out=hidTs[b], in_=ptr[:, b * 256:(b + 1) * 256]), reads=[Bptr2[b]], writes=[BhidT[b]])

                    def tail(ti, i, o, b, slot=slot, ex=ex):
                        hidT = hidTs[b]
                        for half in range(2):
                            for c in range(2):
                                P.op("pe", lambda e, half=half, c=c: e.matmul(
                                    out=Opf[o][:, half * 512:(half + 1) * 512], lhsT=hidT[:, c * 128:(c + 1) * 128],
                                    rhs=Wdn[slot][:, c, half * 512:(half + 1) * 512], start=(c == 0), stop=(c == 1)),
                                    reads=[BhidT[b], Bwd[slot]], writes=[BO[o]])
                        P.op("dve", lambda e: e.tensor_tensor(out=acc[:, ti, :], in0=Opf[o], in1=acc[:, ti, :], op=ALU.add),
                             reads=[BO[o], Bm["acc"]], writes=[Bm["acc"]])

                    infos = []
                    for ti in range(8):
                        i = s_ctr[0] % 2; s_ctr[0] += 1
                        o = o_ctr2[0] % 2; o_ctr2[0] += 1
                        infos.append((ti, i, o, ti % 2))
                    for k in range(8 + 2):
                        if k < 8:
                            head(infos[k][0], infos[k][1])
                        if 0 <= k - 1 < 8:
                            mid(*infos[k - 1])
                        if 0 <= k - 2 < 8:
                            tail(*infos[k - 2])
                if dbg == 52:
                    break
                for ti in range(8):
                    r0 = sbk * 1024 + ti * 128
                    ob = Buf()
                    P.dma("sp", "dfin", lambda e, ti=ti, r0=r0: e.dma_start(out=out.ap()[r0:r0 + 128, :], in_=acc[:, ti, :]),
                          reads=[Bm["acc"]], writes=[ob])
                    out_bufs.append(ob)

        P.wait_all("sp", out_bufs)
        P.finish()
        with nc.allow_non_contiguous_dma(reason="small parameter loads"):
            P.emit()
    return nc


_PARAM_KEYS = ["norm1_g", "w_in", "diff_q_g", "diff_k_g", "lambda_q1", "lambda_k1", "lambda_q2", "lambda_k2",
               "diff_sub_g", "moba_q_g", "moba_k_g", "rel_bias", "w_out", "norm2_g", "router_group",
               "router_expert", "w_gate", "w_up", "w_down"]


def run_cores(inputs, nseq, ncores, stop_after_attn=False, dbg=0):
    nc = build(nseq, stop_after_attn=stop_after_attn, dbg=dbg)
    onehot, cmask, blk = _consts()
    xs = np.ascontiguousarray(inputs["x"], dtype=np.float32).reshape(-1, D_MODEL)
    in_maps = []
    for c in range(ncores):
        m = {k: np.ascontiguousarray(inputs[k], dtype=np.float32) for k in _PARAM_KEYS}
        m["x"] = xs[c * nseq * S_LEN:(c + 1) * nseq * S_LEN]
        m["c_onehot"] = onehot; m["c_blk"] = blk
        in_maps.append(m)
    res = run_bass_kernel_spmd(nc, in_maps, core_ids=list(range(ncores)))
    return np.concatenate([np.asarray(r["out"]) for r in res.results], axis=0)


def kernel(**inputs):
    B = inputs["x"].shape[0]
    o = run_cores(inputs, B // 8, 8)
    return o.reshape(B, S_LEN, D_MODEL).astype(np.float32)
```
